# Optimizing a Trainium2 kernel written in Bass

```python
import jax, jax.numpy as jnp
from jax import lax
import numpy as np

D_MODEL = 1024
BATCH = 8
SEQ = 4096
DEPTH = 2

MEM_LEN = 256
N_EVEN = (DEPTH + 1) // 2
N_ODD = DEPTH // 2
PF_WIDTH = D_MODEL
POOL_GROUPS = 4
POOL_WINDOWS = (2, 4, 8, 16)
POOL_DIM = PF_WIDTH // 2 // POOL_GROUPS
FOURIER_HEADS = 4
FOURIER_DIM = PF_WIDTH // 2 // FOURIER_HEADS
HG_EXPAND = 128
HG_HEADS = D_MODEL // HG_EXPAND
HG_DIM = HG_HEADS * HG_EXPAND
HG_N_PROJ = 5
HG_CHUNK = 64
XA_HEADS = 4
XA_HEAD_DIM = D_MODEL // XA_HEADS
MOE_GROUPS = 4
MOE_PER_GROUP = 8
MOE_EXPERTS = MOE_GROUPS * MOE_PER_GROUP
MOE_TOPK = 2
MOE_HIDDEN = D_MODEL // 2
N_SUBLAYERS = 3
DN_ALPHA = (2.0 * DEPTH) ** 0.25
DN_BETA = (8.0 * DEPTH) ** -0.25
LN_EPS = 1e-5

kernel_name = "hybrid_pool_fourier_hgrn2_hmoe_encoder"


def layer_norm(x, g, b):
    xf = x.astype(jnp.float32)
    mu = jnp.mean(xf, axis=-1, keepdims=True)
    var = jnp.mean(jnp.square(xf - mu), axis=-1, keepdims=True)
    y = (xf - mu) * lax.rsqrt(var + LN_EPS) * g.astype(jnp.float32) + b.astype(jnp.float32)
    return y.astype(x.dtype)


def centred_pool_minus_identity(u):
    S = u.shape[1]
    uf = u.astype(jnp.float32)
    cs = jnp.cumsum(uf, axis=1)
    cs = jnp.concatenate([jnp.zeros_like(cs[:, :1]), cs], axis=1)
    t = np.arange(S)
    outs = []
    for gi, w in enumerate(POOL_WINDOWS):
        lo = np.clip(t - w // 2, 0, S)
        hi = np.clip(t + w // 2, 0, S)
        cnt = (hi - lo).astype(np.float32)
        window_sum = cs[:, hi, gi] - cs[:, lo, gi]
        outs.append(window_sum / cnt[None, :, None])
    pooled = jnp.stack(outs, axis=2)
    return (pooled - uf).astype(u.dtype)


def fourier_heads(u, ln_g):
    uf = u.astype(jnp.float32)
    mu = jnp.mean(uf, axis=-1, keepdims=True)
    var = jnp.mean(jnp.square(uf - mu), axis=-1, keepdims=True)
    un = (uf - mu) * lax.rsqrt(var + LN_EPS) * ln_g.astype(jnp.float32)
    return jnp.real(jnp.fft.fft2(un, axes=(1, 3), norm="ortho")).astype(u.dtype)


def pool_fourier_mixer(x, w_in, pool_w, pool_scale, fourier_ln_g, fourier_w, w_out):
    B, S, _ = x.shape
    half = PF_WIDTH // 2
    u = x @ w_in
    ua = u[..., :half].reshape(B, S, POOL_GROUPS, POOL_DIM)
    ub = u[..., half:].reshape(B, S, FOURIER_HEADS, FOURIER_DIM)
    ya = jnp.einsum('bsgc,gcd->bsgd', centred_pool_minus_identity(ua), pool_w)
    ya = ya.reshape(B, S, half) * pool_scale
    yb = jnp.einsum('bshc,hcd->bshd', fourier_heads(ub, fourier_ln_g), fourier_w)
    yb = yb.reshape(B, S, half)
    return jnp.concatenate([ya, yb], axis=-1) @ w_out


def gla_chunked(q, k, v, logf):
    B, S, H, Dk = q.shape
    Dv = v.shape[-1]
    L = HG_CHUNK
    NC = S // L

    def chunks(a):
        return a.reshape(B, NC, L, H, a.shape[-1]).transpose(0, 3, 1, 2, 4)

    q, k, v, logf = chunks(q), chunks(k), chunks(v), chunks(logf)
    b = jnp.cumsum(logf, axis=3)
    b_ref = b[:, :, :, L // 2:L // 2 + 1]
    qt = q * jnp.exp(b - b_ref)
    kt = k * jnp.exp(b_ref - b)
    scores = jnp.einsum('bhnlk,bhnmk->bhnlm', qt, kt)
    tril = np.tril(np.ones((L, L), dtype=bool))
    scores = jnp.where(tril, scores, 0.0)
    o_intra = jnp.einsum('bhnlm,bhnmv->bhnlv', scores, v)
    b_last = b[:, :, :, -1:]
    u_chunk = jnp.einsum('bhnlk,bhnlv->bhnkv', k * jnp.exp(b_last - b), v)
    decay = jnp.exp(b_last[:, :, :, 0])

    def step(state, inp):
        d, u_c = inp
        return d[..., None] * state + u_c, state

    _, s_prev = lax.scan(step, jnp.zeros((B, H, Dk, Dv), jnp.float32),
                         (jnp.moveaxis(decay, 2, 0), jnp.moveaxis(u_chunk, 2, 0)))
    s_prev = jnp.moveaxis(s_prev, 0, 2)
    o_inter = jnp.einsum('bhnlk,bhnkv->bhnlv', q * jnp.exp(b), s_prev)
    o = o_intra + o_inter
    return o.transpose(0, 2, 3, 1, 4).reshape(B, S, H, Dv)


def hgrn2_mixer(x, w_in, lower_bound, norm_g, w_out):
    B, S, _ = x.shape
    proj = (x @ w_in).astype(jnp.float32).reshape(B, S, HG_N_PROJ, HG_HEADS, HG_EXPAND)
    q = jax.nn.silu(proj[:, :, 0])
    i = proj[:, :, 1]
    g = proj[:, :, 4]
    lb = lower_bound.astype(jnp.float32).reshape(2, HG_HEADS, HG_EXPAND)

    def direction(d, reverse):
        f = lb[d] + (1.0 - lb[d]) * jax.nn.sigmoid(proj[:, :, 2 + d])
        args = (q, 1.0 - f, i, jnp.log(f))
        if reverse:
            return jnp.flip(gla_chunked(*[jnp.flip(a, axis=1) for a in args]), axis=1)
        return gla_chunked(*args)

    o = direction(0, False) + direction(1, True)
    o = o * lax.rsqrt(jnp.mean(jnp.square(o), axis=-1, keepdims=True) + LN_EPS)
    o = o * norm_g.astype(jnp.float32) * jax.nn.silu(g)
    return o.reshape(B, S, HG_DIM).astype(x.dtype) @ w_out


def memory_cross_attention(x, mem, wq, wkv, wo):
    B, S, _ = x.shape
    M = mem.shape[1]
    q = (x @ wq).reshape(B, S, XA_HEADS, XA_HEAD_DIM)
    kv = (mem @ wkv).reshape(B, M, 2, XA_HEADS, XA_HEAD_DIM)
    k, v = kv[:, :, 0], kv[:, :, 1]
    s = jnp.einsum('bshd,bmhd->bhsm', q, k).astype(jnp.float32) * (XA_HEAD_DIM ** -0.5)
    p = jax.nn.softmax(s, axis=-1).astype(x.dtype)
    o = jnp.einsum('bhsm,bmhd->bshd', p, v).reshape(B, S, D_MODEL)
    return o @ wo


def hier_moe(x, w_group, b_group, w_expert, b_expert, w_gate, w_up, w_down):
    B, S, D = x.shape
    T = B * S
    xt = x.reshape(T, D)
    g_logits = (xt @ w_group).astype(jnp.float32) + b_group.astype(jnp.float32)
    p_grp, g_idx = lax.top_k(jax.nn.softmax(g_logits, axis=-1), 1)
    e_logits = ((xt @ w_expert).astype(jnp.float32) + b_expert.astype(jnp.float32))
    e_logits = e_logits.reshape(T, MOE_GROUPS, MOE_PER_GROUP)
    e_sel = jnp.take_along_axis(e_logits, g_idx[:, :, None], axis=1)[:, 0]
    p_exp, e_local = lax.top_k(jax.nn.softmax(e_sel, axis=-1), MOE_TOPK)
    gates = p_grp * p_exp / jnp.sum(p_exp, axis=-1, keepdims=True)
    experts = g_idx * MOE_PER_GROUP + e_local
    e_flat = experts.reshape(-1)
    order = jnp.argsort(e_flat)
    tok_sorted = (jnp.arange(T * MOE_TOPK, dtype=jnp.int32) // MOE_TOPK)[order]
    gate_sorted = gates.reshape(-1)[order]
    sizes = jnp.bincount(e_flat, length=MOE_EXPERTS).astype(jnp.int32)
    xs = xt[tok_sorted]
    h = jax.nn.silu(lax.ragged_dot(xs, w_gate, sizes)) * lax.ragged_dot(xs, w_up, sizes)
    y = lax.ragged_dot(h, w_down, sizes).astype(jnp.float32) * gate_sorted[:, None]
    out = jax.ops.segment_sum(y, tok_sorted, num_segments=T)
    return out.astype(x.dtype).reshape(B, S, D)


def setup_inputs(seed: int = 0) -> dict:
    key = jax.random.key(seed)
    ks = jax.random.split(key, 26)
    D = D_MODEL
    half = PF_WIDTH // 2

    def nrm(k, shape, scale):
        return jax.random.normal(k, shape, jnp.float32) * scale

    xa_wk = nrm(ks[13], (DEPTH, D, D), D ** -0.5)
    xa_wv = nrm(ks[14], (DEPTH, D, D), DN_BETA * D ** -0.5)
    return {
        "x": nrm(ks[0], (BATCH, SEQ, D), 1.0),
        "mem": nrm(ks[1], (BATCH, MEM_LEN, D), 1.0),
        "pf_w_in": nrm(ks[2], (N_EVEN, D, PF_WIDTH), D ** -0.5),
        "pf_pool_w": nrm(ks[3], (N_EVEN, POOL_GROUPS, POOL_DIM, POOL_DIM), POOL_DIM ** -0.5),
        "pf_pool_scale": 1.0 + nrm(ks[4], (N_EVEN, half), 0.02),
        "pf_fourier_ln_g": 1.0 + nrm(ks[5], (N_EVEN, FOURIER_HEADS, FOURIER_DIM), 0.02),
        "pf_fourier_w": nrm(ks[6], (N_EVEN, FOURIER_HEADS, FOURIER_DIM, FOURIER_DIM), FOURIER_DIM ** -0.5),
        "pf_w_out": nrm(ks[7], (N_EVEN, PF_WIDTH, D), DN_BETA * PF_WIDTH ** -0.5),
        "hg_w_in": nrm(ks[8], (N_ODD, D, HG_N_PROJ * HG_DIM), D ** -0.5),
        "hg_lower_bounds": 1.0 + nrm(ks[9], (DEPTH, 2, HG_DIM), 0.1),
        "hg_norm_g": 1.0 + nrm(ks[10], (N_ODD, HG_EXPAND), 0.02),
        "hg_w_out": nrm(ks[11], (N_ODD, HG_DIM, D), DN_BETA * HG_DIM ** -0.5),
        "xa_wq": nrm(ks[12], (DEPTH, D, D), D ** -0.5),
        "xa_wkv": jnp.concatenate([xa_wk, xa_wv], axis=-1),
        "xa_wo": nrm(ks[15], (DEPTH, D, D), DN_BETA * D ** -0.5),
        "moe_w_group": nrm(ks[16], (DEPTH, D, MOE_GROUPS), D ** -0.5),
        "moe_b_group": nrm(ks[17], (DEPTH, MOE_GROUPS), 0.01),
        "moe_w_expert": nrm(ks[18], (DEPTH, D, MOE_EXPERTS), D ** -0.5),
        "moe_b_expert": nrm(ks[19], (DEPTH, MOE_EXPERTS), 0.01),
        "moe_w_gate": nrm(ks[20], (DEPTH, MOE_EXPERTS, D, MOE_HIDDEN), D ** -0.5),
        "moe_w_up": nrm(ks[21], (DEPTH, MOE_EXPERTS, D, MOE_HIDDEN), D ** -0.5),
        "moe_w_down": nrm(ks[22], (DEPTH, MOE_EXPERTS, MOE_HIDDEN, D), DN_BETA * MOE_HIDDEN ** -0.5),
        "ln_g": 1.0 + nrm(ks[23], (DEPTH, N_SUBLAYERS, D), 0.02),
        "ln_b": nrm(ks[24], (DEPTH, N_SUBLAYERS, D), 0.02),
    }


def reference(x, mem, pf_w_in, pf_pool_w, pf_pool_scale, pf_fourier_ln_g, pf_fourier_w, pf_w_out,
              hg_w_in, hg_lower_bounds, hg_norm_g, hg_w_out, xa_wq, xa_wkv, xa_wo,
              moe_w_group, moe_b_group, moe_w_expert, moe_b_expert, moe_w_gate, moe_w_up, moe_w_down,
              ln_g, ln_b):
    lb_all = jnp.cumsum(jax.nn.softmax(hg_lower_bounds.astype(jnp.float32), axis=0), axis=0)
    lb_all = lb_all - lb_all[:1]
    for l in range(DEPTH):
        j = l // 2
        if l % 2 == 0:
            h = pool_fourier_mixer(x, pf_w_in[j], pf_pool_w[j], pf_pool_scale[j],
                                   pf_fourier_ln_g[j], pf_fourier_w[j], pf_w_out[j])
        else:
            h = hgrn2_mixer(x, hg_w_in[j], lb_all[l], hg_norm_g[j], hg_w_out[j])
        x = layer_norm(DN_ALPHA * x + h, ln_g[l, 0], ln_b[l, 0])
        h = memory_cross_attention(x, mem, xa_wq[l], xa_wkv[l], xa_wo[l])
        x = layer_norm(DN_ALPHA * x + h, ln_g[l, 1], ln_b[l, 1])
        h = hier_moe(x, moe_w_group[l], moe_b_group[l], moe_w_expert[l], moe_b_expert[l],
                     moe_w_gate[l], moe_w_up[l], moe_w_down[l])
        x = layer_norm(DN_ALPHA * x + h, ln_g[l, 2], ln_b[l, 2])
    return x
```

```python
import numpy as np
from contextlib import ExitStack
import concourse.bass as bass
import concourse.mybir as mybir
from concourse.bass_utils import run_bass_kernel_spmd

F32 = mybir.dt.float32
BF16 = mybir.dt.bfloat16
I32 = mybir.dt.int32
ALU = mybir.AluOpType
AF = mybir.ActivationFunctionType
AX = mybir.AxisListType

S = 4096
D = 1024
NT = 32
MEM = 256
CAP = 512
NEXP = 32
ALPHA = 4.0 ** 0.25
EPS = 1e-5
EPOCH = 12000
DBG = {"skip_l0": False, "xa_groups": 8, "hg_heads": 8, "skip": ()}
NDS = 8


class Op:
    __slots__ = ("eng", "fn", "reads", "writes", "dma", "seq", "dn", "deps", "waits", "bar", "c", "ns")


class Prog:
    ENGS = ["pe", "act", "dve", "pool", "sp"]

    def __init__(self, nc):
        self.nc = nc
        self.ops = []

    DEFC = {"pe": 1.8, "act": 0.6, "dve": 0.6, "pool": 1.5, "sp": 4.0}

    def add(self, eng, fn, reads=(), writes=(), dma=False, c=None):
        o = Op()
        o.eng, o.fn, o.reads, o.writes, o.dma, o.bar = eng, fn, list(reads), list(writes), dma, False
        o.c = c if c is not None else (4.0 if dma else self.DEFC[eng])
        o.ns = getattr(self, "nosched", False)
        self.ops.append(o)
        return o

    def pe(self, fn, r=(), w=(), c=None):
        return self.add("pe", fn, r, w, c=c)

    def act(self, fn, r=(), w=(), c=None):
        return self.add("act", fn, r, w, c=c)

    def dve(self, fn, r=(), w=(), c=None):
        return self.add("dve", fn, r, w, c=c)

    def pool(self, fn, r=(), w=(), c=None):
        return self.add("pool", fn, r, w, c=c)

    def dma(self, fn, r=(), w=(), q="sp", c=None):
        return self.add(q, fn, r, w, dma=True, c=c)

    def barrier(self):
        for e in self.ENGS:
            o = self.add(e, None)
            o.bar = True

    WIN = {"pe": 48, "act": 48, "dve": 48, "pool": 1, "sp": 48}

    def reorder(self, window=48, sync_lat=0.2):
        ops = self.ops
        new_ops = []
        seg = []

        def flush():
            n = len(seg)
            if n == 0:
                return
            if any(getattr(op, "ns", False) for op in seg):
                new_ops.extend(seg)
                seg.clear()
                return
            last_w, readers = {}, {}
            preds = [set() for _ in range(n)]
            for j, op in enumerate(seg):
                for r in op.reads:
                    if r in last_w:
                        preds[j].add(last_w[r])
                for w in op.writes:
                    if w in last_w:
                        preds[j].add(last_w[w])
                    preds[j].update(readers.get(w, ()))
                preds[j].discard(j)
                for r in op.reads:
                    readers.setdefault(r, []).append(j)
                for w in op.writes:
                    last_w[w] = j
                    readers[w] = []
            queues = {e: [j for j, op in enumerate(seg) if op.eng == e] for e in self.ENGS}
            ptr = {e: 0 for e in self.ENGS}
            done = [False] * n
            fin = [0.0] * n
            t_e = {e: 0.0 for e in self.ENGS}
            left = n
            while left:
                best = None
                for e in self.ENGS:
                    q = queues[e]
                    p = ptr[e]
                    while p < len(q) and done[q[p]]:
                        p += 1
                    ptr[e] = p
                    seen = 0
                    k = p
                    while k < len(q) and seen < self.WIN[e]:
                        j = q[k]
                        k += 1
                        if done[j]:
                            continue
                        seen += 1
                        ok = True
                        rt = 0.0
                        for pr in preds[j]:
                            if not done[pr]:
                                ok = False
                                break
                            f = fin[pr] + sync_lat
                            if f > rt:
                                rt = f
                        if not ok:
                            continue
                        stt = max(t_e[e], rt)
                        key = (stt, j)
                        if best is None or key < best[0]:
                            best = (key, e, j)
                        if stt <= t_e[e]:
                            break
                assert best is not None, "scheduler deadlock"
                (stt, _), e, j = best
                op = seg[j]
                done[j] = True
                left -= 1
                if op.dma:
                    t_e[e] = stt + 0.15
                    fin[j] = stt + op.c
                else:
                    t_e[e] = stt + op.c
                    fin[j] = stt + op.c
                new_ops.append(op)
            seg.clear()

        i = 0
        while i < len(ops):
            if ops[i].bar:
                flush()
                while i < len(ops) and ops[i].bar:
                    new_ops.append(ops[i])
                    i += 1
            else:
                seg.append(ops[i])
                i += 1
        flush()
        self.ops = new_ops

    def emit(self, final_keys=(), reorder=True):
        nc = self.nc
        if reorder:
            self.reorder()
        ops = self.ops
        self.add("sp", None, final_keys, ())
        cnt = {e: 0 for e in self.ENGS}
        dcnt = {e: 0 for e in self.ENGS}
        dma_ops = {e: [] for e in self.ENGS}
        last_c = {e: None for e in self.ENGS}
        epos = {e: 0 for e in self.ENGS}
        for j, op in enumerate(ops):
            if op.dma:
                op.dn = dcnt[op.eng]
                dcnt[op.eng] += 1
                dma_ops[op.eng].append(j)
            elif op.fn is not None:
                epos[op.eng] += 1
                op.seq = epos[op.eng]
        last_w = {}
        readers = {}
        for j, op in enumerate(ops):
            deps = set()
            if op.bar:
                for e in self.ENGS:
                    if last_c[e] is not None:
                        deps.add(last_c[e])
                for e in self.ENGS:
                    lst = [i for i in dma_ops[e] if i < j]
                    deps.update(lst[-NDS:])
            for r in op.reads:
                if r in last_w:
                    deps.add(last_w[r])
            for w in op.writes:
                if w in last_w:
                    deps.add(last_w[w])
                deps.update(readers.get(w, ()))
            if op.dma and op.dn >= NDS:
                deps.add(dma_ops[op.eng][op.dn - NDS])
            deps.discard(j)
            op.deps = deps
            for r in op.reads:
                readers.setdefault(r, []).append(j)
            for w in op.writes:
                last_w[w] = j
                readers[w] = []
            if (not op.dma) and op.fn is not None:
                last_c[op.eng] = j
        wc = {e: {} for e in self.ENGS}
        wd = {e: {} for e in self.ENGS}
        signal = set()
        for j, op in enumerate(ops):
            waits = []
            me = op.eng
            for i in sorted(op.deps):
                d = ops[i]
                if d.dma:
                    k = (d.eng, d.dn % NDS)
                    val = 16 * (d.dn // NDS + 1)
                    if wd[me].get(k, 0) < val:
                        wd[me][k] = val
                        waits.append(("d", i))
                else:
                    if d.fn is None:
                        continue
                    if d.eng == "pe" and me == "pe":
                        continue
                    if wc[me].get(d.eng, 0) < d.seq:
                        wc[me][d.eng] = d.seq
                        waits.append(("c", i))
                        signal.add(i)
            op.waits = waits
        for j, op in enumerate(ops):
            if (not op.dma) and op.fn is not None:
                if j in signal:
                    cnt[op.eng] += 1
                    op.seq = cnt[op.eng]
                else:
                    op.seq = None
        with ExitStack() as st:
            csem = {}
            for e in self.ENGS:
                n_ep = (cnt[e] + EPOCH - 1) // EPOCH
                csem[e] = [st.enter_context(nc.semaphore(f"c_{e}_{k}")) for k in range(max(n_ep, 1))]
            dsem = {}
            for e in self.ENGS:
                if dcnt[e]:
                    dsem[e] = [st.enter_context(nc.semaphore(f"d_{e}_{k}")) for k in range(NDS)]
            for op in ops:
                ww = []
                for (kind, i) in op.waits:
                    d = ops[i]
                    if kind == "d":
                        ww.append((dsem[d.eng][d.dn % NDS], 16 * (d.dn // NDS + 1)))
                    else:
                        ww.append((csem[d.eng][(d.seq - 1) // EPOCH], (d.seq - 1) % EPOCH + 1))
                op.waits = ww
            streams = {e: [op for op in ops if op.eng == e] for e in self.ENGS}
            block = st.enter_context(nc.Block())

            def mk(ename):
                def body(e):
                    for op in streams[ename]:
                        for (sem, val) in op.waits:
                            e.wait_ge(sem, val)
                        if op.fn is None:
                            continue
                        ins = op.fn(e)
                        if op.dma:
                            ins.then_inc(dsem[ename][op.dn % NDS], 16)
                        elif op.seq is not None:
                            ins.then_inc(csem[ename][(op.seq - 1) // EPOCH], 1)
                return body

            block.tensor(mk("pe"))
            block.scalar(mk("act"))
            block.vector(mk("dve"))
            block.gpsimd(mk("pool"))
            block.sync(mk("sp"))
        return cnt, dcnt


class Arena:
    def __init__(self, ap32, nbytes):
        self.ap = ap32
        self.n = nbytes
        self.off = 0
        self.marks = []

    def alloc(self, free_elems, dt):
        sz = 2 if dt == BF16 else 4
        nb = (free_elems * sz + 31) // 32 * 32
        assert self.off + nb <= self.n, f"arena overflow {self.off}+{nb}>{self.n}"
        a = self.ap[:, self.off // 4:(self.off + nb) // 4]
        self.off += nb
        if dt != F32:
            a = a.bitcast(dt)
        return a[:, 0:free_elems]

    def mark(self):
        self.marks.append(self.off)

    def release(self):
        self.off = self.marks.pop()


def build(upto=99, dbg=False):
    nc = bass.Bass("TRN2", target_bir_lowering=False)
    P = Prog(nc)

    def din(name, shape, dt=F32):
        return nc.dram_tensor(name, list(shape), dt, kind="ExternalInput").ap()

    x_d = din("x", [S, D])
    mem_d = din("mem", [MEM, D])
    pf_w_in = din("pf_w_in", [D, D])
    pf_pool_w = din("pf_pool_w", [4, 128, 128])
    pf_pool_scale = din("pf_pool_scale", [512])
    pf_ln_g = din("pf_fourier_ln_g", [4, 128])
    pf_fw = din("pf_fourier_w", [4, 128, 128])
    pf_w_out = din("pf_w_out", [D, D])
    hg_w_in = din("hg_w_in", [D, 5 * D])
    hg_lb = din("hg_lower_bounds", [2, 2, D])
    hg_norm_g = din("hg_norm_g", [128])
    hg_w_out = din("hg_w_out", [D, D])
    xa_wq = din("xa_wq", [2, D, D])
    xa_wkv = din("xa_wkv", [2, D, 2 * D])
    xa_wo = din("xa_wo", [2, D, D])
    moe_wg = din("moe_w_group", [2, D, 4])
    moe_bg = din("moe_b_group", [2, 4])
    moe_we = din("moe_w_expert", [2, D, 32])
    moe_be = din("moe_b_expert", [2, 32])
    moe_gate = din("moe_w_gate", [2, NEXP, D, 512])
    moe_up = din("moe_w_up", [2, NEXP, D, 512])
    moe_down = din("moe_w_down", [2, NEXP, 512, D])
    ln_g = din("ln_g", [2, 3, D])
    ln_b = din("ln_b", [2, 3, D])
    c_cs = din("c_cs", [S, S], BF16)
    c_ss = din("c_ss", [S, S], BF16)
    c_dc = din("c_dc", [128, 256])
    c_invcnt = din("c_invcnt", [4, S])
    c_masks = din("c_masks", [4, 128, 128])
    c_eoff = din("c_eoff", [1, 32])
    c_trash = din("c_trash", [128, 1])
    c_alt = din("c_alt", [128, NT])
    out_d = nc.dram_tensor("out", [S, D], F32, kind="ExternalOutput").ap()
    XR = [nc.dram_tensor(f"xr{k}", [S, D], F32).ap() for k in range(2)]
    Zscr = nc.dram_tensor("zscr", [4, NT, 128, 256], BF16).ap()
    XS = nc.dram_tensor("xs_scr", [NEXP * CAP + 128, D], BF16).ap()
    YS = nc.dram_tensor("ys_scr", [NEXP * CAP + 128, D], F32).ap()
    ONT = nc.dram_tensor("ont_scr", [D, S], BF16).ap()

    st = ExitStack()
    ARENA_BYTES = 206 * 1024
    arena_t = st.enter_context(nc.sbuf_tensor("arena", [128, ARENA_BYTES // 4], F32))
    A = Arena(arena_t[:], ARENA_BYTES)
    psb = [st.enter_context(nc.psum_tensor(f"ps{k}", [128, 512], F32)) for k in range(8)]

    def PS(k):
        return psb[k][:]

    def PSB(k):
        return psb[k][:].bitcast(BF16)

    ident = A.alloc(128, BF16)
    ident32 = A.alloc(128, F32)
    ones_m = A.alloc(128, F32)
    gb = A.alloc(2 * D, F32)
    xT = A.alloc(8 * S, BF16)
    xT3 = xT.rearrange("p (k t) -> p k t", k=8)

    P.pool(lambda e: e.memset(ident32, 0.0), w=["ident32"])
    P.pool(lambda e: e.affine_select(out=ident32, in_=ident32, pattern=[[-1, 128]], compare_op=ALU.not_equal,
                                     fill=1.0, base=0, channel_multiplier=1), r=["ident32"], w=["ident32"])
    P.dve(lambda e: e.tensor_copy(out=ident, in_=ident32), r=["ident32"], w=["ident"])
    P.pool(lambda e: e.memset(ones_m, 1.0 / 128.0), w=["ones_m"])

    def ncd(fn):
        def g(e):
            with nc.allow_non_contiguous_dma(reason="tiny per-partition scalar tables"):
                return fn(e)
        return g

    cast_rr = [0]

    def cast_copy(out, in_, r, w):
        cast_rr[0] ^= 1
        if cast_rr[0]:
            P.act(lambda e: e.copy(out=out, in_=in_), r=r, w=w)
        else:
            P.dve(lambda e: e.tensor_copy(out=out, in_=in_), r=r, w=w)

    def transpose_to(dst3, src_bf, bank, rkeys, wkeys):
        def f(e):
            ins = None
            for k in range(8):
                ins = e.transpose(out=PSB(bank)[:, k * 128:(k + 1) * 128], in_=src_bf[:, k * 128:(k + 1) * 128],
                                  identity=ident)
            return ins
        P.pe(f, r=list(rkeys) + ["ident"], w=[("ps", bank)])
        cast_copy(dst3, PSB(bank).rearrange("p (k t) -> p k t", k=8), [("ps", bank)], wkeys)

    def load_w_bf16(dst3, src2d, kt, n, wkey, rows_per_dma=256):
        kk = max(1, rows_per_dma // 128)
        for k0 in range(0, kt, kk):
            k1 = min(kt, k0 + kk)
            P.dma(lambda e, k0=k0, k1=k1: e.dma_start(
                out=dst3[:, k0:k1, :], in_=src2d[k0 * 128:k1 * 128, :].rearrange("(k p) n -> p k n", p=128)),
                w=[wkey], q="pool")

    def load_gb(l, j):
        P.dma(lambda e: e.dma_start(out=gb[:, 0:D], in_=ln_g[l, j:j + 1, :].broadcast_to([128, D])), w=["gb"])
        P.dma(lambda e: e.dma_start(out=gb[:, D:2 * D], in_=ln_b[l, j:j + 1, :].broadcast_to([128, D])), w=["gb"])

    class Epi:
        def __init__(self):
            self.xr = [A.alloc(D, F32) for _ in range(2)]
            self.y = [A.alloc(D, F32) for _ in range(2)]
            self.xo = [A.alloc(D, F32) for _ in range(2)]
            self.xob = [A.alloc(D, BF16) for _ in range(2)]
            self.st = A.alloc(12, F32)
            self.mv = A.alloc(8, F32)

    def epilogue(E, i, hsrc, hkeys, res_ap, dst_ap, make_xT=True, tbank=7, gmul="pool"):
        b = i % 2
        xr, y, xo, xob = E.xr[b], E.y[b], E.xo[b], E.xob[b]
        kxr, ky, kxo, kxob = ("e_xr", b), ("e_y", b), ("e_xo", b), ("e_xob", b)
        P.dma(lambda e: e.dma_start(out=xr, in_=res_ap[i * 128:(i + 1) * 128, :]), r=["res_dram"], w=[kxr])
        for hh in range(2):
            P.dve(lambda e, hh=hh: e.scalar_tensor_tensor(
                out=y[:, hh * 512:(hh + 1) * 512], in0=xr[:, hh * 512:(hh + 1) * 512], scalar=ALPHA,
                in1=hsrc[hh], op0=ALU.mult, op1=ALU.add), r=[kxr] + list(hkeys), w=[ky])
        P.dve(lambda e: e.bn_stats(out=E.st[:, 0:6], in_=y[:, 0:512]), r=[ky], w=["e_st0"])
        P.dve(lambda e: e.bn_stats(out=E.st[:, 6:12], in_=y[:, 512:1024]), r=[ky], w=["e_st1"])
        P.dve(lambda e: e.bn_aggr(out=E.mv[:, 0:2], in_=E.st[:, 0:12]), r=["e_st0", "e_st1"], w=["e_mv"])
        P.act(lambda e: e.activation(out=E.mv[:, 2:3], in_=E.mv[:, 1:2], func=AF.Ln, bias=EPS), r=["e_mv"], w=["e_sd"], c=0.2)
        P.act(lambda e: e.activation(out=E.mv[:, 3:4], in_=E.mv[:, 2:3], func=AF.Exp, scale=-0.5), r=["e_sd"], w=["e_rs"], c=0.2)
        P.dve(lambda e: e.scalar_tensor_tensor(out=E.mv[:, 4:5], in0=E.mv[:, 0:1], scalar=-1.0, in1=E.mv[:, 3:4],
                                               op0=ALU.mult, op1=ALU.mult), r=["e_mv", "e_rs"], w=["e_nm"])
        P.act(lambda e: e.activation(out=y, in_=y, func=AF.Identity, bias=E.mv[:, 4:5], scale=E.mv[:, 3:4]),
              r=[ky, "e_rs", "e_nm"], w=[ky])
        if gmul == "dve":
            P.dve(lambda e: e.tensor_tensor(out=y, in0=y, in1=gb[:, 0:D], op=ALU.mult), r=[ky, "gb"], w=[ky], c=1.2)
        else:
            P.pool(lambda e: e.tensor_tensor(out=y, in0=y, in1=gb[:, 0:D], op=ALU.mult), r=[ky, "gb"], w=[ky], c=2.4)
        P.pool(lambda e: e.tensor_tensor(out=xo, in0=y, in1=gb[:, D:2 * D], op=ALU.add), r=[ky, "gb"], w=[kxo])
        P.dma(lambda e: e.dma_start(out=dst_ap[i * 128:(i + 1) * 128, :], in_=xo), r=[kxo], w=["dst_dram", ("dst", i)], q="pool")
        if make_xT:
            P.act(lambda e: e.copy(out=xob, in_=xo), r=[kxo], w=[kxob])
            transpose_to(xT3[:, :, i * 128:(i + 1) * 128], xob, tbank, [kxob], [("xT", i)])

    A.mark()
    xb0 = [A.alloc(D, BF16) for _ in range(2)]
    for i in range(NT):
        b = i % 2
        P.dma(lambda e, i=i, b=b: e.dma_start(out=xb0[b], in_=x_d[i * 128:(i + 1) * 128, :]), w=[("xb0", b)], q="pool")
        transpose_to(xT3[:, :, i * 128:(i + 1) * 128], xb0[b], 6 + b, [("xb0", b)], [("xT", i)])
    P.barrier()
    A.release()

    xT_all = [("xT", i) for i in range(NT)]
    cur_res = x_d
    nxt = 0

    def l0_phase(cur_res, dst_res):
        if True:
            A.mark()
            catT = A.alloc(8 * S, BF16)
            catT3 = catT.rearrange("p (k t) -> p k t", k=8)
            A.mark()
            w_in = A.alloc(8 * D, BF16).rearrange("p (k n) -> p k n", k=8)
            load_w_bf16(w_in, pf_w_in, 8, D, "w_in")
            poolw = A.alloc(4 * 128, BF16).rearrange("p (g n) -> p g n", g=4)
            fw = A.alloc(4 * 128, BF16).rearrange("p (g n) -> p g n", g=4)
            dc = A.alloc(256, BF16)
            P.dma(lambda e: e.dma_start(out=poolw, in_=pf_pool_w.rearrange("g c d -> c g d")), w=["poolw"], q="pool")
            P.dma(lambda e: e.dma_start(out=fw, in_=pf_fw.rearrange("g c d -> c g d")), w=["fw"], q="pool")
            P.dma(lambda e: e.dma_start(out=dc, in_=c_dc), w=["dc"], q="pool")
            pscale = A.alloc(4, F32)
            lng = A.alloc(4, F32)
            P.dma(ncd(lambda e: e.dma_start(out=pscale, in_=pf_pool_scale.rearrange("(g d) -> d g", g=4))), w=["pscale"])
            P.dma(ncd(lambda e: e.dma_start(out=lng, in_=pf_ln_g.rearrange("h c -> c h"))), w=["lng"])
            PADW = 16
            A.mark()
            a0 = A.alloc(S + 2 * PADW, F32)
            t1 = A.alloc(1024 + 2 * PADW, F32)
            t2 = A.alloc(1024 + 2 * PADW, F32)
            icnt = A.alloc(1024, F32)
            pm = A.alloc(1024, BF16)
            P.pool(lambda e: e.memset(a0, 0.0), w=["a0"])
            for g in range(4):
                for tb in range(8):
                    bank = tb % 2
                    def mm(e, g=g, tb=tb, bank=bank):
                        ins = None
                        for k in range(8):
                            ins = e.matmul(PS(bank), lhsT=w_in[:, k, g * 128:(g + 1) * 128],
                                           rhs=xT3[:, k, tb * 512:(tb + 1) * 512], start=(k == 0), stop=(k == 7))
                        return ins
                    P.pe(mm, r=["w_in"] + xT_all[tb * 4:tb * 4 + 4], w=[("ps", bank)])
                    P.act(lambda e, tb=tb, bank=bank: e.copy(out=a0[:, PADW + tb * 512:PADW + (tb + 1) * 512], in_=PS(bank)),
                          r=[("ps", bank)], w=["a0"])
                for blk in range(4):
                    s0 = PADW + blk * 1024
                    def lv(buf, lo, hi):
                        return buf[:, PADW + lo:PADW + 1024 + hi]
                    def a0v(lo, hi, s0=s0):
                        return a0[:, s0 + lo:s0 + 1024 + hi]
                    P.dma(lambda e, g=g, blk=blk: e.dma_start(
                        out=icnt, in_=c_invcnt[g:g + 1, blk * 1024:(blk + 1) * 1024].broadcast_to([128, 1024])),
                        w=["icnt"])
                    P.dve(lambda e, a0v=a0v, lv=lv: e.tensor_tensor(out=lv(t1, -8, 8), in0=a0v(-9, 7), in1=a0v(-8, 8), op=ALU.add),
                          r=["a0"], w=["t1"])
                    cur, curk, oth, othk = t1, "t1", t2, "t2"
                    ext = 8
                    for lev in range(1, g + 1):
                        sh = 1 << (lev - 1)
                        ne = ext - 2 * sh if lev < 3 else 0
                        ne = {1: 6, 2: 4, 3: 0}[lev]
                        P.dve(lambda e, cur=cur, oth=oth, sh=sh, ne=ne, lv=lv: e.tensor_tensor(
                            out=lv(oth, -ne, ne), in0=lv(cur, -ne - sh, ne - sh), in1=lv(cur, -ne + sh, ne + sh), op=ALU.add),
                            r=[curk], w=[othk])
                        cur, curk, oth, othk = oth, othk, cur, curk
                        ext = ne
                    P.dve(lambda e, cur=cur, oth=oth, lv=lv: e.tensor_tensor(out=lv(oth, 0, 0), in0=lv(cur, 0, 0), in1=icnt, op=ALU.mult),
                          r=[curk, "icnt"], w=[othk])
                    P.dve(lambda e, oth=oth, lv=lv, a0v=a0v: e.tensor_tensor(out=pm, in0=lv(oth, 0, 0), in1=a0v(0, 0), op=ALU.subtract),
                          r=[othk, "a0"], w=["pm"])
                    for hb in range(2):
                        bank = 2 + hb
                        P.pe(lambda e, g=g, hb=hb, bank=bank: e.matmul(PS(bank), lhsT=poolw[:, g, :], rhs=pm[:, hb * 512:(hb + 1) * 512],
                                                                      start=True, stop=True), r=["pm", "poolw"], w=[("ps", bank)])
                        c0 = blk * 1024 + hb * 512
                        P.act(lambda e, g=g, bank=bank, c0=c0: e.activation(out=catT3[:, g, c0:c0 + 512], in_=PS(bank), func=AF.Identity,
                                                                             scale=pscale[:, g:g + 1]),
                              r=[("ps", bank), "pscale"], w=[("catT", g)])
            P.barrier()
            A.release()
            ub = A.alloc(512, F32)
            dd = A.alloc(512, F32)
            d2 = A.alloc(512, F32)
            rs = A.alloc(512, F32)
            un = A.alloc(512, BF16)
            zs = [A.alloc(512, BF16) for _ in range(2)]
            for h in range(4):
                for tb in range(8):
                    def mm(e, h=h, tb=tb):
                        ins = None
                        for k in range(8):
                            ins = e.matmul(PS(0), lhsT=w_in[:, k, 512 + h * 128:512 + (h + 1) * 128],
                                           rhs=xT3[:, k, tb * 512:(tb + 1) * 512], start=(k == 0), stop=(k == 7))
                        return ins
                    P.pe(mm, r=["w_in"] + xT_all[tb * 4:tb * 4 + 4], w=[("ps", 0)])
                    P.act(lambda e: e.copy(out=ub, in_=PS(0)), r=[("ps", 0)], w=["ub"])
                    P.pe(lambda e: e.matmul(PS(1), lhsT=ones_m, rhs=ub, start=True, stop=True), r=["ub", "ones_m"], w=[("ps", 1)])
                    P.dve(lambda e: e.tensor_tensor(out=dd, in0=ub, in1=PS(1), op=ALU.subtract), r=["ub", ("ps", 1)], w=["dd"])
                    P.act(lambda e: e.activation(out=d2, in_=dd, func=AF.Square), r=["dd"], w=["d2"])
                    P.pe(lambda e: e.matmul(PS(1), lhsT=ones_m, rhs=d2, start=True, stop=True), r=["d2", "ones_m"], w=[("ps", 1)])
                    P.act(lambda e: e.activation(out=rs, in_=PS(1), func=AF.Ln, bias=EPS), r=[("ps", 1)], w=["rs"])
                    P.act(lambda e: e.activation(out=rs, in_=rs, func=AF.Exp, scale=-0.5), r=["rs"], w=["rs"])
                    P.dve(lambda e, h=h: e.scalar_tensor_tensor(out=un, in0=dd, scalar=lng[:, h:h + 1], in1=rs, op0=ALU.mult, op1=ALU.mult),
                          r=["dd", "rs", "lng"], w=["un"])
                    for pr in range(2):
                        bank = 2 + pr
                        def mz(e, pr=pr, bank=bank):
                            ins = None
                            for q in range(2):
                                tt = pr * 2 + q
                                ins = e.matmul(PS(bank)[:, q * 256:(q + 1) * 256], lhsT=un[:, tt * 128:(tt + 1) * 128], rhs=dc,
                                               start=True, stop=True)
                            return ins
                        P.pe(mz, r=["un", "dc"], w=[("ps", bank)])
                        cast_copy(zs[pr], PS(bank), [("ps", bank)], [("zs", pr)])
                        t0 = tb * 4 + pr * 2
                        P.dma(lambda e, h=h, t0=t0, pr=pr: e.dma_start(
                            out=Zscr[h, t0:t0 + 2].rearrange("t p c -> p t c"), in_=zs[pr].rearrange("p (t c) -> p t c", t=2)),
                            r=[("zs", pr)], w=["zscr"])
            P.barrier()
            A.release()
            A.mark()
            fw2 = A.alloc(4 * 128, BF16).rearrange("p (g n) -> p g n", g=4)
            P.dma(lambda e: e.dma_start(out=fw2, in_=pf_fw.rearrange("g c d -> c g d")), w=["fw2"], q="pool")
            zres = xT.rearrange("p (h t c) -> p h t c", h=4, t=NT)
            for h in range(4):
                P.dma(lambda e, h=h: e.dma_start(out=zres[:, h], in_=Zscr[h].rearrange("t p c -> p t c")), r=["zscr"], w=["zres"])
            NCH = 4
            dbuf = [[A.alloc(8 * 512, BF16).rearrange("p (s t) -> p s t", s=8) for _ in range(2)] for _ in range(2)]
            asb = [A.alloc(512, F32) for _ in range(4)]
            ypb = [A.alloc(512, BF16) for _ in range(4)]
            ymb = [A.alloc(512, BF16) for _ in range(4)]
            alt = A.alloc(NT, BF16)
            y2k = A.alloc(4, BF16)
            P.dma(lambda e: e.dma_start(out=alt, in_=c_alt), w=["alt"], q="pool")
            for h in range(4):
                def m2k(e, h=h):
                    ins = None
                    for stile in range(NT):
                        ins = e.matmul(PS(0)[:, h:h + 1], lhsT=zres[:, h, stile, 0:128], rhs=alt[:, stile:stile + 1],
                                       start=(stile == 0), stop=(stile == NT - 1))
                    return ins
                P.pe(m2k, r=["zres", "alt"], w=[("ps", 0)], c=2.5)
            P.dve(lambda e: e.tensor_copy(out=y2k, in_=PS(0)[:, 0:4]), r=[("ps", 0)], w=["y2k"])
            for h in range(4):
                P.pe(lambda e, h=h: e.matmul(PS(0)[:, 8 + h:9 + h], lhsT=fw2[:, h, :], rhs=y2k[:, h:h + 1], start=True, stop=True),
                     r=["y2k", "fw2"], w=[("ps", 0)], c=0.2)
            for h in range(4):
                P.dve(lambda e, h=h: e.tensor_copy(out=catT3[:, 4 + h, 2048:2049], in_=PS(0)[:, 8 + h:9 + h]), r=[("ps", 0)], w=[("catT", 4 + h)], c=0.1)
            cn = 0
            for kb in range(4):
                for sc in range(NCH):
                    bsel = cn % 2
                    cn += 1
                    for m, src in enumerate((c_cs, c_ss)):
                        P.dma(lambda e, m=m, src=src, sc=sc, kb=kb, bsel=bsel: e.dma_start(
                            out=dbuf[bsel][m], in_=src[sc * 1024:(sc + 1) * 1024, kb * 512:(kb + 1) * 512].rearrange("(s p) t -> p s t", p=128)),
                            w=[("dbuf", bsel, m)])
                    for h in range(4):
                        def mm(e, h=h, sc=sc, bsel=bsel):
                            ins = None
                            for s_ in range(8):
                                stile = sc * 8 + s_
                                first = (sc == 0 and s_ == 0)
                                last = (sc == NCH - 1 and s_ == 7)
                                e.matmul(PS(h), lhsT=zres[:, h, stile, 0:128], rhs=dbuf[bsel][0][:, s_, :], start=first, stop=last)
                                ins = e.matmul(PS(4 + h), lhsT=zres[:, h, stile, 128:256], rhs=dbuf[bsel][1][:, s_, :], start=first, stop=last)
                            return ins
                        P.pe(mm, r=["zres", ("dbuf", bsel, 0), ("dbuf", bsel, 1)], w=[("ps", h), ("ps", 4 + h)], c=3.6)
                for h in range(4):
                    P.act(lambda e, h=h: e.copy(out=asb[h], in_=PS(h)), r=[("ps", h)], w=[("asb", h)])
                    P.dve(lambda e, h=h: e.tensor_tensor(out=ypb[h], in0=asb[h], in1=PS(4 + h), op=ALU.add), r=[("asb", h), ("ps", 4 + h)], w=[("ypb", h)])
                    P.dve(lambda e, h=h: e.tensor_tensor(out=ymb[h], in0=asb[h], in1=PS(4 + h), op=ALU.subtract), r=[("asb", h), ("ps", 4 + h)], w=[("ymb", h)])
                    P.pe(lambda e, h=h: e.matmul(PS(h), lhsT=fw2[:, h, :], rhs=ypb[h], start=True, stop=True), r=[("ypb", h), "fw2"], w=[("ps", h)], c=0.3)
                    P.pe(lambda e, h=h: e.matmul(PS(4 + h), lhsT=fw2[:, h, :], rhs=ymb[h], start=True, stop=True), r=[("ymb", h), "fw2"], w=[("ps", 4 + h)], c=0.3)
                    P.act(lambda e, h=h, kb=kb: e.copy(out=catT3[:, 4 + h, kb * 512:(kb + 1) * 512], in_=PS(h)), r=[("ps", h)], w=[("catT", 4 + h)])
                    if kb == 0:
                        P.dve(lambda e, h=h: e.tensor_copy(out=catT3[:, 4 + h, 3585:4096][:, ::-1], in_=PS(4 + h)[:, 1:512]),
                              r=[("ps", 4 + h)], w=[("catT", 4 + h)])
                    else:
                        lo = S - kb * 512 - 511
                        P.dve(lambda e, h=h, lo=lo: e.tensor_copy(out=catT3[:, 4 + h, lo:lo + 512][:, ::-1], in_=PS(4 + h)),
                              r=[("ps", 4 + h)], w=[("catT", 4 + h)])
            P.barrier()
            A.release()
            if dbg:
                dbg_cat = nc.dram_tensor("dbg_cat", [D, S], BF16, kind="ExternalOutput").ap()
                P.dma(lambda e: e.dma_start(out=dbg_cat.rearrange("(k p) t -> p k t", p=128), in_=catT3), r=[("catT", k) for k in range(8)], w=["dbg_cat"])
            A.mark()
            w_out = A.alloc(8 * D, BF16).rearrange("p (k n) -> p k n", k=8)
            load_w_bf16(w_out, pf_w_out, 8, D, "w_out")
            load_gb(0, 0)
            E = Epi()
            for i in range(NT):
                for hh in range(2):
                    bank = (i % 2) * 2 + hh
                    def mm(e, i=i, hh=hh, bank=bank):
                        ins = None
                        for k in range(8):
                            ins = e.matmul(PS(bank), lhsT=catT3[:, k, i * 128:(i + 1) * 128], rhs=w_out[:, k, hh * 512:(hh + 1) * 512],
                                           start=(k == 0), stop=(k == 7))
                        return ins
                    P.pe(mm, r=["w_out"] + [("catT", k) for k in range(8)], w=[("ps", bank)])
                b0 = (i % 2) * 2
                epilogue(E, i, [PS(b0), PS(b0 + 1)], [("ps", b0), ("ps", b0 + 1)], cur_res, dst_res)
            P.barrier()
            A.release()
            A.release()


    def xa_phase(l, cur_res, dst_res):
        if True:
            A.mark()
            wq = A.alloc(8 * D, BF16).rearrange("p (k n) -> p k n", k=8)
            wo = A.alloc(8 * D, BF16).rearrange("p (k n) -> p k n", k=8)
            kT = A.alloc(8 * MEM, BF16).rearrange("p (k m) -> p k m", k=8)
            vtok = A.alloc(2 * D, BF16).rearrange("p (m n) -> p m n", m=2)
            A.mark()
            wkv = A.alloc(8 * 2 * D, BF16).rearrange("p (k n) -> p k n", k=8)
            memb = [A.alloc(D, BF16) for _ in range(2)]
            memT = A.alloc(8 * MEM, BF16).rearrange("p (k m) -> p k m", k=8)
            for j in range(2):
                P.dma(lambda e, j=j: e.dma_start(out=memb[j], in_=mem_d[j * 128:(j + 1) * 128, :]), w=[("memb", j)], q="pool")
                transpose_to(memT[:, :, j * 128:(j + 1) * 128], memb[j], 6 + j, [("memb", j)], ["memT"])
            for cb in range(2):
                for k0 in range(0, 8, 2):
                    P.dma(lambda e, cb=cb, k0=k0: e.dma_start(
                        out=wkv[:, k0:k0 + 2, cb * D:(cb + 1) * D],
                        in_=xa_wkv[l, k0 * 128:(k0 + 2) * 128, cb * D:(cb + 1) * D].rearrange("(k p) n -> p k n", p=128)),
                        w=["wkv"], q="pool")
            for ct in range(8):
                bank = ct % 2
                def mm(e, ct=ct, bank=bank):
                    ins = None
                    for k in range(8):
                        ins = e.matmul(PS(bank)[:, 0:MEM], lhsT=wkv[:, k, ct * 128:(ct + 1) * 128], rhs=memT[:, k, :],
                                       start=(k == 0), stop=(k == 7))
                    return ins
                P.pe(mm, r=["wkv", "memT"], w=[("ps", bank)])
                cast_copy(kT[:, ct, :], PS(bank)[:, 0:MEM], [("ps", bank)], ["kT"])
            for mt in range(2):
                for hb in range(2):
                    bank = 2 + hb
                    def mm(e, mt=mt, hb=hb, bank=bank):
                        ins = None
                        for k in range(8):
                            ins = e.matmul(PS(bank), lhsT=memT[:, k, mt * 128:(mt + 1) * 128],
                                           rhs=wkv[:, k, D + hb * 512:D + (hb + 1) * 512], start=(k == 0), stop=(k == 7))
                        return ins
                    P.pe(mm, r=["wkv", "memT"], w=[("ps", bank)])
                    cast_copy(vtok[:, mt, hb * 512:(hb + 1) * 512], PS(bank), [("ps", bank)], ["vtok"])
            P.nosched = False
            load_w_bf16(wq, xa_wq[l], 8, D, "wq")
            load_w_bf16(wo, xa_wo[l], 8, D, "wo")
            load_gb(l, 1)
            E = Epi()
            qT = A.alloc(8 * 512, BF16).rearrange("p (k t) -> p k t", k=8)
            oT = A.alloc(8 * 512, BF16).rearrange("p (k t) -> p k t", k=8)
            ex = A.alloc(MEM, F32)
            pb = A.alloc(MEM, BF16)
            pT = A.alloc(MEM, BF16).rearrange("p (m t) -> p m t", m=2)
            sm = A.alloc(8, F32)
            SCL = 1.0 / 16.0
            last_layer_xT = True
            for grp in range(DBG["xa_groups"]):
                for ct in range(8):
                    bank = 6
                    def mm(e, ct=ct, grp=grp, bank=bank):
                        ins = None
                        for k in range(8):
                            ins = e.matmul(PS(bank), lhsT=wq[:, k, ct * 128:(ct + 1) * 128], rhs=xT3[:, k, grp * 512:(grp + 1) * 512],
                                           start=(k == 0), stop=(k == 7))
                        return ins
                    P.pe(mm, r=["wq"] + xT_all[grp * 4:grp * 4 + 4], w=[("ps", bank)])
                    cast_copy(qT[:, ct, :], PS(bank), [("ps", bank)], [("qT", ct)])
                for tl in range(4):
                    i = grp * 4 + tl
                    tsl = slice(tl * 128, (tl + 1) * 128)
                    for h in range(4):
                        sc = PS(2 + h % 2)[:, 0:256]
                        ksc = ("ps", 2 + h % 2)
                        def ms(e, h=h, sc=sc, tsl=tsl):
                            ins = None
                            for dk in range(2):
                                ins = e.matmul(sc, lhsT=qT[:, 2 * h + dk, tsl], rhs=kT[:, 2 * h + dk, :], start=(dk == 0), stop=(dk == 1))
                            return ins
                        P.pe(ms, r=[("qT", 2 * h), ("qT", 2 * h + 1), "kT"], w=[ksc])
                        P.dve(lambda e, sc=sc: e.reduce_max(out=sm[:, 0:1], in_=sc, axis=AX.X), r=[ksc], w=["sm0"])
                        P.dve(lambda e: e.tensor_scalar(out=sm[:, 1:2], in0=sm[:, 0:1], scalar1=-SCL, scalar2=None, op0=ALU.mult), r=["sm0"], w=["sm1"])
                        P.act(lambda e, sc=sc: e.activation(out=ex, in_=sc, func=AF.Exp, bias=sm[:, 1:2], scale=SCL), r=[ksc, "sm1"], w=["ex"])
                        P.dve(lambda e: e.reduce_sum(out=sm[:, 2:3], in_=ex, axis=AX.X), r=["ex"], w=["sm2"])
                        P.dve(lambda e: e.reciprocal(out=sm[:, 3:4], in_=sm[:, 2:3]), r=["sm2"], w=["sm3"])
                        P.dve(lambda e: e.tensor_scalar(out=pb, in0=ex, scalar1=sm[:, 3:4], scalar2=None, op0=ALU.mult), r=["ex", "sm3"], w=["pb"])
                        ptq = PSB(4)[:, 0:256]
                        kpt = ("ps", 4)
                        def mt_(e, ptq=ptq):
                            ins = None
                            for mt in range(2):
                                ins = e.transpose(out=ptq[:, mt * 128:(mt + 1) * 128], in_=pb[:, mt * 128:(mt + 1) * 128], identity=ident)
                            return ins
                        P.pe(mt_, r=["pb", "ident"], w=[kpt])
                        cast_copy(pT, ptq.rearrange("p (m t) -> p m t", m=2), [kpt], ["pT"])
                        pv = PS((5, 7)[h % 2])[:, 0:256]
                        kpv = ("ps", (5, 7)[h % 2])
                        def mpv(e, h=h, pv=pv):
                            ins = None
                            for dv in range(2):
                                for mt in range(2):
                                    ins = e.matmul(pv[:, dv * 128:(dv + 1) * 128], lhsT=vtok[:, mt, h * 256 + dv * 128:h * 256 + (dv + 1) * 128],
                                                   rhs=pT[:, mt, :], start=(mt == 0), stop=(mt == 1))
                            return ins
                        P.pe(mpv, r=["pT", "vtok"], w=[kpv])
                        cast_copy(oT[:, 2 * h:2 * h + 2, tsl], pv.rearrange("p (d t) -> p d t", d=2), [kpv], [("oT", tl)])
                    for hh in range(2):
                        def mm(e, hh=hh, tsl=tsl):
                            ins = None
                            for k in range(8):
                                ins = e.matmul(PS(hh), lhsT=oT[:, k, tsl], rhs=wo[:, k, hh * 512:(hh + 1) * 512], start=(k == 0), stop=(k == 7))
                            return ins
                        P.pe(mm, r=["wo", ("oT", tl)], w=[("ps", hh)])
                    epilogue(E, i, [PS(0), PS(1)], [("ps", 0), ("ps", 1)], cur_res, dst_res, make_xT=False)
            P.nosched = False
            P.barrier()
            A.release()
            A.release()

    def moe_phase(l, cur_res, dst_res, want_xT):
        if True:
            A.mark()
            idx = A.alloc(NT * 2, I32 if False else F32).bitcast(I32).rearrange("p (t k) -> p t k", k=2)
            gates = A.alloc(NT * 2, F32).rearrange("p (t k) -> p t k", k=2)
            A.mark()
            wr32 = A.alloc(8 * 36, F32).rearrange("p (k n) -> p k n", k=8)
            P.dma(ncd(lambda e: e.dma_start(out=wr32[:, :, 0:4], in_=moe_wg[l].rearrange("(k p) n -> p k n", p=128))), w=["wr32"])
            P.dma(ncd(lambda e: e.dma_start(out=wr32[:, :, 4:36], in_=moe_we[l].rearrange("(k p) n -> p k n", p=128))), w=["wr32"])
            bias_bc = A.alloc(36, F32)
            P.dma(lambda e: e.dma_start(out=bias_bc[:, 0:4], in_=moe_bg[l:l + 1, :].broadcast_to([128, 4])), w=["bias_bc"])
            P.dma(lambda e: e.dma_start(out=bias_bc[:, 4:36], in_=moe_be[l:l + 1, :].broadcast_to([128, 32])), w=["bias_bc"])
            eoff = A.alloc(32, F32)
            P.dma(lambda e: e.dma_start(out=eoff, in_=c_eoff.broadcast_to([128, 32])), w=["eoff"])
            ustr = A.alloc(128, BF16)
            onesb = A.alloc(128, BF16)
            P.dma(lambda e: e.dma_start(out=ustr, in_=c_masks[2]), w=["ustr"], q="pool")
            P.dma(lambda e: e.dma_start(out=onesb, in_=c_masks[3]), w=["onesb"], q="pool")
            trash = A.alloc(1, F32)
            P.dma(ncd(lambda e: e.dma_start(out=trash, in_=c_trash)), w=["trash"])
            carry = A.alloc(32, F32)
            P.dve(lambda e: e.memset(carry, 0.0), w=["carry"])
            if DBG.get("zero_xs"):
                zt = A.alloc(D, BF16)
                P.pool(lambda e: e.memset(zt, 0.0), w=["zt"])
                for r0 in range(0, NEXP * CAP + 128, 128):
                    P.dma(lambda e, r0=r0: e.dma_start(out=XS[r0:r0 + 128, :], in_=zt), r=["zt"], w=["XS"])
            x2 = [A.alloc(D, F32) for _ in range(2)]
            xb2 = [A.alloc(D, BF16) for _ in range(2)]
            RB = []
            for _p in range(2):
                rb = {}
                rb["x2T"] = A.alloc(8 * 128, F32).rearrange("p (k t) -> p k t", k=8)
                for nm, n_ in (("lg", 36), ("rt", 64), ("maskg", 4), ("esel", 8), ("e2", 8), ("mask1", 8), ("mask2", 8),
                               ("M1", 32), ("M2", 32), ("Ms", 32), ("pos", 32), ("tmp", 32)):
                    rb[nm] = A.alloc(n_, F32)
                rb["Mb"] = A.alloc(32, BF16)
                RB.append(rb)
            def route_tile(i):
                b = i % 2
                rbp = b if DBG.get("route_double", 0) else 0
                rb = RB[rbp]
                x2T, lg, rt, maskg, esel, e2, mask1, mask2 = rb['x2T'], rb['lg'], rb['rt'], rb['maskg'], rb['esel'], rb['e2'], rb['mask1'], rb['mask2']
                M1, M2, Ms, Mb, pos, tmp = rb['M1'], rb['M2'], rb['Ms'], rb['Mb'], rb['pos'], rb['tmp']
                M1v = M1.rearrange('p (g j) -> p g j', g=4)
                M2v = M2.rearrange('p (g j) -> p g j', g=4)
                pb_ = 4 * rbp
                kx2, kxb = ("x2", b), ("xb2", b)
                P.dma(lambda e, i=i, b=b: e.dma_start(out=x2[b], in_=cur_res[i * 128:(i + 1) * 128, :]), w=[kx2])
                P.act(lambda e, b=b: e.copy(out=xb2[b], in_=x2[b]), r=[kx2], w=[kxb])
                for half in range(2):
                    def tr(e, half=half, b=b):
                        ins = None
                        for q in range(4):
                            k = half * 4 + q
                            ins = e.transpose(out=PS(pb_ + half)[:, q * 128:(q + 1) * 128], in_=x2[b][:, k * 128:(k + 1) * 128], identity=ident32)
                        return ins
                    P.pe(tr, r=[kx2, "ident32"], w=[("ps", pb_ + half)])
                    cast_copy(x2T[:, half * 4:(half + 1) * 4, :], PS(pb_ + half).rearrange("p (k t) -> p k t", k=4), [("ps", pb_ + half)], [("x2T", rbp)])
                def mlg(e):
                    ins = None
                    for k in range(8):
                        ins = e.matmul(PS(pb_ + 2)[:, 0:36], lhsT=x2T[:, k, :], rhs=wr32[:, k, :], start=(k == 0), stop=(k == 7))
                    return ins
                P.pe(mlg, r=[("x2T", rbp), "wr32"], w=[("ps", pb_ + 2)])
                V = P.dve
                V(lambda e: e.tensor_tensor(out=lg, in0=PS(pb_ + 2)[:, 0:36], in1=bias_bc, op=ALU.add), r=[("ps", pb_ + 2), "bias_bc"], w=[("lg", rbp)])
                V(lambda e: e.reduce_max(out=rt[:, 0:1], in_=lg[:, 0:4], axis=AX.X), r=[("lg", rbp)], w=[("rt0", rbp)])
                V(lambda e: e.tensor_scalar(out=maskg, in0=lg[:, 0:4], scalar1=rt[:, 0:1], scalar2=None, op0=ALU.is_equal), r=[("lg", rbp), ("rt0", rbp)], w=[("maskg", rbp)])
                V(lambda e: e.tensor_scalar(out=rt[:, 1:2], in0=rt[:, 0:1], scalar1=-1.0, scalar2=None, op0=ALU.mult), r=[("rt0", rbp)], w=[("rt1", rbp)])
                P.act(lambda e: e.activation(out=rt[:, 4:8], in_=lg[:, 0:4], func=AF.Exp, bias=rt[:, 1:2], scale=1.0), r=[("lg", rbp), ("rt1", rbp)], w=[("rt4", rbp)])
                V(lambda e: e.reduce_sum(out=rt[:, 2:3], in_=rt[:, 4:8], axis=AX.X), r=[("rt4", rbp)], w=[("rt2", rbp)])
                V(lambda e: e.reciprocal(out=rt[:, 3:4], in_=rt[:, 2:3]), r=[("rt2", rbp)], w=[("rt3", rbp)])
                V(lambda e: e.tensor_scalar(out=esel, in0=lg[:, 4:12], scalar1=maskg[:, 0:1], scalar2=None, op0=ALU.mult), r=[("lg", rbp), ("maskg", rbp)], w=[("esel", rbp)])
                for g in range(1, 4):
                    V(lambda e, g=g: e.scalar_tensor_tensor(out=esel, in0=lg[:, 4 + 8 * g:12 + 8 * g], scalar=maskg[:, g:g + 1], in1=esel,
                                                            op0=ALU.mult, op1=ALU.add), r=[("lg", rbp), ("maskg", rbp), ("esel", rbp)], w=[("esel", rbp)])
                V(lambda e: e.reduce_max(out=rt[:, 8:9], in_=esel, axis=AX.X), r=[("esel", rbp)], w=[("rt8", rbp)])
                V(lambda e: e.tensor_scalar(out=mask1, in0=esel, scalar1=rt[:, 8:9], scalar2=None, op0=ALU.is_equal), r=[("esel", rbp), ("rt8", rbp)], w=[("mask1", rbp)])
                V(lambda e: e.scalar_tensor_tensor(out=e2, in0=mask1, scalar=-1e30, in1=esel, op0=ALU.mult, op1=ALU.add), r=[("mask1", rbp), ("esel", rbp)], w=[("e2", rbp)])
                V(lambda e: e.reduce_max(out=rt[:, 9:10], in_=e2, axis=AX.X), r=[("e2", rbp)], w=[("rt9", rbp)])
                V(lambda e: e.tensor_scalar(out=mask2, in0=e2, scalar1=rt[:, 9:10], scalar2=None, op0=ALU.is_equal), r=[("e2", rbp), ("rt9", rbp)], w=[("mask2", rbp)])
                V(lambda e: e.tensor_tensor(out=rt[:, 10:11], in0=rt[:, 9:10], in1=rt[:, 8:9], op=ALU.subtract), r=[("rt8", rbp), ("rt9", rbp)], w=[("rt10", rbp)])
                P.act(lambda e: e.activation(out=rt[:, 11:12], in_=rt[:, 10:11], func=AF.Exp), r=[("rt10", rbp)], w=[("rt11", rbp)])
                V(lambda e: e.tensor_scalar(out=rt[:, 12:13], in0=rt[:, 11:12], scalar1=1.0, scalar2=None, op0=ALU.add), r=[("rt11", rbp)], w=[("rt12", rbp)])
                V(lambda e: e.reciprocal(out=rt[:, 13:14], in_=rt[:, 12:13]), r=[("rt12", rbp)], w=[("rt13", rbp)])
                V(lambda e, i=i: e.tensor_tensor(out=gates[:, i, 0:1], in0=rt[:, 3:4], in1=rt[:, 13:14], op=ALU.mult), r=[("rt3", rbp), ("rt13", rbp)], w=["gates"])
                V(lambda e, i=i: e.tensor_tensor(out=gates[:, i, 1:2], in0=gates[:, i, 0:1], in1=rt[:, 11:12], op=ALU.mult), r=["gates", ("rt11", rbp)], w=["gates"])
                for g in range(4):
                    V(lambda e, g=g: e.tensor_scalar(out=M1v[:, g, :], in0=mask1, scalar1=maskg[:, g:g + 1], scalar2=None, op0=ALU.mult),
                      r=[("mask1", rbp), ("maskg", rbp)], w=[("M1", rbp)])
                    V(lambda e, g=g: e.tensor_scalar(out=M2v[:, g, :], in0=mask2, scalar1=maskg[:, g:g + 1], scalar2=None, op0=ALU.mult),
                      r=[("mask2", rbp), ("maskg", rbp)], w=[("M2", rbp)])
                V(lambda e: e.tensor_tensor(out=Ms, in0=M1, in1=M2, op=ALU.add), r=[("M1", rbp), ("M2", rbp)], w=[("Ms", rbp)])
                V(lambda e: e.tensor_copy(out=Mb, in_=Ms), r=[("Ms", rbp)], w=[("Mb", rbp)])
                def mpos(e):
                    e.matmul(PS(pb_ + 3)[:, 0:32], lhsT=ustr, rhs=Mb, start=True, stop=True)
                    return e.matmul(PS(pb_ + 3)[:, 32:64], lhsT=onesb, rhs=Mb, start=True, stop=True)
                P.pe(mpos, r=[("Mb", rbp), "ustr", "onesb"], w=[("ps", pb_ + 3)])
                V(lambda e: e.tensor_tensor(out=pos, in0=PS(pb_ + 3)[:, 0:32], in1=carry, op=ALU.add), r=[("ps", pb_ + 3), "carry"], w=[("pos", rbp)])
                V(lambda e: e.tensor_tensor(out=carry, in0=PS(pb_ + 3)[:, 32:64], in1=carry, op=ALU.add), r=[("ps", pb_ + 3), "carry"], w=["carry"])
                V(lambda e: e.tensor_scalar(out=tmp, in0=pos, scalar1=float(CAP), scalar2=None, op0=ALU.is_ge), r=[("pos", rbp)], w=[("tmp", rbp)])
                V(lambda e: e.tensor_tensor(out=pos, in0=pos, in1=eoff, op=ALU.add), r=[("pos", rbp), "eoff"], w=[("pos", rbp)])
                V(lambda e: e.tensor_scalar(out=Ms, in0=pos, scalar1=-1.0, scalar2=trash[:, 0:1], op0=ALU.mult, op1=ALU.add), r=[("pos", rbp), "trash"], w=[("Ms", rbp)])
                V(lambda e: e.tensor_tensor(out=tmp, in0=tmp, in1=Ms, op=ALU.mult), r=[("tmp", rbp), ("Ms", rbp)], w=[("tmp", rbp)])
                V(lambda e: e.tensor_tensor(out=pos, in0=pos, in1=tmp, op=ALU.add), r=[("pos", rbp), ("tmp", rbp)], w=[("pos", rbp)])
                for kk, Mk in enumerate((M1, M2)):
                    kM = ("M1", rbp) if kk == 0 else ("M2", rbp)
                    V(lambda e, Mk=Mk: e.tensor_tensor(out=tmp, in0=Mk, in1=pos, op=ALU.mult), r=[kM, ("pos", rbp)], w=[("tmp", rbp)])
                    V(lambda e, kk=kk: e.reduce_sum(out=rt[:, 16 + kk:17 + kk], in_=tmp, axis=AX.X), r=[("tmp", rbp)], w=[("rtidx", kk, rbp)])
                    V(lambda e, kk=kk, i=i: e.tensor_copy(out=idx[:, i, kk:kk + 1], in_=rt[:, 16 + kk:17 + kk]), r=[("rtidx", kk, rbp)], w=[("idx", i, kk)])
                    P.dma(lambda e, kk=kk, i=i, b=b: e.indirect_dma_start(
                        out=XS, out_offset=bass.IndirectOffsetOnAxis(idx[:, i, kk:kk + 1], 0), in_=xb2[b], in_offset=None), r=[("idx", i, kk), kxb], w=["XS"], q="pool")
            for i in range(NT):
                route_tile(i)
            P.barrier()
            A.release()
            A.mark()
            NCT = CAP // 128
            xT_f32 = xT.bitcast(F32)
            stg = [xT_f32[:, k * 2048:(k + 1) * 2048] for k in range(6)]
            wgb = [A.alloc(8 * 512, BF16).rearrange("p (k n) -> p k n", k=8) for _ in range(2)]
            wub = [A.alloc(8 * 512, BF16).rearrange("p (k n) -> p k n", k=8) for _ in range(2)]
            wdb = [A.alloc(4 * D, BF16).rearrange("p (k n) -> p k n", k=4) for _ in range(2)]
            xsb = [A.alloc(D, BF16) for _ in range(4)]
            xsT2 = [A.alloc(8 * CAP, BF16).rearrange("p (k t) -> p k t", k=8) for _ in range(2)]
            hT2 = [A.alloc(4 * CAP, BF16).rearrange("p (k t) -> p k t", k=4) for _ in range(2)]
            sg2 = [A.alloc(CAP, F32) for _ in range(2)]
            ysb = [A.alloc(D, F32) for _ in range(4)]
            sn = 0
            for ex_i in range(NEXP):
                b = ex_i % 2
                xsT, hT, sg = xsT2[b], hT2[b], sg2[b]
                kxsT, khT, ksg = ("xsT", b), ("hT", b), ("sg", b)
                chunks = []
                for (wsrc, dstb, nm) in ((moe_gate, wgb, "wg"), (moe_up, wub, "wu")):
                    for c in range(2):
                        chunks.append((wsrc[l, ex_i, c * 512:(c + 1) * 512, :].rearrange("(k p) n -> p k n", p=128),
                                       dstb[b][:, c * 4:(c + 1) * 4, :], (nm, b), 4))
                for c in range(2):
                    chunks.append((moe_down[l, ex_i, c * 256:(c + 1) * 256, :].rearrange("(k p) n -> p k n", p=128),
                                   wdb[b][:, c * 2:(c + 1) * 2, :], ("wd", b), 2))
                for (src, dst, key, kk) in chunks:
                    sb_ = sn % 6
                    sn += 1
                    sview = stg[sb_].rearrange("p (k n) -> p k n", k=kk)
                    P.dma(lambda e, src=src, sview=sview: e.dma_start(out=sview, in_=src), w=[("stg", sb_)], q="pool")
                    if sn % 2 == 0:
                        P.act(lambda e, dst=dst, sview=sview: e.copy(out=dst, in_=sview), r=[("stg", sb_)], w=[key], c=2.0)
                    else:
                        P.dve(lambda e, dst=dst, sview=sview: e.tensor_copy(out=dst, in_=sview), r=[("stg", sb_)], w=[key], c=2.3)
                for stl in range(NCT):
                    xb_ = (ex_i * NCT + stl) % 4
                    r0 = ex_i * CAP + stl * 128
                    P.dma(lambda e, r0=r0, xb_=xb_: e.dma_start(out=xsb[xb_], in_=XS[r0:r0 + 128, :]), r=["XS"], w=[("xsb", xb_)], q="pool")
                    transpose_to(xsT[:, :, stl * 128:(stl + 1) * 128], xsb[xb_], 7, [("xsb", xb_)], [kxsT])
                for ft in range(4):
                    for m, wb_, kw in ((0, wgb, "wg"), (1, wub, "wu")):
                        def mm(e, ft=ft, m=m, wb_=wb_, b=b, xsT=xsT):
                            ins = None
                            for k in range(8):
                                ins = e.matmul(PS(m)[:, 0:CAP], lhsT=wb_[b][:, k, ft * 128:(ft + 1) * 128], rhs=xsT[:, k, :],
                                               start=(k == 0), stop=(k == 7))
                            return ins
                        P.pe(mm, r=[(kw, b), kxsT], w=[("ps", m)])
                    P.act(lambda e, sg=sg: e.activation(out=sg, in_=PS(0)[:, 0:CAP], func=AF.Silu), r=[("ps", 0)], w=[ksg])
                    P.dve(lambda e, ft=ft, sg=sg, hT=hT: e.tensor_tensor(out=hT[:, ft, :], in0=sg, in1=PS(1)[:, 0:CAP], op=ALU.mult), r=[ksg, ("ps", 1)], w=[khT])
                for stl in range(NCT):
                    yb_ = stl % 4
                    for hh in range(2):
                        bank = 2 + (stl % 2) * 2 + hh
                        def mm(e, stl=stl, hh=hh, bank=bank, b=b, hT=hT):
                            ins = None
                            for ft in range(4):
                                ins = e.matmul(PS(bank), lhsT=hT[:, ft, stl * 128:(stl + 1) * 128], rhs=wdb[b][:, ft, hh * 512:(hh + 1) * 512],
                                               start=(ft == 0), stop=(ft == 3))
                            return ins
                        P.pe(mm, r=[khT, ("wd", b)], w=[("ps", bank)])
                        if hh == 0:
                            P.act(lambda e, bank=bank, yb_=yb_: e.copy(out=ysb[yb_][:, 0:512], in_=PS(bank)), r=[("ps", bank)], w=[("ysb", yb_)])
                        else:
                            P.dve(lambda e, bank=bank, yb_=yb_: e.tensor_copy(out=ysb[yb_][:, 512:1024], in_=PS(bank)), r=[("ps", bank)], w=[("ysb", yb_)])
                    r0 = ex_i * CAP + stl * 128
                    P.dma(lambda e, r0=r0, yb_=yb_: e.dma_start(out=YS[r0:r0 + 128, :], in_=ysb[yb_]), r=[("ysb", yb_)], w=["YS"])
            P.barrier()
            A.release()
            A.mark()
            load_gb(l, 2)
            E = Epi()
            y1 = [A.alloc(D, F32) for _ in range(2)]
            y2 = [A.alloc(D, F32) for _ in range(2)]
            hm = [A.alloc(D, F32) for _ in range(2)]
            dst = dst_res
            def gather(i):
                b = i % 2
                P.dma(lambda e, i=i, b=b: e.indirect_dma_start(
                    out=y1[b], out_offset=None, in_=YS, in_offset=bass.IndirectOffsetOnAxis(idx[:, i, 0:1], 0)), r=["YS"], w=[("y1", b)], q="pool")
                P.dma(lambda e, i=i, b=b: e.indirect_dma_start(
                    out=y2[b], out_offset=None, in_=YS, in_offset=bass.IndirectOffsetOnAxis(idx[:, i, 1:2], 0)), r=["YS"], w=[("y2", b)], q="pool")
            gather(0)
            gather(1)
            for i in range(NT):
                b = i % 2
                P.dve(lambda e, i=i, b=b: e.tensor_scalar(out=hm[b], in0=y1[b], scalar1=gates[:, i, 0:1], scalar2=None, op0=ALU.mult),
                      r=[("y1", b)], w=[("hm", b)])
                P.dve(lambda e, i=i, b=b: e.scalar_tensor_tensor(out=hm[b], in0=y2[b], scalar=gates[:, i, 1:2], in1=hm[b], op0=ALU.mult, op1=ALU.add),
                      r=[("y2", b), ("hm", b)], w=[("hm", b)])
                if i + 2 < NT:
                    gather(i + 2)
                epilogue(E, i, [hm[b][:, 0:512], hm[b][:, 512:1024]], [("hm", b)], cur_res, dst, make_xT=want_xT, gmul="dve")
            P.barrier()
            A.release()
            A.release()


    def hg_phase(cur_res, dst_res):
        A.mark()
        A.mark()
        P.nosched = bool(DBG.get("hg_nosched", 0))
        lbraw = A.alloc(32, F32)
        lbt = A.alloc(32, F32)
        lbrows = A.alloc(128, F32)
        P.dma(lambda e: e.dma_start(out=lbrows[0:32, :], in_=hg_lb.rearrange("l d (h k) -> (l d h) k", k=128)), w=["lbrows"])
        P.pe(lambda e: e.transpose(out=PS(0)[:, 0:32], in_=lbrows[0:32, :], identity=ident32[0:32, 0:32]), r=["lbrows", "ident32"], w=[("ps", 0)], c=0.3)
        P.dve(lambda e: e.tensor_copy(out=lbraw, in_=PS(0)[:, 0:32]), r=[("ps", 0)], w=["lbraw"])
        P.dve(lambda e: e.tensor_tensor(out=lbt[:, 0:16], in0=lbraw[:, 16:32], in1=lbraw[:, 0:16], op=ALU.subtract), r=["lbraw"], w=["lbd"])
        P.act(lambda e: e.activation(out=lbt[:, 0:16], in_=lbt[:, 0:16], func=AF.Sigmoid), r=["lbd"], w=["lb"])
        P.dve(lambda e: e.tensor_scalar(out=lbt[:, 16:32], in0=lbt[:, 0:16], scalar1=-1.0, scalar2=1.0, op0=ALU.mult, op1=ALU.add), r=["lb"], w=["oml"])
        ng = A.alloc(1, F32)
        P.dma(ncd(lambda e: e.dma_start(out=ng, in_=hg_norm_g.rearrange("(k o) -> k o", o=1))), w=["ng"])
        cmask = A.alloc(512, F32)
        P.pool(lambda e: e.memset(cmask, 1.0), w=["cmask"])
        P.pool(lambda e: e.memset(cmask.rearrange("p (c l) -> p c l", l=64)[:, :, 0:1], 0.0), r=["cmask"], w=["cmask"])
        mk = [A.alloc(128, F32) for _ in range(2)]
        for d in range(2):
            P.dma(lambda e, d=d: e.dma_start(out=mk[d], in_=c_masks[d]), w=[("mk", d)])
        Sst = [[A.alloc(128, F32) for _ in range(2)] for _ in range(2)]
        s_cur = [0, 0]
        wh = [A.alloc(8 * 5 * 128, BF16).rearrange("p (k q n) -> p k q n", k=8, q=5) for _ in range(2)]
        rst = A.alloc(4 * 512, F32)
        vtok = A.alloc(NT * 128, BF16).rearrange("p (t n) -> p t n", t=NT)
        qT = A.alloc(S, F32)
        oTh = A.alloc(S, F32)
        T = []
        for d in range(2):
            t = {}
            for nm in ("sgm", "lgf", "bb", "kk", "t1", "t2"):
                t[nm] = A.alloc(512, F32)
            t["kbT"] = A.alloc(512, BF16)
            for nm in ("qt", "kt", "qb"):
                t[nm] = [A.alloc(512, BF16) for _ in range(2)]
            t["kbtok"] = [A.alloc(512, BF16).rearrange("p (t n) -> p t n", t=4) for _ in range(2)]
            t["dec"] = [A.alloc(8, F32) for _ in range(2)]
            t["sTm"] = [A.alloc(128, BF16) for _ in range(2)]
            t["Sp"] = [[A.alloc(128, BF16) for _ in range(2)] for _ in range(2)]
            T.append(t)
        o2 = A.alloc(512, F32)
        rsn = A.alloc(512, F32)
        sgt = A.alloc(512, F32)
        onb = [A.alloc(512, BF16) for _ in range(2)]

        def K_(d, nm):
            return ("hg", d, nm)

        def v3(ap):
            return ap.rearrange("p (c l) -> p c l", l=64)

        def prep(h, d, blk, buf, wb, kwb, part="ab"):
            t = T[d]
            t0 = blk * 512
            col = d * 8 + h
            fb = (0, 6)[d]
            qt_, kt_, qb_, kbtok_, dec_ = t["qt"][buf], t["kt"][buf], t["qb"][buf], t["kbtok"][buf], t["dec"][buf]
            def mm(e):
                ins = None
                for k in range(8):
                    ins = e.matmul(PS(fb), lhsT=wb[:, k, 2 + d, :], rhs=xT3[:, k, t0:t0 + 512], start=(k == 0), stop=(k == 7))
                return ins
            if "a" in part:
                P.pe(mm, r=[kwb] + xT_all[blk * 4:blk * 4 + 4], w=[("ps", fb)])
                P.act(lambda e: e.activation(out=t["sgm"], in_=PS(fb), func=AF.Sigmoid), r=[("ps", fb)], w=[K_(d, "sgm"), "sigdone"])
                P.dve(lambda e: e.tensor_scalar(out=t["sgm"], in0=t["sgm"], scalar1=lbt[:, 16 + col:17 + col], scalar2=lbt[:, col:col + 1],
                                                op0=ALU.mult, op1=ALU.add), r=[K_(d, "sgm"), "lb", "oml"], w=[K_(d, "sgm")])
            if "a" in part and ((blk < 4) if d == 0 else (blk >= 4)):
                qblk_, vblk_ = qT[:, t0:t0 + 512], vtok[:, blk * 4:(blk + 1) * 4, :]
                def mq(e):
                    ins = None
                    for k in range(8):
                        ins = e.matmul(PS(fb), lhsT=wb[:, k, 0, :], rhs=xT3[:, k, t0:t0 + 512], start=(k == 0), stop=(k == 7))
                    return ins
                P.pe(mq, r=[kwb] + xT_all[blk * 4:blk * 4 + 4], w=[("ps", fb)])
                P.act(lambda e: e.activation(out=qblk_, in_=PS(fb), func=AF.Sigmoid), r=[("ps", fb)], w=[("qT", blk), "sigdone"])
                P.dve(lambda e: e.tensor_tensor(out=qblk_, in0=qblk_, in1=PS(fb), op=ALU.mult), r=[("qT", blk), ("ps", fb)], w=[("qT", blk)])
                def mv(e):
                    ins = None
                    for tl in range(4):
                        tile = blk * 4 + tl
                        for k in range(8):
                            ins = e.matmul(PS(fb)[:, tl * 128:(tl + 1) * 128], lhsT=xT3[:, k, tile * 128:(tile + 1) * 128], rhs=wb[:, k, 1, :],
                                           start=(k == 0), stop=(k == 7))
                    return ins
                P.pe(mv, r=[kwb] + xT_all[blk * 4:blk * 4 + 4], w=[("ps", fb)], c=2.5)
                cast_copy(vblk_, PS(fb).rearrange("p (t n) -> p t n", t=4), [("ps", fb)], [("vtok", blk)])
            if "b" not in part:
                return
            P.act(lambda e: e.activation(out=t["lgf"], in_=t["sgm"], func=AF.Ln), r=[K_(d, "sgm"), "sigdone"], w=[K_(d, "lgf")])
            P.pool(lambda e: e.tensor_scalar(out=t["kk"], in0=t["sgm"], scalar1=-1.0, scalar2=1.0, op0=ALU.mult, op1=ALU.add),
                   r=[K_(d, "sgm")], w=[K_(d, "kk")])
            if d == 0:
                P.dve(lambda e: e.tensor_tensor_scan(out=t["bb"], data0=cmask, data1=t["lgf"], initial=0.0, op0=ALU.mult, op1=ALU.add),
                      r=["cmask", K_(d, "lgf")], w=[K_(d, "bb")])
                iref, ilast = 32, 63
            else:
                P.dve(lambda e: e.tensor_tensor_scan(out=t["t2"], data0=cmask, data1=t["lgf"], initial=0.0, op0=ALU.mult, op1=ALU.add),
                      r=["cmask", K_(d, "lgf")], w=[K_(d, "t2")])
                P.dve(lambda e: e.scalar_tensor_tensor(out=t["t1"], in0=t["t2"], scalar=-1.0, in1=t["lgf"], op0=ALU.mult, op1=ALU.add),
                      r=[K_(d, "t2"), K_(d, "lgf")], w=[K_(d, "t1")])
                P.dve(lambda e: e.tensor_tensor(out=v3(t["bb"]), in0=v3(t["t1"]), in1=v3(t["t2"])[:, :, 63:64].broadcast_to([128, 8, 64]), op=ALU.add),
                      r=[K_(d, "t1"), K_(d, "t2")], w=[K_(d, "bb")])
                iref, ilast = 31, 0
            bref = v3(t["bb"])[:, :, iref:iref + 1].broadcast_to([128, 8, 64])
            blast = v3(t["bb"])[:, :, ilast:ilast + 1].broadcast_to([128, 8, 64])
            qsl = qT[:, t0:t0 + 512]
            kqb = ("qT", blk)
            kq, kk_, kb_, kkb, kd = K_(d, ("qt", buf)), K_(d, ("kt", buf)), K_(d, ("qb", buf)), K_(d, ("kbtok", buf)), K_(d, ("dec", buf))
            P.pool(lambda e: e.tensor_tensor(out=v3(t["t1"]), in0=v3(t["bb"]), in1=bref, op=ALU.subtract), r=[K_(d, "bb")], w=[K_(d, "t1")])
            P.act(lambda e: e.activation(out=t["t2"], in_=t["t1"], func=AF.Exp), r=[K_(d, "t1")], w=[K_(d, "t2")])
            P.dve(lambda e: e.tensor_tensor(out=qt_, in0=qsl, in1=t["t2"], op=ALU.mult), r=[kqb, K_(d, "t2")], w=[kq])
            P.act(lambda e: e.activation(out=t["t2"], in_=t["t1"], func=AF.Exp, scale=-1.0), r=[K_(d, "t1"), kq], w=[K_(d, "t2")])
            P.pool(lambda e: e.tensor_tensor(out=kt_, in0=t["kk"], in1=t["t2"], op=ALU.mult), r=[K_(d, "kk"), K_(d, "t2")], w=[kk_])
            P.act(lambda e: e.activation(out=t["sgm"], in_=t["bb"], func=AF.Exp), r=[K_(d, "bb")], w=[K_(d, "sgm")])
            P.dve(lambda e: e.tensor_tensor(out=qb_, in0=qsl, in1=t["sgm"], op=ALU.mult), r=[kqb, K_(d, "sgm")], w=[kb_])
            P.pool(lambda e: e.tensor_tensor(out=v3(t["t1"]), in0=v3(t["bb"]), in1=blast, op=ALU.subtract), r=[K_(d, "bb"), K_(d, "t2")], w=[K_(d, "t1")])
            P.act(lambda e: e.activation(out=t["t2"], in_=t["t1"], func=AF.Exp, scale=-1.0), r=[K_(d, "t1"), kk_], w=[K_(d, "t2")])
            P.pool(lambda e: e.tensor_tensor(out=t["kbT"], in0=t["kk"], in1=t["t2"], op=ALU.mult), r=[K_(d, "kk"), K_(d, "t2")], w=[K_(d, "kbT")])
            P.act(lambda e: e.activation(out=dec_, in_=v3(t["bb"])[:, :, ilast], func=AF.Exp), r=[K_(d, "bb")], w=[kd])
            def tr(e):
                ins = None
                for tl in range(4):
                    ins = e.transpose(out=PSB(5)[:, d * 512 + tl * 128:d * 512 + (tl + 1) * 128], in_=t["kbT"][:, tl * 128:(tl + 1) * 128], identity=ident)
                return ins
            P.pe(tr, r=[K_(d, "kbT"), "ident"], w=[("ps", 5)])
            cast_copy(kbtok_, PSB(5)[:, d * 512:(d + 1) * 512].rearrange("p (t n) -> p t n", t=4), [("ps", 5)], [kkb])

        def tile_info(idx, d):
            step, ts = idx // 4, idx % 4
            blk = step if d == 0 else 7 - step
            tl = ts if d == 0 else 3 - ts
            return blk, tl, blk * 4 + tl, step % 2, idx % 2

        def front(idx):
            for d in range(2):
                t = T[d]
                blk, tl, tile, buf, par = tile_info(idx, d)
                tsl = slice(tl * 128, (tl + 1) * 128)
                sT = PS(2)[:, (d * 2 + par) * 128:(d * 2 + par + 1) * 128]
                Ub = PS(3 + par)[:, d * 256:(d + 1) * 256]
                order = (0, 1) if d == 0 else (1, 0)
                def mf(e, t=t, Ub=Ub, order=order, tl=tl, tile=tile, buf=buf, sT=sT, tsl=tsl):
                    ins = None
                    for c in (0, 1):
                        ci = order.index(c)
                        csl = slice(c * 64, (c + 1) * 64)
                        e.matmul(Ub[:, ci * 128:(ci + 1) * 128], lhsT=t["kbtok"][buf][csl, tl, :], rhs=vtok[csl, tile, :], start=True, stop=True)
                        ins = e.matmul(sT[:, c * 64:(c + 1) * 64], lhsT=t["kt"][buf][:, tsl], rhs=t["qt"][buf][:, tl * 128 + c * 64:tl * 128 + (c + 1) * 64],
                                       start=True, stop=True)
                    return ins
                P.pe(mf, r=[K_(d, ("kbtok", buf)), ("vtok", blk), K_(d, ("kt", buf)), K_(d, ("qt", buf))], w=[("ps", 3 + par), ("ps", 2)], c=0.5)
            for d in range(2):
                t = T[d]
                blk, tl, tile, buf, par = tile_info(idx, d)
                sT = PS(2)[:, (d * 2 + par) * 128:(d * 2 + par + 1) * 128]
                P.dve(lambda e, t=t, sT=sT, par=par, d=d: e.tensor_tensor(out=t["sTm"][par], in0=sT, in1=mk[d], op=ALU.mult),
                      r=[("ps", 2), ("mk", d)], w=[K_(d, ("sTm", par))], c=0.2)

        def back(idx):
            for d in range(2):
                t = T[d]
                blk, tl, tile, buf, par = tile_info(idx, d)
                Ub = PS(3 + par)[:, d * 256:(d + 1) * 256]
                order = (0, 1) if d == 0 else (1, 0)
                for ci, c in enumerate(order):
                    cib = tl * 2 + c
                    so, sn_ = s_cur[d], s_cur[d] ^ 1
                    s_cur[d] = sn_
                    P.dve(lambda e, t=t, d=d, ci=ci, cib=cib, Ub=Ub, buf=buf, so=so, sn_=sn_: e.scalar_tensor_tensor(
                        out=Sst[d][sn_], in0=Sst[d][so], scalar=t["dec"][buf][:, cib:cib + 1], in1=Ub[:, ci * 128:(ci + 1) * 128], op0=ALU.mult, op1=ALU.add),
                        r=[K_(d, ("S", so)), K_(d, ("dec", buf)), ("ps", 3 + par)], w=[K_(d, ("S", sn_))], c=0.2)
                    dpar, dslot = (par, 1) if ci == 0 else (par ^ 1, 0)
                    P.act(lambda e, t=t, d=d, dpar=dpar, dslot=dslot, sn_=sn_: e.copy(out=t["Sp"][dpar][dslot], in_=Sst[d][sn_]),
                          r=[K_(d, ("S", sn_))], w=[K_(d, ("Sp", dpar, dslot))], c=0.2)
            for d in range(2):
                t = T[d]
                blk, tl, tile, buf, par = tile_info(idx, d)
                acc = PS((1, 7)[par])[:, d * 128:(d + 1) * 128]
                order = (0, 1) if d == 0 else (1, 0)
                def ma(e, t=t, acc=acc, order=order, tl=tl, tile=tile, buf=buf, par=par):
                    e.matmul(acc, lhsT=vtok[:, tile, :], rhs=t["sTm"][par], start=True, stop=False, skip_group_check=True)
                    ins = None
                    for ci, c in enumerate(order):
                        ins = e.matmul(acc[:, c * 64:(c + 1) * 64], lhsT=t["Sp"][par][ci], rhs=t["qb"][buf][:, tl * 128 + c * 64:tl * 128 + (c + 1) * 64],
                                       start=False, stop=True, skip_group_check=True)
                    return ins
                P.pe(ma, r=[("vtok", blk), K_(d, ("sTm", par)), K_(d, ("Sp", par, 0)), K_(d, ("Sp", par, 1)), K_(d, ("qb", buf))], w=[("ps", (1, 7)[par])], c=0.4)
            for d in range(2):
                blk, tl, tile, buf, par = tile_info(idx, d)
                acc = PS((1, 7)[par])[:, d * 128:(d + 1) * 128]
                osl = oTh[:, tile * 128:(tile + 1) * 128]
                first = (blk < 4) if d == 0 else (blk >= 4)
                if first:
                    P.act(lambda e, osl=osl, acc=acc: e.copy(out=osl, in_=acc), r=[("ps", (1, 7)[par])], w=[("oTh", tile)], c=0.25)
                else:
                    P.dve(lambda e, osl=osl, acc=acc: e.tensor_tensor(out=osl, in0=osl, in1=acc, op=ALU.add),
                          r=[("ps", (1, 7)[par]), ("oTh", tile)], w=[("oTh", tile)], c=0.2)

        def record_list(fn):
            saved = P.ops
            P.ops = []
            fn()
            lst = P.ops
            P.ops = saved
            return lst

        for h in range(DBG["hg_heads"]):
            wb = wh[h % 2]
            kwb = ("wh", h % 2)
            for p_ in range(5):
                P.dma(lambda e, p_=p_, h=h, wb=wb: e.dma_start(
                    out=wb[:, :, p_, :], in_=hg_w_in[:, p_ * D + h * 128:p_ * D + (h + 1) * 128].rearrange("(k q) n -> q k n", q=128)),
                    w=[kwb], q="pool")
            for d in range(2):
                s_cur[d] = 0
                P.dve(lambda e, d=d: e.memset(Sst[d][0], 0.0), w=[K_(d, ("S", 0))])
                P.dve(lambda e, d=d: e.memset(T[d]["Sp"][0][0], 0.0), w=[K_(d, ("Sp", 0, 0))])
            if not DBG.get("hg_manual", 0):
                for step in range(8):
                    for d in range(2):
                        prep(h, d, step if d == 0 else 7 - step, step % 2, wb, kwb, part="a")
                    for d in range(2):
                        prep(h, d, step if d == 0 else 7 - step, step % 2, wb, kwb, part="b")
                    for ts in range(4):
                        front(step * 4 + ts)
                        back(step * 4 + ts)
            for d in range(2):
                if DBG.get("hg_manual", 0):
                    prep(h, d, 0 if d == 0 else 7, 0, wb, kwb)
            for step in (range(8) if DBG.get("hg_manual", 0) else ()):
                pl = []
                if step < 7:
                    pl = record_list(lambda: [prep(h, d, (step + 1) if d == 0 else 7 - (step + 1), (step + 1) % 2, wb, kwb) for d in range(2)])
                nq = (len(pl) + 3) // 4
                if not DBG.get("hg_merge", 1):
                    P.ops.extend(pl)
                    pl = []
                for ts in range(4):
                    idx = step * 4 + ts
                    front(idx)
                    if DBG.get("hg_lag", 1):
                        if idx >= 1:
                            back(idx - 1)
                    else:
                        back(idx)
                    P.ops.extend(pl[ts * nq:(ts + 1) * nq])
            if DBG.get("hg_lag", 1) and DBG.get("hg_manual", 0):
                back(31)
            for half in range(2):
                for tb4 in range(4):
                    tb = half * 4 + tb4
                    sl = slice(tb * 512, (tb + 1) * 512)
                    rsl = slice(tb4 * 512, (tb4 + 1) * 512)
                    okeys = [("oTh", i) for i in range(tb * 4, tb * 4 + 4)]
                    P.act(lambda e, sl=sl: e.activation(out=o2, in_=oTh[:, sl], func=AF.Square), r=okeys, w=["o2"])
                    P.pe(lambda e: e.matmul(PS(7), lhsT=ones_m, rhs=o2, start=True, stop=True), r=["o2", "ones_m"], w=[("ps", 7)], c=0.9)
                    P.act(lambda e: e.activation(out=rsn, in_=PS(7), func=AF.Ln, bias=EPS), r=[("ps", 7)], w=["rsn"])
                    P.act(lambda e, rsl=rsl: e.activation(out=rst[:, rsl], in_=rsn, func=AF.Exp, scale=-0.5), r=["rsn"], w=["rst", "normA"])
                for tb4 in range(4):
                    tb = half * 4 + tb4
                    sl = slice(tb * 512, (tb + 1) * 512)
                    rsl = slice(tb4 * 512, (tb4 + 1) * 512)
                    par = tb % 2
                    okeys = [("oTh", i) for i in range(tb * 4, tb * 4 + 4)]
                    def mm(e, sl=sl, wb=wb):
                        ins = None
                        for k in range(8):
                            ins = e.matmul(PS(6), lhsT=wb[:, k, 4, :], rhs=xT3[:, k, sl], start=(k == 0), stop=(k == 7))
                        return ins
                    P.pe(mm, r=[kwb] + xT_all[tb * 4:tb * 4 + 4], w=[("ps", 6)])
                    P.act(lambda e: e.activation(out=sgt, in_=PS(6), func=AF.Silu), r=[("ps", 6), "normA"], w=["sgt"])
                    P.dve(lambda e, sl=sl, rsl=rsl: e.scalar_tensor_tensor(out=o2, in0=oTh[:, sl], scalar=ng[:, 0:1], in1=rst[:, rsl], op0=ALU.mult, op1=ALU.mult),
                          r=okeys + ["rst", "ng", "o2"], w=["o2"])
                    P.dve(lambda e, par=par: e.tensor_tensor(out=onb[par], in0=o2, in1=sgt, op=ALU.mult), r=["o2", "sgt"], w=[("onb", par)])
                    P.dma(lambda e, h=h, sl=sl, par=par: e.dma_start(out=ONT[h * 128:(h + 1) * 128, sl], in_=onb[par]), r=[("onb", par)], w=["ONT"])
        P.nosched = False
        P.barrier()
        A.release()
        w_out = A.alloc(8 * D, BF16).rearrange("p (k n) -> p k n", k=8)
        load_w_bf16(w_out, hg_w_out, 8, D, "hw_out")
        load_gb(1, 0)
        E = Epi()
        onl = [A.alloc(8 * 512, BF16).rearrange("p (k t) -> p k t", k=8) for _ in range(2)]
        for grp in range(8):
            gb_ = grp % 2
            P.dma(lambda e, grp=grp, gb_=gb_: e.dma_start(out=onl[gb_], in_=ONT[:, grp * 512:(grp + 1) * 512].rearrange("(k p) t -> p k t", p=128)),
                  r=["ONT"], w=[("onl", gb_)])
            for tl in range(4):
                i = grp * 4 + tl
                for hh in range(2):
                    bank = (i % 2) * 2 + hh
                    def mm(e, tl=tl, hh=hh, bank=bank, gb_=gb_):
                        ins = None
                        for k in range(8):
                            ins = e.matmul(PS(bank), lhsT=onl[gb_][:, k, tl * 128:(tl + 1) * 128], rhs=w_out[:, k, hh * 512:(hh + 1) * 512],
                                           start=(k == 0), stop=(k == 7))
                        return ins
                    P.pe(mm, r=["hw_out", ("onl", gb_)], w=[("ps", bank)])
                b0 = (i % 2) * 2
                epilogue(E, i, [PS(b0), PS(b0 + 1)], [("ps", b0), ("ps", b0 + 1)], cur_res, dst_res)
        P.barrier()
        A.release()

    plan = []
    if upto >= 1 and not DBG["skip_l0"]:
        plan.append(("l0", 0))
    if upto >= 2:
        plan.append(("xa", 0))
    if upto >= 3:
        plan.append(("moe", 0))
    if upto >= 4:
        plan.append(("hg", 1))
    if upto >= 5:
        plan.append(("xa", 1))
    if upto >= 6:
        plan.append(("moe", 1))
    plan = [p for p in plan if p[0] not in DBG["skip"]]
    for pi, (kind, l) in enumerate(plan):
        last = (pi == len(plan) - 1)
        dst_res = out_d if last else XR[nxt]
        if kind == "l0":
            l0_phase(cur_res, dst_res)
        elif kind == "xa":
            xa_phase(l, cur_res, dst_res)
        elif kind == "moe":
            moe_phase(l, cur_res, dst_res, want_xT=(l == 0))
        elif kind == "hg":
            hg_phase(cur_res, dst_res)
        cur_res = dst_res
        nxt ^= 1

    final_keys = ["dst_dram"] + [("dst", i) for i in range(NT)]
    if cur_res is not out_d:
        A.mark()
        cp = [A.alloc(D, F32) for _ in range(2)]
        for i in range(NT):
            b = i % 2
            P.dma(lambda e, i=i, b=b: e.dma_start(out=cp[b], in_=cur_res[i * 128:(i + 1) * 128, :]), r=["dst_dram"], w=[("cp", b)])
            P.dma(lambda e, i=i, b=b: e.dma_start(out=out_d[i * 128:(i + 1) * 128, :], in_=cp[b]), r=[("cp", b)], w=["out_final"])
        final_keys.append("out_final")
        A.release()
    cnt, dcnt = P.emit(final_keys=final_keys)
    st.close()
    return nc, cnt, dcnt


def make_consts():
    import ml_dtypes
    s = np.arange(S, dtype=np.int64)
    ang = 2.0 * np.pi * ((s[:, None] * s[None, :]) % S).astype(np.float64) / S
    sc = 1.0 / np.sqrt(S)
    cs = (np.cos(ang) * sc).astype(np.float32).astype(ml_dtypes.bfloat16)
    ss = (-np.sin(ang) * sc).astype(np.float32).astype(ml_dtypes.bfloat16)
    c = np.arange(128, dtype=np.int64)
    angc = 2.0 * np.pi * ((c[:, None] * c[None, :]) % 128).astype(np.float64) / 128
    scc = 1.0 / np.sqrt(128.0)
    dc = np.concatenate([np.cos(angc) * scc, np.sin(angc) * scc], axis=1).astype(np.float32)
    t = np.arange(S)
    invcnt = np.zeros((4, S), np.float32)
    for gi, w in enumerate((2, 4, 8, 16)):
        lo = np.clip(t - w // 2, 0, S)
        hi = np.clip(t + w // 2, 0, S)
        invcnt[gi] = 1.0 / (hi - lo).astype(np.float32)
    m = np.arange(128)
    same = (m[:, None] // 64) == (m[None, :] // 64)
    masks = np.zeros((4, 128, 128), np.float32)
    masks[0] = (same & (m[:, None] <= m[None, :]))
    masks[1] = (same & (m[:, None] >= m[None, :]))
    masks[2] = (m[:, None] < m[None, :])
    masks[3] = 1.0
    eoff = (np.arange(32, dtype=np.float32) * CAP).reshape(1, 32)
    trash = (NEXP * CAP + np.arange(128, dtype=np.float32)).reshape(128, 1)
    altc = np.repeat((sc * np.cos(np.pi * np.arange(128))).astype(np.float32).reshape(128, 1), NT, axis=1)
    return {"c_cs": cs, "c_ss": ss, "c_dc": dc, "c_invcnt": invcnt, "c_masks": masks, "c_eoff": eoff, "c_trash": trash, "c_alt": np.ascontiguousarray(altc)}


_CACHE = {}


def kernel(**inputs):
    if "nc" not in _CACHE:
        _CACHE["nc"] = build()[0]
        _CACHE["consts"] = make_consts()
    nc = _CACHE["nc"]
    consts = _CACHE["consts"]
    in_maps = []
    for b in range(8):
        m = {}
        for k, v in inputs.items():
            v = np.asarray(v)
            if k in ("x", "mem"):
                m[k] = np.ascontiguousarray(v[b])
            elif k in ("pf_w_in", "pf_pool_w", "pf_pool_scale", "pf_fourier_ln_g", "pf_fourier_w", "pf_w_out",
                       "hg_w_in", "hg_norm_g", "hg_w_out"):
                m[k] = np.ascontiguousarray(v[0])
            else:
                m[k] = np.ascontiguousarray(v)
        m.update(consts)
        in_maps.append(m)
    res = run_bass_kernel_spmd(nc, in_maps, core_ids=list(range(8)))
    return np.stack([np.asarray(r["out"]) for r in res.results], axis=0).astype(np.float32)
```

```python
import numpy as np
from contextlib import ExitStack
import concourse.bass as bass
import concourse.mybir as mybir
from concourse.bass_utils import run_bass_kernel_spmd

F32 = mybir.dt.float32
BF16 = mybir.dt.bfloat16
I32 = mybir.dt.int32
ALU = mybir.AluOpType
AF = mybir.ActivationFunctionType
AX = mybir.AxisListType

S = 4096
D = 1024
NT = 32
MEM = 256
CAP = 512
NEXP = 32
ALPHA = 4.0 ** 0.25
EPS = 1e-5
EPOCH = 12000
DBG = {"skip_l0": False, "xa_groups": 8, "hg_heads": 8, "skip": ()}
NDS = 8


class Op:
    __slots__ = ("eng", "fn", "reads", "writes", "dma", "seq", "dn", "deps", "waits", "bar", "c", "ns")


class Prog:
    ENGS = ["pe", "act", "dve", "pool", "sp"]

    def __init__(self, nc):
        self.nc = nc
        self.ops = []

    DEFC = {"pe": 1.8, "act": 0.6, "dve": 0.6, "pool": 1.5, "sp": 4.0}

    def add(self, eng, fn, reads=(), writes=(), dma=False, c=None):
        o = Op()
        o.eng, o.fn, o.reads, o.writes, o.dma, o.bar = eng, fn, list(reads), list(writes), dma, False
        o.c = c if c is not None else (4.0 if dma else self.DEFC[eng])
        o.ns = getattr(self, "nosched", False)
        self.ops.append(o)
        return o

    def pe(self, fn, r=(), w=(), c=None):
        return self.add("pe", fn, r, w, c=c)

    def act(self, fn, r=(), w=(), c=None):
        return self.add("act", fn, r, w, c=c)

    def dve(self, fn, r=(), w=(), c=None):
        return self.add("dve", fn, r, w, c=c)

    def pool(self, fn, r=(), w=(), c=None):
        return self.add("pool", fn, r, w, c=c)

    def dma(self, fn, r=(), w=(), q="sp", c=None):
        return self.add(q, fn, r, w, dma=True, c=c)

    def barrier(self):
        for e in self.ENGS:
            o = self.add(e, None)
            o.bar = True

    WIN = {"pe": 48, "act": 48, "dve": 48, "pool": 1, "sp": 48}

    def reorder(self, window=48, sync_lat=0.2):
        ops = self.ops
        new_ops = []
        seg = []

        def flush():
            n = len(seg)
            if n == 0:
                return
            if any(getattr(op, "ns", False) for op in seg):
                new_ops.extend(seg)
                seg.clear()
                return
            last_w, readers = {}, {}
            preds = [set() for _ in range(n)]
            for j, op in enumerate(seg):
                for r in op.reads:
                    if r in last_w:
                        preds[j].add(last_w[r])
                for w in op.writes:
                    if w in last_w:
                        preds[j].add(last_w[w])
                    preds[j].update(readers.get(w, ()))
                preds[j].discard(j)
                for r in op.reads:
                    readers.setdefault(r, []).append(j)
                for w in op.writes:
                    last_w[w] = j
                    readers[w] = []
            queues = {e: [j for j, op in enumerate(seg) if op.eng == e] for e in self.ENGS}
            ptr = {e: 0 for e in self.ENGS}
            done = [False] * n
            fin = [0.0] * n
            t_e = {e: 0.0 for e in self.ENGS}
            left = n
            while left:
                best = None
                for e in self.ENGS:
                    q = queues[e]
                    p = ptr[e]
                    while p < len(q) and done[q[p]]:
                        p += 1
                    ptr[e] = p
                    seen = 0
                    k = p
                    while k < len(q) and seen < self.WIN[e]:
                        j = q[k]
                        k += 1
                        if done[j]:
                            continue
                        seen += 1
                        ok = True
                        rt = 0.0
                        for pr in preds[j]:
                            if not done[pr]:
                                ok = False
                                break
                            f = fin[pr] + sync_lat
                            if f > rt:
                                rt = f
                        if not ok:
                            continue
                        stt = max(t_e[e], rt)
                        key = (stt, j)
                        if best is None or key < best[0]:
                            best = (key, e, j)
                        if stt <= t_e[e]:
                            break
                assert best is not None, "scheduler deadlock"
                (stt, _), e, j = best
                op = seg[j]
                done[j] = True
                left -= 1
                if op.dma:
                    t_e[e] = stt + 0.15
                    fin[j] = stt + op.c
                else:
                    t_e[e] = stt + op.c
                    fin[j] = stt + op.c
                new_ops.append(op)
            seg.clear()

        i = 0
        while i < len(ops):
            if ops[i].bar:
                flush()
                while i < len(ops) and ops[i].bar:
                    new_ops.append(ops[i])
                    i += 1
            else:
                seg.append(ops[i])
                i += 1
        flush()
        self.ops = new_ops

    def emit(self, final_keys=(), reorder=True):
        nc = self.nc
        if reorder:
            self.reorder()
        ops = self.ops
        self.add("sp", None, final_keys, ())
        cnt = {e: 0 for e in self.ENGS}
        dcnt = {e: 0 for e in self.ENGS}
        dma_ops = {e: [] for e in self.ENGS}
        last_c = {e: None for e in self.ENGS}
        epos = {e: 0 for e in self.ENGS}
        for j, op in enumerate(ops):
            if op.dma:
                op.dn = dcnt[op.eng]
                dcnt[op.eng] += 1
                dma_ops[op.eng].append(j)
            elif op.fn is not None:
                epos[op.eng] += 1
                op.seq = epos[op.eng]
        last_w = {}
        readers = {}
        for j, op in enumerate(ops):
            deps = set()
            if op.bar:
                for e in self.ENGS:
                    if last_c[e] is not None:
                        deps.add(last_c[e])
                for e in self.ENGS:
                    lst = [i for i in dma_ops[e] if i < j]
                    deps.update(lst[-NDS:])
            for r in op.reads:
                if r in last_w:
                    deps.add(last_w[r])
            for w in op.writes:
                if w in last_w:
                    deps.add(last_w[w])
                deps.update(readers.get(w, ()))
            if op.dma and op.dn >= NDS:
                deps.add(dma_ops[op.eng][op.dn - NDS])
            deps.discard(j)
            op.deps = deps
            for r in op.reads:
                readers.setdefault(r, []).append(j)
            for w in op.writes:
                last_w[w] = j
                readers[w] = []
            if (not op.dma) and op.fn is not None:
                last_c[op.eng] = j
        wc = {e: {} for e in self.ENGS}
        wd = {e: {} for e in self.ENGS}
        signal = set()
        for j, op in enumerate(ops):
            waits = []
            me = op.eng
            for i in sorted(op.deps):
                d = ops[i]
                if d.dma:
                    k = (d.eng, d.dn % NDS)
                    val = 16 * (d.dn // NDS + 1)
                    if wd[me].get(k, 0) < val:
                        wd[me][k] = val
                        waits.append(("d", i))
                else:
                    if d.fn is None:
                        continue
                    if d.eng == "pe" and me == "pe":
                        continue
                    if wc[me].get(d.eng, 0) < d.seq:
                        wc[me][d.eng] = d.seq
                        waits.append(("c", i))
                        signal.add(i)
            op.waits = waits
        for j, op in enumerate(ops):
            if (not op.dma) and op.fn is not None:
                if j in signal:
                    cnt[op.eng] += 1
                    op.seq = cnt[op.eng]
                else:
                    op.seq = None
        with ExitStack() as st:
            csem = {}
            for e in self.ENGS:
                n_ep = (cnt[e] + EPOCH - 1) // EPOCH
                csem[e] = [st.enter_context(nc.semaphore(f"c_{e}_{k}")) for k in range(max(n_ep, 1))]
            dsem = {}
            for e in self.ENGS:
                if dcnt[e]:
                    dsem[e] = [st.enter_context(nc.semaphore(f"d_{e}_{k}")) for k in range(NDS)]
            for op in ops:
                ww = []
                for (kind, i) in op.waits:
                    d = ops[i]
                    if kind == "d":
                        ww.append((dsem[d.eng][d.dn % NDS], 16 * (d.dn // NDS + 1)))
                    else:
                        ww.append((csem[d.eng][(d.seq - 1) // EPOCH], (d.seq - 1) % EPOCH + 1))
                op.waits = ww
            streams = {e: [op for op in ops if op.eng == e] for e in self.ENGS}
            block = st.enter_context(nc.Block())

            def mk(ename):
                def body(e):
                    for op in streams[ename]:
                        for (sem, val) in op.waits:
                            e.wait_ge(sem, val)
                        if op.fn is None:
                            continue
                        ins = op.fn(e)
                        if op.dma:
                            ins.then_inc(dsem[ename][op.dn % NDS], 16)
                        elif op.seq is not None:
                            ins.then_inc(csem[ename][(op.seq - 1) // EPOCH], 1)
                return body

            block.tensor(mk("pe"))
            block.scalar(mk("act"))
            block.vector(mk("dve"))
            block.gpsimd(mk("pool"))
            block.sync(mk("sp"))
        return cnt, dcnt


class Arena:
    def __init__(self, ap32, nbytes):
        self.ap = ap32
        self.n = nbytes
        self.off = 0
        self.marks = []

    def alloc(self, free_elems, dt):
        sz = 2 if dt == BF16 else 4
        nb = (free_elems * sz + 31) // 32 * 32
        assert self.off + nb <= self.n, f"arena overflow {self.off}+{nb}>{self.n}"
        a = self.ap[:, self.off // 4:(self.off + nb) // 4]
        self.off += nb
        if dt != F32:
            a = a.bitcast(dt)
        return a[:, 0:free_elems]

    def mark(self):
        self.marks.append(self.off)

    def release(self):
        self.off = self.marks.pop()


def build(upto=99, dbg=False):
    nc = bass.Bass("TRN2", target_bir_lowering=False)
    P = Prog(nc)

    def din(name, shape, dt=F32):
        return nc.dram_tensor(name, list(shape), dt, kind="ExternalInput").ap()

    x_d = din("x", [S, D])
    mem_d = din("mem", [MEM, D])
    pf_w_in = din("pf_w_in", [D, D])
    pf_pool_w = din("pf_pool_w", [4, 128, 128])
    pf_pool_scale = din("pf_pool_scale", [512])
    pf_ln_g = din("pf_fourier_ln_g", [4, 128])
    pf_fw = din("pf_fourier_w", [4, 128, 128])
    pf_w_out = din("pf_w_out", [D, D])
    hg_w_in = din("hg_w_in", [D, 5 * D])
    hg_lb = din("hg_lower_bounds", [2, 2, D])
    hg_norm_g = din("hg_norm_g", [128])
    hg_w_out = din("hg_w_out", [D, D])
    xa_wq = din("xa_wq", [2, D, D])
    xa_wkv = din("xa_wkv", [2, D, 2 * D])
    xa_wo = din("xa_wo", [2, D, D])
    moe_wg = din("moe_w_group", [2, D, 4])
    moe_bg = din("moe_b_group", [2, 4])
    moe_we = din("moe_w_expert", [2, D, 32])
    moe_be = din("moe_b_expert", [2, 32])
    moe_gate = din("moe_w_gate", [2, NEXP, D, 512])
    moe_up = din("moe_w_up", [2, NEXP, D, 512])
    moe_down = din("moe_w_down", [2, NEXP, 512, D])
    ln_g = din("ln_g", [2, 3, D])
    ln_b = din("ln_b", [2, 3, D])
    c_cs = din("c_cs", [S, S], BF16)
    c_ss = din("c_ss", [S, S], BF16)
    c_dc = din("c_dc", [128, 256])
    c_invcnt = din("c_invcnt", [4, S])
    c_masks = din("c_masks", [4, 128, 128])
    c_eoff = din("c_eoff", [1, 32])
    c_trash = din("c_trash", [128, 1])
    c_alt = din("c_alt", [128, NT])
    out_d = nc.dram_tensor("out", [S, D], F32, kind="ExternalOutput").ap()
    XR = [nc.dram_tensor(f"xr{k}", [S, D], F32).ap() for k in range(2)]
    Zscr = nc.dram_tensor("zscr", [4, NT, 128, 256], BF16).ap()
    XS = nc.dram_tensor("xs_scr", [NEXP * CAP + 128, D], BF16).ap()
    YS = nc.dram_tensor("ys_scr", [NEXP * CAP + 128, D], F32).ap()
    ONT = nc.dram_tensor("ont_scr", [D, S], BF16).ap()

    st = ExitStack()
    ARENA_BYTES = 206 * 1024
    arena_t = st.enter_context(nc.sbuf_tensor("arena", [128, ARENA_BYTES // 4], F32))
    A = Arena(arena_t[:], ARENA_BYTES)
    psb = [st.enter_context(nc.psum_tensor(f"ps{k}", [128, 512], F32)) for k in range(8)]

    def PS(k):
        return psb[k][:]

    def PSB(k):
        return psb[k][:].bitcast(BF16)

    ident = A.alloc(128, BF16)
    ident32 = A.alloc(128, F32)
    ones_m = A.alloc(128, F32)
    gb = A.alloc(2 * D, F32)
    xT = A.alloc(8 * S, BF16)
    xT3 = xT.rearrange("p (k t) -> p k t", k=8)

    P.pool(lambda e: e.memset(ident32, 0.0), w=["ident32"])
    P.pool(lambda e: e.affine_select(out=ident32, in_=ident32, pattern=[[-1, 128]], compare_op=ALU.not_equal,
                                     fill=1.0, base=0, channel_multiplier=1), r=["ident32"], w=["ident32"])
    P.dve(lambda e: e.tensor_copy(out=ident, in_=ident32), r=["ident32"], w=["ident"])
    P.pool(lambda e: e.memset(ones_m, 1.0 / 128.0), w=["ones_m"])

    def ncd(fn):
        def g(e):
            with nc.allow_non_contiguous_dma(reason="tiny per-partition scalar tables"):
                return fn(e)
        return g

    cast_rr = [0]

    def cast_copy(out, in_, r, w):
        cast_rr[0] ^= 1
        if cast_rr[0]:
            P.act(lambda e: e.copy(out=out, in_=in_), r=r, w=w)
        else:
            P.dve(lambda e: e.tensor_copy(out=out, in_=in_), r=r, w=w)

    def transpose_to(dst3, src_bf, bank, rkeys, wkeys):
        def f(e):
            ins = None
            for k in range(8):
                ins = e.transpose(out=PSB(bank)[:, k * 128:(k + 1) * 128], in_=src_bf[:, k * 128:(k + 1) * 128],
                                  identity=ident)
            return ins
        P.pe(f, r=list(rkeys) + ["ident"], w=[("ps", bank)])
        cast_copy(dst3, PSB(bank).rearrange("p (k t) -> p k t", k=8), [("ps", bank)], wkeys)

    def load_w_bf16(dst3, src2d, kt, n, wkey, rows_per_dma=256):
        kk = max(1, rows_per_dma // 128)
        for k0 in range(0, kt, kk):
            k1 = min(kt, k0 + kk)
            P.dma(lambda e, k0=k0, k1=k1: e.dma_start(
                out=dst3[:, k0:k1, :], in_=src2d[k0 * 128:k1 * 128, :].rearrange("(k p) n -> p k n", p=128)),
                w=[wkey], q="pool")

    def load_gb(l, j):
        P.dma(lambda e: e.dma_start(out=gb[:, 0:D], in_=ln_g[l, j:j + 1, :].broadcast_to([128, D])), w=["gb"])
        P.dma(lambda e: e.dma_start(out=gb[:, D:2 * D], in_=ln_b[l, j:j + 1, :].broadcast_to([128, D])), w=["gb"])

    class Epi:
        def __init__(self):
            self.xr = [A.alloc(D, F32) for _ in range(2)]
            self.y = [A.alloc(D, F32) for _ in range(2)]
            self.xo = [A.alloc(D, F32) for _ in range(2)]
            self.xob = [A.alloc(D, BF16) for _ in range(2)]
            self.st = A.alloc(12, F32)
            self.mv = A.alloc(8, F32)

    def epilogue(E, i, hsrc, hkeys, res_ap, dst_ap, make_xT=True, tbank=7, gmul="pool"):
        b = i % 2
        xr, y, xo, xob = E.xr[b], E.y[b], E.xo[b], E.xob[b]
        kxr, ky, kxo, kxob = ("e_xr", b), ("e_y", b), ("e_xo", b), ("e_xob", b)
        P.dma(lambda e: e.dma_start(out=xr, in_=res_ap[i * 128:(i + 1) * 128, :]), r=["res_dram"], w=[kxr])
        for hh in range(2):
            P.dve(lambda e, hh=hh: e.scalar_tensor_tensor(
                out=y[:, hh * 512:(hh + 1) * 512], in0=xr[:, hh * 512:(hh + 1) * 512], scalar=ALPHA,
                in1=hsrc[hh], op0=ALU.mult, op1=ALU.add), r=[kxr] + list(hkeys), w=[ky])
        P.dve(lambda e: e.bn_stats(out=E.st[:, 0:6], in_=y[:, 0:512]), r=[ky], w=["e_st0"])
        P.dve(lambda e: e.bn_stats(out=E.st[:, 6:12], in_=y[:, 512:1024]), r=[ky], w=["e_st1"])
        P.dve(lambda e: e.bn_aggr(out=E.mv[:, 0:2], in_=E.st[:, 0:12]), r=["e_st0", "e_st1"], w=["e_mv"])
        P.act(lambda e: e.activation(out=E.mv[:, 2:3], in_=E.mv[:, 1:2], func=AF.Ln, bias=EPS), r=["e_mv"], w=["e_sd"], c=0.2)
        P.act(lambda e: e.activation(out=E.mv[:, 3:4], in_=E.mv[:, 2:3], func=AF.Exp, scale=-0.5), r=["e_sd"], w=["e_rs"], c=0.2)
        P.dve(lambda e: e.scalar_tensor_tensor(out=E.mv[:, 4:5], in0=E.mv[:, 0:1], scalar=-1.0, in1=E.mv[:, 3:4],
                                               op0=ALU.mult, op1=ALU.mult), r=["e_mv", "e_rs"], w=["e_nm"])
        P.act(lambda e: e.activation(out=y, in_=y, func=AF.Identity, bias=E.mv[:, 4:5], scale=E.mv[:, 3:4]),
              r=[ky, "e_rs", "e_nm"], w=[ky])
        if gmul == "dve":
            P.dve(lambda e: e.tensor_tensor(out=y, in0=y, in1=gb[:, 0:D], op=ALU.mult), r=[ky, "gb"], w=[ky], c=1.2)
        else:
            P.pool(lambda e: e.tensor_tensor(out=y, in0=y, in1=gb[:, 0:D], op=ALU.mult), r=[ky, "gb"], w=[ky], c=2.4)
        P.pool(lambda e: e.tensor_tensor(out=xo, in0=y, in1=gb[:, D:2 * D], op=ALU.add), r=[ky, "gb"], w=[kxo])
        P.dma(lambda e: e.dma_start(out=dst_ap[i * 128:(i + 1) * 128, :], in_=xo), r=[kxo], w=["dst_dram", ("dst", i)], q="pool")
        if make_xT:
            P.act(lambda e: e.copy(out=xob, in_=xo), r=[kxo], w=[kxob])
            transpose_to(xT3[:, :, i * 128:(i + 1) * 128], xob, tbank, [kxob], [("xT", i)])

    A.mark()
    xb0 = [A.alloc(D, BF16) for _ in range(2)]
    for i in range(NT):
        b = i % 2
        P.dma(lambda e, i=i, b=b: e.dma_start(out=xb0[b], in_=x_d[i * 128:(i + 1) * 128, :]), w=[("xb0", b)], q="pool")
        transpose_to(xT3[:, :, i * 128:(i + 1) * 128], xb0[b], 6 + b, [("xb0", b)], [("xT", i)])
    P.barrier()
    A.release()

    xT_all = [("xT", i) for i in range(NT)]
    cur_res = x_d
    nxt = 0

    def l0_phase(cur_res, dst_res):
        if True:
            A.mark()
            catT = A.alloc(8 * S, BF16)
            catT3 = catT.rearrange("p (k t) -> p k t", k=8)
            A.mark()
            w_in = A.alloc(8 * D, BF16).rearrange("p (k n) -> p k n", k=8)
            load_w_bf16(w_in, pf_w_in, 8, D, "w_in")
            poolw = A.alloc(4 * 128, BF16).rearrange("p (g n) -> p g n", g=4)
            fw = A.alloc(4 * 128, BF16).rearrange("p (g n) -> p g n", g=4)
            dc = A.alloc(256, BF16)
            P.dma(lambda e: e.dma_start(out=poolw, in_=pf_pool_w.rearrange("g c d -> c g d")), w=["poolw"], q="pool")
            P.dma(lambda e: e.dma_start(out=fw, in_=pf_fw.rearrange("g c d -> c g d")), w=["fw"], q="pool")
            P.dma(lambda e: e.dma_start(out=dc, in_=c_dc), w=["dc"], q="pool")
            pscale = A.alloc(4, F32)
            lng = A.alloc(4, F32)
            P.dma(ncd(lambda e: e.dma_start(out=pscale, in_=pf_pool_scale.rearrange("(g d) -> d g", g=4))), w=["pscale"])
            P.dma(ncd(lambda e: e.dma_start(out=lng, in_=pf_ln_g.rearrange("h c -> c h"))), w=["lng"])
            PADW = 16
            A.mark()
            a0 = A.alloc(S + 2 * PADW, F32)
            t1 = A.alloc(1024 + 2 * PADW, F32)
            t2 = A.alloc(1024 + 2 * PADW, F32)
            icnt = A.alloc(1024, F32)
            pm = A.alloc(1024, BF16)
            P.pool(lambda e: e.memset(a0, 0.0), w=["a0"])
            for g in range(4):
                for tb in range(8):
                    bank = tb % 2
                    def mm(e, g=g, tb=tb, bank=bank):
                        ins = None
                        for k in range(8):
                            ins = e.matmul(PS(bank), lhsT=w_in[:, k, g * 128:(g + 1) * 128],
                                           rhs=xT3[:, k, tb * 512:(tb + 1) * 512], start=(k == 0), stop=(k == 7))
                        return ins
                    P.pe(mm, r=["w_in"] + xT_all[tb * 4:tb * 4 + 4], w=[("ps", bank)])
                    P.act(lambda e, tb=tb, bank=bank: e.copy(out=a0[:, PADW + tb * 512:PADW + (tb + 1) * 512], in_=PS(bank)),
                          r=[("ps", bank)], w=["a0"])
                for blk in range(4):
                    s0 = PADW + blk * 1024
                    def lv(buf, lo, hi):
                        return buf[:, PADW + lo:PADW + 1024 + hi]
                    def a0v(lo, hi, s0=s0):
                        return a0[:, s0 + lo:s0 + 1024 + hi]
                    P.dma(lambda e, g=g, blk=blk: e.dma_start(
                        out=icnt, in_=c_invcnt[g:g + 1, blk * 1024:(blk + 1) * 1024].broadcast_to([128, 1024])),
                        w=["icnt"])
                    P.dve(lambda e, a0v=a0v, lv=lv: e.tensor_tensor(out=lv(t1, -8, 8), in0=a0v(-9, 7), in1=a0v(-8, 8), op=ALU.add),
                          r=["a0"], w=["t1"])
                    cur, curk, oth, othk = t1, "t1", t2, "t2"
                    ext = 8
                    for lev in range(1, g + 1):
                        sh = 1 << (lev - 1)
                        ne = ext - 2 * sh if lev < 3 else 0
                        ne = {1: 6, 2: 4, 3: 0}[lev]
                        P.dve(lambda e, cur=cur, oth=oth, sh=sh, ne=ne, lv=lv: e.tensor_tensor(
                            out=lv(oth, -ne, ne), in0=lv(cur, -ne - sh, ne - sh), in1=lv(cur, -ne + sh, ne + sh), op=ALU.add),
                            r=[curk], w=[othk])
                        cur, curk, oth, othk = oth, othk, cur, curk
                        ext = ne
                    P.dve(lambda e, cur=cur, oth=oth, lv=lv: e.tensor_tensor(out=lv(oth, 0, 0), in0=lv(cur, 0, 0), in1=icnt, op=ALU.mult),
                          r=[curk, "icnt"], w=[othk])
                    P.dve(lambda e, oth=oth, lv=lv, a0v=a0v: e.tensor_tensor(out=pm, in0=lv(oth, 0, 0), in1=a0v(0, 0), op=ALU.subtract),
                          r=[othk, "a0"], w=["pm"])
                    for hb in range(2):
                        bank = 2 + hb
                        P.pe(lambda e, g=g, hb=hb, bank=bank: e.matmul(PS(bank), lhsT=poolw[:, g, :], rhs=pm[:, hb * 512:(hb + 1) * 512],
                                                                      start=True, stop=True), r=["pm", "poolw"], w=[("ps", bank)])
                        c0 = blk * 1024 + hb * 512
                        P.act(lambda e, g=g, bank=bank, c0=c0: e.activation(out=catT3[:, g, c0:c0 + 512], in_=PS(bank), func=AF.Identity,
                                                                             scale=pscale[:, g:g + 1]),
                              r=[("ps", bank), "pscale"], w=[("catT", g)])
            ub = A.alloc(512, F32)
            dd = A.alloc(512, F32)
            d2 = A.alloc(512, F32)
            rs = A.alloc(512, F32)
            un = A.alloc(512, BF16)
            zs = [A.alloc(512, BF16) for _ in range(2)]
            for h in range(4):
                for tb in range(8):
                    def mm(e, h=h, tb=tb):
                        ins = None
                        for k in range(8):
                            ins = e.matmul(PS(0), lhsT=w_in[:, k, 512 + h * 128:512 + (h + 1) * 128],
                                           rhs=xT3[:, k, tb * 512:(tb + 1) * 512], start=(k == 0), stop=(k == 7))
                        return ins
                    P.pe(mm, r=["w_in"] + xT_all[tb * 4:tb * 4 + 4], w=[("ps", 0)])
                    P.act(lambda e: e.copy(out=ub, in_=PS(0)), r=[("ps", 0)], w=["ub"])
                    P.pe(lambda e: e.matmul(PS(1), lhsT=ones_m, rhs=ub, start=True, stop=True), r=["ub", "ones_m"], w=[("ps", 1)])
                    P.dve(lambda e: e.tensor_tensor(out=dd, in0=ub, in1=PS(1), op=ALU.subtract), r=["ub", ("ps", 1)], w=["dd"])
                    P.act(lambda e: e.activation(out=d2, in_=dd, func=AF.Square), r=["dd"], w=["d2"])
                    P.pe(lambda e: e.matmul(PS(1), lhsT=ones_m, rhs=d2, start=True, stop=True), r=["d2", "ones_m"], w=[("ps", 1)])
                    P.act(lambda e: e.activation(out=rs, in_=PS(1), func=AF.Ln, bias=EPS), r=[("ps", 1)], w=["rs"])
                    P.act(lambda e: e.activation(out=rs, in_=rs, func=AF.Exp, scale=-0.5), r=["rs"], w=["rs"])
                    P.dve(lambda e, h=h: e.scalar_tensor_tensor(out=un, in0=dd, scalar=lng[:, h:h + 1], in1=rs, op0=ALU.mult, op1=ALU.mult),
                          r=["dd", "rs", "lng"], w=["un"])
                    for pr in range(2):
                        bank = 2 + pr
                        def mz(e, pr=pr, bank=bank):
                            ins = None
                            for q in range(2):
                                tt = pr * 2 + q
                                ins = e.matmul(PS(bank)[:, q * 256:(q + 1) * 256], lhsT=un[:, tt * 128:(tt + 1) * 128], rhs=dc,
                                               start=True, stop=True)
                            return ins
                        P.pe(mz, r=["un", "dc"], w=[("ps", bank)])
                        cast_copy(zs[pr], PS(bank), [("ps", bank)], [("zs", pr)])
                        t0 = tb * 4 + pr * 2
                        P.dma(lambda e, h=h, t0=t0, pr=pr: e.dma_start(
                            out=Zscr[h, t0:t0 + 2].rearrange("t p c -> p t c"), in_=zs[pr].rearrange("p (t c) -> p t c", t=2)),
                            r=[("zs", pr)], w=["zscr"])
            P.barrier()
            A.release()
            A.release()
            A.mark()
            fw2 = A.alloc(4 * 128, BF16).rearrange("p (g n) -> p g n", g=4)
            P.dma(lambda e: e.dma_start(out=fw2, in_=pf_fw.rearrange("g c d -> c g d")), w=["fw2"], q="pool")
            zres = xT.rearrange("p (h t c) -> p h t c", h=4, t=NT)
            for h in range(4):
                P.dma(lambda e, h=h: e.dma_start(out=zres[:, h], in_=Zscr[h].rearrange("t p c -> p t c")), r=["zscr"], w=["zres"])
            NCH = 4
            dbuf = [[A.alloc(8 * 512, BF16).rearrange("p (s t) -> p s t", s=8) for _ in range(2)] for _ in range(2)]
            asb = [A.alloc(512, F32) for _ in range(4)]
            ypb = [A.alloc(512, BF16) for _ in range(4)]
            ymb = [A.alloc(512, BF16) for _ in range(4)]
            alt = A.alloc(NT, BF16)
            y2k = A.alloc(4, BF16)
            P.dma(lambda e: e.dma_start(out=alt, in_=c_alt), w=["alt"], q="pool")
            for h in range(4):
                def m2k(e, h=h):
                    ins = None
                    for stile in range(NT):
                        ins = e.matmul(PS(0)[:, h:h + 1], lhsT=zres[:, h, stile, 0:128], rhs=alt[:, stile:stile + 1],
                                       start=(stile == 0), stop=(stile == NT - 1))
                    return ins
                P.pe(m2k, r=["zres", "alt"], w=[("ps", 0)], c=2.5)
            P.dve(lambda e: e.tensor_copy(out=y2k, in_=PS(0)[:, 0:4]), r=[("ps", 0)], w=["y2k"])
            for h in range(4):
                P.pe(lambda e, h=h: e.matmul(PS(0)[:, 8 + h:9 + h], lhsT=fw2[:, h, :], rhs=y2k[:, h:h + 1], start=True, stop=True),
                     r=["y2k", "fw2"], w=[("ps", 0)], c=0.2)
            for h in range(4):
                P.dve(lambda e, h=h: e.tensor_copy(out=catT3[:, 4 + h, 2048:2049], in_=PS(0)[:, 8 + h:9 + h]), r=[("ps", 0)], w=[("catT", 4 + h)], c=0.1)
            cn = 0
            for kb in range(4):
                for sc in range(NCH):
                    bsel = cn % 2
                    cn += 1
                    for m, src in enumerate((c_cs, c_ss)):
                        P.dma(lambda e, m=m, src=src, sc=sc, kb=kb, bsel=bsel: e.dma_start(
                            out=dbuf[bsel][m], in_=src[sc * 1024:(sc + 1) * 1024, kb * 512:(kb + 1) * 512].rearrange("(s p) t -> p s t", p=128)),
                            w=[("dbuf", bsel, m)])
                    for h in range(4):
                        def mm(e, h=h, sc=sc, bsel=bsel):
                            ins = None
                            for s_ in range(8):
                                stile = sc * 8 + s_
                                first = (sc == 0 and s_ == 0)
                                last = (sc == NCH - 1 and s_ == 7)
                                e.matmul(PS(h), lhsT=zres[:, h, stile, 0:128], rhs=dbuf[bsel][0][:, s_, :], start=first, stop=last)
                                ins = e.matmul(PS(4 + h), lhsT=zres[:, h, stile, 128:256], rhs=dbuf[bsel][1][:, s_, :], start=first, stop=last)
                            return ins
                        P.pe(mm, r=["zres", ("dbuf", bsel, 0), ("dbuf", bsel, 1)], w=[("ps", h), ("ps", 4 + h)], c=3.6)
                for h in range(4):
                    P.act(lambda e, h=h: e.copy(out=asb[h], in_=PS(h)), r=[("ps", h)], w=[("asb", h)])
                    P.dve(lambda e, h=h: e.tensor_tensor(out=ypb[h], in0=asb[h], in1=PS(4 + h), op=ALU.add), r=[("asb", h), ("ps", 4 + h)], w=[("ypb", h)])
                    P.dve(lambda e, h=h: e.tensor_tensor(out=ymb[h], in0=asb[h], in1=PS(4 + h), op=ALU.subtract), r=[("asb", h), ("ps", 4 + h)], w=[("ymb", h)])
                    P.pe(lambda e, h=h: e.matmul(PS(h), lhsT=fw2[:, h, :], rhs=ypb[h], start=True, stop=True), r=[("ypb", h), "fw2"], w=[("ps", h)], c=0.3)
                    P.pe(lambda e, h=h: e.matmul(PS(4 + h), lhsT=fw2[:, h, :], rhs=ymb[h], start=True, stop=True), r=[("ymb", h), "fw2"], w=[("ps", 4 + h)], c=0.3)
                    P.act(lambda e, h=h, kb=kb: e.copy(out=catT3[:, 4 + h, kb * 512:(kb + 1) * 512], in_=PS(h)), r=[("ps", h)], w=[("catT", 4 + h)])
                    if kb == 0:
                        P.dve(lambda e, h=h: e.tensor_copy(out=catT3[:, 4 + h, 3585:4096][:, ::-1], in_=PS(4 + h)[:, 1:512]),
                              r=[("ps", 4 + h)], w=[("catT", 4 + h)])
                    else:
                        lo = S - kb * 512 - 511
                        P.dve(lambda e, h=h, lo=lo: e.tensor_copy(out=catT3[:, 4 + h, lo:lo + 512][:, ::-1], in_=PS(4 + h)),
                              r=[("ps", 4 + h)], w=[("catT", 4 + h)])
            P.barrier()
            A.release()
            if dbg:
                dbg_cat = nc.dram_tensor("dbg_cat", [D, S], BF16, kind="ExternalOutput").ap()
                P.dma(lambda e: e.dma_start(out=dbg_cat.rearrange("(k p) t -> p k t", p=128), in_=catT3), r=[("catT", k) for k in range(8)], w=["dbg_cat"])
            A.mark()
            w_out = A.alloc(8 * D, BF16).rearrange("p (k n) -> p k n", k=8)
            load_w_bf16(w_out, pf_w_out, 8, D, "w_out")
            load_gb(0, 0)
            E = Epi()
            for i in range(NT):
                for hh in range(2):
                    bank = (i % 2) * 2 + hh
                    def mm(e, i=i, hh=hh, bank=bank):
                        ins = None
                        for k in range(8):
                            ins = e.matmul(PS(bank), lhsT=catT3[:, k, i * 128:(i + 1) * 128], rhs=w_out[:, k, hh * 512:(hh + 1) * 512],
                                           start=(k == 0), stop=(k == 7))
                        return ins
                    P.pe(mm, r=["w_out"] + [("catT", k) for k in range(8)], w=[("ps", bank)])
                b0 = (i % 2) * 2
                epilogue(E, i, [PS(b0), PS(b0 + 1)], [("ps", b0), ("ps", b0 + 1)], cur_res, dst_res)
            P.barrier()
            A.release()
            A.release()


    def xa_phase(l, cur_res, dst_res):
        if True:
            A.mark()
            wq = A.alloc(8 * D, BF16).rearrange("p (k n) -> p k n", k=8)
            wo = A.alloc(8 * D, BF16).rearrange("p (k n) -> p k n", k=8)
            kT = A.alloc(8 * MEM, BF16).rearrange("p (k m) -> p k m", k=8)
            vtok = A.alloc(2 * D, BF16).rearrange("p (m n) -> p m n", m=2)
            A.mark()
            wkv = A.alloc(8 * 2 * D, BF16).rearrange("p (k n) -> p k n", k=8)
            memb = [A.alloc(D, BF16) for _ in range(2)]
            memT = A.alloc(8 * MEM, BF16).rearrange("p (k m) -> p k m", k=8)
            for j in range(2):
                P.dma(lambda e, j=j: e.dma_start(out=memb[j], in_=mem_d[j * 128:(j + 1) * 128, :]), w=[("memb", j)], q="pool")
                transpose_to(memT[:, :, j * 128:(j + 1) * 128], memb[j], 6 + j, [("memb", j)], ["memT"])
            for cb in range(2):
                for k0 in range(0, 8, 2):
                    P.dma(lambda e, cb=cb, k0=k0: e.dma_start(
                        out=wkv[:, k0:k0 + 2, cb * D:(cb + 1) * D],
                        in_=xa_wkv[l, k0 * 128:(k0 + 2) * 128, cb * D:(cb + 1) * D].rearrange("(k p) n -> p k n", p=128)),
                        w=["wkv"], q="pool")
            for ct in range(8):
                bank = ct % 2
                def mm(e, ct=ct, bank=bank):
                    ins = None
                    for k in range(8):
                        ins = e.matmul(PS(bank)[:, 0:MEM], lhsT=wkv[:, k, ct * 128:(ct + 1) * 128], rhs=memT[:, k, :],
                                       start=(k == 0), stop=(k == 7))
                    return ins
                P.pe(mm, r=["wkv", "memT"], w=[("ps", bank)])
                cast_copy(kT[:, ct, :], PS(bank)[:, 0:MEM], [("ps", bank)], ["kT"])
            for mt in range(2):
                for hb in range(2):
                    bank = 2 + hb
                    def mm(e, mt=mt, hb=hb, bank=bank):
                        ins = None
                        for k in range(8):
                            ins = e.matmul(PS(bank), lhsT=memT[:, k, mt * 128:(mt + 1) * 128],
                                           rhs=wkv[:, k, D + hb * 512:D + (hb + 1) * 512], start=(k == 0), stop=(k == 7))
                        return ins
                    P.pe(mm, r=["wkv", "memT"], w=[("ps", bank)])
                    cast_copy(vtok[:, mt, hb * 512:(hb + 1) * 512], PS(bank), [("ps", bank)], ["vtok"])
            P.nosched = False
            load_w_bf16(wq, xa_wq[l], 8, D, "wq")
            load_w_bf16(wo, xa_wo[l], 8, D, "wo")
            load_gb(l, 1)
            E = Epi()
            qT = A.alloc(8 * 512, BF16).rearrange("p (k t) -> p k t", k=8)
            oT = A.alloc(8 * 512, BF16).rearrange("p (k t) -> p k t", k=8)
            ex = A.alloc(MEM, F32)
            pb = A.alloc(MEM, BF16)
            pT = A.alloc(MEM, BF16).rearrange("p (m t) -> p m t", m=2)
            sm = A.alloc(8, F32)
            SCL = 1.0 / 16.0
            last_layer_xT = True
            for grp in range(DBG["xa_groups"]):
                for ct in range(8):
                    bank = 6
                    def mm(e, ct=ct, grp=grp, bank=bank):
                        ins = None
                        for k in range(8):
                            ins = e.matmul(PS(bank), lhsT=wq[:, k, ct * 128:(ct + 1) * 128], rhs=xT3[:, k, grp * 512:(grp + 1) * 512],
                                           start=(k == 0), stop=(k == 7))
                        return ins
                    P.pe(mm, r=["wq"] + xT_all[grp * 4:grp * 4 + 4], w=[("ps", bank)])
                    cast_copy(qT[:, ct, :], PS(bank), [("ps", bank)], [("qT", ct)])
                for tl in range(4):
                    i = grp * 4 + tl
                    tsl = slice(tl * 128, (tl + 1) * 128)
                    for h in range(4):
                        sc = PS(2 + h % 2)[:, 0:256]
                        ksc = ("ps", 2 + h % 2)
                        def ms(e, h=h, sc=sc, tsl=tsl):
                            ins = None
                            for dk in range(2):
                                ins = e.matmul(sc, lhsT=qT[:, 2 * h + dk, tsl], rhs=kT[:, 2 * h + dk, :], start=(dk == 0), stop=(dk == 1))
                            return ins
                        P.pe(ms, r=[("qT", 2 * h), ("qT", 2 * h + 1), "kT"], w=[ksc])
                        P.dve(lambda e, sc=sc: e.reduce_max(out=sm[:, 0:1], in_=sc, axis=AX.X), r=[ksc], w=["sm0"])
                        P.dve(lambda e: e.tensor_scalar(out=sm[:, 1:2], in0=sm[:, 0:1], scalar1=-SCL, scalar2=None, op0=ALU.mult), r=["sm0"], w=["sm1"])
                        P.act(lambda e, sc=sc: e.activation(out=ex, in_=sc, func=AF.Exp, bias=sm[:, 1:2], scale=SCL), r=[ksc, "sm1"], w=["ex"])
                        P.dve(lambda e: e.reduce_sum(out=sm[:, 2:3], in_=ex, axis=AX.X), r=["ex"], w=["sm2"])
                        P.dve(lambda e: e.reciprocal(out=sm[:, 3:4], in_=sm[:, 2:3]), r=["sm2"], w=["sm3"])
                        P.dve(lambda e: e.tensor_scalar(out=pb, in0=ex, scalar1=sm[:, 3:4], scalar2=None, op0=ALU.mult), r=["ex", "sm3"], w=["pb"])
                        ptq = PSB(4)[:, 0:256]
                        kpt = ("ps", 4)
                        def mt_(e, ptq=ptq):
                            ins = None
                            for mt in range(2):
                                ins = e.transpose(out=ptq[:, mt * 128:(mt + 1) * 128], in_=pb[:, mt * 128:(mt + 1) * 128], identity=ident)
                            return ins
                        P.pe(mt_, r=["pb", "ident"], w=[kpt])
                        cast_copy(pT, ptq.rearrange("p (m t) -> p m t", m=2), [kpt], ["pT"])
                        pv = PS((5, 7)[h % 2])[:, 0:256]
                        kpv = ("ps", (5, 7)[h % 2])
                        def mpv(e, h=h, pv=pv):
                            ins = None
                            for dv in range(2):
                                for mt in range(2):
                                    ins = e.matmul(pv[:, dv * 128:(dv + 1) * 128], lhsT=vtok[:, mt, h * 256 + dv * 128:h * 256 + (dv + 1) * 128],
                                                   rhs=pT[:, mt, :], start=(mt == 0), stop=(mt == 1))
                            return ins
                        P.pe(mpv, r=["pT", "vtok"], w=[kpv])
                        cast_copy(oT[:, 2 * h:2 * h + 2, tsl], pv.rearrange("p (d t) -> p d t", d=2), [kpv], [("oT", tl)])
                    for hh in range(2):
                        def mm(e, hh=hh, tsl=tsl):
                            ins = None
                            for k in range(8):
                                ins = e.matmul(PS(hh), lhsT=oT[:, k, tsl], rhs=wo[:, k, hh * 512:(hh + 1) * 512], start=(k == 0), stop=(k == 7))
                            return ins
                        P.pe(mm, r=["wo", ("oT", tl)], w=[("ps", hh)])
                    epilogue(E, i, [PS(0), PS(1)], [("ps", 0), ("ps", 1)], cur_res, dst_res, make_xT=False)
            P.nosched = False
            P.barrier()
            A.release()
            A.release()

    def moe_phase(l, cur_res, dst_res, want_xT):
        if True:
            A.mark()
            idx = A.alloc(NT * 2, I32 if False else F32).bitcast(I32).rearrange("p (t k) -> p t k", k=2)
            gates = A.alloc(NT * 2, F32).rearrange("p (t k) -> p t k", k=2)
            A.mark()
            wr32 = A.alloc(8 * 36, F32).rearrange("p (k n) -> p k n", k=8)
            P.dma(ncd(lambda e: e.dma_start(out=wr32[:, :, 0:4], in_=moe_wg[l].rearrange("(k p) n -> p k n", p=128))), w=["wr32"])
            P.dma(ncd(lambda e: e.dma_start(out=wr32[:, :, 4:36], in_=moe_we[l].rearrange("(k p) n -> p k n", p=128))), w=["wr32"])
            bias_bc = A.alloc(36, F32)
            P.dma(lambda e: e.dma_start(out=bias_bc[:, 0:4], in_=moe_bg[l:l + 1, :].broadcast_to([128, 4])), w=["bias_bc"])
            P.dma(lambda e: e.dma_start(out=bias_bc[:, 4:36], in_=moe_be[l:l + 1, :].broadcast_to([128, 32])), w=["bias_bc"])
            eoff = A.alloc(32, F32)
            P.dma(lambda e: e.dma_start(out=eoff, in_=c_eoff.broadcast_to([128, 32])), w=["eoff"])
            ustr = A.alloc(128, BF16)
            onesb = A.alloc(128, BF16)
            P.dma(lambda e: e.dma_start(out=ustr, in_=c_masks[2]), w=["ustr"], q="pool")
            P.dma(lambda e: e.dma_start(out=onesb, in_=c_masks[3]), w=["onesb"], q="pool")
            trash = A.alloc(1, F32)
            P.dma(ncd(lambda e: e.dma_start(out=trash, in_=c_trash)), w=["trash"])
            carry = A.alloc(32, F32)
            P.dve(lambda e: e.memset(carry, 0.0), w=["carry"])
            if DBG.get("zero_xs"):
                zt = A.alloc(D, BF16)
                P.pool(lambda e: e.memset(zt, 0.0), w=["zt"])
                for r0 in range(0, NEXP * CAP + 128, 128):
                    P.dma(lambda e, r0=r0: e.dma_start(out=XS[r0:r0 + 128, :], in_=zt), r=["zt"], w=["XS"])
            x2 = [A.alloc(D, F32) for _ in range(2)]
            xb2 = [A.alloc(D, BF16) for _ in range(2)]
            RB = []
            for _p in range(2):
                rb = {}
                rb["x2T"] = A.alloc(8 * 128, F32).rearrange("p (k t) -> p k t", k=8)
                for nm, n_ in (("lg", 36), ("rt", 64), ("maskg", 4), ("esel", 8), ("e2", 8), ("mask1", 8), ("mask2", 8),
                               ("M1", 32), ("M2", 32), ("Ms", 32), ("pos", 32), ("tmp", 32)):
                    rb[nm] = A.alloc(n_, F32)
                rb["Mb"] = A.alloc(32, BF16)
                RB.append(rb)
            def route_tile(i):
                b = i % 2
                rbp = b if DBG.get("route_double", 0) else 0
                rb = RB[rbp]
                x2T, lg, rt, maskg, esel, e2, mask1, mask2 = rb['x2T'], rb['lg'], rb['rt'], rb['maskg'], rb['esel'], rb['e2'], rb['mask1'], rb['mask2']
                M1, M2, Ms, Mb, pos, tmp = rb['M1'], rb['M2'], rb['Ms'], rb['Mb'], rb['pos'], rb['tmp']
                M1v = M1.rearrange('p (g j) -> p g j', g=4)
                M2v = M2.rearrange('p (g j) -> p g j', g=4)
                pb_ = 4 * rbp
                kx2, kxb = ("x2", b), ("xb2", b)
                P.dma(lambda e, i=i, b=b: e.dma_start(out=x2[b], in_=cur_res[i * 128:(i + 1) * 128, :]), w=[kx2])
                P.act(lambda e, b=b: e.copy(out=xb2[b], in_=x2[b]), r=[kx2], w=[kxb])
                for half in range(2):
                    def tr(e, half=half, b=b):
                        ins = None
                        for q in range(4):
                            k = half * 4 + q
                            ins = e.transpose(out=PS(pb_ + half)[:, q * 128:(q + 1) * 128], in_=x2[b][:, k * 128:(k + 1) * 128], identity=ident32)
                        return ins
                    P.pe(tr, r=[kx2, "ident32"], w=[("ps", pb_ + half)])
                    cast_copy(x2T[:, half * 4:(half + 1) * 4, :], PS(pb_ + half).rearrange("p (k t) -> p k t", k=4), [("ps", pb_ + half)], [("x2T", rbp)])
                def mlg(e):
                    ins = None
                    for k in range(8):
                        ins = e.matmul(PS(pb_ + 2)[:, 0:36], lhsT=x2T[:, k, :], rhs=wr32[:, k, :], start=(k == 0), stop=(k == 7))
                    return ins
                P.pe(mlg, r=[("x2T", rbp), "wr32"], w=[("ps", pb_ + 2)])
                V = P.dve
                V(lambda e: e.tensor_tensor(out=lg, in0=PS(pb_ + 2)[:, 0:36], in1=bias_bc, op=ALU.add), r=[("ps", pb_ + 2), "bias_bc"], w=[("lg", rbp)])
                V(lambda e: e.reduce_max(out=rt[:, 0:1], in_=lg[:, 0:4], axis=AX.X), r=[("lg", rbp)], w=[("rt0", rbp)])
                V(lambda e: e.tensor_scalar(out=maskg, in0=lg[:, 0:4], scalar1=rt[:, 0:1], scalar2=None, op0=ALU.is_equal), r=[("lg", rbp), ("rt0", rbp)], w=[("maskg", rbp)])
                V(lambda e: e.tensor_scalar(out=rt[:, 1:2], in0=rt[:, 0:1], scalar1=-1.0, scalar2=None, op0=ALU.mult), r=[("rt0", rbp)], w=[("rt1", rbp)])
                P.act(lambda e: e.activation(out=rt[:, 4:8], in_=lg[:, 0:4], func=AF.Exp, bias=rt[:, 1:2], scale=1.0), r=[("lg", rbp), ("rt1", rbp)], w=[("rt4", rbp)])
                V(lambda e: e.reduce_sum(out=rt[:, 2:3], in_=rt[:, 4:8], axis=AX.X), r=[("rt4", rbp)], w=[("rt2", rbp)])
                V(lambda e: e.reciprocal(out=rt[:, 3:4], in_=rt[:, 2:3]), r=[("rt2", rbp)], w=[("rt3", rbp)])
                V(lambda e: e.tensor_scalar(out=esel, in0=lg[:, 4:12], scalar1=maskg[:, 0:1], scalar2=None, op0=ALU.mult), r=[("lg", rbp), ("maskg", rbp)], w=[("esel", rbp)])
                for g in range(1, 4):
                    V(lambda e, g=g: e.scalar_tensor_tensor(out=esel, in0=lg[:, 4 + 8 * g:12 + 8 * g], scalar=maskg[:, g:g + 1], in1=esel,
                                                            op0=ALU.mult, op1=ALU.add), r=[("lg", rbp), ("maskg", rbp), ("esel", rbp)], w=[("esel", rbp)])
                V(lambda e: e.reduce_max(out=rt[:, 8:9], in_=esel, axis=AX.X), r=[("esel", rbp)], w=[("rt8", rbp)])
                V(lambda e: e.tensor_scalar(out=mask1, in0=esel, scalar1=rt[:, 8:9], scalar2=None, op0=ALU.is_equal), r=[("esel", rbp), ("rt8", rbp)], w=[("mask1", rbp)])
                V(lambda e: e.scalar_tensor_tensor(out=e2, in0=mask1, scalar=-1e30, in1=esel, op0=ALU.mult, op1=ALU.add), r=[("mask1", rbp), ("esel", rbp)], w=[("e2", rbp)])
                V(lambda e: e.reduce_max(out=rt[:, 9:10], in_=e2, axis=AX.X), r=[("e2", rbp)], w=[("rt9", rbp)])
                V(lambda e: e.tensor_scalar(out=mask2, in0=e2, scalar1=rt[:, 9:10], scalar2=None, op0=ALU.is_equal), r=[("e2", rbp), ("rt9", rbp)], w=[("mask2", rbp)])
                V(lambda e: e.tensor_tensor(out=rt[:, 10:11], in0=rt[:, 9:10], in1=rt[:, 8:9], op=ALU.subtract), r=[("rt8", rbp), ("rt9", rbp)], w=[("rt10", rbp)])
                P.act(lambda e: e.activation(out=rt[:, 11:12], in_=rt[:, 10:11], func=AF.Exp), r=[("rt10", rbp)], w=[("rt11", rbp)])
                V(lambda e: e.tensor_scalar(out=rt[:, 12:13], in0=rt[:, 11:12], scalar1=1.0, scalar2=None, op0=ALU.add), r=[("rt11", rbp)], w=[("rt12", rbp)])
                V(lambda e: e.reciprocal(out=rt[:, 13:14], in_=rt[:, 12:13]), r=[("rt12", rbp)], w=[("rt13", rbp)])
                V(lambda e, i=i: e.tensor_tensor(out=gates[:, i, 0:1], in0=rt[:, 3:4], in1=rt[:, 13:14], op=ALU.mult), r=[("rt3", rbp), ("rt13", rbp)], w=["gates"])
                V(lambda e, i=i: e.tensor_tensor(out=gates[:, i, 1:2], in0=gates[:, i, 0:1], in1=rt[:, 11:12], op=ALU.mult), r=["gates", ("rt11", rbp)], w=["gates"])
                for g in range(4):
                    V(lambda e, g=g: e.tensor_scalar(out=M1v[:, g, :], in0=mask1, scalar1=maskg[:, g:g + 1], scalar2=None, op0=ALU.mult),
                      r=[("mask1", rbp), ("maskg", rbp)], w=[("M1", rbp)])
                    V(lambda e, g=g: e.tensor_scalar(out=M2v[:, g, :], in0=mask2, scalar1=maskg[:, g:g + 1], scalar2=None, op0=ALU.mult),
                      r=[("mask2", rbp), ("maskg", rbp)], w=[("M2", rbp)])
                V(lambda e: e.tensor_tensor(out=Ms, in0=M1, in1=M2, op=ALU.add), r=[("M1", rbp), ("M2", rbp)], w=[("Ms", rbp)])
                V(lambda e: e.tensor_copy(out=Mb, in_=Ms), r=[("Ms", rbp)], w=[("Mb", rbp)])
                def mpos(e):
                    e.matmul(PS(pb_ + 3)[:, 0:32], lhsT=ustr, rhs=Mb, start=True, stop=True)
                    return e.matmul(PS(pb_ + 3)[:, 32:64], lhsT=onesb, rhs=Mb, start=True, stop=True)
                P.pe(mpos, r=[("Mb", rbp), "ustr", "onesb"], w=[("ps", pb_ + 3)])
                V(lambda e: e.tensor_tensor(out=pos, in0=PS(pb_ + 3)[:, 0:32], in1=carry, op=ALU.add), r=[("ps", pb_ + 3), "carry"], w=[("pos", rbp)])
                V(lambda e: e.tensor_tensor(out=carry, in0=PS(pb_ + 3)[:, 32:64], in1=carry, op=ALU.add), r=[("ps", pb_ + 3), "carry"], w=["carry"])
                V(lambda e: e.tensor_scalar(out=tmp, in0=pos, scalar1=float(CAP), scalar2=None, op0=ALU.is_ge), r=[("pos", rbp)], w=[("tmp", rbp)])
                V(lambda e: e.tensor_tensor(out=pos, in0=pos, in1=eoff, op=ALU.add), r=[("pos", rbp), "eoff"], w=[("pos", rbp)])
                V(lambda e: e.tensor_scalar(out=Ms, in0=pos, scalar1=-1.0, scalar2=trash[:, 0:1], op0=ALU.mult, op1=ALU.add), r=[("pos", rbp), "trash"], w=[("Ms", rbp)])
                V(lambda e: e.tensor_tensor(out=tmp, in0=tmp, in1=Ms, op=ALU.mult), r=[("tmp", rbp), ("Ms", rbp)], w=[("tmp", rbp)])
                V(lambda e: e.tensor_tensor(out=pos, in0=pos, in1=tmp, op=ALU.add), r=[("pos", rbp), ("tmp", rbp)], w=[("pos", rbp)])
                for kk, Mk in enumerate((M1, M2)):
                    kM = ("M1", rbp) if kk == 0 else ("M2", rbp)
                    V(lambda e, Mk=Mk: e.tensor_tensor(out=tmp, in0=Mk, in1=pos, op=ALU.mult), r=[kM, ("pos", rbp)], w=[("tmp", rbp)])
                    V(lambda e, kk=kk: e.reduce_sum(out=rt[:, 16 + kk:17 + kk], in_=tmp, axis=AX.X), r=[("tmp", rbp)], w=[("rtidx", kk, rbp)])
                    V(lambda e, kk=kk, i=i: e.tensor_copy(out=idx[:, i, kk:kk + 1], in_=rt[:, 16 + kk:17 + kk]), r=[("rtidx", kk, rbp)], w=[("idx", i, kk)])
                    P.dma(lambda e, kk=kk, i=i, b=b: e.indirect_dma_start(
                        out=XS, out_offset=bass.IndirectOffsetOnAxis(idx[:, i, kk:kk + 1], 0), in_=xb2[b], in_offset=None), r=[("idx", i, kk), kxb], w=["XS"], q="pool")
            for i in range(NT):
                route_tile(i)
            A.mark()
            NCT = CAP // 128
            xT_f32 = xT.bitcast(F32)
            stg = [xT_f32[:, k * 2048:(k + 1) * 2048] for k in range(6)]
            wgb = [A.alloc(8 * 512, BF16).rearrange("p (k n) -> p k n", k=8) for _ in range(2)]
            wub = [A.alloc(8 * 512, BF16).rearrange("p (k n) -> p k n", k=8) for _ in range(2)]
            wdb = [A.alloc(4 * D, BF16).rearrange("p (k n) -> p k n", k=4) for _ in range(2)]
            xsb = [A.alloc(D, BF16) for _ in range(4)]
            xsT2 = [A.alloc(8 * CAP, BF16).rearrange("p (k t) -> p k t", k=8) for _ in range(2)]
            hT2 = [A.alloc(4 * CAP, BF16).rearrange("p (k t) -> p k t", k=4) for _ in range(2)]
            sg2 = [A.alloc(CAP, F32) for _ in range(2)]
            ysb = [A.alloc(D, F32) for _ in range(4)]
            sn = 0
            for ex_i in range(NEXP):
                b = ex_i % 2
                xsT, hT, sg = xsT2[b], hT2[b], sg2[b]
                kxsT, khT, ksg = ("xsT", b), ("hT", b), ("sg", b)
                chunks = []
                for (wsrc, dstb, nm) in ((moe_gate, wgb, "wg"), (moe_up, wub, "wu")):
                    for c in range(2):
                        chunks.append((wsrc[l, ex_i, c * 512:(c + 1) * 512, :].rearrange("(k p) n -> p k n", p=128),
                                       dstb[b][:, c * 4:(c + 1) * 4, :], (nm, b), 4))
                for c in range(2):
                    chunks.append((moe_down[l, ex_i, c * 256:(c + 1) * 256, :].rearrange("(k p) n -> p k n", p=128),
                                   wdb[b][:, c * 2:(c + 1) * 2, :], ("wd", b), 2))
                for (src, dst, key, kk) in chunks:
                    sb_ = sn % 6
                    sn += 1
                    sview = stg[sb_].rearrange("p (k n) -> p k n", k=kk)
                    P.dma(lambda e, src=src, sview=sview: e.dma_start(out=sview, in_=src), w=[("stg", sb_)], q="pool")
                    if sn % 2 == 0:
                        P.act(lambda e, dst=dst, sview=sview: e.copy(out=dst, in_=sview), r=[("stg", sb_)], w=[key], c=2.0)
                    else:
                        P.dve(lambda e, dst=dst, sview=sview: e.tensor_copy(out=dst, in_=sview), r=[("stg", sb_)], w=[key], c=2.3)
                for stl in range(NCT):
                    xb_ = (ex_i * NCT + stl) % 4
                    r0 = ex_i * CAP + stl * 128
                    P.dma(lambda e, r0=r0, xb_=xb_: e.dma_start(out=xsb[xb_], in_=XS[r0:r0 + 128, :]), r=["XS"], w=[("xsb", xb_)], q="pool")
                    transpose_to(xsT[:, :, stl * 128:(stl + 1) * 128], xsb[xb_], 7, [("xsb", xb_)], [kxsT])
                for ft in range(4):
                    for m, wb_, kw in ((0, wgb, "wg"), (1, wub, "wu")):
                        def mm(e, ft=ft, m=m, wb_=wb_, b=b, xsT=xsT):
                            ins = None
                            for k in range(8):
                                ins = e.matmul(PS(m)[:, 0:CAP], lhsT=wb_[b][:, k, ft * 128:(ft + 1) * 128], rhs=xsT[:, k, :],
                                               start=(k == 0), stop=(k == 7))
                            return ins
                        P.pe(mm, r=[(kw, b), kxsT], w=[("ps", m)])
                    P.act(lambda e, sg=sg: e.activation(out=sg, in_=PS(0)[:, 0:CAP], func=AF.Silu), r=[("ps", 0)], w=[ksg])
                    P.dve(lambda e, ft=ft, sg=sg, hT=hT: e.tensor_tensor(out=hT[:, ft, :], in0=sg, in1=PS(1)[:, 0:CAP], op=ALU.mult), r=[ksg, ("ps", 1)], w=[khT])
                for stl in range(NCT):
                    yb_ = stl % 4
                    for hh in range(2):
                        bank = 2 + (stl % 2) * 2 + hh
                        def mm(e, stl=stl, hh=hh, bank=bank, b=b, hT=hT):
                            ins = None
                            for ft in range(4):
                                ins = e.matmul(PS(bank), lhsT=hT[:, ft, stl * 128:(stl + 1) * 128], rhs=wdb[b][:, ft, hh * 512:(hh + 1) * 512],
                                               start=(ft == 0), stop=(ft == 3))
                            return ins
                        P.pe(mm, r=[khT, ("wd", b)], w=[("ps", bank)])
                        if hh == 0:
                            P.act(lambda e, bank=bank, yb_=yb_: e.copy(out=ysb[yb_][:, 0:512], in_=PS(bank)), r=[("ps", bank)], w=[("ysb", yb_)])
                        else:
                            P.dve(lambda e, bank=bank, yb_=yb_: e.tensor_copy(out=ysb[yb_][:, 512:1024], in_=PS(bank)), r=[("ps", bank)], w=[("ysb", yb_)])
                    r0 = ex_i * CAP + stl * 128
                    P.dma(lambda e, r0=r0, yb_=yb_: e.dma_start(out=YS[r0:r0 + 128, :], in_=ysb[yb_]), r=[("ysb", yb_)], w=["YS"])
            P.barrier()
            A.release()
            A.release()
            A.mark()
            load_gb(l, 2)
            E = Epi()
            y1 = [A.alloc(D, F32) for _ in range(2)]
            y2 = [A.alloc(D, F32) for _ in range(2)]
            hm = [A.alloc(D, F32) for _ in range(2)]
            dst = dst_res
            def gather(i):
                b = i % 2
                P.dma(lambda e, i=i, b=b: e.indirect_dma_start(
                    out=y1[b], out_offset=None, in_=YS, in_offset=bass.IndirectOffsetOnAxis(idx[:, i, 0:1], 0)), r=["YS"], w=[("y1", b)], q="pool")
                P.dma(lambda e, i=i, b=b: e.indirect_dma_start(
                    out=y2[b], out_offset=None, in_=YS, in_offset=bass.IndirectOffsetOnAxis(idx[:, i, 1:2], 0)), r=["YS"], w=[("y2", b)], q="pool")
            gather(0)
            gather(1)
            for i in range(NT):
                b = i % 2
                P.dve(lambda e, i=i, b=b: e.tensor_scalar(out=hm[b], in0=y1[b], scalar1=gates[:, i, 0:1], scalar2=None, op0=ALU.mult),
                      r=[("y1", b)], w=[("hm", b)])
                P.dve(lambda e, i=i, b=b: e.scalar_tensor_tensor(out=hm[b], in0=y2[b], scalar=gates[:, i, 1:2], in1=hm[b], op0=ALU.mult, op1=ALU.add),
                      r=[("y2", b), ("hm", b)], w=[("hm", b)])
                if i + 2 < NT:
                    gather(i + 2)
                epilogue(E, i, [hm[b][:, 0:512], hm[b][:, 512:1024]], [("hm", b)], cur_res, dst, make_xT=want_xT, gmul="dve")
            P.barrier()
            A.release()
            A.release()


    def hg_phase(cur_res, dst_res):
        A.mark()
        A.mark()
        P.nosched = bool(DBG.get("hg_nosched", 0))
        lbraw = A.alloc(32, F32)
        lbt = A.alloc(32, F32)
        lbrows = A.alloc(128, F32)
        P.dma(lambda e: e.dma_start(out=lbrows[0:32, :], in_=hg_lb.rearrange("l d (h k) -> (l d h) k", k=128)), w=["lbrows"])
        P.pe(lambda e: e.transpose(out=PS(0)[:, 0:32], in_=lbrows[0:32, :], identity=ident32[0:32, 0:32]), r=["lbrows", "ident32"], w=[("ps", 0)], c=0.3)
        P.dve(lambda e: e.tensor_copy(out=lbraw, in_=PS(0)[:, 0:32]), r=[("ps", 0)], w=["lbraw"])
        P.dve(lambda e: e.tensor_tensor(out=lbt[:, 0:16], in0=lbraw[:, 16:32], in1=lbraw[:, 0:16], op=ALU.subtract), r=["lbraw"], w=["lbd"])
        P.act(lambda e: e.activation(out=lbt[:, 0:16], in_=lbt[:, 0:16], func=AF.Sigmoid), r=["lbd"], w=["lb"])
        P.dve(lambda e: e.tensor_scalar(out=lbt[:, 16:32], in0=lbt[:, 0:16], scalar1=-1.0, scalar2=1.0, op0=ALU.mult, op1=ALU.add), r=["lb"], w=["oml"])
        ng = A.alloc(1, F32)
        P.dma(ncd(lambda e: e.dma_start(out=ng, in_=hg_norm_g.rearrange("(k o) -> k o", o=1))), w=["ng"])
        cmask = A.alloc(512, F32)
        P.pool(lambda e: e.memset(cmask, 1.0), w=["cmask"])
        P.pool(lambda e: e.memset(cmask.rearrange("p (c l) -> p c l", l=64)[:, :, 0:1], 0.0), r=["cmask"], w=["cmask"])
        mk = [A.alloc(128, F32) for _ in range(2)]
        for d in range(2):
            P.dma(lambda e, d=d: e.dma_start(out=mk[d], in_=c_masks[d]), w=[("mk", d)])
        Sst = [[A.alloc(128, F32) for _ in range(2)] for _ in range(2)]
        s_cur = [0, 0]
        wh = [A.alloc(8 * 5 * 128, BF16).rearrange("p (k q n) -> p k q n", k=8, q=5) for _ in range(2)]
        rst = A.alloc(4 * 512, F32)
        vtok = A.alloc(NT * 128, BF16).rearrange("p (t n) -> p t n", t=NT)
        qT = A.alloc(S, F32)
        oTh = A.alloc(S, F32)
        T = []
        for d in range(2):
            t = {}
            for nm in ("sgm", "lgf", "bb", "kk", "t1", "t2"):
                t[nm] = A.alloc(512, F32)
            t["kbT"] = A.alloc(512, BF16)
            for nm in ("qt", "kt", "qb"):
                t[nm] = [A.alloc(512, BF16) for _ in range(2)]
            t["kbtok"] = [A.alloc(512, BF16).rearrange("p (t n) -> p t n", t=4) for _ in range(2)]
            t["dec"] = [A.alloc(8, F32) for _ in range(2)]
            t["sTm"] = [A.alloc(128, BF16) for _ in range(2)]
            t["Sp"] = [[A.alloc(128, BF16) for _ in range(2)] for _ in range(2)]
            T.append(t)
        o2 = A.alloc(512, F32)
        rsn = A.alloc(512, F32)
        sgt = A.alloc(512, F32)
        onb = [A.alloc(512, BF16) for _ in range(2)]

        def K_(d, nm):
            return ("hg", d, nm)

        def v3(ap):
            return ap.rearrange("p (c l) -> p c l", l=64)

        def prep(h, d, blk, buf, wb, kwb, part="ab"):
            t = T[d]
            t0 = blk * 512
            col = d * 8 + h
            fb = (0, 6)[d]
            qt_, kt_, qb_, kbtok_, dec_ = t["qt"][buf], t["kt"][buf], t["qb"][buf], t["kbtok"][buf], t["dec"][buf]
            def mm(e):
                ins = None
                for k in range(8):
                    ins = e.matmul(PS(fb), lhsT=wb[:, k, 2 + d, :], rhs=xT3[:, k, t0:t0 + 512], start=(k == 0), stop=(k == 7))
                return ins
            if "a" in part:
                P.pe(mm, r=[kwb] + xT_all[blk * 4:blk * 4 + 4], w=[("ps", fb)])
                P.act(lambda e: e.activation(out=t["sgm"], in_=PS(fb), func=AF.Sigmoid), r=[("ps", fb)], w=[K_(d, "sgm"), "sigdone"])
                P.dve(lambda e: e.tensor_scalar(out=t["sgm"], in0=t["sgm"], scalar1=lbt[:, 16 + col:17 + col], scalar2=lbt[:, col:col + 1],
                                                op0=ALU.mult, op1=ALU.add), r=[K_(d, "sgm"), "lb", "oml"], w=[K_(d, "sgm")])
            if "a" in part and ((blk < 4) if d == 0 else (blk >= 4)):
                qblk_, vblk_ = qT[:, t0:t0 + 512], vtok[:, blk * 4:(blk + 1) * 4, :]
                def mq(e):
                    ins = None
                    for k in range(8):
                        ins = e.matmul(PS(fb), lhsT=wb[:, k, 0, :], rhs=xT3[:, k, t0:t0 + 512], start=(k == 0), stop=(k == 7))
                    return ins
                P.pe(mq, r=[kwb] + xT_all[blk * 4:blk * 4 + 4], w=[("ps", fb)])
                P.act(lambda e: e.activation(out=qblk_, in_=PS(fb), func=AF.Sigmoid), r=[("ps", fb)], w=[("qT", blk), "sigdone"])
                P.dve(lambda e: e.tensor_tensor(out=qblk_, in0=qblk_, in1=PS(fb), op=ALU.mult), r=[("qT", blk), ("ps", fb)], w=[("qT", blk)])
                def mv(e):
                    ins = None
                    for tl in range(4):
                        tile = blk * 4 + tl
                        for k in range(8):
                            ins = e.matmul(PS(fb)[:, tl * 128:(tl + 1) * 128], lhsT=xT3[:, k, tile * 128:(tile + 1) * 128], rhs=wb[:, k, 1, :],
                                           start=(k == 0), stop=(k == 7))
                    return ins
                P.pe(mv, r=[kwb] + xT_all[blk * 4:blk * 4 + 4], w=[("ps", fb)], c=2.5)
                cast_copy(vblk_, PS(fb).rearrange("p (t n) -> p t n", t=4), [("ps", fb)], [("vtok", blk)])
            if "b" not in part:
                return
            P.act(lambda e: e.activation(out=t["lgf"], in_=t["sgm"], func=AF.Ln), r=[K_(d, "sgm"), "sigdone"], w=[K_(d, "lgf")])
            P.pool(lambda e: e.tensor_scalar(out=t["kk"], in0=t["sgm"], scalar1=-1.0, scalar2=1.0, op0=ALU.mult, op1=ALU.add),
                   r=[K_(d, "sgm")], w=[K_(d, "kk")])
            if d == 0:
                P.dve(lambda e: e.tensor_tensor_scan(out=t["bb"], data0=cmask, data1=t["lgf"], initial=0.0, op0=ALU.mult, op1=ALU.add),
                      r=["cmask", K_(d, "lgf")], w=[K_(d, "bb")])
                iref, ilast = 32, 63
            else:
                P.dve(lambda e: e.tensor_tensor_scan(out=t["t2"], data0=cmask, data1=t["lgf"], initial=0.0, op0=ALU.mult, op1=ALU.add),
                      r=["cmask", K_(d, "lgf")], w=[K_(d, "t2")])
                P.dve(lambda e: e.scalar_tensor_tensor(out=t["t1"], in0=t["t2"], scalar=-1.0, in1=t["lgf"], op0=ALU.mult, op1=ALU.add),
                      r=[K_(d, "t2"), K_(d, "lgf")], w=[K_(d, "t1")])
                P.dve(lambda e: e.tensor_tensor(out=v3(t["bb"]), in0=v3(t["t1"]), in1=v3(t["t2"])[:, :, 63:64].broadcast_to([128, 8, 64]), op=ALU.add),
                      r=[K_(d, "t1"), K_(d, "t2")], w=[K_(d, "bb")])
                iref, ilast = 31, 0
            bref = v3(t["bb"])[:, :, iref:iref + 1].broadcast_to([128, 8, 64])
            blast = v3(t["bb"])[:, :, ilast:ilast + 1].broadcast_to([128, 8, 64])
            qsl = qT[:, t0:t0 + 512]
            kqb = ("qT", blk)
            kq, kk_, kb_, kkb, kd = K_(d, ("qt", buf)), K_(d, ("kt", buf)), K_(d, ("qb", buf)), K_(d, ("kbtok", buf)), K_(d, ("dec", buf))
            P.pool(lambda e: e.tensor_tensor(out=v3(t["t1"]), in0=v3(t["bb"]), in1=bref, op=ALU.subtract), r=[K_(d, "bb")], w=[K_(d, "t1")])
            P.act(lambda e: e.activation(out=t["t2"], in_=t["t1"], func=AF.Exp), r=[K_(d, "t1")], w=[K_(d, "t2")])
            P.dve(lambda e: e.tensor_tensor(out=qt_, in0=qsl, in1=t["t2"], op=ALU.mult), r=[kqb, K_(d, "t2")], w=[kq])
            P.act(lambda e: e.activation(out=t["t2"], in_=t["t1"], func=AF.Exp, scale=-1.0), r=[K_(d, "t1"), kq], w=[K_(d, "t2")])
            P.pool(lambda e: e.tensor_tensor(out=kt_, in0=t["kk"], in1=t["t2"], op=ALU.mult), r=[K_(d, "kk"), K_(d, "t2")], w=[kk_])
            P.act(lambda e: e.activation(out=t["sgm"], in_=t["bb"], func=AF.Exp), r=[K_(d, "bb")], w=[K_(d, "sgm")])
            P.dve(lambda e: e.tensor_tensor(out=qb_, in0=qsl, in1=t["sgm"], op=ALU.mult), r=[kqb, K_(d, "sgm")], w=[kb_])
            P.pool(lambda e: e.tensor_tensor(out=v3(t["t1"]), in0=v3(t["bb"]), in1=blast, op=ALU.subtract), r=[K_(d, "bb"), K_(d, "t2")], w=[K_(d, "t1")])
            P.act(lambda e: e.activation(out=t["t2"], in_=t["t1"], func=AF.Exp, scale=-1.0), r=[K_(d, "t1"), kk_], w=[K_(d, "t2")])
            P.pool(lambda e: e.tensor_tensor(out=t["kbT"], in0=t["kk"], in1=t["t2"], op=ALU.mult), r=[K_(d, "kk"), K_(d, "t2")], w=[K_(d, "kbT")])
            P.act(lambda e: e.activation(out=dec_, in_=v3(t["bb"])[:, :, ilast], func=AF.Exp), r=[K_(d, "bb")], w=[kd])
            def tr(e):
                ins = None
                for tl in range(4):
                    ins = e.transpose(out=PSB(5)[:, d * 512 + tl * 128:d * 512 + (tl + 1) * 128], in_=t["kbT"][:, tl * 128:(tl + 1) * 128], identity=ident)
                return ins
            P.pe(tr, r=[K_(d, "kbT"), "ident"], w=[("ps", 5)])
            cast_copy(kbtok_, PSB(5)[:, d * 512:(d + 1) * 512].rearrange("p (t n) -> p t n", t=4), [("ps", 5)], [kkb])

        def tile_info(idx, d):
            step, ts = idx // 4, idx % 4
            blk = step if d == 0 else 7 - step
            tl = ts if d == 0 else 3 - ts
            return blk, tl, blk * 4 + tl, step % 2, idx % 2

        def front(idx):
            for d in range(2):
                t = T[d]
                blk, tl, tile, buf, par = tile_info(idx, d)
                tsl = slice(tl * 128, (tl + 1) * 128)
                sT = PS(2)[:, (d * 2 + par) * 128:(d * 2 + par + 1) * 128]
                Ub = PS(3 + par)[:, d * 256:(d + 1) * 256]
                order = (0, 1) if d == 0 else (1, 0)
                def mf(e, t=t, Ub=Ub, order=order, tl=tl, tile=tile, buf=buf, sT=sT, tsl=tsl):
                    ins = None
                    for c in (0, 1):
                        ci = order.index(c)
                        csl = slice(c * 64, (c + 1) * 64)
                        e.matmul(Ub[:, ci * 128:(ci + 1) * 128], lhsT=t["kbtok"][buf][csl, tl, :], rhs=vtok[csl, tile, :], start=True, stop=True)
                        ins = e.matmul(sT[:, c * 64:(c + 1) * 64], lhsT=t["kt"][buf][:, tsl], rhs=t["qt"][buf][:, tl * 128 + c * 64:tl * 128 + (c + 1) * 64],
                                       start=True, stop=True)
                    return ins
                P.pe(mf, r=[K_(d, ("kbtok", buf)), ("vtok", blk), K_(d, ("kt", buf)), K_(d, ("qt", buf))], w=[("ps", 3 + par), ("ps", 2)], c=0.5)
            for d in range(2):
                t = T[d]
                blk, tl, tile, buf, par = tile_info(idx, d)
                sT = PS(2)[:, (d * 2 + par) * 128:(d * 2 + par + 1) * 128]
                P.dve(lambda e, t=t, sT=sT, par=par, d=d: e.tensor_tensor(out=t["sTm"][par], in0=sT, in1=mk[d], op=ALU.mult),
                      r=[("ps", 2), ("mk", d)], w=[K_(d, ("sTm", par))], c=0.2)

        def back(idx):
            for d in range(2):
                t = T[d]
                blk, tl, tile, buf, par = tile_info(idx, d)
                Ub = PS(3 + par)[:, d * 256:(d + 1) * 256]
                order = (0, 1) if d == 0 else (1, 0)
                for ci, c in enumerate(order):
                    cib = tl * 2 + c
                    so, sn_ = s_cur[d], s_cur[d] ^ 1
                    s_cur[d] = sn_
                    P.dve(lambda e, t=t, d=d, ci=ci, cib=cib, Ub=Ub, buf=buf, so=so, sn_=sn_: e.scalar_tensor_tensor(
                        out=Sst[d][sn_], in0=Sst[d][so], scalar=t["dec"][buf][:, cib:cib + 1], in1=Ub[:, ci * 128:(ci + 1) * 128], op0=ALU.mult, op1=ALU.add),
                        r=[K_(d, ("S", so)), K_(d, ("dec", buf)), ("ps", 3 + par)], w=[K_(d, ("S", sn_))], c=0.2)
                    dpar, dslot = (par, 1) if ci == 0 else (par ^ 1, 0)
                    P.act(lambda e, t=t, d=d, dpar=dpar, dslot=dslot, sn_=sn_: e.copy(out=t["Sp"][dpar][dslot], in_=Sst[d][sn_]),
                          r=[K_(d, ("S", sn_))], w=[K_(d, ("Sp", dpar, dslot))], c=0.2)
            for d in range(2):
                t = T[d]
                blk, tl, tile, buf, par = tile_info(idx, d)
                acc = PS((1, 7)[par])[:, d * 128:(d + 1) * 128]
                order = (0, 1) if d == 0 else (1, 0)
                def ma(e, t=t, acc=acc, order=order, tl=tl, tile=tile, buf=buf, par=par):
                    e.matmul(acc, lhsT=vtok[:, tile, :], rhs=t["sTm"][par], start=True, stop=False, skip_group_check=True)
                    ins = None
                    for ci, c in enumerate(order):
                        ins = e.matmul(acc[:, c * 64:(c + 1) * 64], lhsT=t["Sp"][par][ci], rhs=t["qb"][buf][:, tl * 128 + c * 64:tl * 128 + (c + 1) * 64],
                                       start=False, stop=True, skip_group_check=True)
                    return ins
                P.pe(ma, r=[("vtok", blk), K_(d, ("sTm", par)), K_(d, ("Sp", par, 0)), K_(d, ("Sp", par, 1)), K_(d, ("qb", buf))], w=[("ps", (1, 7)[par])], c=0.4)
            for d in range(2):
                blk, tl, tile, buf, par = tile_info(idx, d)
                acc = PS((1, 7)[par])[:, d * 128:(d + 1) * 128]
                osl = oTh[:, tile * 128:(tile + 1) * 128]
                first = (blk < 4) if d == 0 else (blk >= 4)
                if first:
                    P.act(lambda e, osl=osl, acc=acc: e.copy(out=osl, in_=acc), r=[("ps", (1, 7)[par])], w=[("oTh", tile)], c=0.25)
                else:
                    P.dve(lambda e, osl=osl, acc=acc: e.tensor_tensor(out=osl, in0=osl, in1=acc, op=ALU.add),
                          r=[("ps", (1, 7)[par]), ("oTh", tile)], w=[("oTh", tile)], c=0.2)

        def record_list(fn):
            saved = P.ops
            P.ops = []
            fn()
            lst = P.ops
            P.ops = saved
            return lst

        for h in range(DBG["hg_heads"]):
            wb = wh[h % 2]
            kwb = ("wh", h % 2)
            for p_ in range(5):
                P.dma(lambda e, p_=p_, h=h, wb=wb: e.dma_start(
                    out=wb[:, :, p_, :], in_=hg_w_in[:, p_ * D + h * 128:p_ * D + (h + 1) * 128].rearrange("(k q) n -> q k n", q=128)),
                    w=[kwb], q="pool")
            for d in range(2):
                s_cur[d] = 0
                P.dve(lambda e, d=d: e.memset(Sst[d][0], 0.0), w=[K_(d, ("S", 0))])
                P.dve(lambda e, d=d: e.memset(T[d]["Sp"][0][0], 0.0), w=[K_(d, ("Sp", 0, 0))])
            if not DBG.get("hg_manual", 0):
                for step in range(8):
                    for d in range(2):
                        prep(h, d, step if d == 0 else 7 - step, step % 2, wb, kwb, part="a")
                    for d in range(2):
                        prep(h, d, step if d == 0 else 7 - step, step % 2, wb, kwb, part="b")
                    for ts in range(4):
                        front(step * 4 + ts)
                        back(step * 4 + ts)
            for d in range(2):
                if DBG.get("hg_manual", 0):
                    prep(h, d, 0 if d == 0 else 7, 0, wb, kwb)
            for step in (range(8) if DBG.get("hg_manual", 0) else ()):
                pl = []
                if step < 7:
                    pl = record_list(lambda: [prep(h, d, (step + 1) if d == 0 else 7 - (step + 1), (step + 1) % 2, wb, kwb) for d in range(2)])
                nq = (len(pl) + 3) // 4
                if not DBG.get("hg_merge", 1):
                    P.ops.extend(pl)
                    pl = []
                for ts in range(4):
                    idx = step * 4 + ts
                    front(idx)
                    if DBG.get("hg_lag", 1):
                        if idx >= 1:
                            back(idx - 1)
                    else:
                        back(idx)
                    P.ops.extend(pl[ts * nq:(ts + 1) * nq])
            if DBG.get("hg_lag", 1) and DBG.get("hg_manual", 0):
                back(31)
            for half in range(2):
                for tb4 in range(4):
                    tb = half * 4 + tb4
                    sl = slice(tb * 512, (tb + 1) * 512)
                    rsl = slice(tb4 * 512, (tb4 + 1) * 512)
                    okeys = [("oTh", i) for i in range(tb * 4, tb * 4 + 4)]
                    P.act(lambda e, sl=sl: e.activation(out=o2, in_=oTh[:, sl], func=AF.Square), r=okeys, w=["o2"])
                    P.pe(lambda e: e.matmul(PS(7), lhsT=ones_m, rhs=o2, start=True, stop=True), r=["o2", "ones_m"], w=[("ps", 7)], c=0.9)
                    P.act(lambda e: e.activation(out=rsn, in_=PS(7), func=AF.Ln, bias=EPS), r=[("ps", 7)], w=["rsn"])
                    P.act(lambda e, rsl=rsl: e.activation(out=rst[:, rsl], in_=rsn, func=AF.Exp, scale=-0.5), r=["rsn"], w=["rst", "normA"])
                for tb4 in range(4):
                    tb = half * 4 + tb4
                    sl = slice(tb * 512, (tb + 1) * 512)
                    rsl = slice(tb4 * 512, (tb4 + 1) * 512)
                    par = tb % 2
                    okeys = [("oTh", i) for i in range(tb * 4, tb * 4 + 4)]
                    def mm(e, sl=sl, wb=wb):
                        ins = None
                        for k in range(8):
                            ins = e.matmul(PS(6), lhsT=wb[:, k, 4, :], rhs=xT3[:, k, sl], start=(k == 0), stop=(k == 7))
                        return ins
                    P.pe(mm, r=[kwb] + xT_all[tb * 4:tb * 4 + 4], w=[("ps", 6)])
                    P.act(lambda e: e.activation(out=sgt, in_=PS(6), func=AF.Silu), r=[("ps", 6), "normA"], w=["sgt"])
                    P.dve(lambda e, sl=sl, rsl=rsl: e.scalar_tensor_tensor(out=o2, in0=oTh[:, sl], scalar=ng[:, 0:1], in1=rst[:, rsl], op0=ALU.mult, op1=ALU.mult),
                          r=okeys + ["rst", "ng", "o2"], w=["o2"])
                    P.dve(lambda e, par=par: e.tensor_tensor(out=onb[par], in0=o2, in1=sgt, op=ALU.mult), r=["o2", "sgt"], w=[("onb", par)])
                    P.dma(lambda e, h=h, sl=sl, par=par: e.dma_start(out=ONT[h * 128:(h + 1) * 128, sl], in_=onb[par]), r=[("onb", par)], w=["ONT"])
        P.nosched = False
        P.barrier()
        A.release()
        w_out = A.alloc(8 * D, BF16).rearrange("p (k n) -> p k n", k=8)
        load_w_bf16(w_out, hg_w_out, 8, D, "hw_out")
        load_gb(1, 0)
        E = Epi()
        onl = [A.alloc(8 * 512, BF16).rearrange("p (k t) -> p k t", k=8) for _ in range(2)]
        for grp in range(8):
            gb_ = grp % 2
            P.dma(lambda e, grp=grp, gb_=gb_: e.dma_start(out=onl[gb_], in_=ONT[:, grp * 512:(grp + 1) * 512].rearrange("(k p) t -> p k t", p=128)),
                  r=["ONT"], w=[("onl", gb_)])
            for tl in range(4):
                i = grp * 4 + tl
                for hh in range(2):
                    bank = (i % 2) * 2 + hh
                    def mm(e, tl=tl, hh=hh, bank=bank, gb_=gb_):
                        ins = None
                        for k in range(8):
                            ins = e.matmul(PS(bank), lhsT=onl[gb_][:, k, tl * 128:(tl + 1) * 128], rhs=w_out[:, k, hh * 512:(hh + 1) * 512],
                                           start=(k == 0), stop=(k == 7))
                        return ins
                    P.pe(mm, r=["hw_out", ("onl", gb_)], w=[("ps", bank)])
                b0 = (i % 2) * 2
                epilogue(E, i, [PS(b0), PS(b0 + 1)], [("ps", b0), ("ps", b0 + 1)], cur_res, dst_res)
        P.barrier()
        A.release()

    plan = []
    if upto >= 1 and not DBG["skip_l0"]:
        plan.append(("l0", 0))
    if upto >= 2:
        plan.append(("xa", 0))
    if upto >= 3:
        plan.append(("moe", 0))
    if upto >= 4:
        plan.append(("hg", 1))
    if upto >= 5:
        plan.append(("xa", 1))
    if upto >= 6:
        plan.append(("moe", 1))
    plan = [p for p in plan if p[0] not in DBG["skip"]]
    for pi, (kind, l) in enumerate(plan):
        last = (pi == len(plan) - 1)
        dst_res = out_d if last else XR[nxt]
        if kind == "l0":
            l0_phase(cur_res, dst_res)
        elif kind == "xa":
            xa_phase(l, cur_res, dst_res)
        elif kind == "moe":
            moe_phase(l, cur_res, dst_res, want_xT=(l == 0))
        elif kind == "hg":
            hg_phase(cur_res, dst_res)
        cur_res = dst_res
        nxt ^= 1

    final_keys = ["dst_dram"] + [("dst", i) for i in range(NT)]
    if cur_res is not out_d:
        A.mark()
        cp = [A.alloc(D, F32) for _ in range(2)]
        for i in range(NT):
            b = i % 2
            P.dma(lambda e, i=i, b=b: e.dma_start(out=cp[b], in_=cur_res[i * 128:(i + 1) * 128, :]), r=["dst_dram"], w=[("cp", b)])
            P.dma(lambda e, i=i, b=b: e.dma_start(out=out_d[i * 128:(i + 1) * 128, :], in_=cp[b]), r=[("cp", b)], w=["out_final"])
        final_keys.append("out_final")
        A.release()
    cnt, dcnt = P.emit(final_keys=final_keys)
    st.close()
    return nc, cnt, dcnt


def make_consts():
    import ml_dtypes
    s = np.arange(S, dtype=np.int64)
    ang = 2.0 * np.pi * ((s[:, None] * s[None, :]) % S).astype(np.float64) / S
    sc = 1.0 / np.sqrt(S)
    cs = (np.cos(ang) * sc).astype(np.float32).astype(ml_dtypes.bfloat16)
    ss = (-np.sin(ang) * sc).astype(np.float32).astype(ml_dtypes.bfloat16)
    c = np.arange(128, dtype=np.int64)
    angc = 2.0 * np.pi * ((c[:, None] * c[None, :]) % 128).astype(np.float64) / 128
    scc = 1.0 / np.sqrt(128.0)
    dc = np.concatenate([np.cos(angc) * scc, np.sin(angc) * scc], axis=1).astype(np.float32)
    t = np.arange(S)
    invcnt = np.zeros((4, S), np.float32)
    for gi, w in enumerate((2, 4, 8, 16)):
        lo = np.clip(t - w // 2, 0, S)
        hi = np.clip(t + w // 2, 0, S)
        invcnt[gi] = 1.0 / (hi - lo).astype(np.float32)
    m = np.arange(128)
    same = (m[:, None] // 64) == (m[None, :] // 64)
    masks = np.zeros((4, 128, 128), np.float32)
    masks[0] = (same & (m[:, None] <= m[None, :]))
    masks[1] = (same & (m[:, None] >= m[None, :]))
    masks[2] = (m[:, None] < m[None, :])
    masks[3] = 1.0
    eoff = (np.arange(32, dtype=np.float32) * CAP).reshape(1, 32)
    trash = (NEXP * CAP + np.arange(128, dtype=np.float32)).reshape(128, 1)
    altc = np.repeat((sc * np.cos(np.pi * np.arange(128))).astype(np.float32).reshape(128, 1), NT, axis=1)
    return {"c_cs": cs, "c_ss": ss, "c_dc": dc, "c_invcnt": invcnt, "c_masks": masks, "c_eoff": eoff, "c_trash": trash, "c_alt": np.ascontiguousarray(altc)}


_CACHE = {}


def kernel(**inputs):
    if "nc" not in _CACHE:
        _CACHE["nc"] = build()[0]
        _CACHE["consts"] = make_consts()
    nc = _CACHE["nc"]
    consts = _CACHE["consts"]
    in_maps = []
    for b in range(8):
        m = {}
        for k, v in inputs.items():
            v = np.asarray(v)
            if k in ("x", "mem"):
                m[k] = np.ascontiguousarray(v[b])
            elif k in ("pf_w_in", "pf_pool_w", "pf_pool_scale", "pf_fourier_ln_g", "pf_fourier_w", "pf_w_out",
                       "hg_w_in", "hg_norm_g", "hg_w_out"):
                m[k] = np.ascontiguousarray(v[0])
            else:
                m[k] = np.ascontiguousarray(v)
        m.update(consts)
        in_maps.append(m)
    res = run_bass_kernel_spmd(nc, in_maps, core_ids=list(range(8)))
    return np.stack([np.asarray(r["out"]) for r in res.results], axis=0).astype(np.float32)
```

```python
import numpy as np
from contextlib import ExitStack
import concourse.bass as bass
import concourse.mybir as mybir
from concourse.bass_utils import run_bass_kernel_spmd

F32 = mybir.dt.float32
BF16 = mybir.dt.bfloat16
I32 = mybir.dt.int32
ALU = mybir.AluOpType
AF = mybir.ActivationFunctionType
AX = mybir.AxisListType

S = 4096
D = 1024
NT = 32
MEM = 256
CAP = 512
NEXP = 32
ALPHA = 4.0 ** 0.25
EPS = 1e-5
EPOCH = 12000
DBG = {"skip_l0": False, "xa_groups": 8, "hg_heads": 8, "skip": ()}
NDS = 8


class Op:
    __slots__ = ("eng", "fn", "reads", "writes", "dma", "seq", "dn", "deps", "waits", "bar", "c", "ns")


class Prog:
    ENGS = ["pe", "act", "dve", "pool", "sp"]

    def __init__(self, nc):
        self.nc = nc
        self.ops = []

    DEFC = {"pe": 1.8, "act": 0.6, "dve": 0.6, "pool": 1.5, "sp": 4.0}

    def add(self, eng, fn, reads=(), writes=(), dma=False, c=None):
        o = Op()
        o.eng, o.fn, o.reads, o.writes, o.dma, o.bar = eng, fn, list(reads), list(writes), dma, False
        o.c = c if c is not None else (4.0 if dma else self.DEFC[eng])
        o.ns = getattr(self, "nosched", False)
        self.ops.append(o)
        return o

    def pe(self, fn, r=(), w=(), c=None):
        return self.add("pe", fn, r, w, c=c)

    def act(self, fn, r=(), w=(), c=None):
        return self.add("act", fn, r, w, c=c)

    def dve(self, fn, r=(), w=(), c=None):
        return self.add("dve", fn, r, w, c=c)

    def pool(self, fn, r=(), w=(), c=None):
        return self.add("pool", fn, r, w, c=c)

    def dma(self, fn, r=(), w=(), q="sp", c=None):
        return self.add(q, fn, r, w, dma=True, c=c)

    def barrier(self):
        for e in self.ENGS:
            o = self.add(e, None)
            o.bar = True

    WIN = {"pe": 48, "act": 48, "dve": 48, "pool": 1, "sp": 48}

    def reorder(self, window=48, sync_lat=0.2):
        ops = self.ops
        new_ops = []
        seg = []

        def flush():
            n = len(seg)
            if n == 0:
                return
            if any(getattr(op, "ns", False) for op in seg):
                new_ops.extend(seg)
                seg.clear()
                return
            last_w, readers = {}, {}
            preds = [set() for _ in range(n)]
            for j, op in enumerate(seg):
                for r in op.reads:
                    if r in last_w:
                        preds[j].add(last_w[r])
                for w in op.writes:
                    if w in last_w:
                        preds[j].add(last_w[w])
                    preds[j].update(readers.get(w, ()))
                preds[j].discard(j)
                for r in op.reads:
                    readers.setdefault(r, []).append(j)
                for w in op.writes:
                    last_w[w] = j
                    readers[w] = []
            queues = {e: [j for j, op in enumerate(seg) if op.eng == e] for e in self.ENGS}
            ptr = {e: 0 for e in self.ENGS}
            done = [False] * n
            fin = [0.0] * n
            t_e = {e: 0.0 for e in self.ENGS}
            left = n
            while left:
                best = None
                for e in self.ENGS:
                    q = queues[e]
                    p = ptr[e]
                    while p < len(q) and done[q[p]]:
                        p += 1
                    ptr[e] = p
                    seen = 0
                    k = p
                    while k < len(q) and seen < self.WIN[e]:
                        j = q[k]
                        k += 1
                        if done[j]:
                            continue
                        seen += 1
                        ok = True
                        rt = 0.0
                        for pr in preds[j]:
                            if not done[pr]:
                                ok = False
                                break
                            f = fin[pr] + sync_lat
                            if f > rt:
                                rt = f
                        if not ok:
                            continue
                        stt = max(t_e[e], rt)
                        key = (stt, j)
                        if best is None or key < best[0]:
                            best = (key, e, j)
                        if stt <= t_e[e]:
                            break
                assert best is not None, "scheduler deadlock"
                (stt, _), e, j = best
                op = seg[j]
                done[j] = True
                left -= 1
                if op.dma:
                    t_e[e] = stt + 0.15
                    fin[j] = stt + op.c
                else:
                    t_e[e] = stt + op.c
                    fin[j] = stt + op.c
                new_ops.append(op)
            seg.clear()

        i = 0
        while i < len(ops):
            if ops[i].bar:
                flush()
                while i < len(ops) and ops[i].bar:
                    new_ops.append(ops[i])
                    i += 1
            else:
                seg.append(ops[i])
                i += 1
        flush()
        self.ops = new_ops

    def emit(self, final_keys=(), reorder=True):
        nc = self.nc
        if reorder:
            self.reorder()
        ops = self.ops
        self.add("sp", None, final_keys, ())
        cnt = {e: 0 for e in self.ENGS}
        dcnt = {e: 0 for e in self.ENGS}
        dma_ops = {e: [] for e in self.ENGS}
        last_c = {e: None for e in self.ENGS}
        epos = {e: 0 for e in self.ENGS}
        for j, op in enumerate(ops):
            if op.dma:
                op.dn = dcnt[op.eng]
                dcnt[op.eng] += 1
                dma_ops[op.eng].append(j)
            elif op.fn is not None:
                epos[op.eng] += 1
                op.seq = epos[op.eng]
        last_w = {}
        readers = {}
        for j, op in enumerate(ops):
            deps = set()
            if op.bar:
                for e in self.ENGS:
                    if last_c[e] is not None:
                        deps.add(last_c[e])
                for e in self.ENGS:
                    lst = [i for i in dma_ops[e] if i < j]
                    deps.update(lst[-NDS:])
            for r in op.reads:
                if r in last_w:
                    deps.add(last_w[r])
            for w in op.writes:
                if w in last_w:
                    deps.add(last_w[w])
                deps.update(readers.get(w, ()))
            if op.dma and op.dn >= NDS:
                deps.add(dma_ops[op.eng][op.dn - NDS])
            deps.discard(j)
            op.deps = deps
            for r in op.reads:
                readers.setdefault(r, []).append(j)
            for w in op.writes:
                last_w[w] = j
                readers[w] = []
            if (not op.dma) and op.fn is not None:
                last_c[op.eng] = j
        wc = {e: {} for e in self.ENGS}
        wd = {e: {} for e in self.ENGS}
        signal = set()
        for j, op in enumerate(ops):
            waits = []
            me = op.eng
            for i in sorted(op.deps):
                d = ops[i]
                if d.dma:
                    k = (d.eng, d.dn % NDS)
                    val = 16 * (d.dn // NDS + 1)
                    if wd[me].get(k, 0) < val:
                        wd[me][k] = val
                        waits.append(("d", i))
                else:
                    if d.fn is None:
                        continue
                    if d.eng == "pe" and me == "pe":
                        continue
                    if wc[me].get(d.eng, 0) < d.seq:
                        wc[me][d.eng] = d.seq
                        waits.append(("c", i))
                        signal.add(i)
            op.waits = waits
        for j, op in enumerate(ops):
            if (not op.dma) and op.fn is not None:
                if j in signal:
                    cnt[op.eng] += 1
                    op.seq = cnt[op.eng]
                else:
                    op.seq = None
        with ExitStack() as st:
            csem = {}
            for e in self.ENGS:
                n_ep = (cnt[e] + EPOCH - 1) // EPOCH
                csem[e] = [st.enter_context(nc.semaphore(f"c_{e}_{k}")) for k in range(max(n_ep, 1))]
            dsem = {}
            for e in self.ENGS:
                if dcnt[e]:
                    dsem[e] = [st.enter_context(nc.semaphore(f"d_{e}_{k}")) for k in range(NDS)]
            for op in ops:
                ww = []
                for (kind, i) in op.waits:
                    d = ops[i]
                    if kind == "d":
                        ww.append((dsem[d.eng][d.dn % NDS], 16 * (d.dn // NDS + 1)))
                    else:
                        ww.append((csem[d.eng][(d.seq - 1) // EPOCH], (d.seq - 1) % EPOCH + 1))
                op.waits = ww
            streams = {e: [op for op in ops if op.eng == e] for e in self.ENGS}
            block = st.enter_context(nc.Block())

            def mk(ename):
                def body(e):
                    for op in streams[ename]:
                        for (sem, val) in op.waits:
                            e.wait_ge(sem, val)
                        if op.fn is None:
                            continue
                        ins = op.fn(e)
                        if op.dma:
                            ins.then_inc(dsem[ename][op.dn % NDS], 16)
                        elif op.seq is not None:
                            ins.then_inc(csem[ename][(op.seq - 1) // EPOCH], 1)
                return body

            block.tensor(mk("pe"))
            block.scalar(mk("act"))
            block.vector(mk("dve"))
            block.gpsimd(mk("pool"))
            block.sync(mk("sp"))
        return cnt, dcnt


class Arena:
    def __init__(self, ap32, nbytes):
        self.ap = ap32
        self.n = nbytes
        self.off = 0
        self.marks = []

    def alloc(self, free_elems, dt):
        sz = 2 if dt == BF16 else 4
        nb = (free_elems * sz + 31) // 32 * 32
        assert self.off + nb <= self.n, f"arena overflow {self.off}+{nb}>{self.n}"
        a = self.ap[:, self.off // 4:(self.off + nb) // 4]
        self.off += nb
        if dt != F32:
            a = a.bitcast(dt)
        return a[:, 0:free_elems]

    def mark(self):
        self.marks.append(self.off)

    def release(self):
        self.off = self.marks.pop()


def build(upto=99, dbg=False):
    nc = bass.Bass("TRN2", target_bir_lowering=False)
    P = Prog(nc)

    def din(name, shape, dt=F32):
        return nc.dram_tensor(name, list(shape), dt, kind="ExternalInput").ap()

    x_d = din("x", [S, D])
    mem_d = din("mem", [MEM, D])
    pf_w_in = din("pf_w_in", [D, D])
    pf_pool_w = din("pf_pool_w", [4, 128, 128])
    pf_pool_scale = din("pf_pool_scale", [512])
    pf_ln_g = din("pf_fourier_ln_g", [4, 128])
    pf_fw = din("pf_fourier_w", [4, 128, 128])
    pf_w_out = din("pf_w_out", [D, D])
    hg_w_in = din("hg_w_in", [D, 5 * D])
    hg_lb = din("hg_lower_bounds", [2, 2, D])
    hg_norm_g = din("hg_norm_g", [128])
    hg_w_out = din("hg_w_out", [D, D])
    xa_wq = din("xa_wq", [2, D, D])
    xa_wkv = din("xa_wkv", [2, D, 2 * D])
    xa_wo = din("xa_wo", [2, D, D])
    moe_wg = din("moe_w_group", [2, D, 4])
    moe_bg = din("moe_b_group", [2, 4])
    moe_we = din("moe_w_expert", [2, D, 32])
    moe_be = din("moe_b_expert", [2, 32])
    moe_gate = din("moe_w_gate", [2, NEXP, D, 512])
    moe_up = din("moe_w_up", [2, NEXP, D, 512])
    moe_down = din("moe_w_down", [2, NEXP, 512, D])
    ln_g = din("ln_g", [2, 3, D])
    ln_b = din("ln_b", [2, 3, D])
    c_cs = din("c_cs", [S, S], BF16)
    c_ss = din("c_ss", [S, S], BF16)
    c_dc = din("c_dc", [128, 256])
    c_invcnt = din("c_invcnt", [4, S])
    c_masks = din("c_masks", [4, 128, 128])
    c_eoff = din("c_eoff", [1, 32])
    c_trash = din("c_trash", [128, 1])
    c_alt = din("c_alt", [128, NT])
    out_d = nc.dram_tensor("out", [S, D], F32, kind="ExternalOutput").ap()
    XR = [nc.dram_tensor(f"xr{k}", [S, D], F32).ap() for k in range(2)]
    Zscr = nc.dram_tensor("zscr", [4, NT, 128, 256], BF16).ap()
    XS = nc.dram_tensor("xs_scr", [NEXP * CAP + 128, D], BF16).ap()
    YS = nc.dram_tensor("ys_scr", [NEXP * CAP + 128, D], F32).ap()
    ONT = nc.dram_tensor("ont_scr", [D, S], BF16).ap()

    st = ExitStack()
    ARENA_BYTES = 206 * 1024
    arena_t = st.enter_context(nc.sbuf_tensor("arena", [128, ARENA_BYTES // 4], F32))
    A = Arena(arena_t[:], ARENA_BYTES)
    psb = [st.enter_context(nc.psum_tensor(f"ps{k}", [128, 512], F32)) for k in range(8)]

    def PS(k):
        return psb[k][:]

    def PSB(k):
        return psb[k][:].bitcast(BF16)

    ident = A.alloc(128, BF16)
    ident32 = A.alloc(128, F32)
    ones_m = A.alloc(128, F32)
    gb = A.alloc(2 * D, F32)
    xT = A.alloc(8 * S, BF16)
    xT3 = xT.rearrange("p (k t) -> p k t", k=8)

    P.pool(lambda e: e.memset(ident32, 0.0), w=["ident32"])
    P.pool(lambda e: e.affine_select(out=ident32, in_=ident32, pattern=[[-1, 128]], compare_op=ALU.not_equal,
                                     fill=1.0, base=0, channel_multiplier=1), r=["ident32"], w=["ident32"])
    P.dve(lambda e: e.tensor_copy(out=ident, in_=ident32), r=["ident32"], w=["ident"])
    P.pool(lambda e: e.memset(ones_m, 1.0 / 128.0), w=["ones_m"])

    def ncd(fn):
        def g(e):
            with nc.allow_non_contiguous_dma(reason="tiny per-partition scalar tables"):
                return fn(e)
        return g

    cast_rr = [0]

    def cast_copy(out, in_, r, w):
        cast_rr[0] ^= 1
        if cast_rr[0]:
            P.act(lambda e: e.copy(out=out, in_=in_), r=r, w=w)
        else:
            P.dve(lambda e: e.tensor_copy(out=out, in_=in_), r=r, w=w)

    def transpose_to(dst3, src_bf, bank, rkeys, wkeys):
        def f(e):
            ins = None
            for k in range(8):
                ins = e.transpose(out=PSB(bank)[:, k * 128:(k + 1) * 128], in_=src_bf[:, k * 128:(k + 1) * 128],
                                  identity=ident)
            return ins
        P.pe(f, r=list(rkeys) + ["ident"], w=[("ps", bank)])
        cast_copy(dst3, PSB(bank).rearrange("p (k t) -> p k t", k=8), [("ps", bank)], wkeys)

    def load_w_bf16(dst3, src2d, kt, n, wkey, rows_per_dma=256):
        kk = max(1, rows_per_dma // 128)
        for k0 in range(0, kt, kk):
            k1 = min(kt, k0 + kk)
            P.dma(lambda e, k0=k0, k1=k1: e.dma_start(
                out=dst3[:, k0:k1, :], in_=src2d[k0 * 128:k1 * 128, :].rearrange("(k p) n -> p k n", p=128)),
                w=[wkey], q="pool")

    def load_gb(l, j):
        P.dma(lambda e: e.dma_start(out=gb[:, 0:D], in_=ln_g[l, j:j + 1, :].broadcast_to([128, D])), w=["gb"])
        P.dma(lambda e: e.dma_start(out=gb[:, D:2 * D], in_=ln_b[l, j:j + 1, :].broadcast_to([128, D])), w=["gb"])

    class Epi:
        def __init__(self):
            self.xr = [A.alloc(D, F32) for _ in range(2)]
            self.y = [A.alloc(D, F32) for _ in range(2)]
            self.xo = [A.alloc(D, F32) for _ in range(2)]
            self.xob = [A.alloc(D, BF16) for _ in range(2)]
            self.st = A.alloc(12, F32)
            self.mv = A.alloc(8, F32)

    def epilogue(E, i, hsrc, hkeys, res_ap, dst_ap, make_xT=True, tbank=7, gmul="pool"):
        b = i % 2
        xr, y, xo, xob = E.xr[b], E.y[b], E.xo[b], E.xob[b]
        kxr, ky, kxo, kxob = ("e_xr", b), ("e_y", b), ("e_xo", b), ("e_xob", b)
        P.dma(lambda e: e.dma_start(out=xr, in_=res_ap[i * 128:(i + 1) * 128, :]), r=["res_dram"], w=[kxr])
        for hh in range(2):
            P.dve(lambda e, hh=hh: e.scalar_tensor_tensor(
                out=y[:, hh * 512:(hh + 1) * 512], in0=xr[:, hh * 512:(hh + 1) * 512], scalar=ALPHA,
                in1=hsrc[hh], op0=ALU.mult, op1=ALU.add), r=[kxr] + list(hkeys), w=[ky])
        P.dve(lambda e: e.bn_stats(out=E.st[:, 0:6], in_=y[:, 0:512]), r=[ky], w=["e_st0"])
        P.dve(lambda e: e.bn_stats(out=E.st[:, 6:12], in_=y[:, 512:1024]), r=[ky], w=["e_st1"])
        P.dve(lambda e: e.bn_aggr(out=E.mv[:, 0:2], in_=E.st[:, 0:12]), r=["e_st0", "e_st1"], w=["e_mv"])
        P.act(lambda e: e.activation(out=E.mv[:, 2:3], in_=E.mv[:, 1:2], func=AF.Ln, bias=EPS), r=["e_mv"], w=["e_sd"], c=0.2)
        P.act(lambda e: e.activation(out=E.mv[:, 3:4], in_=E.mv[:, 2:3], func=AF.Exp, scale=-0.5), r=["e_sd"], w=["e_rs"], c=0.2)
        P.dve(lambda e: e.scalar_tensor_tensor(out=E.mv[:, 4:5], in0=E.mv[:, 0:1], scalar=-1.0, in1=E.mv[:, 3:4],
                                               op0=ALU.mult, op1=ALU.mult), r=["e_mv", "e_rs"], w=["e_nm"])
        P.act(lambda e: e.activation(out=y, in_=y, func=AF.Identity, bias=E.mv[:, 4:5], scale=E.mv[:, 3:4]),
              r=[ky, "e_rs", "e_nm"], w=[ky])
        if gmul == "dve":
            P.dve(lambda e: e.tensor_tensor(out=y, in0=y, in1=gb[:, 0:D], op=ALU.mult), r=[ky, "gb"], w=[ky], c=1.2)
        else:
            P.pool(lambda e: e.tensor_tensor(out=y, in0=y, in1=gb[:, 0:D], op=ALU.mult), r=[ky, "gb"], w=[ky], c=2.4)
        P.pool(lambda e: e.tensor_tensor(out=xo, in0=y, in1=gb[:, D:2 * D], op=ALU.add), r=[ky, "gb"], w=[kxo])
        P.dma(lambda e: e.dma_start(out=dst_ap[i * 128:(i + 1) * 128, :], in_=xo), r=[kxo], w=["dst_dram", ("dst", i)], q="pool")
        if make_xT:
            P.act(lambda e: e.copy(out=xob, in_=xo), r=[kxo], w=[kxob])
            transpose_to(xT3[:, :, i * 128:(i + 1) * 128], xob, tbank, [kxob], [("xT", i)])

    A.mark()
    xb0 = [A.alloc(D, BF16) for _ in range(2)]
    for i in range(NT):
        b = i % 2
        P.dma(lambda e, i=i, b=b: e.dma_start(out=xb0[b], in_=x_d[i * 128:(i + 1) * 128, :]), w=[("xb0", b)], q="pool")
        transpose_to(xT3[:, :, i * 128:(i + 1) * 128], xb0[b], 6 + b, [("xb0", b)], [("xT", i)])
    P.barrier()
    A.release()

    xT_all = [("xT", i) for i in range(NT)]
    cur_res = x_d
    nxt = 0

    def l0_phase(cur_res, dst_res):
        if True:
            A.mark()
            catT = A.alloc(8 * S, BF16)
            catT3 = catT.rearrange("p (k t) -> p k t", k=8)
            A.mark()
            w_in = A.alloc(8 * D, BF16).rearrange("p (k n) -> p k n", k=8)
            load_w_bf16(w_in, pf_w_in, 8, D, "w_in")
            poolw = A.alloc(4 * 128, BF16).rearrange("p (g n) -> p g n", g=4)
            fw = A.alloc(4 * 128, BF16).rearrange("p (g n) -> p g n", g=4)
            dc = A.alloc(256, BF16)
            P.dma(lambda e: e.dma_start(out=poolw, in_=pf_pool_w.rearrange("g c d -> c g d")), w=["poolw"], q="pool")
            P.dma(lambda e: e.dma_start(out=fw, in_=pf_fw.rearrange("g c d -> c g d")), w=["fw"], q="pool")
            P.dma(lambda e: e.dma_start(out=dc, in_=c_dc), w=["dc"], q="pool")
            pscale = A.alloc(4, F32)
            lng = A.alloc(4, F32)
            P.dma(ncd(lambda e: e.dma_start(out=pscale, in_=pf_pool_scale.rearrange("(g d) -> d g", g=4))), w=["pscale"])
            P.dma(ncd(lambda e: e.dma_start(out=lng, in_=pf_ln_g.rearrange("h c -> c h"))), w=["lng"])
            PADW = 16
            A.mark()
            a0 = A.alloc(S + 2 * PADW, F32)
            t1 = A.alloc(1024 + 2 * PADW, F32)
            t2 = A.alloc(1024 + 2 * PADW, F32)
            icnt = A.alloc(1024, F32)
            pm = A.alloc(1024, BF16)
            P.pool(lambda e: e.memset(a0, 0.0), w=["a0"])
            for g in range(4):
                for tb in range(8):
                    bank = tb % 2
                    def mm(e, g=g, tb=tb, bank=bank):
                        ins = None
                        for k in range(8):
                            ins = e.matmul(PS(bank), lhsT=w_in[:, k, g * 128:(g + 1) * 128],
                                           rhs=xT3[:, k, tb * 512:(tb + 1) * 512], start=(k == 0), stop=(k == 7))
                        return ins
                    P.pe(mm, r=["w_in"] + xT_all[tb * 4:tb * 4 + 4], w=[("ps", bank)])
                    P.act(lambda e, tb=tb, bank=bank: e.copy(out=a0[:, PADW + tb * 512:PADW + (tb + 1) * 512], in_=PS(bank)),
                          r=[("ps", bank)], w=["a0"])
                for blk in range(4):
                    s0 = PADW + blk * 1024
                    def lv(buf, lo, hi):
                        return buf[:, PADW + lo:PADW + 1024 + hi]
                    def a0v(lo, hi, s0=s0):
                        return a0[:, s0 + lo:s0 + 1024 + hi]
                    P.dma(lambda e, g=g, blk=blk: e.dma_start(
                        out=icnt, in_=c_invcnt[g:g + 1, blk * 1024:(blk + 1) * 1024].broadcast_to([128, 1024])),
                        w=["icnt"])
                    P.dve(lambda e, a0v=a0v, lv=lv: e.tensor_tensor(out=lv(t1, -8, 8), in0=a0v(-9, 7), in1=a0v(-8, 8), op=ALU.add),
                          r=["a0"], w=["t1"])
                    cur, curk, oth, othk = t1, "t1", t2, "t2"
                    ext = 8
                    for lev in range(1, g + 1):
                        sh = 1 << (lev - 1)
                        ne = ext - 2 * sh if lev < 3 else 0
                        ne = {1: 6, 2: 4, 3: 0}[lev]
                        P.dve(lambda e, cur=cur, oth=oth, sh=sh, ne=ne, lv=lv: e.tensor_tensor(
                            out=lv(oth, -ne, ne), in0=lv(cur, -ne - sh, ne - sh), in1=lv(cur, -ne + sh, ne + sh), op=ALU.add),
                            r=[curk], w=[othk])
                        cur, curk, oth, othk = oth, othk, cur, curk
                        ext = ne
                    P.dve(lambda e, cur=cur, oth=oth, lv=lv: e.tensor_tensor(out=lv(oth, 0, 0), in0=lv(cur, 0, 0), in1=icnt, op=ALU.mult),
                          r=[curk, "icnt"], w=[othk])
                    P.dve(lambda e, oth=oth, lv=lv, a0v=a0v: e.tensor_tensor(out=pm, in0=lv(oth, 0, 0), in1=a0v(0, 0), op=ALU.subtract),
                          r=[othk, "a0"], w=["pm"])
                    for hb in range(2):
                        bank = 2 + hb
                        P.pe(lambda e, g=g, hb=hb, bank=bank: e.matmul(PS(bank), lhsT=poolw[:, g, :], rhs=pm[:, hb * 512:(hb + 1) * 512],
                                                                      start=True, stop=True), r=["pm", "poolw"], w=[("ps", bank)])
                        c0 = blk * 1024 + hb * 512
                        P.act(lambda e, g=g, bank=bank, c0=c0: e.activation(out=catT3[:, g, c0:c0 + 512], in_=PS(bank), func=AF.Identity,
                                                                             scale=pscale[:, g:g + 1]),
                              r=[("ps", bank), "pscale"], w=[("catT", g)])
            ub = A.alloc(512, F32)
            dd = A.alloc(512, F32)
            d2 = A.alloc(512, F32)
            rs = A.alloc(512, F32)
            un = A.alloc(512, BF16)
            zs = [A.alloc(512, BF16) for _ in range(2)]
            for h in range(4):
                for tb in range(8):
                    def mm(e, h=h, tb=tb):
                        ins = None
                        for k in range(8):
                            ins = e.matmul(PS(0), lhsT=w_in[:, k, 512 + h * 128:512 + (h + 1) * 128],
                                           rhs=xT3[:, k, tb * 512:(tb + 1) * 512], start=(k == 0), stop=(k == 7))
                        return ins
                    P.pe(mm, r=["w_in"] + xT_all[tb * 4:tb * 4 + 4], w=[("ps", 0)])
                    P.act(lambda e: e.copy(out=ub, in_=PS(0)), r=[("ps", 0)], w=["ub"])
                    P.pe(lambda e: e.matmul(PS(1), lhsT=ones_m, rhs=ub, start=True, stop=True), r=["ub", "ones_m"], w=[("ps", 1)])
                    P.dve(lambda e: e.tensor_tensor(out=dd, in0=ub, in1=PS(1), op=ALU.subtract), r=["ub", ("ps", 1)], w=["dd"])
                    P.act(lambda e: e.activation(out=d2, in_=dd, func=AF.Square), r=["dd"], w=["d2"])
                    P.pe(lambda e: e.matmul(PS(1), lhsT=ones_m, rhs=d2, start=True, stop=True), r=["d2", "ones_m"], w=[("ps", 1)])
                    P.act(lambda e: e.activation(out=rs, in_=PS(1), func=AF.Ln, bias=EPS), r=[("ps", 1)], w=["rs"])
                    P.act(lambda e: e.activation(out=rs, in_=rs, func=AF.Exp, scale=-0.5), r=["rs"], w=["rs"])
                    P.dve(lambda e, h=h: e.scalar_tensor_tensor(out=un, in0=dd, scalar=lng[:, h:h + 1], in1=rs, op0=ALU.mult, op1=ALU.mult),
                          r=["dd", "rs", "lng"], w=["un"])
                    for pr in range(2):
                        bank = 2 + pr
                        def mz(e, pr=pr, bank=bank):
                            ins = None
                            for q in range(2):
                                tt = pr * 2 + q
                                ins = e.matmul(PS(bank)[:, q * 256:(q + 1) * 256], lhsT=un[:, tt * 128:(tt + 1) * 128], rhs=dc,
                                               start=True, stop=True)
                            return ins
                        P.pe(mz, r=["un", "dc"], w=[("ps", bank)])
                        cast_copy(zs[pr], PS(bank), [("ps", bank)], [("zs", pr)])
                        t0 = tb * 4 + pr * 2
                        P.dma(lambda e, h=h, t0=t0, pr=pr: e.dma_start(
                            out=Zscr[h, t0:t0 + 2].rearrange("t p c -> p t c"), in_=zs[pr].rearrange("p (t c) -> p t c", t=2)),
                            r=[("zs", pr)], w=["zscr"])
            P.barrier()
            A.release()
            A.release()
            A.mark()
            fw2 = A.alloc(4 * 128, BF16).rearrange("p (g n) -> p g n", g=4)
            P.dma(lambda e: e.dma_start(out=fw2, in_=pf_fw.rearrange("g c d -> c g d")), w=["fw2"], q="pool")
            zres = xT.rearrange("p (h t c) -> p h t c", h=4, t=NT)
            for h in range(4):
                P.dma(lambda e, h=h: e.dma_start(out=zres[:, h], in_=Zscr[h].rearrange("t p c -> p t c")), r=["zscr"], w=["zres"])
            NCH = 4
            dbuf = [[A.alloc(8 * 512, BF16).rearrange("p (s t) -> p s t", s=8) for _ in range(2)] for _ in range(2)]
            asb = [A.alloc(512, F32) for _ in range(4)]
            ypb = [A.alloc(512, BF16) for _ in range(4)]
            ymb = [A.alloc(512, BF16) for _ in range(4)]
            alt = A.alloc(NT, BF16)
            y2k = A.alloc(4, BF16)
            P.dma(lambda e: e.dma_start(out=alt, in_=c_alt), w=["alt"], q="pool")
            for h in range(4):
                def m2k(e, h=h):
                    ins = None
                    for stile in range(NT):
                        ins = e.matmul(PS(0)[:, h:h + 1], lhsT=zres[:, h, stile, 0:128], rhs=alt[:, stile:stile + 1],
                                       start=(stile == 0), stop=(stile == NT - 1))
                    return ins
                P.pe(m2k, r=["zres", "alt"], w=[("ps", 0)], c=2.5)
            P.dve(lambda e: e.tensor_copy(out=y2k, in_=PS(0)[:, 0:4]), r=[("ps", 0)], w=["y2k"])
            for h in range(4):
                P.pe(lambda e, h=h: e.matmul(PS(0)[:, 8 + h:9 + h], lhsT=fw2[:, h, :], rhs=y2k[:, h:h + 1], start=True, stop=True),
                     r=["y2k", "fw2"], w=[("ps", 0)], c=0.2)
            for h in range(4):
                P.dve(lambda e, h=h: e.tensor_copy(out=catT3[:, 4 + h, 2048:2049], in_=PS(0)[:, 8 + h:9 + h]), r=[("ps", 0)], w=[("catT", 4 + h)], c=0.1)
            cn = 0
            for kb in range(4):
                for sc in range(NCH):
                    bsel = cn % 2
                    cn += 1
                    for m, src in enumerate((c_cs, c_ss)):
                        P.dma(lambda e, m=m, src=src, sc=sc, kb=kb, bsel=bsel: e.dma_start(
                            out=dbuf[bsel][m], in_=src[sc * 1024:(sc + 1) * 1024, kb * 512:(kb + 1) * 512].rearrange("(s p) t -> p s t", p=128)),
                            w=[("dbuf", bsel, m)])
                    for h in range(4):
                        def mm(e, h=h, sc=sc, bsel=bsel):
                            ins = None
                            for s_ in range(8):
                                stile = sc * 8 + s_
                                first = (sc == 0 and s_ == 0)
                                last = (sc == NCH - 1 and s_ == 7)
                                e.matmul(PS(h), lhsT=zres[:, h, stile, 0:128], rhs=dbuf[bsel][0][:, s_, :], start=first, stop=last)
                                ins = e.matmul(PS(4 + h), lhsT=zres[:, h, stile, 128:256], rhs=dbuf[bsel][1][:, s_, :], start=first, stop=last)
                            return ins
                        P.pe(mm, r=["zres", ("dbuf", bsel, 0), ("dbuf", bsel, 1)], w=[("ps", h), ("ps", 4 + h)], c=3.6)
                for h in range(4):
                    P.act(lambda e, h=h: e.copy(out=asb[h], in_=PS(h)), r=[("ps", h)], w=[("asb", h)])
                    P.dve(lambda e, h=h: e.tensor_tensor(out=ypb[h], in0=asb[h], in1=PS(4 + h), op=ALU.add), r=[("asb", h), ("ps", 4 + h)], w=[("ypb", h)])
                    P.dve(lambda e, h=h: e.tensor_tensor(out=ymb[h], in0=asb[h], in1=PS(4 + h), op=ALU.subtract), r=[("asb", h), ("ps", 4 + h)], w=[("ymb", h)])
                    P.pe(lambda e, h=h: e.matmul(PS(h), lhsT=fw2[:, h, :], rhs=ypb[h], start=True, stop=True), r=[("ypb", h), "fw2"], w=[("ps", h)], c=0.3)
                    P.pe(lambda e, h=h: e.matmul(PS(4 + h), lhsT=fw2[:, h, :], rhs=ymb[h], start=True, stop=True), r=[("ymb", h), "fw2"], w=[("ps", 4 + h)], c=0.3)
                    P.act(lambda e, h=h, kb=kb: e.copy(out=catT3[:, 4 + h, kb * 512:(kb + 1) * 512], in_=PS(h)), r=[("ps", h)], w=[("catT", 4 + h)])
                    if kb == 0:
                        P.dve(lambda e, h=h: e.tensor_copy(out=catT3[:, 4 + h, 3585:4096][:, ::-1], in_=PS(4 + h)[:, 1:512]),
                              r=[("ps", 4 + h)], w=[("catT", 4 + h)])
                    else:
                        lo = S - kb * 512 - 511
                        P.dve(lambda e, h=h, lo=lo: e.tensor_copy(out=catT3[:, 4 + h, lo:lo + 512][:, ::-1], in_=PS(4 + h)),
                              r=[("ps", 4 + h)], w=[("catT", 4 + h)])
            P.barrier()
            A.release()
            if dbg:
                dbg_cat = nc.dram_tensor("dbg_cat", [D, S], BF16, kind="ExternalOutput").ap()
                P.dma(lambda e: e.dma_start(out=dbg_cat.rearrange("(k p) t -> p k t", p=128), in_=catT3), r=[("catT", k) for k in range(8)], w=["dbg_cat"])
            A.mark()
            w_out = A.alloc(8 * D, BF16).rearrange("p (k n) -> p k n", k=8)
            load_w_bf16(w_out, pf_w_out, 8, D, "w_out")
            load_gb(0, 0)
            E = Epi()
            for i in range(NT):
                for hh in range(2):
                    bank = (i % 2) * 2 + hh
                    def mm(e, i=i, hh=hh, bank=bank):
                        ins = None
                        for k in range(8):
                            ins = e.matmul(PS(bank), lhsT=catT3[:, k, i * 128:(i + 1) * 128], rhs=w_out[:, k, hh * 512:(hh + 1) * 512],
                                           start=(k == 0), stop=(k == 7))
                        return ins
                    P.pe(mm, r=["w_out"] + [("catT", k) for k in range(8)], w=[("ps", bank)])
                b0 = (i % 2) * 2
                epilogue(E, i, [PS(b0), PS(b0 + 1)], [("ps", b0), ("ps", b0 + 1)], cur_res, dst_res)
            P.barrier()
            A.release()
            A.release()


    def xa_phase(l, cur_res, dst_res):
        if True:
            A.mark()
            wq = A.alloc(8 * D, BF16).rearrange("p (k n) -> p k n", k=8)
            wo = A.alloc(8 * D, BF16).rearrange("p (k n) -> p k n", k=8)
            kT = A.alloc(8 * MEM, BF16).rearrange("p (k m) -> p k m", k=8)
            vtok = A.alloc(2 * D, BF16).rearrange("p (m n) -> p m n", m=2)
            A.mark()
            wkv = A.alloc(8 * 2 * D, BF16).rearrange("p (k n) -> p k n", k=8)
            memb = [A.alloc(D, BF16) for _ in range(2)]
            memT = A.alloc(8 * MEM, BF16).rearrange("p (k m) -> p k m", k=8)
            for j in range(2):
                P.dma(lambda e, j=j: e.dma_start(out=memb[j], in_=mem_d[j * 128:(j + 1) * 128, :]), w=[("memb", j)], q="pool")
                transpose_to(memT[:, :, j * 128:(j + 1) * 128], memb[j], 6 + j, [("memb", j)], ["memT"])
            for cb in range(2):
                for k0 in range(0, 8, 2):
                    P.dma(lambda e, cb=cb, k0=k0: e.dma_start(
                        out=wkv[:, k0:k0 + 2, cb * D:(cb + 1) * D],
                        in_=xa_wkv[l, k0 * 128:(k0 + 2) * 128, cb * D:(cb + 1) * D].rearrange("(k p) n -> p k n", p=128)),
                        w=["wkv"], q="pool")
            for ct in range(8):
                bank = ct % 2
                def mm(e, ct=ct, bank=bank):
                    ins = None
                    for k in range(8):
                        ins = e.matmul(PS(bank)[:, 0:MEM], lhsT=wkv[:, k, ct * 128:(ct + 1) * 128], rhs=memT[:, k, :],
                                       start=(k == 0), stop=(k == 7))
                    return ins
                P.pe(mm, r=["wkv", "memT"], w=[("ps", bank)])
                cast_copy(kT[:, ct, :], PS(bank)[:, 0:MEM], [("ps", bank)], ["kT"])
            for mt in range(2):
                for hb in range(2):
                    bank = 2 + hb
                    def mm(e, mt=mt, hb=hb, bank=bank):
                        ins = None
                        for k in range(8):
                            ins = e.matmul(PS(bank), lhsT=memT[:, k, mt * 128:(mt + 1) * 128],
                                           rhs=wkv[:, k, D + hb * 512:D + (hb + 1) * 512], start=(k == 0), stop=(k == 7))
                        return ins
                    P.pe(mm, r=["wkv", "memT"], w=[("ps", bank)])
                    cast_copy(vtok[:, mt, hb * 512:(hb + 1) * 512], PS(bank), [("ps", bank)], ["vtok"])
            P.nosched = False
            load_w_bf16(wq, xa_wq[l], 8, D, "wq")
            load_w_bf16(wo, xa_wo[l], 8, D, "wo")
            load_gb(l, 1)
            E = Epi()
            qT = A.alloc(8 * 512, BF16).rearrange("p (k t) -> p k t", k=8)
            oT = A.alloc(8 * 512, BF16).rearrange("p (k t) -> p k t", k=8)
            ex = A.alloc(MEM, F32)
            pb = A.alloc(MEM, BF16)
            pT = A.alloc(MEM, BF16).rearrange("p (m t) -> p m t", m=2)
            sm = A.alloc(8, F32)
            SCL = 1.0 / 16.0
            last_layer_xT = True
            for grp in range(DBG["xa_groups"]):
                for ct in range(8):
                    bank = 6
                    def mm(e, ct=ct, grp=grp, bank=bank):
                        ins = None
                        for k in range(8):
                            ins = e.matmul(PS(bank), lhsT=wq[:, k, ct * 128:(ct + 1) * 128], rhs=xT3[:, k, grp * 512:(grp + 1) * 512],
                                           start=(k == 0), stop=(k == 7))
                        return ins
                    P.pe(mm, r=["wq"] + xT_all[grp * 4:grp * 4 + 4], w=[("ps", bank)])
                    cast_copy(qT[:, ct, :], PS(bank), [("ps", bank)], [("qT", ct)])
                for tl in range(4):
                    i = grp * 4 + tl
                    tsl = slice(tl * 128, (tl + 1) * 128)
                    for h in range(4):
                        sc = PS(2 + h % 2)[:, 0:256]
                        ksc = ("ps", 2 + h % 2)
                        def ms(e, h=h, sc=sc, tsl=tsl):
                            ins = None
                            for dk in range(2):
                                ins = e.matmul(sc, lhsT=qT[:, 2 * h + dk, tsl], rhs=kT[:, 2 * h + dk, :], start=(dk == 0), stop=(dk == 1))
                            return ins
                        P.pe(ms, r=[("qT", 2 * h), ("qT", 2 * h + 1), "kT"], w=[ksc])
                        P.dve(lambda e, sc=sc: e.reduce_max(out=sm[:, 0:1], in_=sc, axis=AX.X), r=[ksc], w=["sm0"])
                        P.dve(lambda e: e.tensor_scalar(out=sm[:, 1:2], in0=sm[:, 0:1], scalar1=-SCL, scalar2=None, op0=ALU.mult), r=["sm0"], w=["sm1"])
                        P.act(lambda e, sc=sc: e.activation(out=ex, in_=sc, func=AF.Exp, bias=sm[:, 1:2], scale=SCL), r=[ksc, "sm1"], w=["ex"])
                        P.dve(lambda e: e.reduce_sum(out=sm[:, 2:3], in_=ex, axis=AX.X), r=["ex"], w=["sm2"])
                        P.dve(lambda e: e.reciprocal(out=sm[:, 3:4], in_=sm[:, 2:3]), r=["sm2"], w=["sm3"])
                        P.dve(lambda e: e.tensor_scalar(out=pb, in0=ex, scalar1=sm[:, 3:4], scalar2=None, op0=ALU.mult), r=["ex", "sm3"], w=["pb"])
                        ptq = PSB(4)[:, 0:256]
                        kpt = ("ps", 4)
                        def mt_(e, ptq=ptq):
                            ins = None
                            for mt in range(2):
                                ins = e.transpose(out=ptq[:, mt * 128:(mt + 1) * 128], in_=pb[:, mt * 128:(mt + 1) * 128], identity=ident)
                            return ins
                        P.pe(mt_, r=["pb", "ident"], w=[kpt])
                        cast_copy(pT, ptq.rearrange("p (m t) -> p m t", m=2), [kpt], ["pT"])
                        pv = PS((5, 7)[h % 2])[:, 0:256]
                        kpv = ("ps", (5, 7)[h % 2])
                        def mpv(e, h=h, pv=pv):
                            ins = None
                            for dv in range(2):
                                for mt in range(2):
                                    ins = e.matmul(pv[:, dv * 128:(dv + 1) * 128], lhsT=vtok[:, mt, h * 256 + dv * 128:h * 256 + (dv + 1) * 128],
                                                   rhs=pT[:, mt, :], start=(mt == 0), stop=(mt == 1))
                            return ins
                        P.pe(mpv, r=["pT", "vtok"], w=[kpv])
                        cast_copy(oT[:, 2 * h:2 * h + 2, tsl], pv.rearrange("p (d t) -> p d t", d=2), [kpv], [("oT", tl)])
                    for hh in range(2):
                        def mm(e, hh=hh, tsl=tsl):
                            ins = None
                            for k in range(8):
                                ins = e.matmul(PS(hh), lhsT=oT[:, k, tsl], rhs=wo[:, k, hh * 512:(hh + 1) * 512], start=(k == 0), stop=(k == 7))
                            return ins
                        P.pe(mm, r=["wo", ("oT", tl)], w=[("ps", hh)])
                    epilogue(E, i, [PS(0), PS(1)], [("ps", 0), ("ps", 1)], cur_res, dst_res, make_xT=False)
            P.nosched = False
            P.barrier()
            A.release()
            A.release()

    def moe_phase(l, cur_res, dst_res, want_xT):
        if True:
            A.mark()
            idx = A.alloc(NT * 2, I32 if False else F32).bitcast(I32).rearrange("p (t k) -> p t k", k=2)
            gates = A.alloc(NT * 2, F32).rearrange("p (t k) -> p t k", k=2)
            A.mark()
            wr32 = A.alloc(8 * 36, F32).rearrange("p (k n) -> p k n", k=8)
            P.dma(ncd(lambda e: e.dma_start(out=wr32[:, :, 0:4], in_=moe_wg[l].rearrange("(k p) n -> p k n", p=128))), w=["wr32"])
            P.dma(ncd(lambda e: e.dma_start(out=wr32[:, :, 4:36], in_=moe_we[l].rearrange("(k p) n -> p k n", p=128))), w=["wr32"])
            bias_bc = A.alloc(36, F32)
            P.dma(lambda e: e.dma_start(out=bias_bc[:, 0:4], in_=moe_bg[l:l + 1, :].broadcast_to([128, 4])), w=["bias_bc"])
            P.dma(lambda e: e.dma_start(out=bias_bc[:, 4:36], in_=moe_be[l:l + 1, :].broadcast_to([128, 32])), w=["bias_bc"])
            eoff = A.alloc(32, F32)
            P.dma(lambda e: e.dma_start(out=eoff, in_=c_eoff.broadcast_to([128, 32])), w=["eoff"])
            ustr = A.alloc(128, BF16)
            onesb = A.alloc(128, BF16)
            P.dma(lambda e: e.dma_start(out=ustr, in_=c_masks[2]), w=["ustr"], q="pool")
            P.dma(lambda e: e.dma_start(out=onesb, in_=c_masks[3]), w=["onesb"], q="pool")
            trash = A.alloc(1, F32)
            P.dma(ncd(lambda e: e.dma_start(out=trash, in_=c_trash)), w=["trash"])
            carry = A.alloc(32, F32)
            P.dve(lambda e: e.memset(carry, 0.0), w=["carry"])
            if DBG.get("zero_xs"):
                zt = A.alloc(D, BF16)
                P.pool(lambda e: e.memset(zt, 0.0), w=["zt"])
                for r0 in range(0, NEXP * CAP + 128, 128):
                    P.dma(lambda e, r0=r0: e.dma_start(out=XS[r0:r0 + 128, :], in_=zt), r=["zt"], w=["XS"])
            x2 = [A.alloc(D, F32) for _ in range(2)]
            xb2 = [A.alloc(D, BF16) for _ in range(2)]
            RB = []
            for _p in range(2):
                rb = {}
                rb["x2T"] = A.alloc(8 * 128, F32).rearrange("p (k t) -> p k t", k=8)
                for nm, n_ in (("lg", 36), ("rt", 64), ("maskg", 4), ("esel", 8), ("e2", 8), ("mask1", 8), ("mask2", 8),
                               ("M1", 32), ("M2", 32), ("Ms", 32), ("pos", 32), ("tmp", 32)):
                    rb[nm] = A.alloc(n_, F32)
                rb["Mb"] = A.alloc(32, BF16)
                RB.append(rb)
            def route_tile(i):
                b = i % 2
                rbp = b if DBG.get("route_double", 0) else 0
                rb = RB[rbp]
                x2T, lg, rt, maskg, esel, e2, mask1, mask2 = rb['x2T'], rb['lg'], rb['rt'], rb['maskg'], rb['esel'], rb['e2'], rb['mask1'], rb['mask2']
                M1, M2, Ms, Mb, pos, tmp = rb['M1'], rb['M2'], rb['Ms'], rb['Mb'], rb['pos'], rb['tmp']
                M1v = M1.rearrange('p (g j) -> p g j', g=4)
                M2v = M2.rearrange('p (g j) -> p g j', g=4)
                pb_ = 4 * rbp
                kx2, kxb = ("x2", b), ("xb2", b)
                P.dma(lambda e, i=i, b=b: e.dma_start(out=x2[b], in_=cur_res[i * 128:(i + 1) * 128, :]), w=[kx2])
                P.act(lambda e, b=b: e.copy(out=xb2[b], in_=x2[b]), r=[kx2], w=[kxb])
                for half in range(2):
                    def tr(e, half=half, b=b):
                        ins = None
                        for q in range(4):
                            k = half * 4 + q
                            ins = e.transpose(out=PS(pb_ + half)[:, q * 128:(q + 1) * 128], in_=x2[b][:, k * 128:(k + 1) * 128], identity=ident32)
                        return ins
                    P.pe(tr, r=[kx2, "ident32"], w=[("ps", pb_ + half)])
                    cast_copy(x2T[:, half * 4:(half + 1) * 4, :], PS(pb_ + half).rearrange("p (k t) -> p k t", k=4), [("ps", pb_ + half)], [("x2T", rbp)])
                def mlg(e):
                    ins = None
                    for k in range(8):
                        ins = e.matmul(PS(pb_ + 2)[:, 0:36], lhsT=x2T[:, k, :], rhs=wr32[:, k, :], start=(k == 0), stop=(k == 7))
                    return ins
                P.pe(mlg, r=[("x2T", rbp), "wr32"], w=[("ps", pb_ + 2)])
                V = P.dve
                V(lambda e: e.tensor_tensor(out=lg, in0=PS(pb_ + 2)[:, 0:36], in1=bias_bc, op=ALU.add), r=[("ps", pb_ + 2), "bias_bc"], w=[("lg", rbp)])
                V(lambda e: e.reduce_max(out=rt[:, 0:1], in_=lg[:, 0:4], axis=AX.X), r=[("lg", rbp)], w=[("rt0", rbp)])
                V(lambda e: e.tensor_scalar(out=maskg, in0=lg[:, 0:4], scalar1=rt[:, 0:1], scalar2=None, op0=ALU.is_equal), r=[("lg", rbp), ("rt0", rbp)], w=[("maskg", rbp)])
                V(lambda e: e.tensor_scalar(out=rt[:, 1:2], in0=rt[:, 0:1], scalar1=-1.0, scalar2=None, op0=ALU.mult), r=[("rt0", rbp)], w=[("rt1", rbp)])
                P.act(lambda e: e.activation(out=rt[:, 4:8], in_=lg[:, 0:4], func=AF.Exp, bias=rt[:, 1:2], scale=1.0), r=[("lg", rbp), ("rt1", rbp)], w=[("rt4", rbp)])
                V(lambda e: e.reduce_sum(out=rt[:, 2:3], in_=rt[:, 4:8], axis=AX.X), r=[("rt4", rbp)], w=[("rt2", rbp)])
                V(lambda e: e.reciprocal(out=rt[:, 3:4], in_=rt[:, 2:3]), r=[("rt2", rbp)], w=[("rt3", rbp)])
                V(lambda e: e.tensor_scalar(out=esel, in0=lg[:, 4:12], scalar1=maskg[:, 0:1], scalar2=None, op0=ALU.mult), r=[("lg", rbp), ("maskg", rbp)], w=[("esel", rbp)])
                for g in range(1, 4):
                    V(lambda e, g=g: e.scalar_tensor_tensor(out=esel, in0=lg[:, 4 + 8 * g:12 + 8 * g], scalar=maskg[:, g:g + 1], in1=esel,
                                                            op0=ALU.mult, op1=ALU.add), r=[("lg", rbp), ("maskg", rbp), ("esel", rbp)], w=[("esel", rbp)])
                V(lambda e: e.reduce_max(out=rt[:, 8:9], in_=esel, axis=AX.X), r=[("esel", rbp)], w=[("rt8", rbp)])
                V(lambda e: e.tensor_scalar(out=mask1, in0=esel, scalar1=rt[:, 8:9], scalar2=None, op0=ALU.is_equal), r=[("esel", rbp), ("rt8", rbp)], w=[("mask1", rbp)])
                V(lambda e: e.scalar_tensor_tensor(out=e2, in0=mask1, scalar=-1e30, in1=esel, op0=ALU.mult, op1=ALU.add), r=[("mask1", rbp), ("esel", rbp)], w=[("e2", rbp)])
                V(lambda e: e.reduce_max(out=rt[:, 9:10], in_=e2, axis=AX.X), r=[("e2", rbp)], w=[("rt9", rbp)])
                V(lambda e: e.tensor_scalar(out=mask2, in0=e2, scalar1=rt[:, 9:10], scalar2=None, op0=ALU.is_equal), r=[("e2", rbp), ("rt9", rbp)], w=[("mask2", rbp)])
                V(lambda e: e.tensor_tensor(out=rt[:, 10:11], in0=rt[:, 9:10], in1=rt[:, 8:9], op=ALU.subtract), r=[("rt8", rbp), ("rt9", rbp)], w=[("rt10", rbp)])
                P.act(lambda e: e.activation(out=rt[:, 11:12], in_=rt[:, 10:11], func=AF.Exp), r=[("rt10", rbp)], w=[("rt11", rbp)])
                V(lambda e: e.tensor_scalar(out=rt[:, 12:13], in0=rt[:, 11:12], scalar1=1.0, scalar2=None, op0=ALU.add), r=[("rt11", rbp)], w=[("rt12", rbp)])
                V(lambda e: e.reciprocal(out=rt[:, 13:14], in_=rt[:, 12:13]), r=[("rt12", rbp)], w=[("rt13", rbp)])
                V(lambda e, i=i: e.tensor_tensor(out=gates[:, i, 0:1], in0=rt[:, 3:4], in1=rt[:, 13:14], op=ALU.mult), r=[("rt3", rbp), ("rt13", rbp)], w=["gates"])
                V(lambda e, i=i: e.tensor_tensor(out=gates[:, i, 1:2], in0=gates[:, i, 0:1], in1=rt[:, 11:12], op=ALU.mult), r=["gates", ("rt11", rbp)], w=["gates"])
                for g in range(4):
                    V(lambda e, g=g: e.tensor_scalar(out=M1v[:, g, :], in0=mask1, scalar1=maskg[:, g:g + 1], scalar2=None, op0=ALU.mult),
                      r=[("mask1", rbp), ("maskg", rbp)], w=[("M1", rbp)])
                    V(lambda e, g=g: e.tensor_scalar(out=M2v[:, g, :], in0=mask2, scalar1=maskg[:, g:g + 1], scalar2=None, op0=ALU.mult),
                      r=[("mask2", rbp), ("maskg", rbp)], w=[("M2", rbp)])
                V(lambda e: e.tensor_tensor(out=Ms, in0=M1, in1=M2, op=ALU.add), r=[("M1", rbp), ("M2", rbp)], w=[("Ms", rbp)])
                V(lambda e: e.tensor_copy(out=Mb, in_=Ms), r=[("Ms", rbp)], w=[("Mb", rbp)])
                def mpos(e):
                    e.matmul(PS(pb_ + 3)[:, 0:32], lhsT=ustr, rhs=Mb, start=True, stop=True)
                    return e.matmul(PS(pb_ + 3)[:, 32:64], lhsT=onesb, rhs=Mb, start=True, stop=True)
                P.pe(mpos, r=[("Mb", rbp), "ustr", "onesb"], w=[("ps", pb_ + 3)])
                V(lambda e: e.tensor_tensor(out=pos, in0=PS(pb_ + 3)[:, 0:32], in1=carry, op=ALU.add), r=[("ps", pb_ + 3), "carry"], w=[("pos", rbp)])
                V(lambda e: e.tensor_tensor(out=carry, in0=PS(pb_ + 3)[:, 32:64], in1=carry, op=ALU.add), r=[("ps", pb_ + 3), "carry"], w=["carry"])
                V(lambda e: e.tensor_scalar(out=tmp, in0=pos, scalar1=float(CAP), scalar2=None, op0=ALU.is_ge), r=[("pos", rbp)], w=[("tmp", rbp)])
                V(lambda e: e.tensor_tensor(out=pos, in0=pos, in1=eoff, op=ALU.add), r=[("pos", rbp), "eoff"], w=[("pos", rbp)])
                V(lambda e: e.tensor_scalar(out=Ms, in0=pos, scalar1=-1.0, scalar2=trash[:, 0:1], op0=ALU.mult, op1=ALU.add), r=[("pos", rbp), "trash"], w=[("Ms", rbp)])
                V(lambda e: e.tensor_tensor(out=tmp, in0=tmp, in1=Ms, op=ALU.mult), r=[("tmp", rbp), ("Ms", rbp)], w=[("tmp", rbp)])
                V(lambda e: e.tensor_tensor(out=pos, in0=pos, in1=tmp, op=ALU.add), r=[("pos", rbp), ("tmp", rbp)], w=[("pos", rbp)])
                for kk, Mk in enumerate((M1, M2)):
                    kM = ("M1", rbp) if kk == 0 else ("M2", rbp)
                    V(lambda e, Mk=Mk: e.tensor_tensor(out=tmp, in0=Mk, in1=pos, op=ALU.mult), r=[kM, ("pos", rbp)], w=[("tmp", rbp)])
                    V(lambda e, kk=kk: e.reduce_sum(out=rt[:, 16 + kk:17 + kk], in_=tmp, axis=AX.X), r=[("tmp", rbp)], w=[("rtidx", kk, rbp)])
                    V(lambda e, kk=kk, i=i: e.tensor_copy(out=idx[:, i, kk:kk + 1], in_=rt[:, 16 + kk:17 + kk]), r=[("rtidx", kk, rbp)], w=[("idx", i, kk)])
                    P.dma(lambda e, kk=kk, i=i, b=b: e.indirect_dma_start(
                        out=XS, out_offset=bass.IndirectOffsetOnAxis(idx[:, i, kk:kk + 1], 0), in_=xb2[b], in_offset=None), r=[("idx", i, kk), kxb], w=["XS"], q="pool")
            for i in range(NT):
                route_tile(i)
            P.barrier()
            A.release()
            A.mark()
            NCT = CAP // 128
            xT_f32 = xT.bitcast(F32)
            stg = [xT_f32[:, k * 2048:(k + 1) * 2048] for k in range(6)]
            wgb = [A.alloc(8 * 512, BF16).rearrange("p (k n) -> p k n", k=8) for _ in range(2)]
            wub = [A.alloc(8 * 512, BF16).rearrange("p (k n) -> p k n", k=8) for _ in range(2)]
            wdb = [A.alloc(4 * D, BF16).rearrange("p (k n) -> p k n", k=4) for _ in range(2)]
            xsb = [A.alloc(D, BF16) for _ in range(4)]
            xsT2 = [A.alloc(8 * CAP, BF16).rearrange("p (k t) -> p k t", k=8) for _ in range(2)]
            hT2 = [A.alloc(4 * CAP, BF16).rearrange("p (k t) -> p k t", k=4) for _ in range(2)]
            sg2 = [A.alloc(CAP, F32) for _ in range(2)]
            ysb = [A.alloc(D, F32) for _ in range(4)]
            sn = 0
            for ex_i in range(NEXP):
                b = ex_i % 2
                xsT, hT, sg = xsT2[b], hT2[b], sg2[b]
                kxsT, khT, ksg = ("xsT", b), ("hT", b), ("sg", b)
                chunks = []
                for (wsrc, dstb, nm) in ((moe_gate, wgb, "wg"), (moe_up, wub, "wu")):
                    for c in range(2):
                        chunks.append((wsrc[l, ex_i, c * 512:(c + 1) * 512, :].rearrange("(k p) n -> p k n", p=128),
                                       dstb[b][:, c * 4:(c + 1) * 4, :], (nm, b), 4))
                for c in range(2):
                    chunks.append((moe_down[l, ex_i, c * 256:(c + 1) * 256, :].rearrange("(k p) n -> p k n", p=128),
                                   wdb[b][:, c * 2:(c + 1) * 2, :], ("wd", b), 2))
                for (src, dst, key, kk) in chunks:
                    sb_ = sn % 6
                    sn += 1
                    sview = stg[sb_].rearrange("p (k n) -> p k n", k=kk)
                    P.dma(lambda e, src=src, sview=sview: e.dma_start(out=sview, in_=src), w=[("stg", sb_)], q="pool")
                    if sn % 2 == 0:
                        P.act(lambda e, dst=dst, sview=sview: e.copy(out=dst, in_=sview), r=[("stg", sb_)], w=[key], c=2.0)
                    else:
                        P.dve(lambda e, dst=dst, sview=sview: e.tensor_copy(out=dst, in_=sview), r=[("stg", sb_)], w=[key], c=2.3)
                for stl in range(NCT):
                    xb_ = (ex_i * NCT + stl) % 4
                    r0 = ex_i * CAP + stl * 128
                    P.dma(lambda e, r0=r0, xb_=xb_: e.dma_start(out=xsb[xb_], in_=XS[r0:r0 + 128, :]), r=["XS"], w=[("xsb", xb_)], q="pool")
                    transpose_to(xsT[:, :, stl * 128:(stl + 1) * 128], xsb[xb_], 7, [("xsb", xb_)], [kxsT])
                for ft in range(4):
                    for m, wb_, kw in ((0, wgb, "wg"), (1, wub, "wu")):
                        def mm(e, ft=ft, m=m, wb_=wb_, b=b, xsT=xsT):
                            ins = None
                            for k in range(8):
                                ins = e.matmul(PS(m)[:, 0:CAP], lhsT=wb_[b][:, k, ft * 128:(ft + 1) * 128], rhs=xsT[:, k, :],
                                               start=(k == 0), stop=(k == 7))
                            return ins
                        P.pe(mm, r=[(kw, b), kxsT], w=[("ps", m)])
                    P.act(lambda e, sg=sg: e.activation(out=sg, in_=PS(0)[:, 0:CAP], func=AF.Silu), r=[("ps", 0)], w=[ksg])
                    P.dve(lambda e, ft=ft, sg=sg, hT=hT: e.tensor_tensor(out=hT[:, ft, :], in0=sg, in1=PS(1)[:, 0:CAP], op=ALU.mult), r=[ksg, ("ps", 1)], w=[khT])
                for stl in range(NCT):
                    yb_ = stl % 4
                    for hh in range(2):
                        bank = 2 + (stl % 2) * 2 + hh
                        def mm(e, stl=stl, hh=hh, bank=bank, b=b, hT=hT):
                            ins = None
                            for ft in range(4):
                                ins = e.matmul(PS(bank), lhsT=hT[:, ft, stl * 128:(stl + 1) * 128], rhs=wdb[b][:, ft, hh * 512:(hh + 1) * 512],
                                               start=(ft == 0), stop=(ft == 3))
                            return ins
                        P.pe(mm, r=[khT, ("wd", b)], w=[("ps", bank)])
                        if hh == 0:
                            P.act(lambda e, bank=bank, yb_=yb_: e.copy(out=ysb[yb_][:, 0:512], in_=PS(bank)), r=[("ps", bank)], w=[("ysb", yb_)])
                        else:
                            P.dve(lambda e, bank=bank, yb_=yb_: e.tensor_copy(out=ysb[yb_][:, 512:1024], in_=PS(bank)), r=[("ps", bank)], w=[("ysb", yb_)])
                    r0 = ex_i * CAP + stl * 128
                    P.dma(lambda e, r0=r0, yb_=yb_: e.dma_start(out=YS[r0:r0 + 128, :], in_=ysb[yb_]), r=[("ysb", yb_)], w=["YS"])
            P.barrier()
            A.release()
            A.mark()
            load_gb(l, 2)
            E = Epi()
            y1 = [A.alloc(D, F32) for _ in range(2)]
            y2 = [A.alloc(D, F32) for _ in range(2)]
            hm = [A.alloc(D, F32) for _ in range(2)]
            dst = dst_res
            def gather(i):
                b = i % 2
                P.dma(lambda e, i=i, b=b: e.indirect_dma_start(
                    out=y1[b], out_offset=None, in_=YS, in_offset=bass.IndirectOffsetOnAxis(idx[:, i, 0:1], 0)), r=["YS"], w=[("y1", b)], q="pool")
                P.dma(lambda e, i=i, b=b: e.indirect_dma_start(
                    out=y2[b], out_offset=None, in_=YS, in_offset=bass.IndirectOffsetOnAxis(idx[:, i, 1:2], 0)), r=["YS"], w=[("y2", b)], q="pool")
            gather(0)
            gather(1)
            for i in range(NT):
                b = i % 2
                P.dve(lambda e, i=i, b=b: e.tensor_scalar(out=hm[b], in0=y1[b], scalar1=gates[:, i, 0:1], scalar2=None, op0=ALU.mult),
                      r=[("y1", b)], w=[("hm", b)])
                P.dve(lambda e, i=i, b=b: e.scalar_tensor_tensor(out=hm[b], in0=y2[b], scalar=gates[:, i, 1:2], in1=hm[b], op0=ALU.mult, op1=ALU.add),
                      r=[("y2", b), ("hm", b)], w=[("hm", b)])
                if i + 2 < NT:
                    gather(i + 2)
                epilogue(E, i, [hm[b][:, 0:512], hm[b][:, 512:1024]], [("hm", b)], cur_res, dst, make_xT=want_xT, gmul="dve")
            P.barrier()
            A.release()
            A.release()


    def hg_phase(cur_res, dst_res):
        A.mark()
        A.mark()
        P.nosched = bool(DBG.get("hg_nosched", 0))
        lbraw = A.alloc(32, F32)
        lbt = A.alloc(32, F32)
        lbrows = A.alloc(128, F32)
        P.dma(lambda e: e.dma_start(out=lbrows[0:32, :], in_=hg_lb.rearrange("l d (h k) -> (l d h) k", k=128)), w=["lbrows"])
        P.pe(lambda e: e.transpose(out=PS(0)[:, 0:32], in_=lbrows[0:32, :], identity=ident32[0:32, 0:32]), r=["lbrows", "ident32"], w=[("ps", 0)], c=0.3)
        P.dve(lambda e: e.tensor_copy(out=lbraw, in_=PS(0)[:, 0:32]), r=[("ps", 0)], w=["lbraw"])
        P.dve(lambda e: e.tensor_tensor(out=lbt[:, 0:16], in0=lbraw[:, 16:32], in1=lbraw[:, 0:16], op=ALU.subtract), r=["lbraw"], w=["lbd"])
        P.act(lambda e: e.activation(out=lbt[:, 0:16], in_=lbt[:, 0:16], func=AF.Sigmoid), r=["lbd"], w=["lb"])
        P.dve(lambda e: e.tensor_scalar(out=lbt[:, 16:32], in0=lbt[:, 0:16], scalar1=-1.0, scalar2=1.0, op0=ALU.mult, op1=ALU.add), r=["lb"], w=["oml"])
        ng = A.alloc(1, F32)
        P.dma(ncd(lambda e: e.dma_start(out=ng, in_=hg_norm_g.rearrange("(k o) -> k o", o=1))), w=["ng"])
        cmask = A.alloc(512, F32)
        P.pool(lambda e: e.memset(cmask, 1.0), w=["cmask"])
        P.pool(lambda e: e.memset(cmask.rearrange("p (c l) -> p c l", l=64)[:, :, 0:1], 0.0), r=["cmask"], w=["cmask"])
        mk = [A.alloc(128, F32) for _ in range(2)]
        for d in range(2):
            P.dma(lambda e, d=d: e.dma_start(out=mk[d], in_=c_masks[d]), w=[("mk", d)])
        Sst = [[A.alloc(128, F32) for _ in range(2)] for _ in range(2)]
        s_cur = [0, 0]
        wh = [A.alloc(8 * 5 * 128, BF16).rearrange("p (k q n) -> p k q n", k=8, q=5) for _ in range(2)]
        rst = A.alloc(4 * 512, F32)
        vtok = A.alloc(NT * 128, BF16).rearrange("p (t n) -> p t n", t=NT)
        qT = A.alloc(S, F32)
        oTh = A.alloc(S, F32)
        T = []
        for d in range(2):
            t = {}
            for nm in ("sgm", "lgf", "bb", "kk", "t1", "t2"):
                t[nm] = A.alloc(512, F32)
            t["kbT"] = A.alloc(512, BF16)
            for nm in ("qt", "kt", "qb"):
                t[nm] = [A.alloc(512, BF16) for _ in range(2)]
            t["kbtok"] = [A.alloc(512, BF16).rearrange("p (t n) -> p t n", t=4) for _ in range(2)]
            t["dec"] = [A.alloc(8, F32) for _ in range(2)]
            t["sTm"] = [A.alloc(128, BF16) for _ in range(2)]
            t["Sp"] = [[A.alloc(128, BF16) for _ in range(2)] for _ in range(2)]
            T.append(t)
        o2 = A.alloc(512, F32)
        rsn = A.alloc(512, F32)
        sgt = A.alloc(512, F32)
        onb = [A.alloc(512, BF16) for _ in range(2)]

        def K_(d, nm):
            return ("hg", d, nm)

        def v3(ap):
            return ap.rearrange("p (c l) -> p c l", l=64)

        def prep(h, d, blk, buf, wb, kwb, part="ab"):
            t = T[d]
            t0 = blk * 512
            col = d * 8 + h
            fb = (0, 6)[d]
            qt_, kt_, qb_, kbtok_, dec_ = t["qt"][buf], t["kt"][buf], t["qb"][buf], t["kbtok"][buf], t["dec"][buf]
            def mm(e):
                ins = None
                for k in range(8):
                    ins = e.matmul(PS(fb), lhsT=wb[:, k, 2 + d, :], rhs=xT3[:, k, t0:t0 + 512], start=(k == 0), stop=(k == 7))
                return ins
            if "a" in part:
                P.pe(mm, r=[kwb] + xT_all[blk * 4:blk * 4 + 4], w=[("ps", fb)])
                P.act(lambda e: e.activation(out=t["sgm"], in_=PS(fb), func=AF.Sigmoid), r=[("ps", fb)], w=[K_(d, "sgm"), "sigdone"])
                P.dve(lambda e: e.tensor_scalar(out=t["sgm"], in0=t["sgm"], scalar1=lbt[:, 16 + col:17 + col], scalar2=lbt[:, col:col + 1],
                                                op0=ALU.mult, op1=ALU.add), r=[K_(d, "sgm"), "lb", "oml"], w=[K_(d, "sgm")])
            if "a" in part and ((blk < 4) if d == 0 else (blk >= 4)):
                qblk_, vblk_ = qT[:, t0:t0 + 512], vtok[:, blk * 4:(blk + 1) * 4, :]
                def mq(e):
                    ins = None
                    for k in range(8):
                        ins = e.matmul(PS(fb), lhsT=wb[:, k, 0, :], rhs=xT3[:, k, t0:t0 + 512], start=(k == 0), stop=(k == 7))
                    return ins
                P.pe(mq, r=[kwb] + xT_all[blk * 4:blk * 4 + 4], w=[("ps", fb)])
                P.act(lambda e: e.activation(out=qblk_, in_=PS(fb), func=AF.Sigmoid), r=[("ps", fb)], w=[("qT", blk), "sigdone"])
                P.dve(lambda e: e.tensor_tensor(out=qblk_, in0=qblk_, in1=PS(fb), op=ALU.mult), r=[("qT", blk), ("ps", fb)], w=[("qT", blk)])
                def mv(e):
                    ins = None
                    for tl in range(4):
                        tile = blk * 4 + tl
                        for k in range(8):
                            ins = e.matmul(PS(fb)[:, tl * 128:(tl + 1) * 128], lhsT=xT3[:, k, tile * 128:(tile + 1) * 128], rhs=wb[:, k, 1, :],
                                           start=(k == 0), stop=(k == 7))
                    return ins
                P.pe(mv, r=[kwb] + xT_all[blk * 4:blk * 4 + 4], w=[("ps", fb)], c=2.5)
                cast_copy(vblk_, PS(fb).rearrange("p (t n) -> p t n", t=4), [("ps", fb)], [("vtok", blk)])
            if "b" not in part:
                return
            P.act(lambda e: e.activation(out=t["lgf"], in_=t["sgm"], func=AF.Ln), r=[K_(d, "sgm"), "sigdone"], w=[K_(d, "lgf")])
            P.pool(lambda e: e.tensor_scalar(out=t["kk"], in0=t["sgm"], scalar1=-1.0, scalar2=1.0, op0=ALU.mult, op1=ALU.add),
                   r=[K_(d, "sgm")], w=[K_(d, "kk")])
            if d == 0:
                P.dve(lambda e: e.tensor_tensor_scan(out=t["bb"], data0=cmask, data1=t["lgf"], initial=0.0, op0=ALU.mult, op1=ALU.add),
                      r=["cmask", K_(d, "lgf")], w=[K_(d, "bb")])
                iref, ilast = 32, 63
            else:
                P.dve(lambda e: e.tensor_tensor_scan(out=t["t2"], data0=cmask, data1=t["lgf"], initial=0.0, op0=ALU.mult, op1=ALU.add),
                      r=["cmask", K_(d, "lgf")], w=[K_(d, "t2")])
                P.dve(lambda e: e.scalar_tensor_tensor(out=t["t1"], in0=t["t2"], scalar=-1.0, in1=t["lgf"], op0=ALU.mult, op1=ALU.add),
                      r=[K_(d, "t2"), K_(d, "lgf")], w=[K_(d, "t1")])
                P.dve(lambda e: e.tensor_tensor(out=v3(t["bb"]), in0=v3(t["t1"]), in1=v3(t["t2"])[:, :, 63:64].broadcast_to([128, 8, 64]), op=ALU.add),
                      r=[K_(d, "t1"), K_(d, "t2")], w=[K_(d, "bb")])
                iref, ilast = 31, 0
            bref = v3(t["bb"])[:, :, iref:iref + 1].broadcast_to([128, 8, 64])
            blast = v3(t["bb"])[:, :, ilast:ilast + 1].broadcast_to([128, 8, 64])
            qsl = qT[:, t0:t0 + 512]
            kqb = ("qT", blk)
            kq, kk_, kb_, kkb, kd = K_(d, ("qt", buf)), K_(d, ("kt", buf)), K_(d, ("qb", buf)), K_(d, ("kbtok", buf)), K_(d, ("dec", buf))
            P.pool(lambda e: e.tensor_tensor(out=v3(t["t1"]), in0=v3(t["bb"]), in1=bref, op=ALU.subtract), r=[K_(d, "bb")], w=[K_(d, "t1")])
            P.act(lambda e: e.activation(out=t["t2"], in_=t["t1"], func=AF.Exp), r=[K_(d, "t1")], w=[K_(d, "t2")])
            P.dve(lambda e: e.tensor_tensor(out=qt_, in0=qsl, in1=t["t2"], op=ALU.mult), r=[kqb, K_(d, "t2")], w=[kq])
            P.act(lambda e: e.activation(out=t["t2"], in_=t["t1"], func=AF.Exp, scale=-1.0), r=[K_(d, "t1"), kq], w=[K_(d, "t2")])
            P.pool(lambda e: e.tensor_tensor(out=kt_, in0=t["kk"], in1=t["t2"], op=ALU.mult), r=[K_(d, "kk"), K_(d, "t2")], w=[kk_])
            P.act(lambda e: e.activation(out=t["sgm"], in_=t["bb"], func=AF.Exp), r=[K_(d, "bb")], w=[K_(d, "sgm")])
            P.dve(lambda e: e.tensor_tensor(out=qb_, in0=qsl, in1=t["sgm"], op=ALU.mult), r=[kqb, K_(d, "sgm")], w=[kb_])
            P.pool(lambda e: e.tensor_tensor(out=v3(t["t1"]), in0=v3(t["bb"]), in1=blast, op=ALU.subtract), r=[K_(d, "bb"), K_(d, "t2")], w=[K_(d, "t1")])
            P.act(lambda e: e.activation(out=t["t2"], in_=t["t1"], func=AF.Exp, scale=-1.0), r=[K_(d, "t1"), kk_], w=[K_(d, "t2")])
            P.pool(lambda e: e.tensor_tensor(out=t["kbT"], in0=t["kk"], in1=t["t2"], op=ALU.mult), r=[K_(d, "kk"), K_(d, "t2")], w=[K_(d, "kbT")])
            P.act(lambda e: e.activation(out=dec_, in_=v3(t["bb"])[:, :, ilast], func=AF.Exp), r=[K_(d, "bb")], w=[kd])
            def tr(e):
                ins = None
                for tl in range(4):
                    ins = e.transpose(out=PSB(5)[:, d * 512 + tl * 128:d * 512 + (tl + 1) * 128], in_=t["kbT"][:, tl * 128:(tl + 1) * 128], identity=ident)
                return ins
            P.pe(tr, r=[K_(d, "kbT"), "ident"], w=[("ps", 5)])
            cast_copy(kbtok_, PSB(5)[:, d * 512:(d + 1) * 512].rearrange("p (t n) -> p t n", t=4), [("ps", 5)], [kkb])

        def tile_info(idx, d):
            step, ts = idx // 4, idx % 4
            blk = step if d == 0 else 7 - step
            tl = ts if d == 0 else 3 - ts
            return blk, tl, blk * 4 + tl, step % 2, idx % 2

        def front(idx):
            for d in range(2):
                t = T[d]
                blk, tl, tile, buf, par = tile_info(idx, d)
                tsl = slice(tl * 128, (tl + 1) * 128)
                sT = PS(2)[:, (d * 2 + par) * 128:(d * 2 + par + 1) * 128]
                Ub = PS(3 + par)[:, d * 256:(d + 1) * 256]
                order = (0, 1) if d == 0 else (1, 0)
                def mf(e, t=t, Ub=Ub, order=order, tl=tl, tile=tile, buf=buf, sT=sT, tsl=tsl):
                    ins = None
                    for c in (0, 1):
                        ci = order.index(c)
                        csl = slice(c * 64, (c + 1) * 64)
                        e.matmul(Ub[:, ci * 128:(ci + 1) * 128], lhsT=t["kbtok"][buf][csl, tl, :], rhs=vtok[csl, tile, :], start=True, stop=True)
                        ins = e.matmul(sT[:, c * 64:(c + 1) * 64], lhsT=t["kt"][buf][:, tsl], rhs=t["qt"][buf][:, tl * 128 + c * 64:tl * 128 + (c + 1) * 64],
                                       start=True, stop=True)
                    return ins
                P.pe(mf, r=[K_(d, ("kbtok", buf)), ("vtok", blk), K_(d, ("kt", buf)), K_(d, ("qt", buf))], w=[("ps", 3 + par), ("ps", 2)], c=0.5)
            for d in range(2):
                t = T[d]
                blk, tl, tile, buf, par = tile_info(idx, d)
                sT = PS(2)[:, (d * 2 + par) * 128:(d * 2 + par + 1) * 128]
                P.dve(lambda e, t=t, sT=sT, par=par, d=d: e.tensor_tensor(out=t["sTm"][par], in0=sT, in1=mk[d], op=ALU.mult),
                      r=[("ps", 2), ("mk", d)], w=[K_(d, ("sTm", par))], c=0.2)

        def back(idx):
            for d in range(2):
                t = T[d]
                blk, tl, tile, buf, par = tile_info(idx, d)
                Ub = PS(3 + par)[:, d * 256:(d + 1) * 256]
                order = (0, 1) if d == 0 else (1, 0)
                for ci, c in enumerate(order):
                    cib = tl * 2 + c
                    so, sn_ = s_cur[d], s_cur[d] ^ 1
                    s_cur[d] = sn_
                    P.dve(lambda e, t=t, d=d, ci=ci, cib=cib, Ub=Ub, buf=buf, so=so, sn_=sn_: e.scalar_tensor_tensor(
                        out=Sst[d][sn_], in0=Sst[d][so], scalar=t["dec"][buf][:, cib:cib + 1], in1=Ub[:, ci * 128:(ci + 1) * 128], op0=ALU.mult, op1=ALU.add),
                        r=[K_(d, ("S", so)), K_(d, ("dec", buf)), ("ps", 3 + par)], w=[K_(d, ("S", sn_))], c=0.2)
                    dpar, dslot = (par, 1) if ci == 0 else (par ^ 1, 0)
                    P.act(lambda e, t=t, d=d, dpar=dpar, dslot=dslot, sn_=sn_: e.copy(out=t["Sp"][dpar][dslot], in_=Sst[d][sn_]),
                          r=[K_(d, ("S", sn_))], w=[K_(d, ("Sp", dpar, dslot))], c=0.2)
            for d in range(2):
                t = T[d]
                blk, tl, tile, buf, par = tile_info(idx, d)
                acc = PS((1, 7)[par])[:, d * 128:(d + 1) * 128]
                order = (0, 1) if d == 0 else (1, 0)
                def ma(e, t=t, acc=acc, order=order, tl=tl, tile=tile, buf=buf, par=par):
                    e.matmul(acc, lhsT=vtok[:, tile, :], rhs=t["sTm"][par], start=True, stop=False, skip_group_check=True)
                    ins = None
                    for ci, c in enumerate(order):
                        ins = e.matmul(acc[:, c * 64:(c + 1) * 64], lhsT=t["Sp"][par][ci], rhs=t["qb"][buf][:, tl * 128 + c * 64:tl * 128 + (c + 1) * 64],
                                       start=False, stop=True, skip_group_check=True)
                    return ins
                P.pe(ma, r=[("vtok", blk), K_(d, ("sTm", par)), K_(d, ("Sp", par, 0)), K_(d, ("Sp", par, 1)), K_(d, ("qb", buf))], w=[("ps", (1, 7)[par])], c=0.4)
            for d in range(2):
                blk, tl, tile, buf, par = tile_info(idx, d)
                acc = PS((1, 7)[par])[:, d * 128:(d + 1) * 128]
                osl = oTh[:, tile * 128:(tile + 1) * 128]
                first = (blk < 4) if d == 0 else (blk >= 4)
                if first:
                    P.act(lambda e, osl=osl, acc=acc: e.copy(out=osl, in_=acc), r=[("ps", (1, 7)[par])], w=[("oTh", tile)], c=0.25)
                else:
                    P.dve(lambda e, osl=osl, acc=acc: e.tensor_tensor(out=osl, in0=osl, in1=acc, op=ALU.add),
                          r=[("ps", (1, 7)[par]), ("oTh", tile)], w=[("oTh", tile)], c=0.2)

        def record_list(fn):
            saved = P.ops
            P.ops = []
            fn()
            lst = P.ops
            P.ops = saved
            return lst

        for h in range(DBG["hg_heads"]):
            wb = wh[h % 2]
            kwb = ("wh", h % 2)
            for p_ in range(5):
                P.dma(lambda e, p_=p_, h=h, wb=wb: e.dma_start(
                    out=wb[:, :, p_, :], in_=hg_w_in[:, p_ * D + h * 128:p_ * D + (h + 1) * 128].rearrange("(k q) n -> q k n", q=128)),
                    w=[kwb], q="pool")
            for d in range(2):
                s_cur[d] = 0
                P.dve(lambda e, d=d: e.memset(Sst[d][0], 0.0), w=[K_(d, ("S", 0))])
                P.dve(lambda e, d=d: e.memset(T[d]["Sp"][0][0], 0.0), w=[K_(d, ("Sp", 0, 0))])
            if not DBG.get("hg_manual", 0):
                for step in range(8):
                    for d in range(2):
                        prep(h, d, step if d == 0 else 7 - step, step % 2, wb, kwb, part="a")
                    for d in range(2):
                        prep(h, d, step if d == 0 else 7 - step, step % 2, wb, kwb, part="b")
                    for ts in range(4):
                        front(step * 4 + ts)
                        back(step * 4 + ts)
            for d in range(2):
                if DBG.get("hg_manual", 0):
                    prep(h, d, 0 if d == 0 else 7, 0, wb, kwb)
            for step in (range(8) if DBG.get("hg_manual", 0) else ()):
                pl = []
                if step < 7:
                    pl = record_list(lambda: [prep(h, d, (step + 1) if d == 0 else 7 - (step + 1), (step + 1) % 2, wb, kwb) for d in range(2)])
                nq = (len(pl) + 3) // 4
                if not DBG.get("hg_merge", 1):
                    P.ops.extend(pl)
                    pl = []
                for ts in range(4):
                    idx = step * 4 + ts
                    front(idx)
                    if DBG.get("hg_lag", 1):
                        if idx >= 1:
                            back(idx - 1)
                    else:
                        back(idx)
                    P.ops.extend(pl[ts * nq:(ts + 1) * nq])
            if DBG.get("hg_lag", 1) and DBG.get("hg_manual", 0):
                back(31)
            for half in range(2):
                for tb4 in range(4):
                    tb = half * 4 + tb4
                    sl = slice(tb * 512, (tb + 1) * 512)
                    rsl = slice(tb4 * 512, (tb4 + 1) * 512)
                    okeys = [("oTh", i) for i in range(tb * 4, tb * 4 + 4)]
                    P.act(lambda e, sl=sl: e.activation(out=o2, in_=oTh[:, sl], func=AF.Square), r=okeys, w=["o2"])
                    P.pe(lambda e: e.matmul(PS(7), lhsT=ones_m, rhs=o2, start=True, stop=True), r=["o2", "ones_m"], w=[("ps", 7)], c=0.9)
                    P.act(lambda e: e.activation(out=rsn, in_=PS(7), func=AF.Ln, bias=EPS), r=[("ps", 7)], w=["rsn"])
                    P.act(lambda e, rsl=rsl: e.activation(out=rst[:, rsl], in_=rsn, func=AF.Exp, scale=-0.5), r=["rsn"], w=["rst", "normA"])
                for tb4 in range(4):
                    tb = half * 4 + tb4
                    sl = slice(tb * 512, (tb + 1) * 512)
                    rsl = slice(tb4 * 512, (tb4 + 1) * 512)
                    par = tb % 2
                    okeys = [("oTh", i) for i in range(tb * 4, tb * 4 + 4)]
                    def mm(e, sl=sl, wb=wb):
                        ins = None
                        for k in range(8):
                            ins = e.matmul(PS(6), lhsT=wb[:, k, 4, :], rhs=xT3[:, k, sl], start=(k == 0), stop=(k == 7))
                        return ins
                    P.pe(mm, r=[kwb] + xT_all[tb * 4:tb * 4 + 4], w=[("ps", 6)])
                    P.act(lambda e: e.activation(out=sgt, in_=PS(6), func=AF.Silu), r=[("ps", 6), "normA"], w=["sgt"])
                    P.dve(lambda e, sl=sl, rsl=rsl: e.scalar_tensor_tensor(out=o2, in0=oTh[:, sl], scalar=ng[:, 0:1], in1=rst[:, rsl], op0=ALU.mult, op1=ALU.mult),
                          r=okeys + ["rst", "ng", "o2"], w=["o2"])
                    P.dve(lambda e, par=par: e.tensor_tensor(out=onb[par], in0=o2, in1=sgt, op=ALU.mult), r=["o2", "sgt"], w=[("onb", par)])
                    P.dma(lambda e, h=h, sl=sl, par=par: e.dma_start(out=ONT[h * 128:(h + 1) * 128, sl], in_=onb[par]), r=[("onb", par)], w=["ONT"])
        P.nosched = False
        P.barrier()
        A.release()
        w_out = A.alloc(8 * D, BF16).rearrange("p (k n) -> p k n", k=8)
        load_w_bf16(w_out, hg_w_out, 8, D, "hw_out")
        load_gb(1, 0)
        E = Epi()
        onl = [A.alloc(8 * 512, BF16).rearrange("p (k t) -> p k t", k=8) for _ in range(2)]
        for grp in range(8):
            gb_ = grp % 2
            P.dma(lambda e, grp=grp, gb_=gb_: e.dma_start(out=onl[gb_], in_=ONT[:, grp * 512:(grp + 1) * 512].rearrange("(k p) t -> p k t", p=128)),
                  r=["ONT"], w=[("onl", gb_)])
            for tl in range(4):
                i = grp * 4 + tl
                for hh in range(2):
                    bank = (i % 2) * 2 + hh
                    def mm(e, tl=tl, hh=hh, bank=bank, gb_=gb_):
                        ins = None
                        for k in range(8):
                            ins = e.matmul(PS(bank), lhsT=onl[gb_][:, k, tl * 128:(tl + 1) * 128], rhs=w_out[:, k, hh * 512:(hh + 1) * 512],
                                           start=(k == 0), stop=(k == 7))
                        return ins
                    P.pe(mm, r=["hw_out", ("onl", gb_)], w=[("ps", bank)])
                b0 = (i % 2) * 2
                epilogue(E, i, [PS(b0), PS(b0 + 1)], [("ps", b0), ("ps", b0 + 1)], cur_res, dst_res)
        P.barrier()
        A.release()

    plan = []
    if upto >= 1 and not DBG["skip_l0"]:
        plan.append(("l0", 0))
    if upto >= 2:
        plan.append(("xa", 0))
    if upto >= 3:
        plan.append(("moe", 0))
    if upto >= 4:
        plan.append(("hg", 1))
    if upto >= 5:
        plan.append(("xa", 1))
    if upto >= 6:
        plan.append(("moe", 1))
    plan = [p for p in plan if p[0] not in DBG["skip"]]
    for pi, (kind, l) in enumerate(plan):
        last = (pi == len(plan) - 1)
        dst_res = out_d if last else XR[nxt]
        if kind == "l0":
            l0_phase(cur_res, dst_res)
        elif kind == "xa":
            xa_phase(l, cur_res, dst_res)
        elif kind == "moe":
            moe_phase(l, cur_res, dst_res, want_xT=(l == 0))
        elif kind == "hg":
            hg_phase(cur_res, dst_res)
        cur_res = dst_res
        nxt ^= 1

    final_keys = ["dst_dram"] + [("dst", i) for i in range(NT)]
    if cur_res is not out_d:
        A.mark()
        cp = [A.alloc(D, F32) for _ in range(2)]
        for i in range(NT):
            b = i % 2
            P.dma(lambda e, i=i, b=b: e.dma_start(out=cp[b], in_=cur_res[i * 128:(i + 1) * 128, :]), r=["dst_dram"], w=[("cp", b)])
            P.dma(lambda e, i=i, b=b: e.dma_start(out=out_d[i * 128:(i + 1) * 128, :], in_=cp[b]), r=[("cp", b)], w=["out_final"])
        final_keys.append("out_final")
        A.release()
    cnt, dcnt = P.emit(final_keys=final_keys)
    st.close()
    return nc, cnt, dcnt


def make_consts():
    import ml_dtypes
    s = np.arange(S, dtype=np.int64)
    ang = 2.0 * np.pi * ((s[:, None] * s[None, :]) % S).astype(np.float64) / S
    sc = 1.0 / np.sqrt(S)
    cs = (np.cos(ang) * sc).astype(np.float32).astype(ml_dtypes.bfloat16)
    ss = (-np.sin(ang) * sc).astype(np.float32).astype(ml_dtypes.bfloat16)
    c = np.arange(128, dtype=np.int64)
    angc = 2.0 * np.pi * ((c[:, None] * c[None, :]) % 128).astype(np.float64) / 128
    scc = 1.0 / np.sqrt(128.0)
    dc = np.concatenate([np.cos(angc) * scc, np.sin(angc) * scc], axis=1).astype(np.float32)
    t = np.arange(S)
    invcnt = np.zeros((4, S), np.float32)
    for gi, w in enumerate((2, 4, 8, 16)):
        lo = np.clip(t - w // 2, 0, S)
        hi = np.clip(t + w // 2, 0, S)
        invcnt[gi] = 1.0 / (hi - lo).astype(np.float32)
    m = np.arange(128)
    same = (m[:, None] // 64) == (m[None, :] // 64)
    masks = np.zeros((4, 128, 128), np.float32)
    masks[0] = (same & (m[:, None] <= m[None, :]))
    masks[1] = (same & (m[:, None] >= m[None, :]))
    masks[2] = (m[:, None] < m[None, :])
    masks[3] = 1.0
    eoff = (np.arange(32, dtype=np.float32) * CAP).reshape(1, 32)
    trash = (NEXP * CAP + np.arange(128, dtype=np.float32)).reshape(128, 1)
    altc = np.repeat((sc * np.cos(np.pi * np.arange(128))).astype(np.float32).reshape(128, 1), NT, axis=1)
    return {"c_cs": cs, "c_ss": ss, "c_dc": dc, "c_invcnt": invcnt, "c_masks": masks, "c_eoff": eoff, "c_trash": trash, "c_alt": np.ascontiguousarray(altc)}


_CACHE = {}


def kernel(**inputs):
    if "nc" not in _CACHE:
        _CACHE["nc"] = build()[0]
        _CACHE["consts"] = make_consts()
    nc = _CACHE["nc"]
    consts = _CACHE["consts"]
    in_maps = []
    for b in range(8):
        m = {}
        for k, v in inputs.items():
            v = np.asarray(v)
            if k in ("x", "mem"):
                m[k] = np.ascontiguousarray(v[b])
            elif k in ("pf_w_in", "pf_pool_w", "pf_pool_scale", "pf_fourier_ln_g", "pf_fourier_w", "pf_w_out",
                       "hg_w_in", "hg_norm_g", "hg_w_out"):
                m[k] = np.ascontiguousarray(v[0])
            else:
                m[k] = np.ascontiguousarray(v)
        m.update(consts)
        in_maps.append(m)
    res = run_bass_kernel_spmd(nc, in_maps, core_ids=list(range(8)))
    return np.stack([np.asarray(r["out"]) for r in res.results], axis=0).astype(np.float32)
```

```python
import numpy as np
from contextlib import ExitStack
import concourse.bass as bass
import concourse.mybir as mybir
from concourse.bass_utils import run_bass_kernel_spmd

F32 = mybir.dt.float32
BF16 = mybir.dt.bfloat16
I32 = mybir.dt.int32
ALU = mybir.AluOpType
AF = mybir.ActivationFunctionType
AX = mybir.AxisListType

S = 4096
D = 1024
NT = 32
MEM = 256
CAP = 512
NEXP = 32
ALPHA = 4.0 ** 0.25
EPS = 1e-5
EPOCH = 12000
DBG = {"skip_l0": False, "xa_groups": 8, "hg_heads": 8, "skip": ()}
NDS = 8


class Op:
    __slots__ = ("eng", "fn", "reads", "writes", "dma", "seq", "dn", "deps", "waits", "bar", "c", "ns")


class Prog:
    ENGS = ["pe", "act", "dve", "pool", "sp"]

    def __init__(self, nc):
        self.nc = nc
        self.ops = []

    DEFC = {"pe": 1.8, "act": 0.6, "dve": 0.6, "pool": 1.5, "sp": 4.0}

    def add(self, eng, fn, reads=(), writes=(), dma=False, c=None):
        o = Op()
        o.eng, o.fn, o.reads, o.writes, o.dma, o.bar = eng, fn, list(reads), list(writes), dma, False
        o.c = c if c is not None else (4.0 if dma else self.DEFC[eng])
        o.ns = getattr(self, "nosched", False)
        self.ops.append(o)
        return o

    def pe(self, fn, r=(), w=(), c=None):
        return self.add("pe", fn, r, w, c=c)

    def act(self, fn, r=(), w=(), c=None):
        return self.add("act", fn, r, w, c=c)

    def dve(self, fn, r=(), w=(), c=None):
        return self.add("dve", fn, r, w, c=c)

    def pool(self, fn, r=(), w=(), c=None):
        return self.add("pool", fn, r, w, c=c)

    def dma(self, fn, r=(), w=(), q="sp", c=None):
        return self.add(q, fn, r, w, dma=True, c=c)

    def barrier(self):
        for e in self.ENGS:
            o = self.add(e, None)
            o.bar = True

    WIN = {"pe": 48, "act": 48, "dve": 48, "pool": 1, "sp": 48}

    def reorder(self, window=48, sync_lat=0.2):
        ops = self.ops
        new_ops = []
        seg = []

        def flush():
            n = len(seg)
            if n == 0:
                return
            if any(getattr(op, "ns", False) for op in seg):
                new_ops.extend(seg)
                seg.clear()
                return
            last_w, readers = {}, {}
            preds = [set() for _ in range(n)]
            for j, op in enumerate(seg):
                for r in op.reads:
                    if r in last_w:
                        preds[j].add(last_w[r])
                for w in op.writes:
                    if w in last_w:
                        preds[j].add(last_w[w])
                    preds[j].update(readers.get(w, ()))
                preds[j].discard(j)
                for r in op.reads:
                    readers.setdefault(r, []).append(j)
                for w in op.writes:
                    last_w[w] = j
                    readers[w] = []
            queues = {e: [j for j, op in enumerate(seg) if op.eng == e] for e in self.ENGS}
            ptr = {e: 0 for e in self.ENGS}
            done = [False] * n
            fin = [0.0] * n
            t_e = {e: 0.0 for e in self.ENGS}
            left = n
            while left:
                best = None
                for e in self.ENGS:
                    q = queues[e]
                    p = ptr[e]
                    while p < len(q) and done[q[p]]:
                        p += 1
                    ptr[e] = p
                    seen = 0
                    k = p
                    while k < len(q) and seen < self.WIN[e]:
                        j = q[k]
                        k += 1
                        if done[j]:
                            continue
                        seen += 1
                        ok = True
                        rt = 0.0
                        for pr in preds[j]:
                            if not done[pr]:
                                ok = False
                                break
                            f = fin[pr] + sync_lat
                            if f > rt:
                                rt = f
                        if not ok:
                            continue
                        stt = max(t_e[e], rt)
                        key = (stt, j)
                        if best is None or key < best[0]:
                            best = (key, e, j)
                        if stt <= t_e[e]:
                            break
                assert best is not None, "scheduler deadlock"
                (stt, _), e, j = best
                op = seg[j]
                done[j] = True
                left -= 1
                if op.dma:
                    t_e[e] = stt + 0.15
                    fin[j] = stt + op.c
                else:
                    t_e[e] = stt + op.c
                    fin[j] = stt + op.c
                new_ops.append(op)
            seg.clear()

        i = 0
        while i < len(ops):
            if ops[i].bar:
                flush()
                while i < len(ops) and ops[i].bar:
                    new_ops.append(ops[i])
                    i += 1
            else:
                seg.append(ops[i])
                i += 1
        flush()
        self.ops = new_ops

    def emit(self, final_keys=(), reorder=True):
        nc = self.nc
        if reorder:
            self.reorder()
        ops = self.ops
        self.add("sp", None, final_keys, ())
        cnt = {e: 0 for e in self.ENGS}
        dcnt = {e: 0 for e in self.ENGS}
        dma_ops = {e: [] for e in self.ENGS}
        last_c = {e: None for e in self.ENGS}
        epos = {e: 0 for e in self.ENGS}
        for j, op in enumerate(ops):
            if op.dma:
                op.dn = dcnt[op.eng]
                dcnt[op.eng] += 1
                dma_ops[op.eng].append(j)
            elif op.fn is not None:
                epos[op.eng] += 1
                op.seq = epos[op.eng]
        last_w = {}
        readers = {}
        for j, op in enumerate(ops):
            deps = set()
            if op.bar:
                for e in self.ENGS:
                    if last_c[e] is not None:
                        deps.add(last_c[e])
                for e in self.ENGS:
                    lst = [i for i in dma_ops[e] if i < j]
                    deps.update(lst[-NDS:])
            for r in op.reads:
                if r in last_w:
                    deps.add(last_w[r])
            for w in op.writes:
                if w in last_w:
                    deps.add(last_w[w])
                deps.update(readers.get(w, ()))
            if op.dma and op.dn >= NDS:
                deps.add(dma_ops[op.eng][op.dn - NDS])
            deps.discard(j)
            op.deps = deps
            for r in op.reads:
                readers.setdefault(r, []).append(j)
            for w in op.writes:
                last_w[w] = j
                readers[w] = []
            if (not op.dma) and op.fn is not None:
                last_c[op.eng] = j
        wc = {e: {} for e in self.ENGS}
        wd = {e: {} for e in self.ENGS}
        signal = set()
        for j, op in enumerate(ops):
            waits = []
            me = op.eng
            for i in sorted(op.deps):
                d = ops[i]
                if d.dma:
                    k = (d.eng, d.dn % NDS)
                    val = 16 * (d.dn // NDS + 1)
                    if wd[me].get(k, 0) < val:
                        wd[me][k] = val
                        waits.append(("d", i))
                else:
                    if d.fn is None:
                        continue
                    if d.eng == "pe" and me == "pe":
                        continue
                    if wc[me].get(d.eng, 0) < d.seq:
                        wc[me][d.eng] = d.seq
                        waits.append(("c", i))
                        signal.add(i)
            op.waits = waits
        for j, op in enumerate(ops):
            if (not op.dma) and op.fn is not None:
                if j in signal:
                    cnt[op.eng] += 1
                    op.seq = cnt[op.eng]
                else:
                    op.seq = None
        with ExitStack() as st:
            csem = {}
            for e in self.ENGS:
                n_ep = (cnt[e] + EPOCH - 1) // EPOCH
                csem[e] = [st.enter_context(nc.semaphore(f"c_{e}_{k}")) for k in range(max(n_ep, 1))]
            dsem = {}
            for e in self.ENGS:
                if dcnt[e]:
                    dsem[e] = [st.enter_context(nc.semaphore(f"d_{e}_{k}")) for k in range(NDS)]
            for op in ops:
                ww = []
                for (kind, i) in op.waits:
                    d = ops[i]
                    if kind == "d":
                        ww.append((dsem[d.eng][d.dn % NDS], 16 * (d.dn // NDS + 1)))
                    else:
                        ww.append((csem[d.eng][(d.seq - 1) // EPOCH], (d.seq - 1) % EPOCH + 1))
                op.waits = ww
            streams = {e: [op for op in ops if op.eng == e] for e in self.ENGS}
            block = st.enter_context(nc.Block())

            def mk(ename):
                def body(e):
                    for op in streams[ename]:
                        for (sem, val) in op.waits:
                            e.wait_ge(sem, val)
                        if op.fn is None:
                            continue
                        ins = op.fn(e)
                        if op.dma:
                            ins.then_inc(dsem[ename][op.dn % NDS], 16)
                        elif op.seq is not None:
                            ins.then_inc(csem[ename][(op.seq - 1) // EPOCH], 1)
                return body

            block.tensor(mk("pe"))
            block.scalar(mk("act"))
            block.vector(mk("dve"))
            block.gpsimd(mk("pool"))
            block.sync(mk("sp"))
        return cnt, dcnt


class Arena:
    def __init__(self, ap32, nbytes):
        self.ap = ap32
        self.n = nbytes
        self.off = 0
        self.marks = []

    def alloc(self, free_elems, dt):
        sz = 2 if dt == BF16 else 4
        nb = (free_elems * sz + 31) // 32 * 32
        assert self.off + nb <= self.n, f"arena overflow {self.off}+{nb}>{self.n}"
        a = self.ap[:, self.off // 4:(self.off + nb) // 4]
        self.off += nb
        if dt != F32:
            a = a.bitcast(dt)
        return a[:, 0:free_elems]

    def mark(self):
        self.marks.append(self.off)

    def release(self):
        self.off = self.marks.pop()


def build(upto=99, dbg=False):
    nc = bass.Bass("TRN2", target_bir_lowering=False)
    P = Prog(nc)

    def din(name, shape, dt=F32):
        return nc.dram_tensor(name, list(shape), dt, kind="ExternalInput").ap()

    x_d = din("x", [S, D])
    mem_d = din("mem", [MEM, D])
    pf_w_in = din("pf_w_in", [D, D])
    pf_pool_w = din("pf_pool_w", [4, 128, 128])
    pf_pool_scale = din("pf_pool_scale", [512])
    pf_ln_g = din("pf_fourier_ln_g", [4, 128])
    pf_fw = din("pf_fourier_w", [4, 128, 128])
    pf_w_out = din("pf_w_out", [D, D])
    hg_w_in = din("hg_w_in", [D, 5 * D])
    hg_lb = din("hg_lower_bounds", [2, 2, D])
    hg_norm_g = din("hg_norm_g", [128])
    hg_w_out = din("hg_w_out", [D, D])
    xa_wq = din("xa_wq", [2, D, D])
    xa_wkv = din("xa_wkv", [2, D, 2 * D])
    xa_wo = din("xa_wo", [2, D, D])
    moe_wg = din("moe_w_group", [2, D, 4])
    moe_bg = din("moe_b_group", [2, 4])
    moe_we = din("moe_w_expert", [2, D, 32])
    moe_be = din("moe_b_expert", [2, 32])
    moe_gate = din("moe_w_gate", [2, NEXP, D, 512])
    moe_up = din("moe_w_up", [2, NEXP, D, 512])
    moe_down = din("moe_w_down", [2, NEXP, 512, D])
    ln_g = din("ln_g", [2, 3, D])
    ln_b = din("ln_b", [2, 3, D])
    c_cs = din("c_cs", [S, S], BF16)
    c_ss = din("c_ss", [S, S], BF16)
    c_dc = din("c_dc", [128, 256])
    c_invcnt = din("c_invcnt", [4, S])
    c_masks = din("c_masks", [4, 128, 128])
    c_eoff = din("c_eoff", [1, 32])
    c_trash = din("c_trash", [128, 1])
    c_alt = din("c_alt", [128, NT])
    out_d = nc.dram_tensor("out", [S, D], F32, kind="ExternalOutput").ap()
    XR = [nc.dram_tensor(f"xr{k}", [S, D], F32).ap() for k in range(2)]
    Zscr = nc.dram_tensor("zscr", [4, NT, 128, 256], BF16).ap()
    XS = nc.dram_tensor("xs_scr", [NEXP * CAP + 128, D], BF16).ap()
    YS = nc.dram_tensor("ys_scr", [NEXP * CAP + 128, D], F32).ap()
    ONT = nc.dram_tensor("ont_scr", [D, S], BF16).ap()

    st = ExitStack()
    ARENA_BYTES = 206 * 1024
    arena_t = st.enter_context(nc.sbuf_tensor("arena", [128, ARENA_BYTES // 4], F32))
    A = Arena(arena_t[:], ARENA_BYTES)
    psb = [st.enter_context(nc.psum_tensor(f"ps{k}", [128, 512], F32)) for k in range(8)]

    def PS(k):
        return psb[k][:]

    def PSB(k):
        return psb[k][:].bitcast(BF16)

    ident = A.alloc(128, BF16)
    ident32 = A.alloc(128, F32)
    ones_m = A.alloc(128, F32)
    gb = A.alloc(2 * D, F32)
    xT = A.alloc(8 * S, BF16)
    xT3 = xT.rearrange("p (k t) -> p k t", k=8)

    P.pool(lambda e: e.memset(ident32, 0.0), w=["ident32"])
    P.pool(lambda e: e.affine_select(out=ident32, in_=ident32, pattern=[[-1, 128]], compare_op=ALU.not_equal,
                                     fill=1.0, base=0, channel_multiplier=1), r=["ident32"], w=["ident32"])
    P.dve(lambda e: e.tensor_copy(out=ident, in_=ident32), r=["ident32"], w=["ident"])
    P.pool(lambda e: e.memset(ones_m, 1.0 / 128.0), w=["ones_m"])

    def ncd(fn):
        def g(e):
            with nc.allow_non_contiguous_dma(reason="tiny per-partition scalar tables"):
                return fn(e)
        return g

    cast_rr = [0]

    def cast_copy(out, in_, r, w):
        cast_rr[0] ^= 1
        if cast_rr[0]:
            P.act(lambda e: e.copy(out=out, in_=in_), r=r, w=w)
        else:
            P.dve(lambda e: e.tensor_copy(out=out, in_=in_), r=r, w=w)

    def transpose_to(dst3, src_bf, bank, rkeys, wkeys):
        def f(e):
            ins = None
            for k in range(8):
                ins = e.transpose(out=PSB(bank)[:, k * 128:(k + 1) * 128], in_=src_bf[:, k * 128:(k + 1) * 128],
                                  identity=ident)
            return ins
        P.pe(f, r=list(rkeys) + ["ident"], w=[("ps", bank)])
        cast_copy(dst3, PSB(bank).rearrange("p (k t) -> p k t", k=8), [("ps", bank)], wkeys)

    def load_w_bf16(dst3, src2d, kt, n, wkey, rows_per_dma=256):
        kk = max(1, rows_per_dma // 128)
        for k0 in range(0, kt, kk):
            k1 = min(kt, k0 + kk)
            P.dma(lambda e, k0=k0, k1=k1: e.dma_start(
                out=dst3[:, k0:k1, :], in_=src2d[k0 * 128:k1 * 128, :].rearrange("(k p) n -> p k n", p=128)),
                w=[wkey], q="pool")

    def load_gb(l, j):
        P.dma(lambda e: e.dma_start(out=gb[:, 0:D], in_=ln_g[l, j:j + 1, :].broadcast_to([128, D])), w=["gb"])
        P.dma(lambda e: e.dma_start(out=gb[:, D:2 * D], in_=ln_b[l, j:j + 1, :].broadcast_to([128, D])), w=["gb"])

    class Epi:
        def __init__(self):
            self.xr = [A.alloc(D, F32) for _ in range(2)]
            self.y = [A.alloc(D, F32) for _ in range(2)]
            self.xo = [A.alloc(D, F32) for _ in range(2)]
            self.xob = [A.alloc(D, BF16) for _ in range(2)]
            self.st = A.alloc(12, F32)
            self.mv = A.alloc(8, F32)

    def epilogue(E, i, hsrc, hkeys, res_ap, dst_ap, make_xT=True, tbank=7, gmul="pool"):
        b = i % 2
        xr, y, xo, xob = E.xr[b], E.y[b], E.xo[b], E.xob[b]
        kxr, ky, kxo, kxob = ("e_xr", b), ("e_y", b), ("e_xo", b), ("e_xob", b)
        P.dma(lambda e: e.dma_start(out=xr, in_=res_ap[i * 128:(i + 1) * 128, :]), r=["res_dram"], w=[kxr])
        for hh in range(2):
            P.dve(lambda e, hh=hh: e.scalar_tensor_tensor(
                out=y[:, hh * 512:(hh + 1) * 512], in0=xr[:, hh * 512:(hh + 1) * 512], scalar=ALPHA,
                in1=hsrc[hh], op0=ALU.mult, op1=ALU.add), r=[kxr] + list(hkeys), w=[ky])
        P.dve(lambda e: e.bn_stats(out=E.st[:, 0:6], in_=y[:, 0:512]), r=[ky], w=["e_st0"])
        P.dve(lambda e: e.bn_stats(out=E.st[:, 6:12], in_=y[:, 512:1024]), r=[ky], w=["e_st1"])
        P.dve(lambda e: e.bn_aggr(out=E.mv[:, 0:2], in_=E.st[:, 0:12]), r=["e_st0", "e_st1"], w=["e_mv"])
        P.act(lambda e: e.activation(out=E.mv[:, 2:3], in_=E.mv[:, 1:2], func=AF.Ln, bias=EPS), r=["e_mv"], w=["e_sd"], c=0.2)
        P.act(lambda e: e.activation(out=E.mv[:, 3:4], in_=E.mv[:, 2:3], func=AF.Exp, scale=-0.5), r=["e_sd"], w=["e_rs"], c=0.2)
        P.dve(lambda e: e.scalar_tensor_tensor(out=E.mv[:, 4:5], in0=E.mv[:, 0:1], scalar=-1.0, in1=E.mv[:, 3:4],
                                               op0=ALU.mult, op1=ALU.mult), r=["e_mv", "e_rs"], w=["e_nm"])
        P.act(lambda e: e.activation(out=y, in_=y, func=AF.Identity, bias=E.mv[:, 4:5], scale=E.mv[:, 3:4]),
              r=[ky, "e_rs", "e_nm"], w=[ky])
        if gmul == "dve":
            P.dve(lambda e: e.tensor_tensor(out=y, in0=y, in1=gb[:, 0:D], op=ALU.mult), r=[ky, "gb"], w=[ky], c=1.2)
        else:
            P.pool(lambda e: e.tensor_tensor(out=y, in0=y, in1=gb[:, 0:D], op=ALU.mult), r=[ky, "gb"], w=[ky], c=2.4)
        P.pool(lambda e: e.tensor_tensor(out=xo, in0=y, in1=gb[:, D:2 * D], op=ALU.add), r=[ky, "gb"], w=[kxo])
        P.dma(lambda e: e.dma_start(out=dst_ap[i * 128:(i + 1) * 128, :], in_=xo), r=[kxo], w=[("dst", i)], q="pool")
        if make_xT:
            P.act(lambda e: e.copy(out=xob, in_=xo), r=[kxo], w=[kxob])
            transpose_to(xT3[:, :, i * 128:(i + 1) * 128], xob, tbank, [kxob], [("xT", i)])

    A.mark()
    xb0 = [A.alloc(D, BF16) for _ in range(2)]
    for i in range(NT):
        b = i % 2
        P.dma(lambda e, i=i, b=b: e.dma_start(out=xb0[b], in_=x_d[i * 128:(i + 1) * 128, :]), w=[("xb0", b)], q="pool")
        transpose_to(xT3[:, :, i * 128:(i + 1) * 128], xb0[b], 6 + b, [("xb0", b)], [("xT", i)])
    P.barrier()
    A.release()

    xT_all = [("xT", i) for i in range(NT)]
    cur_res = x_d
    nxt = 0

    def l0_phase(cur_res, dst_res):
        if True:
            A.mark()
            catT = A.alloc(8 * S, BF16)
            catT3 = catT.rearrange("p (k t) -> p k t", k=8)
            A.mark()
            w_in = A.alloc(8 * D, BF16).rearrange("p (k n) -> p k n", k=8)
            load_w_bf16(w_in, pf_w_in, 8, D, "w_in")
            poolw = A.alloc(4 * 128, BF16).rearrange("p (g n) -> p g n", g=4)
            fw = A.alloc(4 * 128, BF16).rearrange("p (g n) -> p g n", g=4)
            dc = A.alloc(256, BF16)
            P.dma(lambda e: e.dma_start(out=poolw, in_=pf_pool_w.rearrange("g c d -> c g d")), w=["poolw"], q="pool")
            P.dma(lambda e: e.dma_start(out=fw, in_=pf_fw.rearrange("g c d -> c g d")), w=["fw"], q="pool")
            P.dma(lambda e: e.dma_start(out=dc, in_=c_dc), w=["dc"], q="pool")
            pscale = A.alloc(4, F32)
            lng = A.alloc(4, F32)
            P.dma(ncd(lambda e: e.dma_start(out=pscale, in_=pf_pool_scale.rearrange("(g d) -> d g", g=4))), w=["pscale"])
            P.dma(ncd(lambda e: e.dma_start(out=lng, in_=pf_ln_g.rearrange("h c -> c h"))), w=["lng"])
            PADW = 16
            A.mark()
            a0 = A.alloc(S + 2 * PADW, F32)
            t1 = A.alloc(1024 + 2 * PADW, F32)
            t2 = A.alloc(1024 + 2 * PADW, F32)
            icnt = A.alloc(1024, F32)
            pm = A.alloc(1024, BF16)
            P.pool(lambda e: e.memset(a0, 0.0), w=["a0"])
            for g in range(4):
                for tb in range(8):
                    bank = tb % 2
                    def mm(e, g=g, tb=tb, bank=bank):
                        ins = None
                        for k in range(8):
                            ins = e.matmul(PS(bank), lhsT=w_in[:, k, g * 128:(g + 1) * 128],
                                           rhs=xT3[:, k, tb * 512:(tb + 1) * 512], start=(k == 0), stop=(k == 7))
                        return ins
                    P.pe(mm, r=["w_in"] + xT_all[tb * 4:tb * 4 + 4], w=[("ps", bank)])
                    P.act(lambda e, tb=tb, bank=bank: e.copy(out=a0[:, PADW + tb * 512:PADW + (tb + 1) * 512], in_=PS(bank)),
                          r=[("ps", bank)], w=["a0"])
                for blk in range(4):
                    s0 = PADW + blk * 1024
                    def lv(buf, lo, hi):
                        return buf[:, PADW + lo:PADW + 1024 + hi]
                    def a0v(lo, hi, s0=s0):
                        return a0[:, s0 + lo:s0 + 1024 + hi]
                    P.dma(lambda e, g=g, blk=blk: e.dma_start(
                        out=icnt, in_=c_invcnt[g:g + 1, blk * 1024:(blk + 1) * 1024].broadcast_to([128, 1024])),
                        w=["icnt"])
                    P.dve(lambda e, a0v=a0v, lv=lv: e.tensor_tensor(out=lv(t1, -8, 8), in0=a0v(-9, 7), in1=a0v(-8, 8), op=ALU.add),
                          r=["a0"], w=["t1"])
                    cur, curk, oth, othk = t1, "t1", t2, "t2"
                    ext = 8
                    for lev in range(1, g + 1):
                        sh = 1 << (lev - 1)
                        ne = ext - 2 * sh if lev < 3 else 0
                        ne = {1: 6, 2: 4, 3: 0}[lev]
                        P.dve(lambda e, cur=cur, oth=oth, sh=sh, ne=ne, lv=lv: e.tensor_tensor(
                            out=lv(oth, -ne, ne), in0=lv(cur, -ne - sh, ne - sh), in1=lv(cur, -ne + sh, ne + sh), op=ALU.add),
                            r=[curk], w=[othk])
                        cur, curk, oth, othk = oth, othk, cur, curk
                        ext = ne
                    P.dve(lambda e, cur=cur, oth=oth, lv=lv: e.tensor_tensor(out=lv(oth, 0, 0), in0=lv(cur, 0, 0), in1=icnt, op=ALU.mult),
                          r=[curk, "icnt"], w=[othk])
                    P.dve(lambda e, oth=oth, lv=lv, a0v=a0v: e.tensor_tensor(out=pm, in0=lv(oth, 0, 0), in1=a0v(0, 0), op=ALU.subtract),
                          r=[othk, "a0"], w=["pm"])
                    for hb in range(2):
                        bank = 2 + hb
                        P.pe(lambda e, g=g, hb=hb, bank=bank: e.matmul(PS(bank), lhsT=poolw[:, g, :], rhs=pm[:, hb * 512:(hb + 1) * 512],
                                                                      start=True, stop=True), r=["pm", "poolw"], w=[("ps", bank)])
                        c0 = blk * 1024 + hb * 512
                        P.act(lambda e, g=g, bank=bank, c0=c0: e.activation(out=catT3[:, g, c0:c0 + 512], in_=PS(bank), func=AF.Identity,
                                                                             scale=pscale[:, g:g + 1]),
                              r=[("ps", bank), "pscale"], w=[("catT", g)])
            ub = A.alloc(512, F32)
            dd = A.alloc(512, F32)
            d2 = A.alloc(512, F32)
            rs = A.alloc(512, F32)
            un = A.alloc(512, BF16)
            zs = [A.alloc(512, BF16) for _ in range(2)]
            for h in range(4):
                for tb in range(8):
                    def mm(e, h=h, tb=tb):
                        ins = None
                        for k in range(8):
                            ins = e.matmul(PS(0), lhsT=w_in[:, k, 512 + h * 128:512 + (h + 1) * 128],
                                           rhs=xT3[:, k, tb * 512:(tb + 1) * 512], start=(k == 0), stop=(k == 7))
                        return ins
                    P.pe(mm, r=["w_in"] + xT_all[tb * 4:tb * 4 + 4], w=[("ps", 0)])
                    P.act(lambda e: e.copy(out=ub, in_=PS(0)), r=[("ps", 0)], w=["ub"])
                    P.pe(lambda e: e.matmul(PS(1), lhsT=ones_m, rhs=ub, start=True, stop=True), r=["ub", "ones_m"], w=[("ps", 1)])
                    P.dve(lambda e: e.tensor_tensor(out=dd, in0=ub, in1=PS(1), op=ALU.subtract), r=["ub", ("ps", 1)], w=["dd"])
                    P.act(lambda e: e.activation(out=d2, in_=dd, func=AF.Square), r=["dd"], w=["d2"])
                    P.pe(lambda e: e.matmul(PS(1), lhsT=ones_m, rhs=d2, start=True, stop=True), r=["d2", "ones_m"], w=[("ps", 1)])
                    P.act(lambda e: e.activation(out=rs, in_=PS(1), func=AF.Ln, bias=EPS), r=[("ps", 1)], w=["rs"])
                    P.act(lambda e: e.activation(out=rs, in_=rs, func=AF.Exp, scale=-0.5), r=["rs"], w=["rs"])
                    P.dve(lambda e, h=h: e.scalar_tensor_tensor(out=un, in0=dd, scalar=lng[:, h:h + 1], in1=rs, op0=ALU.mult, op1=ALU.mult),
                          r=["dd", "rs", "lng"], w=["un"])
                    for pr in range(2):
                        bank = 2 + pr
                        def mz(e, pr=pr, bank=bank):
                            ins = None
                            for q in range(2):
                                tt = pr * 2 + q
                                ins = e.matmul(PS(bank)[:, q * 256:(q + 1) * 256], lhsT=un[:, tt * 128:(tt + 1) * 128], rhs=dc,
                                               start=True, stop=True)
                            return ins
                        P.pe(mz, r=["un", "dc"], w=[("ps", bank)])
                        cast_copy(zs[pr], PS(bank), [("ps", bank)], [("zs", pr)])
                        t0 = tb * 4 + pr * 2
                        P.dma(lambda e, h=h, t0=t0, pr=pr: e.dma_start(
                            out=Zscr[h, t0:t0 + 2].rearrange("t p c -> p t c"), in_=zs[pr].rearrange("p (t c) -> p t c", t=2)),
                            r=[("zs", pr)], w=[("zscr", h, t0)])
            P.barrier()
            A.release()
            A.release()
            A.mark()
            fw2 = A.alloc(4 * 128, BF16).rearrange("p (g n) -> p g n", g=4)
            P.dma(lambda e: e.dma_start(out=fw2, in_=pf_fw.rearrange("g c d -> c g d")), w=["fw2"], q="pool")
            zres = xT.rearrange("p (h t c) -> p h t c", h=4, t=NT)
            for h in range(4):
                P.dma(lambda e, h=h: e.dma_start(out=zres[:, h], in_=Zscr[h].rearrange("t p c -> p t c")), r=["zscr"], w=["zres"])
            NCH = 4
            dbuf = [[A.alloc(8 * 512, BF16).rearrange("p (s t) -> p s t", s=8) for _ in range(2)] for _ in range(2)]
            asb = [A.alloc(512, F32) for _ in range(4)]
            ypb = [A.alloc(512, BF16) for _ in range(4)]
            ymb = [A.alloc(512, BF16) for _ in range(4)]
            alt = A.alloc(NT, BF16)
            y2k = A.alloc(4, BF16)
            P.dma(lambda e: e.dma_start(out=alt, in_=c_alt), w=["alt"], q="pool")
            for h in range(4):
                def m2k(e, h=h):
                    ins = None
                    for stile in range(NT):
                        ins = e.matmul(PS(0)[:, h:h + 1], lhsT=zres[:, h, stile, 0:128], rhs=alt[:, stile:stile + 1],
                                       start=(stile == 0), stop=(stile == NT - 1))
                    return ins
                P.pe(m2k, r=["zres", "alt"], w=[("ps", 0)], c=2.5)
            P.dve(lambda e: e.tensor_copy(out=y2k, in_=PS(0)[:, 0:4]), r=[("ps", 0)], w=["y2k"])
            for h in range(4):
                P.pe(lambda e, h=h: e.matmul(PS(0)[:, 8 + h:9 + h], lhsT=fw2[:, h, :], rhs=y2k[:, h:h + 1], start=True, stop=True),
                     r=["y2k", "fw2"], w=[("ps", 0)], c=0.2)
            for h in range(4):
                P.dve(lambda e, h=h: e.tensor_copy(out=catT3[:, 4 + h, 2048:2049], in_=PS(0)[:, 8 + h:9 + h]), r=[("ps", 0)], w=[("catT", 4 + h)], c=0.1)
            cn = 0
            for kb in range(4):
                for sc in range(NCH):
                    bsel = cn % 2
                    cn += 1
                    for m, src in enumerate((c_cs, c_ss)):
                        P.dma(lambda e, m=m, src=src, sc=sc, kb=kb, bsel=bsel: e.dma_start(
                            out=dbuf[bsel][m], in_=src[sc * 1024:(sc + 1) * 1024, kb * 512:(kb + 1) * 512].rearrange("(s p) t -> p s t", p=128)),
                            w=[("dbuf", bsel, m)])
                    for h in range(4):
                        def mm(e, h=h, sc=sc, bsel=bsel):
                            ins = None
                            for s_ in range(8):
                                stile = sc * 8 + s_
                                first = (sc == 0 and s_ == 0)
                                last = (sc == NCH - 1 and s_ == 7)
                                e.matmul(PS(h), lhsT=zres[:, h, stile, 0:128], rhs=dbuf[bsel][0][:, s_, :], start=first, stop=last)
                                ins = e.matmul(PS(4 + h), lhsT=zres[:, h, stile, 128:256], rhs=dbuf[bsel][1][:, s_, :], start=first, stop=last)
                            return ins
                        P.pe(mm, r=["zres", ("dbuf", bsel, 0), ("dbuf", bsel, 1)], w=[("ps", h), ("ps", 4 + h)], c=3.6)
                for h in range(4):
                    P.act(lambda e, h=h: e.copy(out=asb[h], in_=PS(h)), r=[("ps", h)], w=[("asb", h)])
                    P.dve(lambda e, h=h: e.tensor_tensor(out=ypb[h], in0=asb[h], in1=PS(4 + h), op=ALU.add), r=[("asb", h), ("ps", 4 + h)], w=[("ypb", h)])
                    P.dve(lambda e, h=h: e.tensor_tensor(out=ymb[h], in0=asb[h], in1=PS(4 + h), op=ALU.subtract), r=[("asb", h), ("ps", 4 + h)], w=[("ymb", h)])
                    P.pe(lambda e, h=h: e.matmul(PS(h), lhsT=fw2[:, h, :], rhs=ypb[h], start=True, stop=True), r=[("ypb", h), "fw2"], w=[("ps", h)], c=0.3)
                    P.pe(lambda e, h=h: e.matmul(PS(4 + h), lhsT=fw2[:, h, :], rhs=ymb[h], start=True, stop=True), r=[("ymb", h), "fw2"], w=[("ps", 4 + h)], c=0.3)
                    P.act(lambda e, h=h, kb=kb: e.copy(out=catT3[:, 4 + h, kb * 512:(kb + 1) * 512], in_=PS(h)), r=[("ps", h)], w=[("catT", 4 + h)])
                    if kb == 0:
                        P.dve(lambda e, h=h: e.tensor_copy(out=catT3[:, 4 + h, 3585:4096][:, ::-1], in_=PS(4 + h)[:, 1:512]),
                              r=[("ps", 4 + h)], w=[("catT", 4 + h)])
                    else:
                        lo = S - kb * 512 - 511
                        P.dve(lambda e, h=h, lo=lo: e.tensor_copy(out=catT3[:, 4 + h, lo:lo + 512][:, ::-1], in_=PS(4 + h)),
                              r=[("ps", 4 + h)], w=[("catT", 4 + h)])
            P.barrier()
            A.release()
            if dbg:
                dbg_cat = nc.dram_tensor("dbg_cat", [D, S], BF16, kind="ExternalOutput").ap()
                P.dma(lambda e: e.dma_start(out=dbg_cat.rearrange("(k p) t -> p k t", p=128), in_=catT3), r=[("catT", k) for k in range(8)], w=["dbg_cat"])
            A.mark()
            w_out = A.alloc(8 * D, BF16).rearrange("p (k n) -> p k n", k=8)
            load_w_bf16(w_out, pf_w_out, 8, D, "w_out")
            load_gb(0, 0)
            E = Epi()
            for i in range(NT):
                for hh in range(2):
                    bank = (i % 2) * 2 + hh
                    def mm(e, i=i, hh=hh, bank=bank):
                        ins = None
                        for k in range(8):
                            ins = e.matmul(PS(bank), lhsT=catT3[:, k, i * 128:(i + 1) * 128], rhs=w_out[:, k, hh * 512:(hh + 1) * 512],
                                           start=(k == 0), stop=(k == 7))
                        return ins
                    P.pe(mm, r=["w_out"] + [("catT", k) for k in range(8)], w=[("ps", bank)])
                b0 = (i % 2) * 2
                epilogue(E, i, [PS(b0), PS(b0 + 1)], [("ps", b0), ("ps", b0 + 1)], cur_res, dst_res)
            P.barrier()
            A.release()
            A.release()


    def xa_phase(l, cur_res, dst_res):
        if True:
            A.mark()
            wq = A.alloc(8 * D, BF16).rearrange("p (k n) -> p k n", k=8)
            wo = A.alloc(8 * D, BF16).rearrange("p (k n) -> p k n", k=8)
            kT = A.alloc(8 * MEM, BF16).rearrange("p (k m) -> p k m", k=8)
            vtok = A.alloc(2 * D, BF16).rearrange("p (m n) -> p m n", m=2)
            A.mark()
            wkv = A.alloc(8 * 2 * D, BF16).rearrange("p (k n) -> p k n", k=8)
            memb = [A.alloc(D, BF16) for _ in range(2)]
            memT = A.alloc(8 * MEM, BF16).rearrange("p (k m) -> p k m", k=8)
            for j in range(2):
                P.dma(lambda e, j=j: e.dma_start(out=memb[j], in_=mem_d[j * 128:(j + 1) * 128, :]), w=[("memb", j)], q="pool")
                transpose_to(memT[:, :, j * 128:(j + 1) * 128], memb[j], 6 + j, [("memb", j)], ["memT"])
            for cb in range(2):
                for k0 in range(0, 8, 2):
                    P.dma(lambda e, cb=cb, k0=k0: e.dma_start(
                        out=wkv[:, k0:k0 + 2, cb * D:(cb + 1) * D],
                        in_=xa_wkv[l, k0 * 128:(k0 + 2) * 128, cb * D:(cb + 1) * D].rearrange("(k p) n -> p k n", p=128)),
                        w=["wkv"], q="pool")
            for ct in range(8):
                bank = ct % 2
                def mm(e, ct=ct, bank=bank):
                    ins = None
                    for k in range(8):
                        ins = e.matmul(PS(bank)[:, 0:MEM], lhsT=wkv[:, k, ct * 128:(ct + 1) * 128], rhs=memT[:, k, :],
                                       start=(k == 0), stop=(k == 7))
                    return ins
                P.pe(mm, r=["wkv", "memT"], w=[("ps", bank)])
                cast_copy(kT[:, ct, :], PS(bank)[:, 0:MEM], [("ps", bank)], ["kT"])
            for mt in range(2):
                for hb in range(2):
                    bank = 2 + hb
                    def mm(e, mt=mt, hb=hb, bank=bank):
                        ins = None
                        for k in range(8):
                            ins = e.matmul(PS(bank), lhsT=memT[:, k, mt * 128:(mt + 1) * 128],
                                           rhs=wkv[:, k, D + hb * 512:D + (hb + 1) * 512], start=(k == 0), stop=(k == 7))
                        return ins
                    P.pe(mm, r=["wkv", "memT"], w=[("ps", bank)])
                    cast_copy(vtok[:, mt, hb * 512:(hb + 1) * 512], PS(bank), [("ps", bank)], ["vtok"])
            P.nosched = False
            load_w_bf16(wq, xa_wq[l], 8, D, "wq")
            load_w_bf16(wo, xa_wo[l], 8, D, "wo")
            load_gb(l, 1)
            E = Epi()
            qT = A.alloc(8 * 512, BF16).rearrange("p (k t) -> p k t", k=8)
            oT = A.alloc(8 * 512, BF16).rearrange("p (k t) -> p k t", k=8)
            ex = A.alloc(MEM, F32)
            pb = A.alloc(MEM, BF16)
            pT = A.alloc(MEM, BF16).rearrange("p (m t) -> p m t", m=2)
            sm = A.alloc(8, F32)
            SCL = 1.0 / 16.0
            last_layer_xT = True
            for grp in range(DBG["xa_groups"]):
                for ct in range(8):
                    bank = 6
                    def mm(e, ct=ct, grp=grp, bank=bank):
                        ins = None
                        for k in range(8):
                            ins = e.matmul(PS(bank), lhsT=wq[:, k, ct * 128:(ct + 1) * 128], rhs=xT3[:, k, grp * 512:(grp + 1) * 512],
                                           start=(k == 0), stop=(k == 7))
                        return ins
                    P.pe(mm, r=["wq"] + xT_all[grp * 4:grp * 4 + 4], w=[("ps", bank)])
                    cast_copy(qT[:, ct, :], PS(bank), [("ps", bank)], [("qT", ct)])
                for tl in range(4):
                    i = grp * 4 + tl
                    tsl = slice(tl * 128, (tl + 1) * 128)
                    for h in range(4):
                        sc = PS(2 + h % 2)[:, 0:256]
                        ksc = ("ps", 2 + h % 2)
                        def ms(e, h=h, sc=sc, tsl=tsl):
                            ins = None
                            for dk in range(2):
                                ins = e.matmul(sc, lhsT=qT[:, 2 * h + dk, tsl], rhs=kT[:, 2 * h + dk, :], start=(dk == 0), stop=(dk == 1))
                            return ins
                        P.pe(ms, r=[("qT", 2 * h), ("qT", 2 * h + 1), "kT"], w=[ksc])
                        P.dve(lambda e, sc=sc: e.reduce_max(out=sm[:, 0:1], in_=sc, axis=AX.X), r=[ksc], w=["sm0"])
                        P.dve(lambda e: e.tensor_scalar(out=sm[:, 1:2], in0=sm[:, 0:1], scalar1=-SCL, scalar2=None, op0=ALU.mult), r=["sm0"], w=["sm1"])
                        P.act(lambda e, sc=sc: e.activation(out=ex, in_=sc, func=AF.Exp, bias=sm[:, 1:2], scale=SCL), r=[ksc, "sm1"], w=["ex"])
                        P.dve(lambda e: e.reduce_sum(out=sm[:, 2:3], in_=ex, axis=AX.X), r=["ex"], w=["sm2"])
                        P.dve(lambda e: e.reciprocal(out=sm[:, 3:4], in_=sm[:, 2:3]), r=["sm2"], w=["sm3"])
                        P.dve(lambda e: e.tensor_scalar(out=pb, in0=ex, scalar1=sm[:, 3:4], scalar2=None, op0=ALU.mult), r=["ex", "sm3"], w=["pb"])
                        ptq = PSB(4)[:, 0:256]
                        kpt = ("ps", 4)
                        def mt_(e, ptq=ptq):
                            ins = None
                            for mt in range(2):
                                ins = e.transpose(out=ptq[:, mt * 128:(mt + 1) * 128], in_=pb[:, mt * 128:(mt + 1) * 128], identity=ident)
                            return ins
                        P.pe(mt_, r=["pb", "ident"], w=[kpt])
                        cast_copy(pT, ptq.rearrange("p (m t) -> p m t", m=2), [kpt], ["pT"])
                        pv = PS((5, 7)[h % 2])[:, 0:256]
                        kpv = ("ps", (5, 7)[h % 2])
                        def mpv(e, h=h, pv=pv):
                            ins = None
                            for dv in range(2):
                                for mt in range(2):
                                    ins = e.matmul(pv[:, dv * 128:(dv + 1) * 128], lhsT=vtok[:, mt, h * 256 + dv * 128:h * 256 + (dv + 1) * 128],
                                                   rhs=pT[:, mt, :], start=(mt == 0), stop=(mt == 1))
                            return ins
                        P.pe(mpv, r=["pT", "vtok"], w=[kpv])
                        cast_copy(oT[:, 2 * h:2 * h + 2, tsl], pv.rearrange("p (d t) -> p d t", d=2), [kpv], [("oT", tl)])
                    for hh in range(2):
                        def mm(e, hh=hh, tsl=tsl):
                            ins = None
                            for k in range(8):
                                ins = e.matmul(PS(hh), lhsT=oT[:, k, tsl], rhs=wo[:, k, hh * 512:(hh + 1) * 512], start=(k == 0), stop=(k == 7))
                            return ins
                        P.pe(mm, r=["wo", ("oT", tl)], w=[("ps", hh)])
                    epilogue(E, i, [PS(0), PS(1)], [("ps", 0), ("ps", 1)], cur_res, dst_res, make_xT=False)
            P.nosched = False
            P.barrier()
            A.release()
            A.release()

    def moe_phase(l, cur_res, dst_res, want_xT):
        if True:
            A.mark()
            idx = A.alloc(NT * 2, I32 if False else F32).bitcast(I32).rearrange("p (t k) -> p t k", k=2)
            gates = A.alloc(NT * 2, F32).rearrange("p (t k) -> p t k", k=2)
            A.mark()
            wr32 = A.alloc(8 * 36, F32).rearrange("p (k n) -> p k n", k=8)
            P.dma(ncd(lambda e: e.dma_start(out=wr32[:, :, 0:4], in_=moe_wg[l].rearrange("(k p) n -> p k n", p=128))), w=["wr32"])
            P.dma(ncd(lambda e: e.dma_start(out=wr32[:, :, 4:36], in_=moe_we[l].rearrange("(k p) n -> p k n", p=128))), w=["wr32"])
            bias_bc = A.alloc(36, F32)
            P.dma(lambda e: e.dma_start(out=bias_bc[:, 0:4], in_=moe_bg[l:l + 1, :].broadcast_to([128, 4])), w=["bias_bc"])
            P.dma(lambda e: e.dma_start(out=bias_bc[:, 4:36], in_=moe_be[l:l + 1, :].broadcast_to([128, 32])), w=["bias_bc"])
            eoff = A.alloc(32, F32)
            P.dma(lambda e: e.dma_start(out=eoff, in_=c_eoff.broadcast_to([128, 32])), w=["eoff"])
            ustr = A.alloc(128, BF16)
            onesb = A.alloc(128, BF16)
            P.dma(lambda e: e.dma_start(out=ustr, in_=c_masks[2]), w=["ustr"], q="pool")
            P.dma(lambda e: e.dma_start(out=onesb, in_=c_masks[3]), w=["onesb"], q="pool")
            trash = A.alloc(1, F32)
            P.dma(ncd(lambda e: e.dma_start(out=trash, in_=c_trash)), w=["trash"])
            carry = A.alloc(32, F32)
            P.dve(lambda e: e.memset(carry, 0.0), w=["carry"])
            if DBG.get("zero_xs"):
                zt = A.alloc(D, BF16)
                P.pool(lambda e: e.memset(zt, 0.0), w=["zt"])
                for r0 in range(0, NEXP * CAP + 128, 128):
                    P.dma(lambda e, r0=r0: e.dma_start(out=XS[r0:r0 + 128, :], in_=zt), r=["zt"], w=["XS"])
            x2 = [A.alloc(D, F32) for _ in range(2)]
            xb2 = [A.alloc(D, BF16) for _ in range(2)]
            RB = []
            for _p in range(2):
                rb = {}
                rb["x2T"] = A.alloc(8 * 128, F32).rearrange("p (k t) -> p k t", k=8)
                for nm, n_ in (("lg", 36), ("rt", 64), ("maskg", 4), ("esel", 8), ("e2", 8), ("mask1", 8), ("mask2", 8),
                               ("M1", 32), ("M2", 32), ("Ms", 32), ("pos", 32), ("tmp", 32)):
                    rb[nm] = A.alloc(n_, F32)
                rb["Mb"] = A.alloc(32, BF16)
                RB.append(rb)
            def route_tile(i):
                b = i % 2
                rbp = b if DBG.get("route_double", 0) else 0
                rb = RB[rbp]
                x2T, lg, rt, maskg, esel, e2, mask1, mask2 = rb['x2T'], rb['lg'], rb['rt'], rb['maskg'], rb['esel'], rb['e2'], rb['mask1'], rb['mask2']
                M1, M2, Ms, Mb, pos, tmp = rb['M1'], rb['M2'], rb['Ms'], rb['Mb'], rb['pos'], rb['tmp']
                M1v = M1.rearrange('p (g j) -> p g j', g=4)
                M2v = M2.rearrange('p (g j) -> p g j', g=4)
                pb_ = 4 * rbp
                kx2, kxb = ("x2", b), ("xb2", b)
                P.dma(lambda e, i=i, b=b: e.dma_start(out=x2[b], in_=cur_res[i * 128:(i + 1) * 128, :]), w=[kx2])
                P.act(lambda e, b=b: e.copy(out=xb2[b], in_=x2[b]), r=[kx2], w=[kxb])
                for half in range(2):
                    def tr(e, half=half, b=b):
                        ins = None
                        for q in range(4):
                            k = half * 4 + q
                            ins = e.transpose(out=PS(pb_ + half)[:, q * 128:(q + 1) * 128], in_=x2[b][:, k * 128:(k + 1) * 128], identity=ident32)
                        return ins
                    P.pe(tr, r=[kx2, "ident32"], w=[("ps", pb_ + half)])
                    cast_copy(x2T[:, half * 4:(half + 1) * 4, :], PS(pb_ + half).rearrange("p (k t) -> p k t", k=4), [("ps", pb_ + half)], [("x2T", rbp)])
                def mlg(e):
                    ins = None
                    for k in range(8):
                        ins = e.matmul(PS(pb_ + 2)[:, 0:36], lhsT=x2T[:, k, :], rhs=wr32[:, k, :], start=(k == 0), stop=(k == 7))
                    return ins
                P.pe(mlg, r=[("x2T", rbp), "wr32"], w=[("ps", pb_ + 2)])
                V = P.dve
                V(lambda e: e.tensor_tensor(out=lg, in0=PS(pb_ + 2)[:, 0:36], in1=bias_bc, op=ALU.add), r=[("ps", pb_ + 2), "bias_bc"], w=[("lg", rbp)])
                V(lambda e: e.reduce_max(out=rt[:, 0:1], in_=lg[:, 0:4], axis=AX.X), r=[("lg", rbp)], w=[("rt0", rbp)])
                V(lambda e: e.tensor_scalar(out=maskg, in0=lg[:, 0:4], scalar1=rt[:, 0:1], scalar2=None, op0=ALU.is_equal), r=[("lg", rbp), ("rt0", rbp)], w=[("maskg", rbp)])
                V(lambda e: e.tensor_scalar(out=rt[:, 1:2], in0=rt[:, 0:1], scalar1=-1.0, scalar2=None, op0=ALU.mult), r=[("rt0", rbp)], w=[("rt1", rbp)])
                P.act(lambda e: e.activation(out=rt[:, 4:8], in_=lg[:, 0:4], func=AF.Exp, bias=rt[:, 1:2], scale=1.0), r=[("lg", rbp), ("rt1", rbp)], w=[("rt4", rbp)])
                V(lambda e: e.reduce_sum(out=rt[:, 2:3], in_=rt[:, 4:8], axis=AX.X), r=[("rt4", rbp)], w=[("rt2", rbp)])
                V(lambda e: e.reciprocal(out=rt[:, 3:4], in_=rt[:, 2:3]), r=[("rt2", rbp)], w=[("rt3", rbp)])
                V(lambda e: e.tensor_scalar(out=esel, in0=lg[:, 4:12], scalar1=maskg[:, 0:1], scalar2=None, op0=ALU.mult), r=[("lg", rbp), ("maskg", rbp)], w=[("esel", rbp)])
                for g in range(1, 4):
                    V(lambda e, g=g: e.scalar_tensor_tensor(out=esel, in0=lg[:, 4 + 8 * g:12 + 8 * g], scalar=maskg[:, g:g + 1], in1=esel,
                                                            op0=ALU.mult, op1=ALU.add), r=[("lg", rbp), ("maskg", rbp), ("esel", rbp)], w=[("esel", rbp)])
                V(lambda e: e.reduce_max(out=rt[:, 8:9], in_=esel, axis=AX.X), r=[("esel", rbp)], w=[("rt8", rbp)])
                V(lambda e: e.tensor_scalar(out=mask1, in0=esel, scalar1=rt[:, 8:9], scalar2=None, op0=ALU.is_equal), r=[("esel", rbp), ("rt8", rbp)], w=[("mask1", rbp)])
                V(lambda e: e.scalar_tensor_tensor(out=e2, in0=mask1, scalar=-1e30, in1=esel, op0=ALU.mult, op1=ALU.add), r=[("mask1", rbp), ("esel", rbp)], w=[("e2", rbp)])
                V(lambda e: e.reduce_max(out=rt[:, 9:10], in_=e2, axis=AX.X), r=[("e2", rbp)], w=[("rt9", rbp)])
                V(lambda e: e.tensor_scalar(out=mask2, in0=e2, scalar1=rt[:, 9:10], scalar2=None, op0=ALU.is_equal), r=[("e2", rbp), ("rt9", rbp)], w=[("mask2", rbp)])
                V(lambda e: e.tensor_tensor(out=rt[:, 10:11], in0=rt[:, 9:10], in1=rt[:, 8:9], op=ALU.subtract), r=[("rt8", rbp), ("rt9", rbp)], w=[("rt10", rbp)])
                P.act(lambda e: e.activation(out=rt[:, 11:12], in_=rt[:, 10:11], func=AF.Exp), r=[("rt10", rbp)], w=[("rt11", rbp)])
                V(lambda e: e.tensor_scalar(out=rt[:, 12:13], in0=rt[:, 11:12], scalar1=1.0, scalar2=None, op0=ALU.add), r=[("rt11", rbp)], w=[("rt12", rbp)])
                V(lambda e: e.reciprocal(out=rt[:, 13:14], in_=rt[:, 12:13]), r=[("rt12", rbp)], w=[("rt13", rbp)])
                V(lambda e, i=i: e.tensor_tensor(out=gates[:, i, 0:1], in0=rt[:, 3:4], in1=rt[:, 13:14], op=ALU.mult), r=[("rt3", rbp), ("rt13", rbp)], w=["gates"])
                V(lambda e, i=i: e.tensor_tensor(out=gates[:, i, 1:2], in0=gates[:, i, 0:1], in1=rt[:, 11:12], op=ALU.mult), r=["gates", ("rt11", rbp)], w=["gates"])
                for g in range(4):
                    V(lambda e, g=g: e.tensor_scalar(out=M1v[:, g, :], in0=mask1, scalar1=maskg[:, g:g + 1], scalar2=None, op0=ALU.mult),
                      r=[("mask1", rbp), ("maskg", rbp)], w=[("M1", rbp)])
                    V(lambda e, g=g: e.tensor_scalar(out=M2v[:, g, :], in0=mask2, scalar1=maskg[:, g:g + 1], scalar2=None, op0=ALU.mult),
                      r=[("mask2", rbp), ("maskg", rbp)], w=[("M2", rbp)])
                V(lambda e: e.tensor_tensor(out=Ms, in0=M1, in1=M2, op=ALU.add), r=[("M1", rbp), ("M2", rbp)], w=[("Ms", rbp)])
                V(lambda e: e.tensor_copy(out=Mb, in_=Ms), r=[("Ms", rbp)], w=[("Mb", rbp)])
                def mpos(e):
                    e.matmul(PS(pb_ + 3)[:, 0:32], lhsT=ustr, rhs=Mb, start=True, stop=True)
                    return e.matmul(PS(pb_ + 3)[:, 32:64], lhsT=onesb, rhs=Mb, start=True, stop=True)
                P.pe(mpos, r=[("Mb", rbp), "ustr", "onesb"], w=[("ps", pb_ + 3)])
                V(lambda e: e.tensor_tensor(out=pos, in0=PS(pb_ + 3)[:, 0:32], in1=carry, op=ALU.add), r=[("ps", pb_ + 3), "carry"], w=[("pos", rbp)])
                V(lambda e: e.tensor_tensor(out=carry, in0=PS(pb_ + 3)[:, 32:64], in1=carry, op=ALU.add), r=[("ps", pb_ + 3), "carry"], w=["carry"])
                V(lambda e: e.tensor_scalar(out=tmp, in0=pos, scalar1=float(CAP), scalar2=None, op0=ALU.is_ge), r=[("pos", rbp)], w=[("tmp", rbp)])
                V(lambda e: e.tensor_tensor(out=pos, in0=pos, in1=eoff, op=ALU.add), r=[("pos", rbp), "eoff"], w=[("pos", rbp)])
                V(lambda e: e.tensor_scalar(out=Ms, in0=pos, scalar1=-1.0, scalar2=trash[:, 0:1], op0=ALU.mult, op1=ALU.add), r=[("pos", rbp), "trash"], w=[("Ms", rbp)])
                V(lambda e: e.tensor_tensor(out=tmp, in0=tmp, in1=Ms, op=ALU.mult), r=[("tmp", rbp), ("Ms", rbp)], w=[("tmp", rbp)])
                V(lambda e: e.tensor_tensor(out=pos, in0=pos, in1=tmp, op=ALU.add), r=[("pos", rbp), ("tmp", rbp)], w=[("pos", rbp)])
                for kk, Mk in enumerate((M1, M2)):
                    kM = ("M1", rbp) if kk == 0 else ("M2", rbp)
                    V(lambda e, Mk=Mk: e.tensor_tensor(out=tmp, in0=Mk, in1=pos, op=ALU.mult), r=[kM, ("pos", rbp)], w=[("tmp", rbp)])
                    V(lambda e, kk=kk: e.reduce_sum(out=rt[:, 16 + kk:17 + kk], in_=tmp, axis=AX.X), r=[("tmp", rbp)], w=[("rtidx", kk, rbp)])
                    V(lambda e, kk=kk, i=i: e.tensor_copy(out=idx[:, i, kk:kk + 1], in_=rt[:, 16 + kk:17 + kk]), r=[("rtidx", kk, rbp)], w=[("idx", i, kk)])
                    P.dma(lambda e, kk=kk, i=i, b=b: e.indirect_dma_start(
                        out=XS, out_offset=bass.IndirectOffsetOnAxis(idx[:, i, kk:kk + 1], 0), in_=xb2[b], in_offset=None), r=[("idx", i, kk), kxb], w=[("XS", i, kk)], q="pool")
            for i in range(NT):
                route_tile(i)
            P.barrier()
            A.release()
            A.mark()
            NCT = CAP // 128
            xT_f32 = xT.bitcast(F32)
            stg = [xT_f32[:, k * 2048:(k + 1) * 2048] for k in range(6)]
            wgb = [A.alloc(8 * 512, BF16).rearrange("p (k n) -> p k n", k=8) for _ in range(2)]
            wub = [A.alloc(8 * 512, BF16).rearrange("p (k n) -> p k n", k=8) for _ in range(2)]
            wdb = [A.alloc(4 * D, BF16).rearrange("p (k n) -> p k n", k=4) for _ in range(2)]
            xsb = [A.alloc(D, BF16) for _ in range(4)]
            xsT2 = [A.alloc(8 * CAP, BF16).rearrange("p (k t) -> p k t", k=8) for _ in range(2)]
            hT2 = [A.alloc(4 * CAP, BF16).rearrange("p (k t) -> p k t", k=4) for _ in range(2)]
            sg2 = [A.alloc(CAP, F32) for _ in range(2)]
            ysb = [A.alloc(D, F32) for _ in range(4)]
            sn = 0
            for ex_i in range(NEXP):
                b = ex_i % 2
                xsT, hT, sg = xsT2[b], hT2[b], sg2[b]
                kxsT, khT, ksg = ("xsT", b), ("hT", b), ("sg", b)
                chunks = []
                for (wsrc, dstb, nm) in ((moe_gate, wgb, "wg"), (moe_up, wub, "wu")):
                    for c in range(2):
                        chunks.append((wsrc[l, ex_i, c * 512:(c + 1) * 512, :].rearrange("(k p) n -> p k n", p=128),
                                       dstb[b][:, c * 4:(c + 1) * 4, :], (nm, b), 4))
                for c in range(2):
                    chunks.append((moe_down[l, ex_i, c * 256:(c + 1) * 256, :].rearrange("(k p) n -> p k n", p=128),
                                   wdb[b][:, c * 2:(c + 1) * 2, :], ("wd", b), 2))
                for (src, dst, key, kk) in chunks:
                    sb_ = sn % 6
                    sn += 1
                    sview = stg[sb_].rearrange("p (k n) -> p k n", k=kk)
                    P.dma(lambda e, src=src, sview=sview: e.dma_start(out=sview, in_=src), w=[("stg", sb_)], q="pool")
                    if sn % 2 == 0:
                        P.act(lambda e, dst=dst, sview=sview: e.copy(out=dst, in_=sview), r=[("stg", sb_)], w=[key], c=2.0)
                    else:
                        P.dve(lambda e, dst=dst, sview=sview: e.tensor_copy(out=dst, in_=sview), r=[("stg", sb_)], w=[key], c=2.3)
                for stl in range(NCT):
                    xb_ = (ex_i * NCT + stl) % 4
                    r0 = ex_i * CAP + stl * 128
                    P.dma(lambda e, r0=r0, xb_=xb_: e.dma_start(out=xsb[xb_], in_=XS[r0:r0 + 128, :]), r=["XS"], w=[("xsb", xb_)], q="pool")
                    transpose_to(xsT[:, :, stl * 128:(stl + 1) * 128], xsb[xb_], 7, [("xsb", xb_)], [kxsT])
                for ft in range(4):
                    for m, wb_, kw in ((0, wgb, "wg"), (1, wub, "wu")):
                        def mm(e, ft=ft, m=m, wb_=wb_, b=b, xsT=xsT):
                            ins = None
                            for k in range(8):
                                ins = e.matmul(PS(m)[:, 0:CAP], lhsT=wb_[b][:, k, ft * 128:(ft + 1) * 128], rhs=xsT[:, k, :],
                                               start=(k == 0), stop=(k == 7))
                            return ins
                        P.pe(mm, r=[(kw, b), kxsT], w=[("ps", m)])
                    P.act(lambda e, sg=sg: e.activation(out=sg, in_=PS(0)[:, 0:CAP], func=AF.Silu), r=[("ps", 0)], w=[ksg])
                    P.dve(lambda e, ft=ft, sg=sg, hT=hT: e.tensor_tensor(out=hT[:, ft, :], in0=sg, in1=PS(1)[:, 0:CAP], op=ALU.mult), r=[ksg, ("ps", 1)], w=[khT])
                for stl in range(NCT):
                    yb_ = stl % 4
                    for hh in range(2):
                        bank = 2 + (stl % 2) * 2 + hh
                        def mm(e, stl=stl, hh=hh, bank=bank, b=b, hT=hT):
                            ins = None
                            for ft in range(4):
                                ins = e.matmul(PS(bank), lhsT=hT[:, ft, stl * 128:(stl + 1) * 128], rhs=wdb[b][:, ft, hh * 512:(hh + 1) * 512],
                                               start=(ft == 0), stop=(ft == 3))
                            return ins
                        P.pe(mm, r=[khT, ("wd", b)], w=[("ps", bank)])
                        if hh == 0:
                            P.act(lambda e, bank=bank, yb_=yb_: e.copy(out=ysb[yb_][:, 0:512], in_=PS(bank)), r=[("ps", bank)], w=[("ysb", yb_)])
                        else:
                            P.dve(lambda e, bank=bank, yb_=yb_: e.tensor_copy(out=ysb[yb_][:, 512:1024], in_=PS(bank)), r=[("ps", bank)], w=[("ysb", yb_)])
                    r0 = ex_i * CAP + stl * 128
                    P.dma(lambda e, r0=r0, yb_=yb_: e.dma_start(out=YS[r0:r0 + 128, :], in_=ysb[yb_]), r=[("ysb", yb_)], w=[("YS", r0)])
            P.barrier()
            A.release()
            A.mark()
            load_gb(l, 2)
            E = Epi()
            y1 = [A.alloc(D, F32) for _ in range(2)]
            y2 = [A.alloc(D, F32) for _ in range(2)]
            hm = [A.alloc(D, F32) for _ in range(2)]
            dst = dst_res
            def gather(i):
                b = i % 2
                P.dma(lambda e, i=i, b=b: e.indirect_dma_start(
                    out=y1[b], out_offset=None, in_=YS, in_offset=bass.IndirectOffsetOnAxis(idx[:, i, 0:1], 0)), r=["YS"], w=[("y1", b)], q="pool")
                P.dma(lambda e, i=i, b=b: e.indirect_dma_start(
                    out=y2[b], out_offset=None, in_=YS, in_offset=bass.IndirectOffsetOnAxis(idx[:, i, 1:2], 0)), r=["YS"], w=[("y2", b)], q="pool")
            gather(0)
            gather(1)
            for i in range(NT):
                b = i % 2
                P.dve(lambda e, i=i, b=b: e.tensor_scalar(out=hm[b], in0=y1[b], scalar1=gates[:, i, 0:1], scalar2=None, op0=ALU.mult),
                      r=[("y1", b)], w=[("hm", b)])
                P.dve(lambda e, i=i, b=b: e.scalar_tensor_tensor(out=hm[b], in0=y2[b], scalar=gates[:, i, 1:2], in1=hm[b], op0=ALU.mult, op1=ALU.add),
                      r=[("y2", b), ("hm", b)], w=[("hm", b)])
                if i + 2 < NT:
                    gather(i + 2)
                epilogue(E, i, [hm[b][:, 0:512], hm[b][:, 512:1024]], [("hm", b)], cur_res, dst, make_xT=want_xT, gmul="dve")
            P.barrier()
            A.release()
            A.release()


    def hg_phase(cur_res, dst_res):
        A.mark()
        A.mark()
        P.nosched = bool(DBG.get("hg_nosched", 0))
        lbraw = A.alloc(32, F32)
        lbt = A.alloc(32, F32)
        lbrows = A.alloc(128, F32)
        P.dma(lambda e: e.dma_start(out=lbrows[0:32, :], in_=hg_lb.rearrange("l d (h k) -> (l d h) k", k=128)), w=["lbrows"])
        P.pe(lambda e: e.transpose(out=PS(0)[:, 0:32], in_=lbrows[0:32, :], identity=ident32[0:32, 0:32]), r=["lbrows", "ident32"], w=[("ps", 0)], c=0.3)
        P.dve(lambda e: e.tensor_copy(out=lbraw, in_=PS(0)[:, 0:32]), r=[("ps", 0)], w=["lbraw"])
        P.dve(lambda e: e.tensor_tensor(out=lbt[:, 0:16], in0=lbraw[:, 16:32], in1=lbraw[:, 0:16], op=ALU.subtract), r=["lbraw"], w=["lbd"])
        P.act(lambda e: e.activation(out=lbt[:, 0:16], in_=lbt[:, 0:16], func=AF.Sigmoid), r=["lbd"], w=["lb"])
        P.dve(lambda e: e.tensor_scalar(out=lbt[:, 16:32], in0=lbt[:, 0:16], scalar1=-1.0, scalar2=1.0, op0=ALU.mult, op1=ALU.add), r=["lb"], w=["oml"])
        ng = A.alloc(1, F32)
        P.dma(ncd(lambda e: e.dma_start(out=ng, in_=hg_norm_g.rearrange("(k o) -> k o", o=1))), w=["ng"])
        cmask = A.alloc(512, F32)
        P.pool(lambda e: e.memset(cmask, 1.0), w=["cmask"])
        P.pool(lambda e: e.memset(cmask.rearrange("p (c l) -> p c l", l=64)[:, :, 0:1], 0.0), r=["cmask"], w=["cmask"])
        mk = [A.alloc(128, F32) for _ in range(2)]
        for d in range(2):
            P.dma(lambda e, d=d: e.dma_start(out=mk[d], in_=c_masks[d]), w=[("mk", d)])
        Sst = [[A.alloc(128, F32) for _ in range(2)] for _ in range(2)]
        s_cur = [0, 0]
        wh = [A.alloc(8 * 5 * 128, BF16).rearrange("p (k q n) -> p k q n", k=8, q=5) for _ in range(2)]
        rst = A.alloc(4 * 512, F32)
        vtok = A.alloc(NT * 128, BF16).rearrange("p (t n) -> p t n", t=NT)
        qT = A.alloc(S, F32)
        oTh = A.alloc(S, F32)
        T = []
        for d in range(2):
            t = {}
            for nm in ("sgm", "lgf", "bb", "kk", "t1", "t2"):
                t[nm] = A.alloc(512, F32)
            t["kbT"] = A.alloc(512, BF16)
            for nm in ("qt", "kt", "qb"):
                t[nm] = [A.alloc(512, BF16) for _ in range(2)]
            t["kbtok"] = [A.alloc(512, BF16).rearrange("p (t n) -> p t n", t=4) for _ in range(2)]
            t["dec"] = [A.alloc(8, F32) for _ in range(2)]
            t["sTm"] = [A.alloc(128, BF16) for _ in range(2)]
            t["Sp"] = [[A.alloc(128, BF16) for _ in range(2)] for _ in range(2)]
            T.append(t)
        o2 = A.alloc(512, F32)
        rsn = A.alloc(512, F32)
        sgt = A.alloc(512, F32)
        onb = [A.alloc(512, BF16) for _ in range(2)]

        def K_(d, nm):
            return ("hg", d, nm)

        def v3(ap):
            return ap.rearrange("p (c l) -> p c l", l=64)

        def prep(h, d, blk, buf, wb, kwb, part="ab"):
            t = T[d]
            t0 = blk * 512
            col = d * 8 + h
            fb = (0, 6)[d]
            qt_, kt_, qb_, kbtok_, dec_ = t["qt"][buf], t["kt"][buf], t["qb"][buf], t["kbtok"][buf], t["dec"][buf]
            def mm(e):
                ins = None
                for k in range(8):
                    ins = e.matmul(PS(fb), lhsT=wb[:, k, 2 + d, :], rhs=xT3[:, k, t0:t0 + 512], start=(k == 0), stop=(k == 7))
                return ins
            if "a" in part:
                P.pe(mm, r=[kwb] + xT_all[blk * 4:blk * 4 + 4], w=[("ps", fb)])
                P.act(lambda e: e.activation(out=t["sgm"], in_=PS(fb), func=AF.Sigmoid), r=[("ps", fb)], w=[K_(d, "sgm"), "sigdone"])
                P.dve(lambda e: e.tensor_scalar(out=t["sgm"], in0=t["sgm"], scalar1=lbt[:, 16 + col:17 + col], scalar2=lbt[:, col:col + 1],
                                                op0=ALU.mult, op1=ALU.add), r=[K_(d, "sgm"), "lb", "oml"], w=[K_(d, "sgm")])
            if "a" in part and ((blk < 4) if d == 0 else (blk >= 4)):
                qblk_, vblk_ = qT[:, t0:t0 + 512], vtok[:, blk * 4:(blk + 1) * 4, :]
                def mq(e):
                    ins = None
                    for k in range(8):
                        ins = e.matmul(PS(fb), lhsT=wb[:, k, 0, :], rhs=xT3[:, k, t0:t0 + 512], start=(k == 0), stop=(k == 7))
                    return ins
                P.pe(mq, r=[kwb] + xT_all[blk * 4:blk * 4 + 4], w=[("ps", fb)])
                P.act(lambda e: e.activation(out=qblk_, in_=PS(fb), func=AF.Sigmoid), r=[("ps", fb)], w=[("qT", blk), "sigdone"])
                P.dve(lambda e: e.tensor_tensor(out=qblk_, in0=qblk_, in1=PS(fb), op=ALU.mult), r=[("qT", blk), ("ps", fb)], w=[("qT", blk)])
                def mv(e):
                    ins = None
                    for tl in range(4):
                        tile = blk * 4 + tl
                        for k in range(8):
                            ins = e.matmul(PS(fb)[:, tl * 128:(tl + 1) * 128], lhsT=xT3[:, k, tile * 128:(tile + 1) * 128], rhs=wb[:, k, 1, :],
                                           start=(k == 0), stop=(k == 7))
                    return ins
                P.pe(mv, r=[kwb] + xT_all[blk * 4:blk * 4 + 4], w=[("ps", fb)], c=2.5)
                cast_copy(vblk_, PS(fb).rearrange("p (t n) -> p t n", t=4), [("ps", fb)], [("vtok", blk)])
            if "b" not in part:
                return
            P.act(lambda e: e.activation(out=t["lgf"], in_=t["sgm"], func=AF.Ln), r=[K_(d, "sgm"), "sigdone"], w=[K_(d, "lgf")])
            P.pool(lambda e: e.tensor_scalar(out=t["kk"], in0=t["sgm"], scalar1=-1.0, scalar2=1.0, op0=ALU.mult, op1=ALU.add),
                   r=[K_(d, "sgm")], w=[K_(d, "kk")])
            if d == 0:
                P.dve(lambda e: e.tensor_tensor_scan(out=t["bb"], data0=cmask, data1=t["lgf"], initial=0.0, op0=ALU.mult, op1=ALU.add),
                      r=["cmask", K_(d, "lgf")], w=[K_(d, "bb")])
                iref, ilast = 32, 63
            else:
                P.dve(lambda e: e.tensor_tensor_scan(out=t["t2"], data0=cmask, data1=t["lgf"], initial=0.0, op0=ALU.mult, op1=ALU.add),
                      r=["cmask", K_(d, "lgf")], w=[K_(d, "t2")])
                P.dve(lambda e: e.scalar_tensor_tensor(out=t["t1"], in0=t["t2"], scalar=-1.0, in1=t["lgf"], op0=ALU.mult, op1=ALU.add),
                      r=[K_(d, "t2"), K_(d, "lgf")], w=[K_(d, "t1")])
                P.dve(lambda e: e.tensor_tensor(out=v3(t["bb"]), in0=v3(t["t1"]), in1=v3(t["t2"])[:, :, 63:64].broadcast_to([128, 8, 64]), op=ALU.add),
                      r=[K_(d, "t1"), K_(d, "t2")], w=[K_(d, "bb")])
                iref, ilast = 31, 0
            bref = v3(t["bb"])[:, :, iref:iref + 1].broadcast_to([128, 8, 64])
            blast = v3(t["bb"])[:, :, ilast:ilast + 1].broadcast_to([128, 8, 64])
            qsl = qT[:, t0:t0 + 512]
            kqb = ("qT", blk)
            kq, kk_, kb_, kkb, kd = K_(d, ("qt", buf)), K_(d, ("kt", buf)), K_(d, ("qb", buf)), K_(d, ("kbtok", buf)), K_(d, ("dec", buf))
            P.pool(lambda e: e.tensor_tensor(out=v3(t["t1"]), in0=v3(t["bb"]), in1=bref, op=ALU.subtract), r=[K_(d, "bb")], w=[K_(d, "t1")])
            P.act(lambda e: e.activation(out=t["t2"], in_=t["t1"], func=AF.Exp), r=[K_(d, "t1")], w=[K_(d, "t2")])
            P.dve(lambda e: e.tensor_tensor(out=qt_, in0=qsl, in1=t["t2"], op=ALU.mult), r=[kqb, K_(d, "t2")], w=[kq])
            P.act(lambda e: e.activation(out=t["t2"], in_=t["t1"], func=AF.Exp, scale=-1.0), r=[K_(d, "t1"), kq], w=[K_(d, "t2")])
            P.pool(lambda e: e.tensor_tensor(out=kt_, in0=t["kk"], in1=t["t2"], op=ALU.mult), r=[K_(d, "kk"), K_(d, "t2")], w=[kk_])
            P.act(lambda e: e.activation(out=t["sgm"], in_=t["bb"], func=AF.Exp), r=[K_(d, "bb")], w=[K_(d, "sgm")])
            P.dve(lambda e: e.tensor_tensor(out=qb_, in0=qsl, in1=t["sgm"], op=ALU.mult), r=[kqb, K_(d, "sgm")], w=[kb_])
            P.pool(lambda e: e.tensor_tensor(out=v3(t["t1"]), in0=v3(t["bb"]), in1=blast, op=ALU.subtract), r=[K_(d, "bb"), K_(d, "t2")], w=[K_(d, "t1")])
            P.act(lambda e: e.activation(out=t["t2"], in_=t["t1"], func=AF.Exp, scale=-1.0), r=[K_(d, "t1"), kk_], w=[K_(d, "t2")])
            P.pool(lambda e: e.tensor_tensor(out=t["kbT"], in0=t["kk"], in1=t["t2"], op=ALU.mult), r=[K_(d, "kk"), K_(d, "t2")], w=[K_(d, "kbT")])
            P.act(lambda e: e.activation(out=dec_, in_=v3(t["bb"])[:, :, ilast], func=AF.Exp), r=[K_(d, "bb")], w=[kd])
            def tr(e):
                ins = None
                for tl in range(4):
                    ins = e.transpose(out=PSB(5)[:, d * 512 + tl * 128:d * 512 + (tl + 1) * 128], in_=t["kbT"][:, tl * 128:(tl + 1) * 128], identity=ident)
                return ins
            P.pe(tr, r=[K_(d, "kbT"), "ident"], w=[("ps", 5)])
            cast_copy(kbtok_, PSB(5)[:, d * 512:(d + 1) * 512].rearrange("p (t n) -> p t n", t=4), [("ps", 5)], [kkb])

        def tile_info(idx, d):
            step, ts = idx // 4, idx % 4
            blk = step if d == 0 else 7 - step
            tl = ts if d == 0 else 3 - ts
            return blk, tl, blk * 4 + tl, step % 2, idx % 2

        def front(idx):
            for d in range(2):
                t = T[d]
                blk, tl, tile, buf, par = tile_info(idx, d)
                tsl = slice(tl * 128, (tl + 1) * 128)
                sT = PS(2)[:, (d * 2 + par) * 128:(d * 2 + par + 1) * 128]
                Ub = PS(3 + par)[:, d * 256:(d + 1) * 256]
                order = (0, 1) if d == 0 else (1, 0)
                def mf(e, t=t, Ub=Ub, order=order, tl=tl, tile=tile, buf=buf, sT=sT, tsl=tsl):
                    ins = None
                    for c in (0, 1):
                        ci = order.index(c)
                        csl = slice(c * 64, (c + 1) * 64)
                        e.matmul(Ub[:, ci * 128:(ci + 1) * 128], lhsT=t["kbtok"][buf][csl, tl, :], rhs=vtok[csl, tile, :], start=True, stop=True)
                        ins = e.matmul(sT[:, c * 64:(c + 1) * 64], lhsT=t["kt"][buf][:, tsl], rhs=t["qt"][buf][:, tl * 128 + c * 64:tl * 128 + (c + 1) * 64],
                                       start=True, stop=True)
                    return ins
                P.pe(mf, r=[K_(d, ("kbtok", buf)), ("vtok", blk), K_(d, ("kt", buf)), K_(d, ("qt", buf))], w=[("ps", 3 + par), ("ps", 2)], c=0.5)
            for d in range(2):
                t = T[d]
                blk, tl, tile, buf, par = tile_info(idx, d)
                sT = PS(2)[:, (d * 2 + par) * 128:(d * 2 + par + 1) * 128]
                P.dve(lambda e, t=t, sT=sT, par=par, d=d: e.tensor_tensor(out=t["sTm"][par], in0=sT, in1=mk[d], op=ALU.mult),
                      r=[("ps", 2), ("mk", d)], w=[K_(d, ("sTm", par))], c=0.2)

        def back(idx):
            for d in range(2):
                t = T[d]
                blk, tl, tile, buf, par = tile_info(idx, d)
                Ub = PS(3 + par)[:, d * 256:(d + 1) * 256]
                order = (0, 1) if d == 0 else (1, 0)
                for ci, c in enumerate(order):
                    cib = tl * 2 + c
                    so, sn_ = s_cur[d], s_cur[d] ^ 1
                    s_cur[d] = sn_
                    P.dve(lambda e, t=t, d=d, ci=ci, cib=cib, Ub=Ub, buf=buf, so=so, sn_=sn_: e.scalar_tensor_tensor(
                        out=Sst[d][sn_], in0=Sst[d][so], scalar=t["dec"][buf][:, cib:cib + 1], in1=Ub[:, ci * 128:(ci + 1) * 128], op0=ALU.mult, op1=ALU.add),
                        r=[K_(d, ("S", so)), K_(d, ("dec", buf)), ("ps", 3 + par)], w=[K_(d, ("S", sn_))], c=0.2)
                    dpar, dslot = (par, 1) if ci == 0 else (par ^ 1, 0)
                    P.act(lambda e, t=t, d=d, dpar=dpar, dslot=dslot, sn_=sn_: e.copy(out=t["Sp"][dpar][dslot], in_=Sst[d][sn_]),
                          r=[K_(d, ("S", sn_))], w=[K_(d, ("Sp", dpar, dslot))], c=0.2)
            for d in range(2):
                t = T[d]
                blk, tl, tile, buf, par = tile_info(idx, d)
                acc = PS((1, 7)[par])[:, d * 128:(d + 1) * 128]
                order = (0, 1) if d == 0 else (1, 0)
                def ma(e, t=t, acc=acc, order=order, tl=tl, tile=tile, buf=buf, par=par):
                    e.matmul(acc, lhsT=vtok[:, tile, :], rhs=t["sTm"][par], start=True, stop=False, skip_group_check=True)
                    ins = None
                    for ci, c in enumerate(order):
                        ins = e.matmul(acc[:, c * 64:(c + 1) * 64], lhsT=t["Sp"][par][ci], rhs=t["qb"][buf][:, tl * 128 + c * 64:tl * 128 + (c + 1) * 64],
                                       start=False, stop=True, skip_group_check=True)
                    return ins
                P.pe(ma, r=[("vtok", blk), K_(d, ("sTm", par)), K_(d, ("Sp", par, 0)), K_(d, ("Sp", par, 1)), K_(d, ("qb", buf))], w=[("ps", (1, 7)[par])], c=0.4)
            for d in range(2):
                blk, tl, tile, buf, par = tile_info(idx, d)
                acc = PS((1, 7)[par])[:, d * 128:(d + 1) * 128]
                osl = oTh[:, tile * 128:(tile + 1) * 128]
                first = (blk < 4) if d == 0 else (blk >= 4)
                if first:
                    P.act(lambda e, osl=osl, acc=acc: e.copy(out=osl, in_=acc), r=[("ps", (1, 7)[par])], w=[("oTh", tile)], c=0.25)
                else:
                    P.dve(lambda e, osl=osl, acc=acc: e.tensor_tensor(out=osl, in0=osl, in1=acc, op=ALU.add),
                          r=[("ps", (1, 7)[par]), ("oTh", tile)], w=[("oTh", tile)], c=0.2)

        def record_list(fn):
            saved = P.ops
            P.ops = []
            fn()
            lst = P.ops
            P.ops = saved
            return lst

        for h in range(DBG["hg_heads"]):
            wb = wh[h % 2]
            kwb = ("wh", h % 2)
            for p_ in range(5):
                P.dma(lambda e, p_=p_, h=h, wb=wb: e.dma_start(
                    out=wb[:, :, p_, :], in_=hg_w_in[:, p_ * D + h * 128:p_ * D + (h + 1) * 128].rearrange("(k q) n -> q k n", q=128)),
                    w=[kwb], q="pool")
            for d in range(2):
                s_cur[d] = 0
                P.dve(lambda e, d=d: e.memset(Sst[d][0], 0.0), w=[K_(d, ("S", 0))])
                P.dve(lambda e, d=d: e.memset(T[d]["Sp"][0][0], 0.0), w=[K_(d, ("Sp", 0, 0))])
            if not DBG.get("hg_manual", 0):
                for step in range(8):
                    for d in range(2):
                        prep(h, d, step if d == 0 else 7 - step, step % 2, wb, kwb, part="a")
                    for d in range(2):
                        prep(h, d, step if d == 0 else 7 - step, step % 2, wb, kwb, part="b")
                    for ts in range(4):
                        front(step * 4 + ts)
                        back(step * 4 + ts)
            for d in range(2):
                if DBG.get("hg_manual", 0):
                    prep(h, d, 0 if d == 0 else 7, 0, wb, kwb)
            for step in (range(8) if DBG.get("hg_manual", 0) else ()):
                pl = []
                if step < 7:
                    pl = record_list(lambda: [prep(h, d, (step + 1) if d == 0 else 7 - (step + 1), (step + 1) % 2, wb, kwb) for d in range(2)])
                nq = (len(pl) + 3) // 4
                if not DBG.get("hg_merge", 1):
                    P.ops.extend(pl)
                    pl = []
                for ts in range(4):
                    idx = step * 4 + ts
                    front(idx)
                    if DBG.get("hg_lag", 1):
                        if idx >= 1:
                            back(idx - 1)
                    else:
                        back(idx)
                    P.ops.extend(pl[ts * nq:(ts + 1) * nq])
            if DBG.get("hg_lag", 1) and DBG.get("hg_manual", 0):
                back(31)
            for half in range(2):
                for tb4 in range(4):
                    tb = half * 4 + tb4
                    sl = slice(tb * 512, (tb + 1) * 512)
                    rsl = slice(tb4 * 512, (tb4 + 1) * 512)
                    okeys = [("oTh", i) for i in range(tb * 4, tb * 4 + 4)]
                    P.act(lambda e, sl=sl: e.activation(out=o2, in_=oTh[:, sl], func=AF.Square), r=okeys, w=["o2"])
                    P.pe(lambda e: e.matmul(PS(7), lhsT=ones_m, rhs=o2, start=True, stop=True), r=["o2", "ones_m"], w=[("ps", 7)], c=0.9)
                    P.act(lambda e: e.activation(out=rsn, in_=PS(7), func=AF.Ln, bias=EPS), r=[("ps", 7)], w=["rsn"])
                    P.act(lambda e, rsl=rsl: e.activation(out=rst[:, rsl], in_=rsn, func=AF.Exp, scale=-0.5), r=["rsn"], w=["rst", "normA"])
                for tb4 in range(4):
                    tb = half * 4 + tb4
                    sl = slice(tb * 512, (tb + 1) * 512)
                    rsl = slice(tb4 * 512, (tb4 + 1) * 512)
                    par = tb % 2
                    okeys = [("oTh", i) for i in range(tb * 4, tb * 4 + 4)]
                    def mm(e, sl=sl, wb=wb):
                        ins = None
                        for k in range(8):
                            ins = e.matmul(PS(6), lhsT=wb[:, k, 4, :], rhs=xT3[:, k, sl], start=(k == 0), stop=(k == 7))
                        return ins
                    P.pe(mm, r=[kwb] + xT_all[tb * 4:tb * 4 + 4], w=[("ps", 6)])
                    P.act(lambda e: e.activation(out=sgt, in_=PS(6), func=AF.Silu), r=[("ps", 6), "normA"], w=["sgt"])
                    P.dve(lambda e, sl=sl, rsl=rsl: e.scalar_tensor_tensor(out=o2, in0=oTh[:, sl], scalar=ng[:, 0:1], in1=rst[:, rsl], op0=ALU.mult, op1=ALU.mult),
                          r=okeys + ["rst", "ng", "o2"], w=["o2"])
                    P.dve(lambda e, par=par: e.tensor_tensor(out=onb[par], in0=o2, in1=sgt, op=ALU.mult), r=["o2", "sgt"], w=[("onb", par)])
                    P.dma(lambda e, h=h, sl=sl, par=par: e.dma_start(out=ONT[h * 128:(h + 1) * 128, sl], in_=onb[par]), r=[("onb", par)], w=[("ONT", h, tb)])
        P.nosched = False
        P.barrier()
        A.release()
        w_out = A.alloc(8 * D, BF16).rearrange("p (k n) -> p k n", k=8)
        load_w_bf16(w_out, hg_w_out, 8, D, "hw_out")
        load_gb(1, 0)
        E = Epi()
        onl = [A.alloc(8 * 512, BF16).rearrange("p (k t) -> p k t", k=8) for _ in range(2)]
        for grp in range(8):
            gb_ = grp % 2
            P.dma(lambda e, grp=grp, gb_=gb_: e.dma_start(out=onl[gb_], in_=ONT[:, grp * 512:(grp + 1) * 512].rearrange("(k p) t -> p k t", p=128)),
                  r=["ONT"], w=[("onl", gb_)])
            for tl in range(4):
                i = grp * 4 + tl
                for hh in range(2):
                    bank = (i % 2) * 2 + hh
                    def mm(e, tl=tl, hh=hh, bank=bank, gb_=gb_):
                        ins = None
                        for k in range(8):
                            ins = e.matmul(PS(bank), lhsT=onl[gb_][:, k, tl * 128:(tl + 1) * 128], rhs=w_out[:, k, hh * 512:(hh + 1) * 512],
                                           start=(k == 0), stop=(k == 7))
                        return ins
                    P.pe(mm, r=["hw_out", ("onl", gb_)], w=[("ps", bank)])
                b0 = (i % 2) * 2
                epilogue(E, i, [PS(b0), PS(b0 + 1)], [("ps", b0), ("ps", b0 + 1)], cur_res, dst_res)
        P.barrier()
        A.release()

    plan = []
    if upto >= 1 and not DBG["skip_l0"]:
        plan.append(("l0", 0))
    if upto >= 2:
        plan.append(("xa", 0))
    if upto >= 3:
        plan.append(("moe", 0))
    if upto >= 4:
        plan.append(("hg", 1))
    if upto >= 5:
        plan.append(("xa", 1))
    if upto >= 6:
        plan.append(("moe", 1))
    plan = [p for p in plan if p[0] not in DBG["skip"]]
    for pi, (kind, l) in enumerate(plan):
        last = (pi == len(plan) - 1)
        dst_res = out_d if last else XR[nxt]
        if kind == "l0":
            l0_phase(cur_res, dst_res)
        elif kind == "xa":
            xa_phase(l, cur_res, dst_res)
        elif kind == "moe":
            moe_phase(l, cur_res, dst_res, want_xT=(l == 0))
        elif kind == "hg":
            hg_phase(cur_res, dst_res)
        cur_res = dst_res
        nxt ^= 1

    final_keys = ["dst_dram"] + [("dst", i) for i in range(NT)]
    if cur_res is not out_d:
        A.mark()
        cp = [A.alloc(D, F32) for _ in range(2)]
        for i in range(NT):
            b = i % 2
            P.dma(lambda e, i=i, b=b: e.dma_start(out=cp[b], in_=cur_res[i * 128:(i + 1) * 128, :]), r=["dst_dram"], w=[("cp", b)])
            P.dma(lambda e, i=i, b=b: e.dma_start(out=out_d[i * 128:(i + 1) * 128, :], in_=cp[b]), r=[("cp", b)], w=["out_final"])
        final_keys.append("out_final")
        A.release()
    cnt, dcnt = P.emit(final_keys=final_keys)
    st.close()
    return nc, cnt, dcnt


def make_consts():
    import ml_dtypes
    s = np.arange(S, dtype=np.int64)
    ang = 2.0 * np.pi * ((s[:, None] * s[None, :]) % S).astype(np.float64) / S
    sc = 1.0 / np.sqrt(S)
    cs = (np.cos(ang) * sc).astype(np.float32).astype(ml_dtypes.bfloat16)
    ss = (-np.sin(ang) * sc).astype(np.float32).astype(ml_dtypes.bfloat16)
    c = np.arange(128, dtype=np.int64)
    angc = 2.0 * np.pi * ((c[:, None] * c[None, :]) % 128).astype(np.float64) / 128
    scc = 1.0 / np.sqrt(128.0)
    dc = np.concatenate([np.cos(angc) * scc, np.sin(angc) * scc], axis=1).astype(np.float32)
    t = np.arange(S)
    invcnt = np.zeros((4, S), np.float32)
    for gi, w in enumerate((2, 4, 8, 16)):
        lo = np.clip(t - w // 2, 0, S)
        hi = np.clip(t + w // 2, 0, S)
        invcnt[gi] = 1.0 / (hi - lo).astype(np.float32)
    m = np.arange(128)
    same = (m[:, None] // 64) == (m[None, :] // 64)
    masks = np.zeros((4, 128, 128), np.float32)
    masks[0] = (same & (m[:, None] <= m[None, :]))
    masks[1] = (same & (m[:, None] >= m[None, :]))
    masks[2] = (m[:, None] < m[None, :])
    masks[3] = 1.0
    eoff = (np.arange(32, dtype=np.float32) * CAP).reshape(1, 32)
    trash = (NEXP * CAP + np.arange(128, dtype=np.float32)).reshape(128, 1)
    altc = np.repeat((sc * np.cos(np.pi * np.arange(128))).astype(np.float32).reshape(128, 1), NT, axis=1)
    return {"c_cs": cs, "c_ss": ss, "c_dc": dc, "c_invcnt": invcnt, "c_masks": masks, "c_eoff": eoff, "c_trash": trash, "c_alt": np.ascontiguousarray(altc)}


_CACHE = {}


def kernel(**inputs):
    if "nc" not in _CACHE:
        _CACHE["nc"] = build()[0]
        _CACHE["consts"] = make_consts()
    nc = _CACHE["nc"]
    consts = _CACHE["consts"]
    in_maps = []
    for b in range(8):
        m = {}
        for k, v in inputs.items():
            v = np.asarray(v)
            if k in ("x", "mem"):
                m[k] = np.ascontiguousarray(v[b])
            elif k in ("pf_w_in", "pf_pool_w", "pf_pool_scale", "pf_fourier_ln_g", "pf_fourier_w", "pf_w_out",
                       "hg_w_in", "hg_norm_g", "hg_w_out"):
                m[k] = np.ascontiguousarray(v[0])
            else:
                m[k] = np.ascontiguousarray(v)
        m.update(consts)
        in_maps.append(m)
    res = run_bass_kernel_spmd(nc, in_maps, core_ids=list(range(8)))
    return np.stack([np.asarray(r["out"]) for r in res.results], axis=0).astype(np.float32)
```

```python
import numpy as np
from contextlib import ExitStack
import concourse.bass as bass
import concourse.mybir as mybir
from concourse.bass_utils import run_bass_kernel_spmd

F32 = mybir.dt.float32
BF16 = mybir.dt.bfloat16
I32 = mybir.dt.int32
ALU = mybir.AluOpType
AF = mybir.ActivationFunctionType
AX = mybir.AxisListType

S = 4096
D = 1024
NT = 32
MEM = 256
CAP = 512
NEXP = 32
ALPHA = 4.0 ** 0.25
EPS = 1e-5
EPOCH = 12000
DBG = {"skip_l0": False, "xa_groups": 8, "hg_heads": 8, "skip": ()}
NDS = 8


class Op:
    __slots__ = ("eng", "fn", "reads", "writes", "dma", "seq", "dn", "deps", "waits", "bar", "c", "ns")


class Prog:
    ENGS = ["pe", "act", "dve", "pool", "sp"]

    def __init__(self, nc):
        self.nc = nc
        self.ops = []

    DEFC = {"pe": 1.8, "act": 0.6, "dve": 0.6, "pool": 1.5, "sp": 4.0}

    def add(self, eng, fn, reads=(), writes=(), dma=False, c=None):
        o = Op()
        o.eng, o.fn, o.reads, o.writes, o.dma, o.bar = eng, fn, list(reads), list(writes), dma, False
        o.c = c if c is not None else (4.0 if dma else self.DEFC[eng])
        o.ns = getattr(self, "nosched", False)
        self.ops.append(o)
        return o

    def pe(self, fn, r=(), w=(), c=None):
        return self.add("pe", fn, r, w, c=c)

    def act(self, fn, r=(), w=(), c=None):
        return self.add("act", fn, r, w, c=c)

    def dve(self, fn, r=(), w=(), c=None):
        return self.add("dve", fn, r, w, c=c)

    def pool(self, fn, r=(), w=(), c=None):
        return self.add("pool", fn, r, w, c=c)

    def dma(self, fn, r=(), w=(), q="sp", c=None):
        return self.add(q, fn, r, w, dma=True, c=c)

    def barrier(self):
        for e in self.ENGS:
            o = self.add(e, None)
            o.bar = True

    WIN = {"pe": 48, "act": 48, "dve": 48, "pool": 1, "sp": 48}

    def reorder(self, window=48, sync_lat=0.2):
        ops = self.ops
        new_ops = []
        seg = []

        def flush():
            n = len(seg)
            if n == 0:
                return
            if any(getattr(op, "ns", False) for op in seg):
                new_ops.extend(seg)
                seg.clear()
                return
            last_w, readers = {}, {}
            preds = [set() for _ in range(n)]
            for j, op in enumerate(seg):
                for r in op.reads:
                    if r in last_w:
                        preds[j].add(last_w[r])
                for w in op.writes:
                    if w in last_w:
                        preds[j].add(last_w[w])
                    preds[j].update(readers.get(w, ()))
                preds[j].discard(j)
                for r in op.reads:
                    readers.setdefault(r, []).append(j)
                for w in op.writes:
                    last_w[w] = j
                    readers[w] = []
            queues = {e: [j for j, op in enumerate(seg) if op.eng == e] for e in self.ENGS}
            ptr = {e: 0 for e in self.ENGS}
            done = [False] * n
            fin = [0.0] * n
            t_e = {e: 0.0 for e in self.ENGS}
            left = n
            while left:
                best = None
                for e in self.ENGS:
                    q = queues[e]
                    p = ptr[e]
                    while p < len(q) and done[q[p]]:
                        p += 1
                    ptr[e] = p
                    seen = 0
                    k = p
                    while k < len(q) and seen < self.WIN[e]:
                        j = q[k]
                        k += 1
                        if done[j]:
                            continue
                        seen += 1
                        ok = True
                        rt = 0.0
                        for pr in preds[j]:
                            if not done[pr]:
                                ok = False
                                break
                            f = fin[pr] + sync_lat
                            if f > rt:
                                rt = f
                        if not ok:
                            continue
                        stt = max(t_e[e], rt)
                        key = (stt, j)
                        if best is None or key < best[0]:
                            best = (key, e, j)
                        if stt <= t_e[e]:
                            break
                assert best is not None, "scheduler deadlock"
                (stt, _), e, j = best
                op = seg[j]
                done[j] = True
                left -= 1
                if op.dma:
                    t_e[e] = stt + 0.15
                    fin[j] = stt + op.c
                else:
                    t_e[e] = stt + op.c
                    fin[j] = stt + op.c
                new_ops.append(op)
            seg.clear()

        i = 0
        while i < len(ops):
            if ops[i].bar:
                flush()
                while i < len(ops) and ops[i].bar:
                    new_ops.append(ops[i])
                    i += 1
            else:
                seg.append(ops[i])
                i += 1
        flush()
        self.ops = new_ops

    def emit(self, final_keys=(), reorder=True):
        nc = self.nc
        if reorder:
            self.reorder()
        ops = self.ops
        self.add("sp", None, final_keys, ())
        cnt = {e: 0 for e in self.ENGS}
        dcnt = {e: 0 for e in self.ENGS}
        dma_ops = {e: [] for e in self.ENGS}
        last_c = {e: None for e in self.ENGS}
        epos = {e: 0 for e in self.ENGS}
        for j, op in enumerate(ops):
            if op.dma:
                op.dn = dcnt[op.eng]
                dcnt[op.eng] += 1
                dma_ops[op.eng].append(j)
            elif op.fn is not None:
                epos[op.eng] += 1
                op.seq = epos[op.eng]
        last_w = {}
        readers = {}
        for j, op in enumerate(ops):
            deps = set()
            if op.bar:
                for e in self.ENGS:
                    if last_c[e] is not None:
                        deps.add(last_c[e])
                for e in self.ENGS:
                    lst = [i for i in dma_ops[e] if i < j]
                    deps.update(lst[-NDS:])
            for r in op.reads:
                if r in last_w:
                    deps.add(last_w[r])
            for w in op.writes:
                if w in last_w:
                    deps.add(last_w[w])
                deps.update(readers.get(w, ()))
            if op.dma and op.dn >= NDS:
                deps.add(dma_ops[op.eng][op.dn - NDS])
            deps.discard(j)
            op.deps = deps
            for r in op.reads:
                readers.setdefault(r, []).append(j)
            for w in op.writes:
                last_w[w] = j
                readers[w] = []
            if (not op.dma) and op.fn is not None:
                last_c[op.eng] = j
        wc = {e: {} for e in self.ENGS}
        wd = {e: {} for e in self.ENGS}
        signal = set()
        for j, op in enumerate(ops):
            waits = []
            me = op.eng
            for i in sorted(op.deps):
                d = ops[i]
                if d.dma:
                    k = (d.eng, d.dn % NDS)
                    val = 16 * (d.dn // NDS + 1)
                    if wd[me].get(k, 0) < val:
                        wd[me][k] = val
                        waits.append(("d", i))
                else:
                    if d.fn is None:
                        continue
                    if d.eng == "pe" and me == "pe":
                        continue
                    if wc[me].get(d.eng, 0) < d.seq:
                        wc[me][d.eng] = d.seq
                        waits.append(("c", i))
                        signal.add(i)
            op.waits = waits
        for j, op in enumerate(ops):
            if (not op.dma) and op.fn is not None:
                if j in signal:
                    cnt[op.eng] += 1
                    op.seq = cnt[op.eng]
                else:
                    op.seq = None
        with ExitStack() as st:
            csem = {}
            for e in self.ENGS:
                n_ep = (cnt[e] + EPOCH - 1) // EPOCH
                csem[e] = [st.enter_context(nc.semaphore(f"c_{e}_{k}")) for k in range(max(n_ep, 1))]
            dsem = {}
            for e in self.ENGS:
                if dcnt[e]:
                    dsem[e] = [st.enter_context(nc.semaphore(f"d_{e}_{k}")) for k in range(NDS)]
            for op in ops:
                ww = []
                for (kind, i) in op.waits:
                    d = ops[i]
                    if kind == "d":
                        ww.append((dsem[d.eng][d.dn % NDS], 16 * (d.dn // NDS + 1)))
                    else:
                        ww.append((csem[d.eng][(d.seq - 1) // EPOCH], (d.seq - 1) % EPOCH + 1))
                op.waits = ww
            streams = {e: [op for op in ops if op.eng == e] for e in self.ENGS}
            block = st.enter_context(nc.Block())

            def mk(ename):
                def body(e):
                    for op in streams[ename]:
                        for (sem, val) in op.waits:
                            e.wait_ge(sem, val)
                        if op.fn is None:
                            continue
                        ins = op.fn(e)
                        if op.dma:
                            ins.then_inc(dsem[ename][op.dn % NDS], 16)
                        elif op.seq is not None:
                            ins.then_inc(csem[ename][(op.seq - 1) // EPOCH], 1)
                return body

            block.tensor(mk("pe"))
            block.scalar(mk("act"))
            block.vector(mk("dve"))
            block.gpsimd(mk("pool"))
            block.sync(mk("sp"))
        return cnt, dcnt


class Arena:
    def __init__(self, ap32, nbytes):
        self.ap = ap32
        self.n = nbytes
        self.off = 0
        self.marks = []

    def alloc(self, free_elems, dt):
        sz = 2 if dt == BF16 else 4
        nb = (free_elems * sz + 31) // 32 * 32
        assert self.off + nb <= self.n, f"arena overflow {self.off}+{nb}>{self.n}"
        a = self.ap[:, self.off // 4:(self.off + nb) // 4]
        self.off += nb
        if dt != F32:
            a = a.bitcast(dt)
        return a[:, 0:free_elems]

    def mark(self):
        self.marks.append(self.off)

    def release(self):
        self.off = self.marks.pop()


def build(upto=99, dbg=False):
    nc = bass.Bass("TRN2", target_bir_lowering=False)
    P = Prog(nc)

    def din(name, shape, dt=F32):
        return nc.dram_tensor(name, list(shape), dt, kind="ExternalInput").ap()

    x_d = din("x", [S, D])
    mem_d = din("mem", [MEM, D])
    pf_w_in = din("pf_w_in", [D, D])
    pf_pool_w = din("pf_pool_w", [4, 128, 128])
    pf_pool_scale = din("pf_pool_scale", [512])
    pf_ln_g = din("pf_fourier_ln_g", [4, 128])
    pf_fw = din("pf_fourier_w", [4, 128, 128])
    pf_w_out = din("pf_w_out", [D, D])
    hg_w_in = din("hg_w_in", [D, 5 * D])
    hg_lb = din("hg_lower_bounds", [2, 2, D])
    hg_norm_g = din("hg_norm_g", [128])
    hg_w_out = din("hg_w_out", [D, D])
    xa_wq = din("xa_wq", [2, D, D])
    xa_wkv = din("xa_wkv", [2, D, 2 * D])
    xa_wo = din("xa_wo", [2, D, D])
    moe_wg = din("moe_w_group", [2, D, 4])
    moe_bg = din("moe_b_group", [2, 4])
    moe_we = din("moe_w_expert", [2, D, 32])
    moe_be = din("moe_b_expert", [2, 32])
    moe_gate = din("moe_w_gate", [2, NEXP, D, 512])
    moe_up = din("moe_w_up", [2, NEXP, D, 512])
    moe_down = din("moe_w_down", [2, NEXP, 512, D])
    ln_g = din("ln_g", [2, 3, D])
    ln_b = din("ln_b", [2, 3, D])
    c_cs = din("c_cs", [S, S], BF16)
    c_ss = din("c_ss", [S, S], BF16)
    c_dc = din("c_dc", [128, 256])
    c_invcnt = din("c_invcnt", [4, S])
    c_masks = din("c_masks", [4, 128, 128])
    c_eoff = din("c_eoff", [1, 32])
    c_trash = din("c_trash", [128, 1])
    c_alt = din("c_alt", [128, NT])
    out_d = nc.dram_tensor("out", [S, D], F32, kind="ExternalOutput").ap()
    XR = [nc.dram_tensor(f"xr{k}", [S, D], F32).ap() for k in range(2)]
    Zscr = nc.dram_tensor("zscr", [4, NT, 128, 256], BF16).ap()
    XS = nc.dram_tensor("xs_scr", [NEXP * CAP + 128, D], BF16).ap()
    YS = nc.dram_tensor("ys_scr", [NEXP * CAP + 128, D], F32).ap()
    ONT = nc.dram_tensor("ont_scr", [D, S], BF16).ap()

    st = ExitStack()
    ARENA_BYTES = 206 * 1024
    arena_t = st.enter_context(nc.sbuf_tensor("arena", [128, ARENA_BYTES // 4], F32))
    A = Arena(arena_t[:], ARENA_BYTES)
    psb = [st.enter_context(nc.psum_tensor(f"ps{k}", [128, 512], F32)) for k in range(8)]

    def PS(k):
        return psb[k][:]

    def PSB(k):
        return psb[k][:].bitcast(BF16)

    ident = A.alloc(128, BF16)
    ident32 = A.alloc(128, F32)
    ones_m = A.alloc(128, F32)
    gb = A.alloc(2 * D, F32)
    xT = A.alloc(8 * S, BF16)
    xT3 = xT.rearrange("p (k t) -> p k t", k=8)

    P.pool(lambda e: e.memset(ident32, 0.0), w=["ident32"])
    P.pool(lambda e: e.affine_select(out=ident32, in_=ident32, pattern=[[-1, 128]], compare_op=ALU.not_equal,
                                     fill=1.0, base=0, channel_multiplier=1), r=["ident32"], w=["ident32"])
    P.dve(lambda e: e.tensor_copy(out=ident, in_=ident32), r=["ident32"], w=["ident"])
    P.pool(lambda e: e.memset(ones_m, 1.0 / 128.0), w=["ones_m"])

    def ncd(fn):
        def g(e):
            with nc.allow_non_contiguous_dma(reason="tiny per-partition scalar tables"):
                return fn(e)
        return g

    cast_rr = [0]
    cast_force = [None]

    def cast_copy(out, in_, r, w):
        cast_rr[0] ^= 1
        if cast_force[0] == "act" or (cast_force[0] is None and cast_rr[0]):
            P.act(lambda e: e.copy(out=out, in_=in_), r=r, w=w)
        else:
            P.dve(lambda e: e.tensor_copy(out=out, in_=in_), r=r, w=w)

    def transpose_to(dst3, src_bf, bank, rkeys, wkeys):
        def f(e):
            ins = None
            for k in range(8):
                ins = e.transpose(out=PSB(bank)[:, k * 128:(k + 1) * 128], in_=src_bf[:, k * 128:(k + 1) * 128],
                                  identity=ident)
            return ins
        P.pe(f, r=list(rkeys) + ["ident"], w=[("ps", bank)])
        cast_copy(dst3, PSB(bank).rearrange("p (k t) -> p k t", k=8), [("ps", bank)], wkeys)

    def load_w_bf16(dst3, src2d, kt, n, wkey, rows_per_dma=256):
        kk = max(1, rows_per_dma // 128)
        for k0 in range(0, kt, kk):
            k1 = min(kt, k0 + kk)
            P.dma(lambda e, k0=k0, k1=k1: e.dma_start(
                out=dst3[:, k0:k1, :], in_=src2d[k0 * 128:k1 * 128, :].rearrange("(k p) n -> p k n", p=128)),
                w=[wkey], q="pool")

    def load_gb(l, j):
        P.dma(lambda e: e.dma_start(out=gb[:, 0:D], in_=ln_g[l, j:j + 1, :].broadcast_to([128, D])), w=["gb"])
        P.dma(lambda e: e.dma_start(out=gb[:, D:2 * D], in_=ln_b[l, j:j + 1, :].broadcast_to([128, D])), w=["gb"])

    class Epi:
        def __init__(self):
            self.xr = [A.alloc(D, F32) for _ in range(2)]
            self.y = [A.alloc(D, F32) for _ in range(2)]
            self.xo = [A.alloc(D, F32) for _ in range(2)]
            self.xob = [A.alloc(D, BF16) for _ in range(2)]
            self.st = A.alloc(12, F32)
            self.mv = A.alloc(8, F32)

    def epilogue(E, i, hsrc, hkeys, res_ap, dst_ap, make_xT=True, tbank=7, gmul="pool"):
        b = i % 2
        xr, y, xo, xob = E.xr[b], E.y[b], E.xo[b], E.xob[b]
        kxr, ky, kxo, kxob = ("e_xr", b), ("e_y", b), ("e_xo", b), ("e_xob", b)
        P.dma(lambda e: e.dma_start(out=xr, in_=res_ap[i * 128:(i + 1) * 128, :]), r=["res_dram"], w=[kxr])
        for hh in range(2):
            P.dve(lambda e, hh=hh: e.scalar_tensor_tensor(
                out=y[:, hh * 512:(hh + 1) * 512], in0=xr[:, hh * 512:(hh + 1) * 512], scalar=ALPHA,
                in1=hsrc[hh], op0=ALU.mult, op1=ALU.add), r=[kxr] + list(hkeys), w=[ky])
        P.dve(lambda e: e.bn_stats(out=E.st[:, 0:6], in_=y[:, 0:512]), r=[ky], w=["e_st0"])
        P.dve(lambda e: e.bn_stats(out=E.st[:, 6:12], in_=y[:, 512:1024]), r=[ky], w=["e_st1"])
        P.dve(lambda e: e.bn_aggr(out=E.mv[:, 0:2], in_=E.st[:, 0:12]), r=["e_st0", "e_st1"], w=["e_mv"])
        P.act(lambda e: e.activation(out=E.mv[:, 2:3], in_=E.mv[:, 1:2], func=AF.Ln, bias=EPS), r=["e_mv"], w=["e_sd"], c=0.2)
        P.act(lambda e: e.activation(out=E.mv[:, 3:4], in_=E.mv[:, 2:3], func=AF.Exp, scale=-0.5), r=["e_sd"], w=["e_rs"], c=0.2)
        P.dve(lambda e: e.scalar_tensor_tensor(out=E.mv[:, 4:5], in0=E.mv[:, 0:1], scalar=-1.0, in1=E.mv[:, 3:4],
                                               op0=ALU.mult, op1=ALU.mult), r=["e_mv", "e_rs"], w=["e_nm"])
        P.act(lambda e: e.activation(out=y, in_=y, func=AF.Identity, bias=E.mv[:, 4:5], scale=E.mv[:, 3:4]),
              r=[ky, "e_rs", "e_nm"], w=[ky])
        if gmul == "dve":
            P.dve(lambda e: e.tensor_tensor(out=y, in0=y, in1=gb[:, 0:D], op=ALU.mult), r=[ky, "gb"], w=[ky], c=1.2)
        else:
            P.pool(lambda e: e.tensor_tensor(out=y, in0=y, in1=gb[:, 0:D], op=ALU.mult), r=[ky, "gb"], w=[ky], c=2.4)
        P.pool(lambda e: e.tensor_tensor(out=xo, in0=y, in1=gb[:, D:2 * D], op=ALU.add), r=[ky, "gb"], w=[kxo])
        P.dma(lambda e: e.dma_start(out=dst_ap[i * 128:(i + 1) * 128, :], in_=xo), r=[kxo], w=["dst_dram", ("dst", i)], q="pool")
        if make_xT:
            P.act(lambda e: e.copy(out=xob, in_=xo), r=[kxo], w=[kxob])
            transpose_to(xT3[:, :, i * 128:(i + 1) * 128], xob, tbank, [kxob], [("xT", i)])

    A.mark()
    xb0 = [A.alloc(D, BF16) for _ in range(2)]
    for i in range(NT):
        b = i % 2
        P.dma(lambda e, i=i, b=b: e.dma_start(out=xb0[b], in_=x_d[i * 128:(i + 1) * 128, :]), w=[("xb0", b)], q="pool")
        transpose_to(xT3[:, :, i * 128:(i + 1) * 128], xb0[b], 6 + b, [("xb0", b)], [("xT", i)])
    P.barrier()
    A.release()

    xT_all = [("xT", i) for i in range(NT)]
    cur_res = x_d
    nxt = 0

    def l0_phase(cur_res, dst_res):
        if True:
            A.mark()
            catT = A.alloc(8 * S, BF16)
            catT3 = catT.rearrange("p (k t) -> p k t", k=8)
            A.mark()
            w_in = A.alloc(8 * D, BF16).rearrange("p (k n) -> p k n", k=8)
            load_w_bf16(w_in, pf_w_in, 8, D, "w_in")
            poolw = A.alloc(4 * 128, BF16).rearrange("p (g n) -> p g n", g=4)
            fw = A.alloc(4 * 128, BF16).rearrange("p (g n) -> p g n", g=4)
            dc = A.alloc(256, BF16)
            P.dma(lambda e: e.dma_start(out=poolw, in_=pf_pool_w.rearrange("g c d -> c g d")), w=["poolw"], q="pool")
            P.dma(lambda e: e.dma_start(out=fw, in_=pf_fw.rearrange("g c d -> c g d")), w=["fw"], q="pool")
            P.dma(lambda e: e.dma_start(out=dc, in_=c_dc), w=["dc"], q="pool")
            pscale = A.alloc(4, F32)
            lng = A.alloc(4, F32)
            P.dma(ncd(lambda e: e.dma_start(out=pscale, in_=pf_pool_scale.rearrange("(g d) -> d g", g=4))), w=["pscale"])
            P.dma(ncd(lambda e: e.dma_start(out=lng, in_=pf_ln_g.rearrange("h c -> c h"))), w=["lng"])
            PADW = 16
            A.mark()
            a0 = A.alloc(S + 2 * PADW, F32)
            t1 = A.alloc(1024 + 2 * PADW, F32)
            t2 = A.alloc(1024 + 2 * PADW, F32)
            icnt = A.alloc(1024, F32)
            pm = A.alloc(1024, BF16)
            P.pool(lambda e: e.memset(a0, 0.0), w=["a0"])
            for g in range(4):
                for tb in range(8):
                    bank = tb % 2
                    def mm(e, g=g, tb=tb, bank=bank):
                        ins = None
                        for k in range(8):
                            ins = e.matmul(PS(bank), lhsT=w_in[:, k, g * 128:(g + 1) * 128],
                                           rhs=xT3[:, k, tb * 512:(tb + 1) * 512], start=(k == 0), stop=(k == 7))
                        return ins
                    P.pe(mm, r=["w_in"] + xT_all[tb * 4:tb * 4 + 4], w=[("ps", bank)])
                    P.act(lambda e, tb=tb, bank=bank: e.copy(out=a0[:, PADW + tb * 512:PADW + (tb + 1) * 512], in_=PS(bank)),
                          r=[("ps", bank)], w=["a0"])
                for blk in range(4):
                    s0 = PADW + blk * 1024
                    def lv(buf, lo, hi):
                        return buf[:, PADW + lo:PADW + 1024 + hi]
                    def a0v(lo, hi, s0=s0):
                        return a0[:, s0 + lo:s0 + 1024 + hi]
                    P.dma(lambda e, g=g, blk=blk: e.dma_start(
                        out=icnt, in_=c_invcnt[g:g + 1, blk * 1024:(blk + 1) * 1024].broadcast_to([128, 1024])),
                        w=["icnt"])
                    P.dve(lambda e, a0v=a0v, lv=lv: e.tensor_tensor(out=lv(t1, -8, 8), in0=a0v(-9, 7), in1=a0v(-8, 8), op=ALU.add),
                          r=["a0"], w=["t1"])
                    cur, curk, oth, othk = t1, "t1", t2, "t2"
                    ext = 8
                    for lev in range(1, g + 1):
                        sh = 1 << (lev - 1)
                        ne = ext - 2 * sh if lev < 3 else 0
                        ne = {1: 6, 2: 4, 3: 0}[lev]
                        P.dve(lambda e, cur=cur, oth=oth, sh=sh, ne=ne, lv=lv: e.tensor_tensor(
                            out=lv(oth, -ne, ne), in0=lv(cur, -ne - sh, ne - sh), in1=lv(cur, -ne + sh, ne + sh), op=ALU.add),
                            r=[curk], w=[othk])
                        cur, curk, oth, othk = oth, othk, cur, curk
                        ext = ne
                    P.dve(lambda e, cur=cur, oth=oth, lv=lv: e.tensor_tensor(out=lv(oth, 0, 0), in0=lv(cur, 0, 0), in1=icnt, op=ALU.mult),
                          r=[curk, "icnt"], w=[othk])
                    P.dve(lambda e, oth=oth, lv=lv, a0v=a0v: e.tensor_tensor(out=pm, in0=lv(oth, 0, 0), in1=a0v(0, 0), op=ALU.subtract),
                          r=[othk, "a0"], w=["pm"])
                    for hb in range(2):
                        bank = 2 + hb
                        P.pe(lambda e, g=g, hb=hb, bank=bank: e.matmul(PS(bank), lhsT=poolw[:, g, :], rhs=pm[:, hb * 512:(hb + 1) * 512],
                                                                      start=True, stop=True), r=["pm", "poolw"], w=[("ps", bank)])
                        c0 = blk * 1024 + hb * 512
                        P.act(lambda e, g=g, bank=bank, c0=c0: e.activation(out=catT3[:, g, c0:c0 + 512], in_=PS(bank), func=AF.Identity,
                                                                             scale=pscale[:, g:g + 1]),
                              r=[("ps", bank), "pscale"], w=[("catT", g)])
            ub = A.alloc(512, F32)
            dd = A.alloc(512, F32)
            d2 = A.alloc(512, F32)
            rs = A.alloc(512, F32)
            un = A.alloc(512, BF16)
            zs = [A.alloc(512, BF16) for _ in range(2)]
            for h in range(4):
                for tb in range(8):
                    def mm(e, h=h, tb=tb):
                        ins = None
                        for k in range(8):
                            ins = e.matmul(PS(0), lhsT=w_in[:, k, 512 + h * 128:512 + (h + 1) * 128],
                                           rhs=xT3[:, k, tb * 512:(tb + 1) * 512], start=(k == 0), stop=(k == 7))
                        return ins
                    P.pe(mm, r=["w_in"] + xT_all[tb * 4:tb * 4 + 4], w=[("ps", 0)])
                    P.act(lambda e: e.copy(out=ub, in_=PS(0)), r=[("ps", 0)], w=["ub"])
                    P.pe(lambda e: e.matmul(PS(1), lhsT=ones_m, rhs=ub, start=True, stop=True), r=["ub", "ones_m"], w=[("ps", 1)])
                    P.dve(lambda e: e.tensor_tensor(out=dd, in0=ub, in1=PS(1), op=ALU.subtract), r=["ub", ("ps", 1)], w=["dd"])
                    P.act(lambda e: e.activation(out=d2, in_=dd, func=AF.Square), r=["dd"], w=["d2"])
                    P.pe(lambda e: e.matmul(PS(1), lhsT=ones_m, rhs=d2, start=True, stop=True), r=["d2", "ones_m"], w=[("ps", 1)])
                    P.act(lambda e: e.activation(out=rs, in_=PS(1), func=AF.Ln, bias=EPS), r=[("ps", 1)], w=["rs"])
                    P.act(lambda e: e.activation(out=rs, in_=rs, func=AF.Exp, scale=-0.5), r=["rs"], w=["rs"])
                    P.dve(lambda e, h=h: e.scalar_tensor_tensor(out=un, in0=dd, scalar=lng[:, h:h + 1], in1=rs, op0=ALU.mult, op1=ALU.mult),
                          r=["dd", "rs", "lng"], w=["un"])
                    for pr in range(2):
                        bank = 2 + pr
                        def mz(e, pr=pr, bank=bank):
                            ins = None
                            for q in range(2):
                                tt = pr * 2 + q
                                ins = e.matmul(PS(bank)[:, q * 256:(q + 1) * 256], lhsT=un[:, tt * 128:(tt + 1) * 128], rhs=dc,
                                               start=True, stop=True)
                            return ins
                        P.pe(mz, r=["un", "dc"], w=[("ps", bank)])
                        cast_copy(zs[pr], PS(bank), [("ps", bank)], [("zs", pr)])
                        t0 = tb * 4 + pr * 2
                        P.dma(lambda e, h=h, t0=t0, pr=pr: e.dma_start(
                            out=Zscr[h, t0:t0 + 2].rearrange("t p c -> p t c"), in_=zs[pr].rearrange("p (t c) -> p t c", t=2)),
                            r=[("zs", pr)], w=["zscr"])
            P.barrier()
            A.release()
            A.release()
            A.mark()
            fw2 = A.alloc(4 * 128, BF16).rearrange("p (g n) -> p g n", g=4)
            P.dma(lambda e: e.dma_start(out=fw2, in_=pf_fw.rearrange("g c d -> c g d")), w=["fw2"], q="pool")
            zres = xT.rearrange("p (h t c) -> p h t c", h=4, t=NT)
            for h in range(4):
                P.dma(lambda e, h=h: e.dma_start(out=zres[:, h], in_=Zscr[h].rearrange("t p c -> p t c")), r=["zscr"], w=["zres"])
            NCH = 4
            dbuf = [[A.alloc(8 * 512, BF16).rearrange("p (s t) -> p s t", s=8) for _ in range(2)] for _ in range(2)]
            asb = [A.alloc(512, F32) for _ in range(4)]
            ypb = [A.alloc(512, BF16) for _ in range(4)]
            ymb = [A.alloc(512, BF16) for _ in range(4)]
            alt = A.alloc(NT, BF16)
            y2k = A.alloc(4, BF16)
            P.dma(lambda e: e.dma_start(out=alt, in_=c_alt), w=["alt"], q="pool")
            for h in range(4):
                def m2k(e, h=h):
                    ins = None
                    for stile in range(NT):
                        ins = e.matmul(PS(0)[:, h:h + 1], lhsT=zres[:, h, stile, 0:128], rhs=alt[:, stile:stile + 1],
                                       start=(stile == 0), stop=(stile == NT - 1))
                    return ins
                P.pe(m2k, r=["zres", "alt"], w=[("ps", 0)], c=2.5)
            P.dve(lambda e: e.tensor_copy(out=y2k, in_=PS(0)[:, 0:4]), r=[("ps", 0)], w=["y2k"])
            for h in range(4):
                P.pe(lambda e, h=h: e.matmul(PS(0)[:, 8 + h:9 + h], lhsT=fw2[:, h, :], rhs=y2k[:, h:h + 1], start=True, stop=True),
                     r=["y2k", "fw2"], w=[("ps", 0)], c=0.2)
            for h in range(4):
                P.dve(lambda e, h=h: e.tensor_copy(out=catT3[:, 4 + h, 2048:2049], in_=PS(0)[:, 8 + h:9 + h]), r=[("ps", 0)], w=[("catT", 4 + h)], c=0.1)
            cn = 0
            for kb in range(4):
                for sc in range(NCH):
                    bsel = cn % 2
                    cn += 1
                    for m, src in enumerate((c_cs, c_ss)):
                        P.dma(lambda e, m=m, src=src, sc=sc, kb=kb, bsel=bsel: e.dma_start(
                            out=dbuf[bsel][m], in_=src[sc * 1024:(sc + 1) * 1024, kb * 512:(kb + 1) * 512].rearrange("(s p) t -> p s t", p=128)),
                            w=[("dbuf", bsel, m)])
                    for h in range(4):
                        def mm(e, h=h, sc=sc, bsel=bsel):
                            ins = None
                            for s_ in range(8):
                                stile = sc * 8 + s_
                                first = (sc == 0 and s_ == 0)
                                last = (sc == NCH - 1 and s_ == 7)
                                e.matmul(PS(h), lhsT=zres[:, h, stile, 0:128], rhs=dbuf[bsel][0][:, s_, :], start=first, stop=last)
                                ins = e.matmul(PS(4 + h), lhsT=zres[:, h, stile, 128:256], rhs=dbuf[bsel][1][:, s_, :], start=first, stop=last)
                            return ins
                        P.pe(mm, r=["zres", ("dbuf", bsel, 0), ("dbuf", bsel, 1)], w=[("ps", h), ("ps", 4 + h)], c=3.6)
                for h in range(4):
                    P.act(lambda e, h=h: e.copy(out=asb[h], in_=PS(h)), r=[("ps", h)], w=[("asb", h)])
                    P.dve(lambda e, h=h: e.tensor_tensor(out=ypb[h], in0=asb[h], in1=PS(4 + h), op=ALU.add), r=[("asb", h), ("ps", 4 + h)], w=[("ypb", h)])
                    P.dve(lambda e, h=h: e.tensor_tensor(out=ymb[h], in0=asb[h], in1=PS(4 + h), op=ALU.subtract), r=[("asb", h), ("ps", 4 + h)], w=[("ymb", h)])
                    P.pe(lambda e, h=h: e.matmul(PS(h), lhsT=fw2[:, h, :], rhs=ypb[h], start=True, stop=True), r=[("ypb", h), "fw2"], w=[("ps", h)], c=0.3)
                    P.pe(lambda e, h=h: e.matmul(PS(4 + h), lhsT=fw2[:, h, :], rhs=ymb[h], start=True, stop=True), r=[("ymb", h), "fw2"], w=[("ps", 4 + h)], c=0.3)
                    P.act(lambda e, h=h, kb=kb: e.copy(out=catT3[:, 4 + h, kb * 512:(kb + 1) * 512], in_=PS(h)), r=[("ps", h)], w=[("catT", 4 + h)])
                    if kb == 0:
                        P.dve(lambda e, h=h: e.tensor_copy(out=catT3[:, 4 + h, 3585:4096][:, ::-1], in_=PS(4 + h)[:, 1:512]),
                              r=[("ps", 4 + h)], w=[("catT", 4 + h)])
                    else:
                        lo = S - kb * 512 - 511
                        P.dve(lambda e, h=h, lo=lo: e.tensor_copy(out=catT3[:, 4 + h, lo:lo + 512][:, ::-1], in_=PS(4 + h)),
                              r=[("ps", 4 + h)], w=[("catT", 4 + h)])
            P.barrier()
            A.release()
            if dbg:
                dbg_cat = nc.dram_tensor("dbg_cat", [D, S], BF16, kind="ExternalOutput").ap()
                P.dma(lambda e: e.dma_start(out=dbg_cat.rearrange("(k p) t -> p k t", p=128), in_=catT3), r=[("catT", k) for k in range(8)], w=["dbg_cat"])
            A.mark()
            w_out = A.alloc(8 * D, BF16).rearrange("p (k n) -> p k n", k=8)
            load_w_bf16(w_out, pf_w_out, 8, D, "w_out")
            load_gb(0, 0)
            E = Epi()
            for i in range(NT):
                for hh in range(2):
                    bank = (i % 2) * 2 + hh
                    def mm(e, i=i, hh=hh, bank=bank):
                        ins = None
                        for k in range(8):
                            ins = e.matmul(PS(bank), lhsT=catT3[:, k, i * 128:(i + 1) * 128], rhs=w_out[:, k, hh * 512:(hh + 1) * 512],
                                           start=(k == 0), stop=(k == 7))
                        return ins
                    P.pe(mm, r=["w_out"] + [("catT", k) for k in range(8)], w=[("ps", bank)])
                b0 = (i % 2) * 2
                epilogue(E, i, [PS(b0), PS(b0 + 1)], [("ps", b0), ("ps", b0 + 1)], cur_res, dst_res)
            P.barrier()
            A.release()
            A.release()


    def xa_phase(l, cur_res, dst_res):
        if True:
            A.mark()
            wq = A.alloc(8 * D, BF16).rearrange("p (k n) -> p k n", k=8)
            wo = A.alloc(8 * D, BF16).rearrange("p (k n) -> p k n", k=8)
            kT = A.alloc(8 * MEM, BF16).rearrange("p (k m) -> p k m", k=8)
            vtok = A.alloc(2 * D, BF16).rearrange("p (m n) -> p m n", m=2)
            A.mark()
            wkv = A.alloc(8 * 2 * D, BF16).rearrange("p (k n) -> p k n", k=8)
            memb = [A.alloc(D, BF16) for _ in range(2)]
            memT = A.alloc(8 * MEM, BF16).rearrange("p (k m) -> p k m", k=8)
            for j in range(2):
                P.dma(lambda e, j=j: e.dma_start(out=memb[j], in_=mem_d[j * 128:(j + 1) * 128, :]), w=[("memb", j)], q="pool")
                transpose_to(memT[:, :, j * 128:(j + 1) * 128], memb[j], 6 + j, [("memb", j)], ["memT"])
            for cb in range(2):
                for k0 in range(0, 8, 2):
                    P.dma(lambda e, cb=cb, k0=k0: e.dma_start(
                        out=wkv[:, k0:k0 + 2, cb * D:(cb + 1) * D],
                        in_=xa_wkv[l, k0 * 128:(k0 + 2) * 128, cb * D:(cb + 1) * D].rearrange("(k p) n -> p k n", p=128)),
                        w=["wkv"], q="pool")
            for ct in range(8):
                bank = ct % 2
                def mm(e, ct=ct, bank=bank):
                    ins = None
                    for k in range(8):
                        ins = e.matmul(PS(bank)[:, 0:MEM], lhsT=wkv[:, k, ct * 128:(ct + 1) * 128], rhs=memT[:, k, :],
                                       start=(k == 0), stop=(k == 7))
                    return ins
                P.pe(mm, r=["wkv", "memT"], w=[("ps", bank)])
                cast_copy(kT[:, ct, :], PS(bank)[:, 0:MEM], [("ps", bank)], ["kT"])
            for mt in range(2):
                for hb in range(2):
                    bank = 2 + hb
                    def mm(e, mt=mt, hb=hb, bank=bank):
                        ins = None
                        for k in range(8):
                            ins = e.matmul(PS(bank), lhsT=memT[:, k, mt * 128:(mt + 1) * 128],
                                           rhs=wkv[:, k, D + hb * 512:D + (hb + 1) * 512], start=(k == 0), stop=(k == 7))
                        return ins
                    P.pe(mm, r=["wkv", "memT"], w=[("ps", bank)])
                    cast_copy(vtok[:, mt, hb * 512:(hb + 1) * 512], PS(bank), [("ps", bank)], ["vtok"])
            P.nosched = False
            load_w_bf16(wq, xa_wq[l], 8, D, "wq")
            load_w_bf16(wo, xa_wo[l], 8, D, "wo")
            load_gb(l, 1)
            E = Epi()
            qT = A.alloc(8 * 512, BF16).rearrange("p (k t) -> p k t", k=8)
            oT = A.alloc(8 * 512, BF16).rearrange("p (k t) -> p k t", k=8)
            ex = A.alloc(MEM, F32)
            pb = A.alloc(MEM, BF16)
            pT = A.alloc(MEM, BF16).rearrange("p (m t) -> p m t", m=2)
            sm = A.alloc(8, F32)
            SCL = 1.0 / 16.0
            cast_force[0] = "act"
            last_layer_xT = True
            for grp in range(DBG["xa_groups"]):
                for ct in range(8):
                    bank = 6
                    def mm(e, ct=ct, grp=grp, bank=bank):
                        ins = None
                        for k in range(8):
                            ins = e.matmul(PS(bank), lhsT=wq[:, k, ct * 128:(ct + 1) * 128], rhs=xT3[:, k, grp * 512:(grp + 1) * 512],
                                           start=(k == 0), stop=(k == 7))
                        return ins
                    P.pe(mm, r=["wq"] + xT_all[grp * 4:grp * 4 + 4], w=[("ps", bank)])
                    cast_copy(qT[:, ct, :], PS(bank), [("ps", bank)], [("qT", ct)])
                for tl in range(4):
                    i = grp * 4 + tl
                    tsl = slice(tl * 128, (tl + 1) * 128)
                    for h in range(4):
                        sc = PS(2 + h % 2)[:, 0:256]
                        ksc = ("ps", 2 + h % 2)
                        def ms(e, h=h, sc=sc, tsl=tsl):
                            ins = None
                            for dk in range(2):
                                ins = e.matmul(sc, lhsT=qT[:, 2 * h + dk, tsl], rhs=kT[:, 2 * h + dk, :], start=(dk == 0), stop=(dk == 1))
                            return ins
                        P.pe(ms, r=[("qT", 2 * h), ("qT", 2 * h + 1), "kT"], w=[ksc])
                        P.dve(lambda e, sc=sc: e.reduce_max(out=sm[:, 0:1], in_=sc, axis=AX.X), r=[ksc], w=["sm0"])
                        P.dve(lambda e: e.tensor_scalar(out=sm[:, 1:2], in0=sm[:, 0:1], scalar1=-SCL, scalar2=None, op0=ALU.mult), r=["sm0"], w=["sm1"])
                        P.act(lambda e, sc=sc: e.activation(out=ex, in_=sc, func=AF.Exp, bias=sm[:, 1:2], scale=SCL), r=[ksc, "sm1"], w=["ex"])
                        P.dve(lambda e: e.reduce_sum(out=sm[:, 2:3], in_=ex, axis=AX.X), r=["ex"], w=["sm2"])
                        P.dve(lambda e: e.reciprocal(out=sm[:, 3:4], in_=sm[:, 2:3]), r=["sm2"], w=["sm3"])
                        P.dve(lambda e: e.tensor_scalar(out=pb, in0=ex, scalar1=sm[:, 3:4], scalar2=None, op0=ALU.mult), r=["ex", "sm3"], w=["pb"])
                        ptq = PSB(4)[:, 0:256]
                        kpt = ("ps", 4)
                        def mt_(e, ptq=ptq):
                            ins = None
                            for mt in range(2):
                                ins = e.transpose(out=ptq[:, mt * 128:(mt + 1) * 128], in_=pb[:, mt * 128:(mt + 1) * 128], identity=ident)
                            return ins
                        P.pe(mt_, r=["pb", "ident"], w=[kpt])
                        cast_copy(pT, ptq.rearrange("p (m t) -> p m t", m=2), [kpt], ["pT"])
                        pv = PS((5, 7)[h % 2])[:, 0:256]
                        kpv = ("ps", (5, 7)[h % 2])
                        def mpv(e, h=h, pv=pv):
                            ins = None
                            for dv in range(2):
                                for mt in range(2):
                                    ins = e.matmul(pv[:, dv * 128:(dv + 1) * 128], lhsT=vtok[:, mt, h * 256 + dv * 128:h * 256 + (dv + 1) * 128],
                                                   rhs=pT[:, mt, :], start=(mt == 0), stop=(mt == 1))
                            return ins
                        P.pe(mpv, r=["pT", "vtok"], w=[kpv])
                        cast_copy(oT[:, 2 * h:2 * h + 2, tsl], pv.rearrange("p (d t) -> p d t", d=2), [kpv], [("oT", tl)])
                    for hh in range(2):
                        def mm(e, hh=hh, tsl=tsl):
                            ins = None
                            for k in range(8):
                                ins = e.matmul(PS(hh), lhsT=oT[:, k, tsl], rhs=wo[:, k, hh * 512:(hh + 1) * 512], start=(k == 0), stop=(k == 7))
                            return ins
                        P.pe(mm, r=["wo", ("oT", tl)], w=[("ps", hh)])
                    epilogue(E, i, [PS(0), PS(1)], [("ps", 0), ("ps", 1)], cur_res, dst_res, make_xT=False)
            cast_force[0] = None
            P.nosched = False
            P.barrier()
            A.release()
            A.release()

    def moe_phase(l, cur_res, dst_res, want_xT):
        if True:
            A.mark()
            idx = A.alloc(NT * 2, I32 if False else F32).bitcast(I32).rearrange("p (t k) -> p t k", k=2)
            gates = A.alloc(NT * 2, F32).rearrange("p (t k) -> p t k", k=2)
            A.mark()
            wr32 = A.alloc(8 * 36, F32).rearrange("p (k n) -> p k n", k=8)
            P.dma(ncd(lambda e: e.dma_start(out=wr32[:, :, 0:4], in_=moe_wg[l].rearrange("(k p) n -> p k n", p=128))), w=["wr32"])
            P.dma(ncd(lambda e: e.dma_start(out=wr32[:, :, 4:36], in_=moe_we[l].rearrange("(k p) n -> p k n", p=128))), w=["wr32"])
            bias_bc = A.alloc(36, F32)
            P.dma(lambda e: e.dma_start(out=bias_bc[:, 0:4], in_=moe_bg[l:l + 1, :].broadcast_to([128, 4])), w=["bias_bc"])
            P.dma(lambda e: e.dma_start(out=bias_bc[:, 4:36], in_=moe_be[l:l + 1, :].broadcast_to([128, 32])), w=["bias_bc"])
            eoff = A.alloc(32, F32)
            P.dma(lambda e: e.dma_start(out=eoff, in_=c_eoff.broadcast_to([128, 32])), w=["eoff"])
            ustr = A.alloc(128, BF16)
            onesb = A.alloc(128, BF16)
            P.dma(lambda e: e.dma_start(out=ustr, in_=c_masks[2]), w=["ustr"], q="pool")
            P.dma(lambda e: e.dma_start(out=onesb, in_=c_masks[3]), w=["onesb"], q="pool")
            trash = A.alloc(1, F32)
            P.dma(ncd(lambda e: e.dma_start(out=trash, in_=c_trash)), w=["trash"])
            carry = A.alloc(32, F32)
            P.dve(lambda e: e.memset(carry, 0.0), w=["carry"])
            if DBG.get("zero_xs"):
                zt = A.alloc(D, BF16)
                P.pool(lambda e: e.memset(zt, 0.0), w=["zt"])
                for r0 in range(0, NEXP * CAP + 128, 128):
                    P.dma(lambda e, r0=r0: e.dma_start(out=XS[r0:r0 + 128, :], in_=zt), r=["zt"], w=["XS"])
            x2 = [A.alloc(D, F32) for _ in range(2)]
            xb2 = [A.alloc(D, BF16) for _ in range(2)]
            RB = []
            for _p in range(2):
                rb = {}
                rb["x2T"] = A.alloc(8 * 128, F32).rearrange("p (k t) -> p k t", k=8)
                for nm, n_ in (("lg", 36), ("rt", 64), ("maskg", 4), ("esel", 8), ("e2", 8), ("mask1", 8), ("mask2", 8),
                               ("M1", 32), ("M2", 32), ("Ms", 32), ("pos", 32), ("tmp", 32)):
                    rb[nm] = A.alloc(n_, F32)
                rb["Mb"] = A.alloc(32, BF16)
                RB.append(rb)
            def route_tile(i):
                b = i % 2
                rbp = b if DBG.get("route_double", 0) else 0
                rb = RB[rbp]
                x2T, lg, rt, maskg, esel, e2, mask1, mask2 = rb['x2T'], rb['lg'], rb['rt'], rb['maskg'], rb['esel'], rb['e2'], rb['mask1'], rb['mask2']
                M1, M2, Ms, Mb, pos, tmp = rb['M1'], rb['M2'], rb['Ms'], rb['Mb'], rb['pos'], rb['tmp']
                M1v = M1.rearrange('p (g j) -> p g j', g=4)
                M2v = M2.rearrange('p (g j) -> p g j', g=4)
                pb_ = 4 * rbp
                kx2, kxb = ("x2", b), ("xb2", b)
                P.dma(lambda e, i=i, b=b: e.dma_start(out=x2[b], in_=cur_res[i * 128:(i + 1) * 128, :]), w=[kx2])
                P.act(lambda e, b=b: e.copy(out=xb2[b], in_=x2[b]), r=[kx2], w=[kxb])
                for half in range(2):
                    def tr(e, half=half, b=b):
                        ins = None
                        for q in range(4):
                            k = half * 4 + q
                            ins = e.transpose(out=PS(pb_ + half)[:, q * 128:(q + 1) * 128], in_=x2[b][:, k * 128:(k + 1) * 128], identity=ident32)
                        return ins
                    P.pe(tr, r=[kx2, "ident32"], w=[("ps", pb_ + half)])
                    cast_copy(x2T[:, half * 4:(half + 1) * 4, :], PS(pb_ + half).rearrange("p (k t) -> p k t", k=4), [("ps", pb_ + half)], [("x2T", rbp)])
                def mlg(e):
                    ins = None
                    for k in range(8):
                        ins = e.matmul(PS(pb_ + 2)[:, 0:36], lhsT=x2T[:, k, :], rhs=wr32[:, k, :], start=(k == 0), stop=(k == 7))
                    return ins
                P.pe(mlg, r=[("x2T", rbp), "wr32"], w=[("ps", pb_ + 2)])
                V = P.dve
                V(lambda e: e.tensor_tensor(out=lg, in0=PS(pb_ + 2)[:, 0:36], in1=bias_bc, op=ALU.add), r=[("ps", pb_ + 2), "bias_bc"], w=[("lg", rbp)])
                V(lambda e: e.reduce_max(out=rt[:, 0:1], in_=lg[:, 0:4], axis=AX.X), r=[("lg", rbp)], w=[("rt0", rbp)])
                V(lambda e: e.tensor_scalar(out=maskg, in0=lg[:, 0:4], scalar1=rt[:, 0:1], scalar2=None, op0=ALU.is_equal), r=[("lg", rbp), ("rt0", rbp)], w=[("maskg", rbp)])
                V(lambda e: e.tensor_scalar(out=rt[:, 1:2], in0=rt[:, 0:1], scalar1=-1.0, scalar2=None, op0=ALU.mult), r=[("rt0", rbp)], w=[("rt1", rbp)])
                P.act(lambda e: e.activation(out=rt[:, 4:8], in_=lg[:, 0:4], func=AF.Exp, bias=rt[:, 1:2], scale=1.0), r=[("lg", rbp), ("rt1", rbp)], w=[("rt4", rbp)])
                V(lambda e: e.reduce_sum(out=rt[:, 2:3], in_=rt[:, 4:8], axis=AX.X), r=[("rt4", rbp)], w=[("rt2", rbp)])
                V(lambda e: e.reciprocal(out=rt[:, 3:4], in_=rt[:, 2:3]), r=[("rt2", rbp)], w=[("rt3", rbp)])
                V(lambda e: e.tensor_scalar(out=esel, in0=lg[:, 4:12], scalar1=maskg[:, 0:1], scalar2=None, op0=ALU.mult), r=[("lg", rbp), ("maskg", rbp)], w=[("esel", rbp)])
                for g in range(1, 4):
                    V(lambda e, g=g: e.scalar_tensor_tensor(out=esel, in0=lg[:, 4 + 8 * g:12 + 8 * g], scalar=maskg[:, g:g + 1], in1=esel,
                                                            op0=ALU.mult, op1=ALU.add), r=[("lg", rbp), ("maskg", rbp), ("esel", rbp)], w=[("esel", rbp)])
                V(lambda e: e.reduce_max(out=rt[:, 8:9], in_=esel, axis=AX.X), r=[("esel", rbp)], w=[("rt8", rbp)])
                V(lambda e: e.tensor_scalar(out=mask1, in0=esel, scalar1=rt[:, 8:9], scalar2=None, op0=ALU.is_equal), r=[("esel", rbp), ("rt8", rbp)], w=[("mask1", rbp)])
                V(lambda e: e.scalar_tensor_tensor(out=e2, in0=mask1, scalar=-1e30, in1=esel, op0=ALU.mult, op1=ALU.add), r=[("mask1", rbp), ("esel", rbp)], w=[("e2", rbp)])
                V(lambda e: e.reduce_max(out=rt[:, 9:10], in_=e2, axis=AX.X), r=[("e2", rbp)], w=[("rt9", rbp)])
                V(lambda e: e.tensor_scalar(out=mask2, in0=e2, scalar1=rt[:, 9:10], scalar2=None, op0=ALU.is_equal), r=[("e2", rbp), ("rt9", rbp)], w=[("mask2", rbp)])
                V(lambda e: e.tensor_tensor(out=rt[:, 10:11], in0=rt[:, 9:10], in1=rt[:, 8:9], op=ALU.subtract), r=[("rt8", rbp), ("rt9", rbp)], w=[("rt10", rbp)])
                P.act(lambda e: e.activation(out=rt[:, 11:12], in_=rt[:, 10:11], func=AF.Exp), r=[("rt10", rbp)], w=[("rt11", rbp)])
                V(lambda e: e.tensor_scalar(out=rt[:, 12:13], in0=rt[:, 11:12], scalar1=1.0, scalar2=None, op0=ALU.add), r=[("rt11", rbp)], w=[("rt12", rbp)])
                V(lambda e: e.reciprocal(out=rt[:, 13:14], in_=rt[:, 12:13]), r=[("rt12", rbp)], w=[("rt13", rbp)])
                V(lambda e, i=i: e.tensor_tensor(out=gates[:, i, 0:1], in0=rt[:, 3:4], in1=rt[:, 13:14], op=ALU.mult), r=[("rt3", rbp), ("rt13", rbp)], w=["gates"])
                V(lambda e, i=i: e.tensor_tensor(out=gates[:, i, 1:2], in0=gates[:, i, 0:1], in1=rt[:, 11:12], op=ALU.mult), r=["gates", ("rt11", rbp)], w=["gates"])
                for g in range(4):
                    V(lambda e, g=g: e.tensor_scalar(out=M1v[:, g, :], in0=mask1, scalar1=maskg[:, g:g + 1], scalar2=None, op0=ALU.mult),
                      r=[("mask1", rbp), ("maskg", rbp)], w=[("M1", rbp)])
                    V(lambda e, g=g: e.tensor_scalar(out=M2v[:, g, :], in0=mask2, scalar1=maskg[:, g:g + 1], scalar2=None, op0=ALU.mult),
                      r=[("mask2", rbp), ("maskg", rbp)], w=[("M2", rbp)])
                V(lambda e: e.tensor_tensor(out=Ms, in0=M1, in1=M2, op=ALU.add), r=[("M1", rbp), ("M2", rbp)], w=[("Ms", rbp)])
                V(lambda e: e.tensor_copy(out=Mb, in_=Ms), r=[("Ms", rbp)], w=[("Mb", rbp)])
                def mpos(e):
                    e.matmul(PS(pb_ + 3)[:, 0:32], lhsT=ustr, rhs=Mb, start=True, stop=True)
                    return e.matmul(PS(pb_ + 3)[:, 32:64], lhsT=onesb, rhs=Mb, start=True, stop=True)
                P.pe(mpos, r=[("Mb", rbp), "ustr", "onesb"], w=[("ps", pb_ + 3)])
                V(lambda e: e.tensor_tensor(out=pos, in0=PS(pb_ + 3)[:, 0:32], in1=carry, op=ALU.add), r=[("ps", pb_ + 3), "carry"], w=[("pos", rbp)])
                V(lambda e: e.tensor_tensor(out=carry, in0=PS(pb_ + 3)[:, 32:64], in1=carry, op=ALU.add), r=[("ps", pb_ + 3), "carry"], w=["carry"])
                V(lambda e: e.tensor_scalar(out=tmp, in0=pos, scalar1=float(CAP), scalar2=None, op0=ALU.is_ge), r=[("pos", rbp)], w=[("tmp", rbp)])
                V(lambda e: e.tensor_tensor(out=pos, in0=pos, in1=eoff, op=ALU.add), r=[("pos", rbp), "eoff"], w=[("pos", rbp)])
                V(lambda e: e.tensor_scalar(out=Ms, in0=pos, scalar1=-1.0, scalar2=trash[:, 0:1], op0=ALU.mult, op1=ALU.add), r=[("pos", rbp), "trash"], w=[("Ms", rbp)])
                V(lambda e: e.tensor_tensor(out=tmp, in0=tmp, in1=Ms, op=ALU.mult), r=[("tmp", rbp), ("Ms", rbp)], w=[("tmp", rbp)])
                V(lambda e: e.tensor_tensor(out=pos, in0=pos, in1=tmp, op=ALU.add), r=[("pos", rbp), ("tmp", rbp)], w=[("pos", rbp)])
                for kk, Mk in enumerate((M1, M2)):
                    kM = ("M1", rbp) if kk == 0 else ("M2", rbp)
                    V(lambda e, Mk=Mk: e.tensor_tensor(out=tmp, in0=Mk, in1=pos, op=ALU.mult), r=[kM, ("pos", rbp)], w=[("tmp", rbp)])
                    V(lambda e, kk=kk: e.reduce_sum(out=rt[:, 16 + kk:17 + kk], in_=tmp, axis=AX.X), r=[("tmp", rbp)], w=[("rtidx", kk, rbp)])
                    V(lambda e, kk=kk, i=i: e.tensor_copy(out=idx[:, i, kk:kk + 1], in_=rt[:, 16 + kk:17 + kk]), r=[("rtidx", kk, rbp)], w=[("idx", i, kk)])
                    P.dma(lambda e, kk=kk, i=i, b=b: e.indirect_dma_start(
                        out=XS, out_offset=bass.IndirectOffsetOnAxis(idx[:, i, kk:kk + 1], 0), in_=xb2[b], in_offset=None), r=[("idx", i, kk), kxb], w=["XS"], q="pool")
            for i in range(NT):
                route_tile(i)
            P.barrier()
            A.release()
            A.mark()
            NCT = CAP // 128
            xT_f32 = xT.bitcast(F32)
            stg = [xT_f32[:, k * 2048:(k + 1) * 2048] for k in range(6)]
            wgb = [A.alloc(8 * 512, BF16).rearrange("p (k n) -> p k n", k=8) for _ in range(2)]
            wub = [A.alloc(8 * 512, BF16).rearrange("p (k n) -> p k n", k=8) for _ in range(2)]
            wdb = [A.alloc(4 * D, BF16).rearrange("p (k n) -> p k n", k=4) for _ in range(2)]
            xsb = [A.alloc(D, BF16) for _ in range(4)]
            xsT2 = [A.alloc(8 * CAP, BF16).rearrange("p (k t) -> p k t", k=8) for _ in range(2)]
            hT2 = [A.alloc(4 * CAP, BF16).rearrange("p (k t) -> p k t", k=4) for _ in range(2)]
            sg2 = [A.alloc(CAP, F32) for _ in range(2)]
            ysb = [A.alloc(D, F32) for _ in range(4)]
            sn = 0
            for ex_i in range(NEXP):
                b = ex_i % 2
                xsT, hT, sg = xsT2[b], hT2[b], sg2[b]
                kxsT, khT, ksg = ("xsT", b), ("hT", b), ("sg", b)
                chunks = []
                for (wsrc, dstb, nm) in ((moe_gate, wgb, "wg"), (moe_up, wub, "wu")):
                    for c in range(2):
                        chunks.append((wsrc[l, ex_i, c * 512:(c + 1) * 512, :].rearrange("(k p) n -> p k n", p=128),
                                       dstb[b][:, c * 4:(c + 1) * 4, :], (nm, b), 4))
                for c in range(2):
                    chunks.append((moe_down[l, ex_i, c * 256:(c + 1) * 256, :].rearrange("(k p) n -> p k n", p=128),
                                   wdb[b][:, c * 2:(c + 1) * 2, :], ("wd", b), 2))
                for (src, dst, key, kk) in chunks:
                    sb_ = sn % 6
                    sn += 1
                    sview = stg[sb_].rearrange("p (k n) -> p k n", k=kk)
                    P.dma(lambda e, src=src, sview=sview: e.dma_start(out=sview, in_=src), w=[("stg", sb_)], q="pool")
                    if sn % 2 == 0:
                        P.act(lambda e, dst=dst, sview=sview: e.copy(out=dst, in_=sview), r=[("stg", sb_)], w=[key], c=2.0)
                    else:
                        P.dve(lambda e, dst=dst, sview=sview: e.tensor_copy(out=dst, in_=sview), r=[("stg", sb_)], w=[key], c=2.3)
                for stl in range(NCT):
                    xb_ = (ex_i * NCT + stl) % 4
                    r0 = ex_i * CAP + stl * 128
                    P.dma(lambda e, r0=r0, xb_=xb_: e.dma_start(out=xsb[xb_], in_=XS[r0:r0 + 128, :]), r=["XS"], w=[("xsb", xb_)], q="pool")
                    transpose_to(xsT[:, :, stl * 128:(stl + 1) * 128], xsb[xb_], 7, [("xsb", xb_)], [kxsT])
                for ft in range(4):
                    for m, wb_, kw in ((0, wgb, "wg"), (1, wub, "wu")):
                        def mm(e, ft=ft, m=m, wb_=wb_, b=b, xsT=xsT):
                            ins = None
                            for k in range(8):
                                ins = e.matmul(PS(m)[:, 0:CAP], lhsT=wb_[b][:, k, ft * 128:(ft + 1) * 128], rhs=xsT[:, k, :],
                                               start=(k == 0), stop=(k == 7))
                            return ins
                        P.pe(mm, r=[(kw, b), kxsT], w=[("ps", m)])
                    P.act(lambda e, sg=sg: e.activation(out=sg, in_=PS(0)[:, 0:CAP], func=AF.Silu), r=[("ps", 0)], w=[ksg])
                    P.dve(lambda e, ft=ft, sg=sg, hT=hT: e.tensor_tensor(out=hT[:, ft, :], in0=sg, in1=PS(1)[:, 0:CAP], op=ALU.mult), r=[ksg, ("ps", 1)], w=[khT])
                for stl in range(NCT):
                    yb_ = stl % 4
                    for hh in range(2):
                        bank = 2 + (stl % 2) * 2 + hh
                        def mm(e, stl=stl, hh=hh, bank=bank, b=b, hT=hT):
                            ins = None
                            for ft in range(4):
                                ins = e.matmul(PS(bank), lhsT=hT[:, ft, stl * 128:(stl + 1) * 128], rhs=wdb[b][:, ft, hh * 512:(hh + 1) * 512],
                                               start=(ft == 0), stop=(ft == 3))
                            return ins
                        P.pe(mm, r=[khT, ("wd", b)], w=[("ps", bank)])
                        if hh == 0:
                            P.act(lambda e, bank=bank, yb_=yb_: e.copy(out=ysb[yb_][:, 0:512], in_=PS(bank)), r=[("ps", bank)], w=[("ysb", yb_)])
                        else:
                            P.dve(lambda e, bank=bank, yb_=yb_: e.tensor_copy(out=ysb[yb_][:, 512:1024], in_=PS(bank)), r=[("ps", bank)], w=[("ysb", yb_)])
                    r0 = ex_i * CAP + stl * 128
                    P.dma(lambda e, r0=r0, yb_=yb_: e.dma_start(out=YS[r0:r0 + 128, :], in_=ysb[yb_]), r=[("ysb", yb_)], w=["YS"])
            P.barrier()
            A.release()
            A.mark()
            load_gb(l, 2)
            E = Epi()
            y1 = [A.alloc(D, F32) for _ in range(2)]
            y2 = [A.alloc(D, F32) for _ in range(2)]
            hm = [A.alloc(D, F32) for _ in range(2)]
            dst = dst_res
            def gather(i):
                b = i % 2
                P.dma(lambda e, i=i, b=b: e.indirect_dma_start(
                    out=y1[b], out_offset=None, in_=YS, in_offset=bass.IndirectOffsetOnAxis(idx[:, i, 0:1], 0)), r=["YS"], w=[("y1", b)], q="pool")
                P.dma(lambda e, i=i, b=b: e.indirect_dma_start(
                    out=y2[b], out_offset=None, in_=YS, in_offset=bass.IndirectOffsetOnAxis(idx[:, i, 1:2], 0)), r=["YS"], w=[("y2", b)], q="pool")
            gather(0)
            gather(1)
            for i in range(NT):
                b = i % 2
                P.dve(lambda e, i=i, b=b: e.tensor_scalar(out=hm[b], in0=y1[b], scalar1=gates[:, i, 0:1], scalar2=None, op0=ALU.mult),
                      r=[("y1", b)], w=[("hm", b)])
                P.dve(lambda e, i=i, b=b: e.scalar_tensor_tensor(out=hm[b], in0=y2[b], scalar=gates[:, i, 1:2], in1=hm[b], op0=ALU.mult, op1=ALU.add),
                      r=[("y2", b), ("hm", b)], w=[("hm", b)])
                if i + 2 < NT:
                    gather(i + 2)
                epilogue(E, i, [hm[b][:, 0:512], hm[b][:, 512:1024]], [("hm", b)], cur_res, dst, make_xT=want_xT, gmul="dve")
            P.barrier()
            A.release()
            A.release()


    def hg_phase(cur_res, dst_res):
        A.mark()
        A.mark()
        P.nosched = bool(DBG.get("hg_nosched", 0))
        lbraw = A.alloc(32, F32)
        lbt = A.alloc(32, F32)
        lbrows = A.alloc(128, F32)
        P.dma(lambda e: e.dma_start(out=lbrows[0:32, :], in_=hg_lb.rearrange("l d (h k) -> (l d h) k", k=128)), w=["lbrows"])
        P.pe(lambda e: e.transpose(out=PS(0)[:, 0:32], in_=lbrows[0:32, :], identity=ident32[0:32, 0:32]), r=["lbrows", "ident32"], w=[("ps", 0)], c=0.3)
        P.dve(lambda e: e.tensor_copy(out=lbraw, in_=PS(0)[:, 0:32]), r=[("ps", 0)], w=["lbraw"])
        P.dve(lambda e: e.tensor_tensor(out=lbt[:, 0:16], in0=lbraw[:, 16:32], in1=lbraw[:, 0:16], op=ALU.subtract), r=["lbraw"], w=["lbd"])
        P.act(lambda e: e.activation(out=lbt[:, 0:16], in_=lbt[:, 0:16], func=AF.Sigmoid), r=["lbd"], w=["lb"])
        P.dve(lambda e: e.tensor_scalar(out=lbt[:, 16:32], in0=lbt[:, 0:16], scalar1=-1.0, scalar2=1.0, op0=ALU.mult, op1=ALU.add), r=["lb"], w=["oml"])
        ng = A.alloc(1, F32)
        P.dma(ncd(lambda e: e.dma_start(out=ng, in_=hg_norm_g.rearrange("(k o) -> k o", o=1))), w=["ng"])
        cmask = A.alloc(512, F32)
        P.pool(lambda e: e.memset(cmask, 1.0), w=["cmask"])
        P.pool(lambda e: e.memset(cmask.rearrange("p (c l) -> p c l", l=64)[:, :, 0:1], 0.0), r=["cmask"], w=["cmask"])
        mk = [A.alloc(128, F32) for _ in range(2)]
        for d in range(2):
            P.dma(lambda e, d=d: e.dma_start(out=mk[d], in_=c_masks[d]), w=[("mk", d)])
        Sst = [[A.alloc(128, F32) for _ in range(2)] for _ in range(2)]
        s_cur = [0, 0]
        wh = [A.alloc(8 * 5 * 128, BF16).rearrange("p (k q n) -> p k q n", k=8, q=5) for _ in range(2)]
        rst = A.alloc(4 * 512, F32)
        vtok = A.alloc(NT * 128, BF16).rearrange("p (t n) -> p t n", t=NT)
        qT = A.alloc(S, F32)
        oTh = A.alloc(S, F32)
        T = []
        for d in range(2):
            t = {}
            for nm in ("sgm", "lgf", "bb", "kk", "t1", "t2"):
                t[nm] = A.alloc(512, F32)
            t["kbT"] = A.alloc(512, BF16)
            for nm in ("qt", "kt", "qb"):
                t[nm] = [A.alloc(512, BF16) for _ in range(2)]
            t["kbtok"] = [A.alloc(512, BF16).rearrange("p (t n) -> p t n", t=4) for _ in range(2)]
            t["dec"] = [A.alloc(8, F32) for _ in range(2)]
            t["sTm"] = [A.alloc(128, BF16) for _ in range(2)]
            t["Sp"] = [[A.alloc(128, BF16) for _ in range(2)] for _ in range(2)]
            T.append(t)
        o2 = A.alloc(512, F32)
        rsn = A.alloc(512, F32)
        sgt = A.alloc(512, F32)
        onb = [A.alloc(512, BF16) for _ in range(2)]

        def K_(d, nm):
            return ("hg", d, nm)

        def v3(ap):
            return ap.rearrange("p (c l) -> p c l", l=64)

        def prep(h, d, blk, buf, wb, kwb, part="ab"):
            t = T[d]
            t0 = blk * 512
            col = d * 8 + h
            fb = (0, 6)[d]
            qt_, kt_, qb_, kbtok_, dec_ = t["qt"][buf], t["kt"][buf], t["qb"][buf], t["kbtok"][buf], t["dec"][buf]
            def mm(e):
                ins = None
                for k in range(8):
                    ins = e.matmul(PS(fb), lhsT=wb[:, k, 2 + d, :], rhs=xT3[:, k, t0:t0 + 512], start=(k == 0), stop=(k == 7))
                return ins
            if "a" in part:
                P.pe(mm, r=[kwb] + xT_all[blk * 4:blk * 4 + 4], w=[("ps", fb)])
                P.act(lambda e: e.activation(out=t["sgm"], in_=PS(fb), func=AF.Sigmoid), r=[("ps", fb)], w=[K_(d, "sgm"), "sigdone"])
                P.dve(lambda e: e.tensor_scalar(out=t["sgm"], in0=t["sgm"], scalar1=lbt[:, 16 + col:17 + col], scalar2=lbt[:, col:col + 1],
                                                op0=ALU.mult, op1=ALU.add), r=[K_(d, "sgm"), "lb", "oml"], w=[K_(d, "sgm")])
            if "a" in part and ((blk < 4) if d == 0 else (blk >= 4)):
                qblk_, vblk_ = qT[:, t0:t0 + 512], vtok[:, blk * 4:(blk + 1) * 4, :]
                def mq(e):
                    ins = None
                    for k in range(8):
                        ins = e.matmul(PS(fb), lhsT=wb[:, k, 0, :], rhs=xT3[:, k, t0:t0 + 512], start=(k == 0), stop=(k == 7))
                    return ins
                P.pe(mq, r=[kwb] + xT_all[blk * 4:blk * 4 + 4], w=[("ps", fb)])
                P.act(lambda e: e.activation(out=qblk_, in_=PS(fb), func=AF.Sigmoid), r=[("ps", fb)], w=[("qT", blk), "sigdone"])
                P.dve(lambda e: e.tensor_tensor(out=qblk_, in0=qblk_, in1=PS(fb), op=ALU.mult), r=[("qT", blk), ("ps", fb)], w=[("qT", blk)])
                def mv(e):
                    ins = None
                    for tl in range(4):
                        tile = blk * 4 + tl
                        for k in range(8):
                            ins = e.matmul(PS(fb)[:, tl * 128:(tl + 1) * 128], lhsT=xT3[:, k, tile * 128:(tile + 1) * 128], rhs=wb[:, k, 1, :],
                                           start=(k == 0), stop=(k == 7))
                    return ins
                P.pe(mv, r=[kwb] + xT_all[blk * 4:blk * 4 + 4], w=[("ps", fb)], c=2.5)
                cast_copy(vblk_, PS(fb).rearrange("p (t n) -> p t n", t=4), [("ps", fb)], [("vtok", blk)])
            if "b" not in part:
                return
            P.act(lambda e: e.activation(out=t["lgf"], in_=t["sgm"], func=AF.Ln), r=[K_(d, "sgm"), "sigdone"], w=[K_(d, "lgf")])
            P.pool(lambda e: e.tensor_scalar(out=t["kk"], in0=t["sgm"], scalar1=-1.0, scalar2=1.0, op0=ALU.mult, op1=ALU.add),
                   r=[K_(d, "sgm")], w=[K_(d, "kk")])
            if d == 0:
                P.dve(lambda e: e.tensor_tensor_scan(out=t["bb"], data0=cmask, data1=t["lgf"], initial=0.0, op0=ALU.mult, op1=ALU.add),
                      r=["cmask", K_(d, "lgf")], w=[K_(d, "bb")])
                iref, ilast = 32, 63
            else:
                P.dve(lambda e: e.tensor_tensor_scan(out=t["t2"], data0=cmask, data1=t["lgf"], initial=0.0, op0=ALU.mult, op1=ALU.add),
                      r=["cmask", K_(d, "lgf")], w=[K_(d, "t2")])
                P.dve(lambda e: e.scalar_tensor_tensor(out=t["t1"], in0=t["t2"], scalar=-1.0, in1=t["lgf"], op0=ALU.mult, op1=ALU.add),
                      r=[K_(d, "t2"), K_(d, "lgf")], w=[K_(d, "t1")])
                P.dve(lambda e: e.tensor_tensor(out=v3(t["bb"]), in0=v3(t["t1"]), in1=v3(t["t2"])[:, :, 63:64].broadcast_to([128, 8, 64]), op=ALU.add),
                      r=[K_(d, "t1"), K_(d, "t2")], w=[K_(d, "bb")])
                iref, ilast = 31, 0
            bref = v3(t["bb"])[:, :, iref:iref + 1].broadcast_to([128, 8, 64])
            blast = v3(t["bb"])[:, :, ilast:ilast + 1].broadcast_to([128, 8, 64])
            qsl = qT[:, t0:t0 + 512]
            kqb = ("qT", blk)
            kq, kk_, kb_, kkb, kd = K_(d, ("qt", buf)), K_(d, ("kt", buf)), K_(d, ("qb", buf)), K_(d, ("kbtok", buf)), K_(d, ("dec", buf))
            P.pool(lambda e: e.tensor_tensor(out=v3(t["t1"]), in0=v3(t["bb"]), in1=bref, op=ALU.subtract), r=[K_(d, "bb")], w=[K_(d, "t1")])
            P.act(lambda e: e.activation(out=t["t2"], in_=t["t1"], func=AF.Exp), r=[K_(d, "t1")], w=[K_(d, "t2")])
            P.dve(lambda e: e.tensor_tensor(out=qt_, in0=qsl, in1=t["t2"], op=ALU.mult), r=[kqb, K_(d, "t2")], w=[kq])
            P.act(lambda e: e.activation(out=t["t2"], in_=t["t1"], func=AF.Exp, scale=-1.0), r=[K_(d, "t1"), kq], w=[K_(d, "t2")])
            P.pool(lambda e: e.tensor_tensor(out=kt_, in0=t["kk"], in1=t["t2"], op=ALU.mult), r=[K_(d, "kk"), K_(d, "t2")], w=[kk_])
            P.act(lambda e: e.activation(out=t["sgm"], in_=t["bb"], func=AF.Exp), r=[K_(d, "bb")], w=[K_(d, "sgm")])
            P.dve(lambda e: e.tensor_tensor(out=qb_, in0=qsl, in1=t["sgm"], op=ALU.mult), r=[kqb, K_(d, "sgm")], w=[kb_])
            P.pool(lambda e: e.tensor_tensor(out=v3(t["t1"]), in0=v3(t["bb"]), in1=blast, op=ALU.subtract), r=[K_(d, "bb"), K_(d, "t2")], w=[K_(d, "t1")])
            P.act(lambda e: e.activation(out=t["t2"], in_=t["t1"], func=AF.Exp, scale=-1.0), r=[K_(d, "t1"), kk_], w=[K_(d, "t2")])
            P.pool(lambda e: e.tensor_tensor(out=t["kbT"], in0=t["kk"], in1=t["t2"], op=ALU.mult), r=[K_(d, "kk"), K_(d, "t2")], w=[K_(d, "kbT")])
            P.act(lambda e: e.activation(out=dec_, in_=v3(t["bb"])[:, :, ilast], func=AF.Exp), r=[K_(d, "bb")], w=[kd])
            def tr(e):
                ins = None
                for tl in range(4):
                    ins = e.transpose(out=PSB(5)[:, d * 512 + tl * 128:d * 512 + (tl + 1) * 128], in_=t["kbT"][:, tl * 128:(tl + 1) * 128], identity=ident)
                return ins
            P.pe(tr, r=[K_(d, "kbT"), "ident"], w=[("ps", 5)])
            cast_copy(kbtok_, PSB(5)[:, d * 512:(d + 1) * 512].rearrange("p (t n) -> p t n", t=4), [("ps", 5)], [kkb])

        def tile_info(idx, d):
            step, ts = idx // 4, idx % 4
            blk = step if d == 0 else 7 - step
            tl = ts if d == 0 else 3 - ts
            return blk, tl, blk * 4 + tl, step % 2, idx % 2

        def front(idx):
            for d in range(2):
                t = T[d]
                blk, tl, tile, buf, par = tile_info(idx, d)
                tsl = slice(tl * 128, (tl + 1) * 128)
                sT = PS(2)[:, (d * 2 + par) * 128:(d * 2 + par + 1) * 128]
                Ub = PS(3 + par)[:, d * 256:(d + 1) * 256]
                order = (0, 1) if d == 0 else (1, 0)
                def mf(e, t=t, Ub=Ub, order=order, tl=tl, tile=tile, buf=buf, sT=sT, tsl=tsl):
                    ins = None
                    for c in (0, 1):
                        ci = order.index(c)
                        csl = slice(c * 64, (c + 1) * 64)
                        e.matmul(Ub[:, ci * 128:(ci + 1) * 128], lhsT=t["kbtok"][buf][csl, tl, :], rhs=vtok[csl, tile, :], start=True, stop=True)
                        ins = e.matmul(sT[:, c * 64:(c + 1) * 64], lhsT=t["kt"][buf][:, tsl], rhs=t["qt"][buf][:, tl * 128 + c * 64:tl * 128 + (c + 1) * 64],
                                       start=True, stop=True)
                    return ins
                P.pe(mf, r=[K_(d, ("kbtok", buf)), ("vtok", blk), K_(d, ("kt", buf)), K_(d, ("qt", buf))], w=[("ps", 3 + par), ("ps", 2)], c=0.5)
            for d in range(2):
                t = T[d]
                blk, tl, tile, buf, par = tile_info(idx, d)
                sT = PS(2)[:, (d * 2 + par) * 128:(d * 2 + par + 1) * 128]
                P.dve(lambda e, t=t, sT=sT, par=par, d=d: e.tensor_tensor(out=t["sTm"][par], in0=sT, in1=mk[d], op=ALU.mult),
                      r=[("ps", 2), ("mk", d)], w=[K_(d, ("sTm", par))], c=0.2)

        def back(idx):
            for d in range(2):
                t = T[d]
                blk, tl, tile, buf, par = tile_info(idx, d)
                Ub = PS(3 + par)[:, d * 256:(d + 1) * 256]
                order = (0, 1) if d == 0 else (1, 0)
                for ci, c in enumerate(order):
                    cib = tl * 2 + c
                    so, sn_ = s_cur[d], s_cur[d] ^ 1
                    s_cur[d] = sn_
                    P.dve(lambda e, t=t, d=d, ci=ci, cib=cib, Ub=Ub, buf=buf, so=so, sn_=sn_: e.scalar_tensor_tensor(
                        out=Sst[d][sn_], in0=Sst[d][so], scalar=t["dec"][buf][:, cib:cib + 1], in1=Ub[:, ci * 128:(ci + 1) * 128], op0=ALU.mult, op1=ALU.add),
                        r=[K_(d, ("S", so)), K_(d, ("dec", buf)), ("ps", 3 + par)], w=[K_(d, ("S", sn_))], c=0.2)
                    dpar, dslot = (par, 1) if ci == 0 else (par ^ 1, 0)
                    P.act(lambda e, t=t, d=d, dpar=dpar, dslot=dslot, sn_=sn_: e.copy(out=t["Sp"][dpar][dslot], in_=Sst[d][sn_]),
                          r=[K_(d, ("S", sn_))], w=[K_(d, ("Sp", dpar, dslot))], c=0.2)
            for d in range(2):
                t = T[d]
                blk, tl, tile, buf, par = tile_info(idx, d)
                acc = PS((1, 7)[par])[:, d * 128:(d + 1) * 128]
                order = (0, 1) if d == 0 else (1, 0)
                def ma(e, t=t, acc=acc, order=order, tl=tl, tile=tile, buf=buf, par=par):
                    e.matmul(acc, lhsT=vtok[:, tile, :], rhs=t["sTm"][par], start=True, stop=False, skip_group_check=True)
                    ins = None
                    for ci, c in enumerate(order):
                        ins = e.matmul(acc[:, c * 64:(c + 1) * 64], lhsT=t["Sp"][par][ci], rhs=t["qb"][buf][:, tl * 128 + c * 64:tl * 128 + (c + 1) * 64],
                                       start=False, stop=True, skip_group_check=True)
                    return ins
                P.pe(ma, r=[("vtok", blk), K_(d, ("sTm", par)), K_(d, ("Sp", par, 0)), K_(d, ("Sp", par, 1)), K_(d, ("qb", buf))], w=[("ps", (1, 7)[par])], c=0.4)
            for d in range(2):
                blk, tl, tile, buf, par = tile_info(idx, d)
                acc = PS((1, 7)[par])[:, d * 128:(d + 1) * 128]
                osl = oTh[:, tile * 128:(tile + 1) * 128]
                first = (blk < 4) if d == 0 else (blk >= 4)
                if first:
                    P.act(lambda e, osl=osl, acc=acc: e.copy(out=osl, in_=acc), r=[("ps", (1, 7)[par])], w=[("oTh", tile)], c=0.25)
                else:
                    P.dve(lambda e, osl=osl, acc=acc: e.tensor_tensor(out=osl, in0=osl, in1=acc, op=ALU.add),
                          r=[("ps", (1, 7)[par]), ("oTh", tile)], w=[("oTh", tile)], c=0.2)

        def record_list(fn):
            saved = P.ops
            P.ops = []
            fn()
            lst = P.ops
            P.ops = saved
            return lst

        for h in range(DBG["hg_heads"]):
            wb = wh[h % 2]
            kwb = ("wh", h % 2)
            for p_ in range(5):
                P.dma(lambda e, p_=p_, h=h, wb=wb: e.dma_start(
                    out=wb[:, :, p_, :], in_=hg_w_in[:, p_ * D + h * 128:p_ * D + (h + 1) * 128].rearrange("(k q) n -> q k n", q=128)),
                    w=[kwb], q="pool")
            for d in range(2):
                s_cur[d] = 0
                P.dve(lambda e, d=d: e.memset(Sst[d][0], 0.0), w=[K_(d, ("S", 0))])
                P.dve(lambda e, d=d: e.memset(T[d]["Sp"][0][0], 0.0), w=[K_(d, ("Sp", 0, 0))])
            if not DBG.get("hg_manual", 0):
                for step in range(8):
                    for d in range(2):
                        prep(h, d, step if d == 0 else 7 - step, step % 2, wb, kwb, part="a")
                    for d in range(2):
                        prep(h, d, step if d == 0 else 7 - step, step % 2, wb, kwb, part="b")
                    for ts in range(4):
                        front(step * 4 + ts)
                        back(step * 4 + ts)
            for d in range(2):
                if DBG.get("hg_manual", 0):
                    prep(h, d, 0 if d == 0 else 7, 0, wb, kwb)
            for step in (range(8) if DBG.get("hg_manual", 0) else ()):
                pl = []
                if step < 7:
                    pl = record_list(lambda: [prep(h, d, (step + 1) if d == 0 else 7 - (step + 1), (step + 1) % 2, wb, kwb) for d in range(2)])
                nq = (len(pl) + 3) // 4
                if not DBG.get("hg_merge", 1):
                    P.ops.extend(pl)
                    pl = []
                for ts in range(4):
                    idx = step * 4 + ts
                    front(idx)
                    if DBG.get("hg_lag", 1):
                        if idx >= 1:
                            back(idx - 1)
                    else:
                        back(idx)
                    P.ops.extend(pl[ts * nq:(ts + 1) * nq])
            if DBG.get("hg_lag", 1) and DBG.get("hg_manual", 0):
                back(31)
            for half in range(2):
                for tb4 in range(4):
                    tb = half * 4 + tb4
                    sl = slice(tb * 512, (tb + 1) * 512)
                    rsl = slice(tb4 * 512, (tb4 + 1) * 512)
                    okeys = [("oTh", i) for i in range(tb * 4, tb * 4 + 4)]
                    P.act(lambda e, sl=sl: e.activation(out=o2, in_=oTh[:, sl], func=AF.Square), r=okeys, w=["o2"])
                    P.pe(lambda e: e.matmul(PS(7), lhsT=ones_m, rhs=o2, start=True, stop=True), r=["o2", "ones_m"], w=[("ps", 7)], c=0.9)
                    P.act(lambda e: e.activation(out=rsn, in_=PS(7), func=AF.Ln, bias=EPS), r=[("ps", 7)], w=["rsn"])
                    P.act(lambda e, rsl=rsl: e.activation(out=rst[:, rsl], in_=rsn, func=AF.Exp, scale=-0.5), r=["rsn"], w=["rst", "normA"])
                for tb4 in range(4):
                    tb = half * 4 + tb4
                    sl = slice(tb * 512, (tb + 1) * 512)
                    rsl = slice(tb4 * 512, (tb4 + 1) * 512)
                    par = tb % 2
                    okeys = [("oTh", i) for i in range(tb * 4, tb * 4 + 4)]
                    def mm(e, sl=sl, wb=wb):
                        ins = None
                        for k in range(8):
                            ins = e.matmul(PS(6), lhsT=wb[:, k, 4, :], rhs=xT3[:, k, sl], start=(k == 0), stop=(k == 7))
                        return ins
                    P.pe(mm, r=[kwb] + xT_all[tb * 4:tb * 4 + 4], w=[("ps", 6)])
                    P.act(lambda e: e.activation(out=sgt, in_=PS(6), func=AF.Silu), r=[("ps", 6), "normA"], w=["sgt"])
                    P.dve(lambda e, sl=sl, rsl=rsl: e.scalar_tensor_tensor(out=o2, in0=oTh[:, sl], scalar=ng[:, 0:1], in1=rst[:, rsl], op0=ALU.mult, op1=ALU.mult),
                          r=okeys + ["rst", "ng", "o2"], w=["o2"])
                    P.dve(lambda e, par=par: e.tensor_tensor(out=onb[par], in0=o2, in1=sgt, op=ALU.mult), r=["o2", "sgt"], w=[("onb", par)])
                    P.dma(lambda e, h=h, sl=sl, par=par: e.dma_start(out=ONT[h * 128:(h + 1) * 128, sl], in_=onb[par]), r=[("onb", par)], w=["ONT"])
        P.nosched = False
        P.barrier()
        A.release()
        w_out = A.alloc(8 * D, BF16).rearrange("p (k n) -> p k n", k=8)
        load_w_bf16(w_out, hg_w_out, 8, D, "hw_out")
        load_gb(1, 0)
        E = Epi()
        onl = [A.alloc(8 * 512, BF16).rearrange("p (k t) -> p k t", k=8) for _ in range(2)]
        for grp in range(8):
            gb_ = grp % 2
            P.dma(lambda e, grp=grp, gb_=gb_: e.dma_start(out=onl[gb_], in_=ONT[:, grp * 512:(grp + 1) * 512].rearrange("(k p) t -> p k t", p=128)),
                  r=["ONT"], w=[("onl", gb_)])
            for tl in range(4):
                i = grp * 4 + tl
                for hh in range(2):
                    bank = (i % 2) * 2 + hh
                    def mm(e, tl=tl, hh=hh, bank=bank, gb_=gb_):
                        ins = None
                        for k in range(8):
                            ins = e.matmul(PS(bank), lhsT=onl[gb_][:, k, tl * 128:(tl + 1) * 128], rhs=w_out[:, k, hh * 512:(hh + 1) * 512],
                                           start=(k == 0), stop=(k == 7))
                        return ins
                    P.pe(mm, r=["hw_out", ("onl", gb_)], w=[("ps", bank)])
                b0 = (i % 2) * 2
                epilogue(E, i, [PS(b0), PS(b0 + 1)], [("ps", b0), ("ps", b0 + 1)], cur_res, dst_res)
        P.barrier()
        A.release()

    plan = []
    if upto >= 1 and not DBG["skip_l0"]:
        plan.append(("l0", 0))
    if upto >= 2:
        plan.append(("xa", 0))
    if upto >= 3:
        plan.append(("moe", 0))
    if upto >= 4:
        plan.append(("hg", 1))
    if upto >= 5:
        plan.append(("xa", 1))
    if upto >= 6:
        plan.append(("moe", 1))
    plan = [p for p in plan if p[0] not in DBG["skip"]]
    for pi, (kind, l) in enumerate(plan):
        last = (pi == len(plan) - 1)
        dst_res = out_d if last else XR[nxt]
        if kind == "l0":
            l0_phase(cur_res, dst_res)
        elif kind == "xa":
            xa_phase(l, cur_res, dst_res)
        elif kind == "moe":
            moe_phase(l, cur_res, dst_res, want_xT=(l == 0))
        elif kind == "hg":
            hg_phase(cur_res, dst_res)
        cur_res = dst_res
        nxt ^= 1

    final_keys = ["dst_dram"] + [("dst", i) for i in range(NT)]
    if cur_res is not out_d:
        A.mark()
        cp = [A.alloc(D, F32) for _ in range(2)]
        for i in range(NT):
            b = i % 2
            P.dma(lambda e, i=i, b=b: e.dma_start(out=cp[b], in_=cur_res[i * 128:(i + 1) * 128, :]), r=["dst_dram"], w=[("cp", b)])
            P.dma(lambda e, i=i, b=b: e.dma_start(out=out_d[i * 128:(i + 1) * 128, :], in_=cp[b]), r=[("cp", b)], w=["out_final"])
        final_keys.append("out_final")
        A.release()
    cnt, dcnt = P.emit(final_keys=final_keys)
    st.close()
    return nc, cnt, dcnt


def make_consts():
    import ml_dtypes
    s = np.arange(S, dtype=np.int64)
    ang = 2.0 * np.pi * ((s[:, None] * s[None, :]) % S).astype(np.float64) / S
    sc = 1.0 / np.sqrt(S)
    cs = (np.cos(ang) * sc).astype(np.float32).astype(ml_dtypes.bfloat16)
    ss = (-np.sin(ang) * sc).astype(np.float32).astype(ml_dtypes.bfloat16)
    c = np.arange(128, dtype=np.int64)
    angc = 2.0 * np.pi * ((c[:, None] * c[None, :]) % 128).astype(np.float64) / 128
    scc = 1.0 / np.sqrt(128.0)
    dc = np.concatenate([np.cos(angc) * scc, np.sin(angc) * scc], axis=1).astype(np.float32)
    t = np.arange(S)
    invcnt = np.zeros((4, S), np.float32)
    for gi, w in enumerate((2, 4, 8, 16)):
        lo = np.clip(t - w // 2, 0, S)
        hi = np.clip(t + w // 2, 0, S)
        invcnt[gi] = 1.0 / (hi - lo).astype(np.float32)
    m = np.arange(128)
    same = (m[:, None] // 64) == (m[None, :] // 64)
    masks = np.zeros((4, 128, 128), np.float32)
    masks[0] = (same & (m[:, None] <= m[None, :]))
    masks[1] = (same & (m[:, None] >= m[None, :]))
    masks[2] = (m[:, None] < m[None, :])
    masks[3] = 1.0
    eoff = (np.arange(32, dtype=np.float32) * CAP).reshape(1, 32)
    trash = (NEXP * CAP + np.arange(128, dtype=np.float32)).reshape(128, 1)
    altc = np.repeat((sc * np.cos(np.pi * np.arange(128))).astype(np.float32).reshape(128, 1), NT, axis=1)
    return {"c_cs": cs, "c_ss": ss, "c_dc": dc, "c_invcnt": invcnt, "c_masks": masks, "c_eoff": eoff, "c_trash": trash, "c_alt": np.ascontiguousarray(altc)}


_CACHE = {}


def kernel(**inputs):
    if "nc" not in _CACHE:
        _CACHE["nc"] = build()[0]
        _CACHE["consts"] = make_consts()
    nc = _CACHE["nc"]
    consts = _CACHE["consts"]
    in_maps = []
    for b in range(8):
        m = {}
        for k, v in inputs.items():
            v = np.asarray(v)
            if k in ("x", "mem"):
                m[k] = np.ascontiguousarray(v[b])
            elif k in ("pf_w_in", "pf_pool_w", "pf_pool_scale", "pf_fourier_ln_g", "pf_fourier_w", "pf_w_out",
                       "hg_w_in", "hg_norm_g", "hg_w_out"):
                m[k] = np.ascontiguousarray(v[0])
            else:
                m[k] = np.ascontiguousarray(v)
        m.update(consts)
        in_maps.append(m)
    res = run_bass_kernel_spmd(nc, in_maps, core_ids=list(range(8)))
    return np.stack([np.asarray(r["out"]) for r in res.results], axis=0).astype(np.float32)
```
